# Optimizing a Trainium2 kernel written in Bass

```python
import math
import jax, jax.numpy as jnp
from jax import lax
import numpy as np

D_MODEL = 1024
BATCH = 4
SEQ = 4096
DEPTH = 4

GRID_W = 64
CTX_LEN = 256
N_MIXERS = 3
CHUNK = 64
CONV_K = 3
Q_BLOCK = 128
ROPE_BASE = 10000.0
EPS = 1e-6
ADA_CHUNKS = 6

GDN_HEADS = 8
GDN_HEAD_K = 128
GDN_HEAD_V = 128
GDN_KW = GDN_HEADS * GDN_HEAD_K
GDN_VW = GDN_HEADS * GDN_HEAD_V
GDN_IN = 2 * GDN_KW + 2 * GDN_VW + 4 * GDN_HEADS

SSD_INNER = 2 * D_MODEL
SSD_HEAD_DIM = 64
SSD_HEADS = SSD_INNER // SSD_HEAD_DIM
SSD_GROUPS = 8
SSD_STATE = 128
SSD_GN = SSD_GROUPS * SSD_STATE
SSD_CONV_CH = SSD_INNER + 2 * SSD_GN
SSD_IN = SSD_INNER + SSD_CONV_CH + 2 * SSD_HEADS

MLA_HEADS = 16
MLA_NOPE = 64
MLA_ROPE = 32
MLA_V = 64
MLA_Q_RANK = 768
MLA_KV_RANK = 256
MLA_IN = MLA_Q_RANK + MLA_KV_RANK + MLA_ROPE

N_EXPERTS = 16
EXPERT_FF = 1024
EC_CAPACITY = 2

kernel_name = "hybrid_gdn_ssd_mla_ecmoe_diffusion_trunk"

F32 = jnp.float32


def rms_norm(x, g):
    xf = x.astype(F32)
    y = xf * lax.rsqrt(jnp.mean(xf * xf, axis=-1, keepdims=True) + EPS)
    return (y * g.astype(F32)).astype(x.dtype)


def _l2norm(x):
    xf = x.astype(F32)
    return (xf * lax.rsqrt(jnp.sum(xf * xf, axis=-1, keepdims=True) + EPS)).astype(x.dtype)


def _rev(a):
    return jnp.flip(a, axis=1)


def depthwise_conv(x, w):
    k = w.shape[0]
    return lax.conv_general_dilated(
        x, w[:, None, :].astype(x.dtype), window_strides=(1,), padding=[((k - 1) // 2, k // 2)],
        dimension_numbers=('NWC', 'WIO', 'NWC'), feature_group_count=x.shape[-1])


def ada_chunks(cvec, w, b):
    m = jax.nn.silu(cvec) @ w + b
    return jnp.split(m, ADA_CHUNKS, axis=-1)


def modulate(h, shift, scale):
    return h * (1 + scale) + shift


def axial_rope(x, rows, cols):
    def rot(xp, pos):
        d = xp.shape[-1]
        half = d // 2
        inv = ROPE_BASE ** (-jnp.arange(half, dtype=F32) * 2.0 / d)
        ang = pos.astype(F32)[:, None] * inv[None, :]
        cos = jnp.cos(ang)[None, :, None, :]
        sin = jnp.sin(ang)[None, :, None, :]
        x1 = xp[..., :half].astype(F32)
        x2 = xp[..., half:].astype(F32)
        return jnp.concatenate([x1 * cos - x2 * sin, x1 * sin + x2 * cos], -1).astype(xp.dtype)
    r = x.shape[-1] // 2
    return jnp.concatenate([rot(x[..., :r], rows), rot(x[..., r:], cols)], -1)


def gated_delta_scan(q, k, v, g, beta, s0):
    b, t, h, dk = q.shape
    dv = v.shape[-1]
    n = t // CHUNK

    def chunk(a):
        a = a.astype(F32).reshape((b, n, CHUNK) + a.shape[2:])
        return jnp.swapaxes(jnp.swapaxes(a, 0, 1), 2, 3)

    qc = chunk(q) * (dk ** -0.5)
    kc, vc, gc, bc = chunk(k), chunk(v), chunk(g), chunk(beta)
    gcum = jnp.cumsum(gc, axis=-1)
    tril = jnp.tril(jnp.ones((CHUNK, CHUNK), bool))
    strict = jnp.tril(jnp.ones((CHUNK, CHUNK), bool), -1)
    decay = jnp.exp(jnp.where(tril, gcum[..., :, None] - gcum[..., None, :], -jnp.inf))
    kb = kc * bc[..., None]
    lower = jnp.where(strict, jnp.einsum('nbhid,nbhjd->nbhij', kb, kc) * decay, 0.0)
    eye = jnp.broadcast_to(jnp.eye(CHUNK, dtype=F32), lower.shape)
    rhs = jnp.concatenate([vc * bc[..., None], kb * jnp.exp(gcum)[..., None]], axis=-1)
    sol = lax.linalg.triangular_solve(eye + lower, rhs, left_side=True, lower=True, unit_diagonal=True)
    u, w = sol[..., :dv], sol[..., dv:]
    attn = jnp.where(tril, jnp.einsum('nbhid,nbhjd->nbhij', qc, kc) * decay, 0.0)

    def step(S, inp):
        q_i, k_i, u_i, w_i, a_i, g_i = inp
        v_new = u_i - jnp.einsum('bhck,bhkv->bhcv', w_i, S)
        o = (jnp.einsum('bhck,bhkv->bhcv', q_i * jnp.exp(g_i)[..., None], S)
             + jnp.einsum('bhij,bhjv->bhiv', a_i, v_new))
        g_last = g_i[..., -1]
        S = (S * jnp.exp(g_last)[..., None, None]
             + jnp.einsum('bhck,bhcv->bhkv', k_i * jnp.exp(g_last[..., None] - g_i)[..., None], v_new))
        return S, o

    S, o = lax.scan(step, s0, (qc, kc, u, w, attn, gcum))
    o = jnp.swapaxes(jnp.swapaxes(o, 2, 3), 0, 1).reshape(b, t, h, dv)
    return o, S


def gdn_mixer(hx, hc, w_in, conv_w, a_log, dt_bias, norm_g, w_out):
    neg_a = -jnp.exp(a_log.astype(F32))

    def project(h):
        b, t, _ = h.shape
        p = h @ w_in
        qkv = jax.nn.silu(depthwise_conv(p[..., :2 * GDN_KW + GDN_VW], conv_w))
        q = _l2norm(qkv[..., :GDN_KW].reshape(b, t, GDN_HEADS, GDN_HEAD_K))
        k = _l2norm(qkv[..., GDN_KW:2 * GDN_KW].reshape(b, t, GDN_HEADS, GDN_HEAD_K))
        v = qkv[..., 2 * GDN_KW:].reshape(b, t, GDN_HEADS, GDN_HEAD_V)
        off = 2 * GDN_KW + GDN_VW
        z = p[..., off:off + GDN_VW].reshape(b, t, GDN_HEADS, GDN_HEAD_V)
        off += GDN_VW
        beta = jax.nn.sigmoid(p[..., off:off + 2 * GDN_HEADS].astype(F32)).reshape(b, t, 2, GDN_HEADS)
        off += 2 * GDN_HEADS
        g = neg_a * jax.nn.softplus(p[..., off:].astype(F32).reshape(b, t, 2, GDN_HEADS) + dt_bias.astype(F32))
        return q, k, v, z, g, beta

    def mix(h, s0f, s0b):
        b, t, _ = h.shape
        q, k, v, z, g, beta = project(h)
        of, sf = gated_delta_scan(q, k, v, g[:, :, 0], beta[:, :, 0], s0f)
        ob, sb = gated_delta_scan(_rev(q), _rev(k), _rev(v), _rev(g[:, :, 1]), _rev(beta[:, :, 1]), s0b)
        o = (of + _rev(ob)).astype(h.dtype)
        o = rms_norm(o, norm_g) * jax.nn.silu(z)
        return o.reshape(b, t, GDN_VW) @ w_out, sf, sb

    s0 = jnp.zeros((hc.shape[0], GDN_HEADS, GDN_HEAD_K, GDN_HEAD_V), F32)
    oc, sf, sb = mix(hc, s0, s0)
    ox, _, _ = mix(hx, sf, sb)
    return ox, oc


def ssd_scan(xs, dt, A, bm, cm, h0):
    b, t = xs.shape[:2]
    n = t // CHUNK

    def chunk(a):
        return jnp.swapaxes(a.astype(F32).reshape((b, n, CHUNK) + a.shape[2:]), 0, 1)

    tril = jnp.tril(jnp.ones((CHUNK, CHUNK), bool))[None, :, :, None, None]

    def step(h, inp):
        x_i, dt_i, b_i, c_i = inp
        acs = jnp.cumsum(dt_i * A, axis=1)
        seg = jnp.where(tril, acs[:, :, None] - acs[:, None, :], -jnp.inf)
        wts = jnp.einsum('btgn,bsgn->btsg', c_i, b_i)[..., None] * jnp.exp(seg) * dt_i[:, None]
        y = (jnp.einsum('btsgr,bsgrp->btgrp', wts, x_i)
             + jnp.einsum('btgn,bgrpn->btgrp', c_i, h) * jnp.exp(acs)[..., None])
        to_end = jnp.exp(acs[:, -1:] - acs) * dt_i
        h = (h * jnp.exp(acs[:, -1])[..., None, None]
             + jnp.einsum('bsgn,bsgr,bsgrp->bgrpn', b_i, to_end, x_i))
        return h, y

    h, y = lax.scan(step, h0, (chunk(xs), chunk(dt), chunk(bm), chunk(cm)))
    return jnp.swapaxes(y, 0, 1).reshape(xs.shape), h


def ssd_mixer(hx, hc, w_in, conv_w, conv_b, a_log, dt_bias, d_skip, norm_g, w_out):
    r = SSD_HEADS // SSD_GROUPS
    A = -jnp.exp(a_log.astype(F32)).reshape(2, SSD_GROUPS, r)

    def project(h):
        b, t, _ = h.shape
        p = h @ w_in
        z = p[..., :SSD_INNER]
        xbc = jax.nn.silu(depthwise_conv(p[..., SSD_INNER:SSD_INNER + SSD_CONV_CH], conv_w) + conv_b)
        dt = jax.nn.softplus((p[..., SSD_INNER + SSD_CONV_CH:] + dt_bias.reshape(-1)).astype(F32))
        dt = dt.reshape(b, t, 2, SSD_GROUPS, r)
        xs = xbc[..., :SSD_INNER].reshape(b, t, SSD_GROUPS, r, SSD_HEAD_DIM)
        bm = xbc[..., SSD_INNER:SSD_INNER + SSD_GN].reshape(b, t, SSD_GROUPS, SSD_STATE)
        cm = xbc[..., SSD_INNER + SSD_GN:].reshape(b, t, SSD_GROUPS, SSD_STATE)
        return z, xs, bm, cm, dt

    def mix(h, h0f, h0b):
        b, t, _ = h.shape
        z, xs, bm, cm, dt = project(h)
        yf, hf = ssd_scan(xs, dt[:, :, 0], A[0], bm, cm, h0f)
        yb, hb = ssd_scan(_rev(xs), _rev(dt[:, :, 1]), A[1], _rev(bm), _rev(cm), h0b)
        y = yf + _rev(yb) + d_skip.astype(F32).reshape(SSD_GROUPS, r)[..., None] * xs.astype(F32)
        y = y.reshape(b, t, SSD_INNER).astype(h.dtype) * jax.nn.silu(z)
        return rms_norm(y, norm_g) @ w_out, hf, hb

    h0 = jnp.zeros((hc.shape[0], SSD_GROUPS, r, SSD_HEAD_DIM, SSD_STATE), F32)
    oc, hf, hb = mix(hc, h0, h0)
    ox, _, _ = mix(hx, hf, hb)
    return ox, oc


def block_attention(q_nope, q_rope, k_nope, k_rope, v):
    b, t, h, _ = q_nope.shape
    nb = t // Q_BLOCK
    scale = (MLA_NOPE + MLA_ROPE) ** -0.5

    def blocks(a):
        return jnp.swapaxes(a.reshape((b, nb, Q_BLOCK) + a.shape[2:]), 0, 1)

    def one(args):
        qn, qr = args
        s = (jnp.einsum('bqhd,bkhd->bhqk', qn, k_nope).astype(F32)
             + jnp.einsum('bqhr,bkr->bhqk', qr, k_rope).astype(F32)) * scale
        p = jax.nn.softmax(s, axis=-1).astype(v.dtype)
        return jnp.einsum('bhqk,bkhd->bqhd', p, v)

    o = lax.map(one, (blocks(q_nope), blocks(q_rope)))
    return jnp.swapaxes(o, 0, 1).reshape(b, t, h, v.shape[-1])


def mla_mixer(hx, hc, w_in, q_norm_g, w_uq, kv_norm_g, w_ukv, w_out, rows, cols):
    def project(h, positioned):
        b, t, _ = h.shape
        p = h @ w_in
        cq = p[..., :MLA_Q_RANK]
        ckv = p[..., MLA_Q_RANK:MLA_Q_RANK + MLA_KV_RANK]
        kr = p[..., MLA_Q_RANK + MLA_KV_RANK:][:, :, None, :]
        q = (rms_norm(cq, q_norm_g) @ w_uq).reshape(b, t, MLA_HEADS, MLA_NOPE + MLA_ROPE)
        kv = (rms_norm(ckv, kv_norm_g) @ w_ukv).reshape(b, t, MLA_HEADS, MLA_NOPE + MLA_V)
        q_nope, q_rope = q[..., :MLA_NOPE], q[..., MLA_NOPE:]
        k_nope, v = kv[..., :MLA_NOPE], kv[..., MLA_NOPE:]
        if positioned:
            q_rope = axial_rope(q_rope, rows, cols)
            kr = axial_rope(kr, rows, cols)
        return q_nope, q_rope, k_nope, kr[:, :, 0], v

    qn_c, qr_c, kn_c, kr_c, v_c = project(hc, False)
    qn_x, qr_x, kn_x, kr_x, v_x = project(hx, True)
    oc = block_attention(qn_c, qr_c, kn_c, kr_c, v_c)
    ox = block_attention(qn_x, qr_x,
                         jnp.concatenate([kn_c, kn_x], axis=1),
                         jnp.concatenate([kr_c, kr_x], axis=1),
                         jnp.concatenate([v_c, v_x], axis=1))
    bx, tx = hx.shape[:2]
    bc, tc = hc.shape[:2]
    return (ox.reshape(bx, tx, MLA_HEADS * MLA_V) @ w_out,
            oc.reshape(bc, tc, MLA_HEADS * MLA_V) @ w_out)


def ec_moe(h, w_router, w_gate, w_up, w_down):
    b, n, _ = h.shape
    cap = EC_CAPACITY * n // N_EXPERTS
    aff = jax.nn.softmax((h @ w_router).astype(F32), axis=-1)
    gate, idx = lax.top_k(jnp.swapaxes(aff, 1, 2), cap)
    bidx = jnp.arange(b)[:, None, None]
    xs = h[bidx, idx]
    hid = jax.nn.silu(jnp.einsum('becd,edf->becf', xs, w_gate)) * jnp.einsum('becd,edf->becf', xs, w_up)
    y = jnp.einsum('becf,efd->becd', hid, w_down) * gate.astype(h.dtype)[..., None]
    return jnp.zeros_like(h).at[bidx, idx].add(y)


def setup_inputs(seed: int = 0) -> dict:
    key = jax.random.key(seed)
    ks = iter(jax.random.split(key, 40))
    n_a = len(range(0, DEPTH, N_MIXERS))
    n_b = len(range(1, DEPTH, N_MIXERS))
    n_c = len(range(2, DEPTH, N_MIXERS))

    def nrm(shape, scale):
        return jax.random.normal(next(ks), shape, F32) * scale

    def gain(shape):
        return 1.0 + nrm(shape, 0.05)

    def a_log(shape):
        return jnp.log(jax.random.uniform(next(ks), shape, F32, 1.0, 16.0))

    def dt_bias(shape):
        dt = jnp.exp(jax.random.uniform(next(ks), shape, F32, math.log(1e-3), math.log(1e-1)))
        return dt + jnp.log(-jnp.expm1(-dt))

    d = D_MODEL
    return {
        "x": nrm((BATCH, SEQ, d), 1.0),
        "c": nrm((BATCH, d), 1.0),
        "ctx": nrm((BATCH, CTX_LEN, d), 1.0),
        "c_ctx": nrm((d,), 1.0),
        "ada_w": nrm((DEPTH, d, ADA_CHUNKS * d), 0.5 * d ** -0.5),
        "ada_b": nrm((DEPTH, ADA_CHUNKS * d), 0.02),
        "norm1_g": gain((DEPTH, d)),
        "norm2_g": gain((DEPTH, d)),
        "router_w": nrm((DEPTH, d, N_EXPERTS), d ** -0.5),
        "moe_w_gate": nrm((DEPTH, N_EXPERTS, d, EXPERT_FF), d ** -0.5),
        "moe_w_up": nrm((DEPTH, N_EXPERTS, d, EXPERT_FF), d ** -0.5),
        "moe_w_down": nrm((DEPTH, N_EXPERTS, EXPERT_FF, d), EXPERT_FF ** -0.5),
        "gdn_w_in": nrm((n_a, d, GDN_IN), d ** -0.5),
        "gdn_conv_w": nrm((n_a, CONV_K, 2 * GDN_KW + GDN_VW), CONV_K ** -0.5),
        "gdn_a_log": a_log((n_a, 2, GDN_HEADS)),
        "gdn_dt_bias": dt_bias((n_a, 2, GDN_HEADS)),
        "gdn_norm_g": gain((n_a, GDN_HEAD_V)),
        "gdn_w_out": nrm((n_a, GDN_VW, d), GDN_VW ** -0.5),
        "ssd_w_in": nrm((n_b, d, SSD_IN), d ** -0.5),
        "ssd_conv_w": nrm((n_b, CONV_K, SSD_CONV_CH), CONV_K ** -0.5),
        "ssd_conv_b": nrm((n_b, SSD_CONV_CH), 0.02),
        "ssd_a_log": a_log((n_b, 2, SSD_HEADS)),
        "ssd_dt_bias": dt_bias((n_b, 2, SSD_HEADS)),
        "ssd_d": 1.0 + nrm((n_b, SSD_HEADS), 0.1),
        "ssd_norm_g": gain((n_b, SSD_INNER)),
        "ssd_w_out": nrm((n_b, SSD_INNER, d), SSD_INNER ** -0.5),
        "mla_w_in": nrm((n_c, d, MLA_IN), d ** -0.5),
        "mla_q_norm_g": gain((n_c, MLA_Q_RANK)),
        "mla_w_uq": nrm((n_c, MLA_Q_RANK, MLA_HEADS * (MLA_NOPE + MLA_ROPE)), MLA_Q_RANK ** -0.5),
        "mla_kv_norm_g": gain((n_c, MLA_KV_RANK)),
        "mla_w_ukv": nrm((n_c, MLA_KV_RANK, MLA_HEADS * (MLA_NOPE + MLA_V)), MLA_KV_RANK ** -0.5),
        "mla_w_out": nrm((n_c, MLA_HEADS * MLA_V, d), (MLA_HEADS * MLA_V) ** -0.5),
        "final_norm_g": gain((d,)),
    }


def reference(x, c, ctx, c_ctx, ada_w, ada_b, norm1_g, norm2_g, router_w, moe_w_gate, moe_w_up, moe_w_down,
              gdn_w_in, gdn_conv_w, gdn_a_log, gdn_dt_bias, gdn_norm_g, gdn_w_out,
              ssd_w_in, ssd_conv_w, ssd_conv_b, ssd_a_log, ssd_dt_bias, ssd_d, ssd_norm_g, ssd_w_out,
              mla_w_in, mla_q_norm_g, mla_w_uq, mla_kv_norm_g, mla_w_ukv, mla_w_out, final_norm_g):
    t = x.shape[1]
    ROWS = t // GRID_W
    rows = jnp.repeat(jnp.arange(ROWS, dtype=jnp.int32), GRID_W)
    cols = jnp.tile(jnp.arange(GRID_W, dtype=jnp.int32), ROWS)

    for i in range(DEPTH):
        last = i == DEPTH - 1
        mx = [m[:, None, :] for m in ada_chunks(c, ada_w[i], ada_b[i])]
        mc = ada_chunks(c_ctx, ada_w[i], ada_b[i])
        hx = modulate(rms_norm(x, norm1_g[i]), mx[0], mx[1])
        hc = modulate(rms_norm(ctx, norm1_g[i]), mc[0], mc[1])
        kind = i % N_MIXERS
        j = i // N_MIXERS
        if kind == 0:
            ox, oc = gdn_mixer(hx, hc, gdn_w_in[j], gdn_conv_w[j], gdn_a_log[j], gdn_dt_bias[j],
                               gdn_norm_g[j], gdn_w_out[j])
        elif kind == 1:
            ox, oc = ssd_mixer(hx, hc, ssd_w_in[j], ssd_conv_w[j], ssd_conv_b[j], ssd_a_log[j],
                               ssd_dt_bias[j], ssd_d[j], ssd_norm_g[j], ssd_w_out[j])
        else:
            ox, oc = mla_mixer(hx, hc, mla_w_in[j], mla_q_norm_g[j], mla_w_uq[j], mla_kv_norm_g[j],
                               mla_w_ukv[j], mla_w_out[j], rows, cols)
        x = x + mx[2] * ox
        hx = modulate(rms_norm(x, norm2_g[i]), mx[3], mx[4])
        x = x + mx[5] * ec_moe(hx, router_w[i], moe_w_gate[i], moe_w_up[i], moe_w_down[i])
        if not last:
            ctx = ctx + mc[2] * oc
            hc = modulate(rms_norm(ctx, norm2_g[i]), mc[3], mc[4])
            ctx = ctx + mc[5] * ec_moe(hc, router_w[i], moe_w_gate[i], moe_w_up[i], moe_w_down[i])

    return rms_norm(x, final_norm_g)
```

```python
import numpy as np
from contextlib import ExitStack
import concourse.bass as bass
import concourse.mybir as mybir
from concourse.bass_utils import run_bass_kernel_spmd

F32 = mybir.dt.float32
AF = mybir.ActivationFunctionType
ALU = mybir.AluOpType
AX = mybir.AxisListType

SEM_EPOCH = 20000
N_DMA_RING = 8
D = 1024
NE = 16


class Buf:
    __slots__ = ("name", "w", "r", "excl")

    def __init__(self, name=""):
        self.name = name
        self.w = None
        self.r = {}
        self.excl = False


def PBuf():
    b = Buf()
    b.excl = True
    return b


class Prog:
    def __init__(self, nc, es):
        self.nc = nc
        self.es = es
        self.eng = {"pe": nc.tensor, "act": nc.scalar, "dve": nc.vector, "pool": nc.gpsimd, "sp": nc.sync}
        self.sem = {}
        self.cnt = {}
        self.semown = {}
        self.seen = {e: {} for e in self.eng}
        self.nsem = 0
        self.ninst = 0
        self.allsems = []
        for e in self.eng:
            self._new_sem(e)
        self.dring = {}
        self.dpos = {}

    def _mk(self, tag):
        self.nsem += 1
        return self.es.enter_context(self.nc.semaphore(f"{tag}{self.nsem}"))

    def _new_sem(self, e):
        s = self._mk("s" + e)
        self.sem[e] = s
        self.cnt[e] = 0
        self.semown[id(s)] = e

    def _wait(self, e, tok):
        s, v = tok
        if e == "pe" and self.semown.get(id(s)) == "pe":
            return
        if self.seen[e].get(id(s), 0) >= v:
            return
        self.eng[e].wait_ge(s, v)
        self.ninst += 1
        self.seen[e][id(s)] = v

    def _deps(self, e, reads, writes):
        for b in reads:
            if b.w is not None:
                self._wait(e, b.w)
            if b.excl:
                for tok in list(b.r.values()):
                    if self.semown.get(id(tok[0])) != e:
                        self._wait(e, tok)
        for b in writes:
            if b.w is not None:
                self._wait(e, b.w)
            for tok in list(b.r.values()):
                self._wait(e, tok)

    def _mark(self, tok, reads, writes):
        s, v = tok
        for b in reads:
            b.r[id(s)] = tok
        for b in writes:
            b.w = tok
            b.r = {}

    def op(self, e, fn, reads=(), writes=()):
        self._deps(e, reads, writes)
        ins = fn(self.eng[e])
        if self.cnt[e] >= SEM_EPOCH:
            self._new_sem(e)
        self.cnt[e] += 1
        ins.then_inc(self.sem[e], 1)
        self.ninst += 1
        self._mark((self.sem[e], self.cnt[e]), reads, writes)
        return ins

    def dma(self, q, out, in_, reads=(), writes=(), **kw):
        self._deps(q, reads, writes)
        if q not in self.dring:
            self.dring[q] = [[self._mk("d" + q), 0] for _ in range(N_DMA_RING)]
            self.dpos[q] = 0
        slot = self.dring[q][self.dpos[q] % N_DMA_RING]
        self.dpos[q] += 1
        if slot[1] > 0:
            self._wait(q, (slot[0], slot[1]))
        slot[1] += 16
        self.eng[q].dma_start(out=out, in_=in_, **kw).then_inc(slot[0], 16)
        self.ninst += 1
        self._mark((slot[0], slot[1]), reads, writes)

    def barrier(self):
        toks = [(self.sem[f], self.cnt[f]) for f in self.eng if self.cnt[f] > 0]
        for q in self.dring:
            toks += [(s, v) for s, v in self.dring[q] if v > 0]
        for e in self.eng:
            for t in toks:
                if self.semown.get(id(t[0])) == e:
                    continue
                self._wait(e, t)

    def finish(self, bufs, e="sp"):
        for b in bufs:
            if b.w is not None:
                self._wait(e, b.w)


class Ctx:
    def __init__(self, nc, es):
        self.nc = nc
        self.es = es
        self.p = Prog(nc, es)
        self.n = 0

    def sb(self, shape, es=None, dt=F32):
        self.n += 1
        return (es or self.es).enter_context(self.nc.sbuf_tensor(f"sb{self.n}", list(shape), dt))

    def ps(self, shape, es=None):
        self.n += 1
        return (es or self.es).enter_context(self.nc.psum_tensor(f"ps{self.n}", list(shape), F32))

    def din(self, name, shape):
        return self.nc.dram_tensor(name, list(shape), F32, kind="ExternalInput").ap()

    def dout(self, name, shape):
        return self.nc.dram_tensor(name, list(shape), F32, kind="ExternalOutput").ap()


def load_bc(c, q, dst, vec_row, buf):
    c.p.dma(q, dst, vec_row.partition_broadcast(128), writes=[buf])


def norm_mod_T(c, K, xt, xb, G, S, hT_dst, hb):
    p = c.p
    p.op("act", lambda e: e.activation(K["junk"][:], xt, AF.Square, accum_out=K["ss"][:, 0:1]),
         reads=[xb], writes=[K["bjunk"], K["bss"]])
    p.op("act", lambda e: e.activation(K["ss"][:, 1:2], K["ss"][:, 0:1], AF.Sqrt, bias=K["eps"][:, 0:1], scale=1.0 / D),
         reads=[K["bss"]], writes=[K["bss"]])
    p.op("dve", lambda e: e.reciprocal(K["ss"][:, 2:3], K["ss"][:, 1:2]), reads=[K["bss"]], writes=[K["bss"]])
    if G is not None:
        p.op("dve", lambda e: e.scalar_tensor_tensor(K["h"][:], xt, K["ss"][:, 2:3], G[0][:], ALU.mult, ALU.mult),
             reads=[xb, K["bss"], G[1]], writes=[K["bh"]])
    else:
        p.op("dve", lambda e: e.tensor_scalar(K["h"][:], xt, K["ss"][:, 2:3], None, ALU.mult),
             reads=[xb, K["bss"]], writes=[K["bh"]])
    if S is not None:
        p.op("pool", lambda e: e.tensor_add(K["h"][:], K["h"][:], S[0][:]), reads=[K["bh"], S[1]], writes=[K["bh"]])
    if hT_dst is None:
        return
    for half in range(2):
        for cc in range(4):
            ch = half * 4 + cc
            p.op("pe", lambda e: e.transpose(K["ptr"][:, cc * 128:(cc + 1) * 128], K["h"][:, ch * 128:(ch + 1) * 128], K["ident"][:]),
                 reads=[K["bh"], K["bident"]], writes=[K["bptr"]])
        p.op("act", lambda e: e.copy(hT_dst[:, half * 4:(half + 1) * 4, :], K["ptr"][:].rearrange("p (c t) -> p c t", c=4)),
             reads=[K["bptr"]], writes=[hb])


def common_consts(c, ident_d):
    K = {}
    K["ident"] = c.sb([128, 128]); K["bident"] = Buf()
    c.p.dma("pool", K["ident"][:], ident_d, writes=[K["bident"]])
    K["junk"] = c.sb([128, D]); K["bjunk"] = Buf()
    K["h"] = c.sb([128, D]); K["bh"] = Buf()
    K["ss"] = c.sb([128, 4]); K["bss"] = Buf()
    K["eps"] = c.sb([128, 1]); K["beps"] = Buf()
    c.p.op("dve", lambda e: e.memset(K["eps"][:], 1e-6), writes=[K["beps"], K["bss"]])
    K["ptr"] = c.ps([128, 512]); K["bptr"] = PBuf()
    return K


GRP_PAIR = [[0, 1], [2, 3], [4, 5], [6, 7]]
GRP_QUAD = [[0, 2, 4, 6], [1, 3, 5, 7]]
GRP_ALL = [list(range(8))]
NTOK = 4352
NT = 34


def new_nc():
    return bass.Bass("TRN2", target_bir_lowering=False, num_devices=8)


def allgather(c, src, dst, groups, reads, writes):
    p = c.p
    p._deps("pool", reads, writes)
    if not hasattr(c, "_cc_sem"):
        c._cc_sem = p._mk("cc")
        c._cc_cnt = 0
    if c._cc_cnt > 0:
        p._wait("pool", (c._cc_sem, c._cc_cnt))
    c._cc_cnt += 1
    c.nc.gpsimd.collective_compute("AllGather", ALU.bypass, replica_groups=groups, ins=[src.opt()], outs=[dst.opt()]).then_inc(c._cc_sem)
    p.ninst += 1
    p._mark((c._cc_sem, c._cc_cnt), reads, writes)


def dram(c, shape):
    c.n += 1
    return c.nc.dram_tensor(f"dr{c.n}", list(shape), F32).ap()


def gather_w(c, name, rows, cols, groups=GRP_QUAD):
    g = len(groups[0])
    ext = c.din(name, [rows // g, cols])
    src = dram(c, [rows // g, cols])
    full = dram(c, [rows, cols])
    bs, bf = Buf(), Buf()
    c.p.dma("sp", src, ext, writes=[bs])
    allgather(c, src, full, groups, [bs], [bf])
    return full, bf


def emit_ada(c, K, cvec, ada_w_full, baw, ada_b):
    p = c.p
    vecs = dram(c, [4, 2, 6 * D])
    bv = Buf()
    es = ExitStack()
    with es:
        sc = c.sb([128, 8, 2], es); bsc = Buf()
        craw = c.sb([16, 128], es); bcraw = Buf()
        p.dma("pool", craw[:], cvec.rearrange("k (c p) -> (k c) p", p=128), writes=[bcraw])
        p.op("pe", lambda e: e.transpose(K["ptr"][:, 0:16], craw[:], K["ident"][0:16, 0:16]), reads=[bcraw, K["bident"]], writes=[K["bptr"]])
        p.op("act", lambda e: e.activation(sc[:].rearrange("p c k -> p k c"), K["ptr"][:, 0:16].rearrange("p (k c) -> p k c", k=2), AF.Silu),
             reads=[K["bptr"]], writes=[bsc])
        ab = c.sb([2, 6 * D], es); bab = Buf()
        orow = c.sb([2, 6 * D], es); borow = Buf()
        wt = [c.sb([128, 8, 512], es) for _ in range(2)]; bwt = [Buf(), Buf()]
        pa = [c.ps([128, 512], es) for _ in range(2)]; bpa = [PBuf(), PBuf()]
        n = 0
        for i in range(4):
            for k in range(2):
                p.dma("pool", ab[k:k + 1, :], ada_b[i:i + 1, :], writes=[bab])
            for g in range(12):
                j = n % 2
                n += 1
                p.dma("sp", wt[j][:], ada_w_full[i * D:(i + 1) * D, g * 512:(g + 1) * 512].rearrange("(c p) f -> p c f", p=128),
                      reads=[baw], writes=[bwt[j]])
                for ch in range(8):
                    p.op("pe", lambda e: e.matmul(pa[j][0:2, :], sc[:, ch, :], wt[j][:, ch, :], start=(ch == 0), stop=(ch == 7)),
                         reads=[bsc, bwt[j]], writes=[bpa[j]])
                p.op("dve", lambda e: e.tensor_add(orow[:, g * 512:(g + 1) * 512], pa[j][0:2, :], ab[:, g * 512:(g + 1) * 512]),
                     reads=[bpa[j], bab], writes=[borow])
            p.dma("pool", vecs[i], orow[:], reads=[borow], writes=[bv])
        p.barrier()
    return vecs, bv


def load_mods(c, es, vecs, bv, i, base, norm_g_row):
    p = c.p
    g2 = c.sb([128, D], es); bg2 = Buf()
    load_bc(c, "pool", g2[:], norm_g_row, bg2)
    mods = {}
    for k, kind in enumerate("xc"):
        t = []
        for j in range(3):
            tt = c.sb([128, D], es); bt = Buf()
            p.dma("pool", tt[:], vecs[i, k:k + 1, (base + j) * D:(base + j + 1) * D].partition_broadcast(128), reads=[bv], writes=[bt])
            t.append((tt, bt))
        Sh, Sc, Ga = t
        p.op("dve", lambda e: e.scalar_tensor_tensor(Sc[0][:], Sc[0][:], 1.0, g2[:], ALU.add, ALU.mult),
             reads=[bg2, Sc[1]], writes=[Sc[1]])
        mods[kind] = (Sc, Sh, Ga)
    return mods


def tile_kind(t):
    return "x" if t < 32 else "c"


def emit_residual(c, K, X, bX, P, bP, mods):
    p = c.p
    G = dram(c, [2 * NTOK, D]); bG = Buf()
    allgather(c, P, G, GRP_PAIR, bP, [bG])
    es = ExitStack()
    with es:
        xt = [c.sb([128, D], es) for _ in range(2)]; bxt = [Buf(), Buf()]
        ga = [c.sb([128, D], es) for _ in range(2)]; bga = [Buf(), Buf()]
        gb = [c.sb([128, D], es) for _ in range(2)]; bgb = [Buf(), Buf()]
        for t in range(NT):
            i = t % 2
            Ga = mods[tile_kind(t)][2]
            p.dma("sp", xt[i][:], X[t * 128:(t + 1) * 128, :], reads=[bX[t]], writes=[bxt[i]])
            p.dma("sp", ga[i][:], G[t * 128:(t + 1) * 128, :], reads=[bG], writes=[bga[i]])
            p.dma("sp", gb[i][:], G[NTOK + t * 128:NTOK + (t + 1) * 128, :], reads=[bG], writes=[bgb[i]])
            p.op("pool", lambda e: e.tensor_add(ga[i][:], ga[i][:], gb[i][:]), reads=[bga[i], bgb[i]], writes=[bga[i]])
            if ssa is not None:
                p.dma("sp", sst[:, 0:1], ssa[sl, :], reads=rr, writes=[bsst])
                p.dma("sp", sst[:, 1:2], ssb[sl, :], reads=rr, writes=[bsst])
                p.op("dve", lambda e: e.tensor_add(sst[:, 2:3], sst[:, 0:1], sst[:, 1:2]), reads=[bsst], writes=[bsst])
                p.op("act", lambda e: e.activation(sst[:, 3:4], sst[:, 2:3], AF.Sqrt, bias=K["eps"][:, 0:1], scale=1.0 / 2048), reads=[bsst], writes=[bsst])
                p.op("dve", lambda e: e.reciprocal(sst[:, 4:5], sst[:, 3:4]), reads=[bsst], writes=[bsst])
                p.op("dve", lambda e: e.scalar_tensor_tensor(ga[i][:], ga[i][:], sst[:, 4:5], Ga[0][:], ALU.mult, ALU.mult),
                     reads=[bga[i], Ga[1], bsst], writes=[bga[i]])
            else:
                p.op("dve", lambda e: e.tensor_mul(ga[i][:], ga[i][:], Ga[0][:]), reads=[bga[i], Ga[1]], writes=[bga[i]])
            p.op("dve", lambda e: e.tensor_add(xt[i][:], xt[i][:], ga[i][:]), reads=[bga[i], bxt[i]], writes=[bxt[i]])
            p.dma("pool", X[t * 128:(t + 1) * 128, :], xt[i][:], reads=[bxt[i]], writes=[bX[t]])
        p.barrier()


CAP_X = 512
CAP_C = 32
N_BISECT = 36
NEH = 8


def emit_moe(c, K, X, bX, mods, wr, wg, wu, wd, bw, P, bP):
    p = c.p
    es0 = ExitStack()
    with es0:
        wr_sb = c.sb([128, 8, NE], es0); bwr = Buf()
        p.dma("pool", wr_sb[:], wr.rearrange("(c p) e -> p c e", p=128), writes=[bwr])
        ones16 = c.sb([16, 128], es0); bones = Buf()
        p.op("dve", lambda e: e.memset(ones16[:], 1.0), writes=[bones])
        xt = [c.sb([128, D], es0) for _ in range(2)]
        bxt = [Buf() for _ in range(2)]
        pmisc = c.ps([128, 512], es0); bpm = PBuf()
        sm = c.sb([128, 8], es0); bsm = Buf()
        ex = c.sb([128, NE], es0); bex = Buf()
        thr_bc = {k: c.sb([128, NE], es0) for k in "xc"}
        bthr = {k: Buf() for k in "xc"}

        def router(hT_src, hb, aff_dst, baff):
            for ch in range(8):
                p.op("pe", lambda e: e.matmul(pmisc[:, 0:NE], hT_src[:, ch, :], wr_sb[:, ch, :], start=(ch == 0), stop=(ch == 7)),
                     reads=[hb, bwr], writes=[bpm])
            p.op("dve", lambda e: e.reduce_max(sm[:, 0:1], pmisc[:, 0:NE], AX.X), reads=[bpm], writes=[bsm])
            p.op("dve", lambda e: e.tensor_scalar(sm[:, 1:2], sm[:, 0:1], -1.0, None, ALU.mult), reads=[bsm], writes=[bsm])
            p.op("act", lambda e: e.activation(ex[:], pmisc[:, 0:NE], AF.Exp, bias=sm[:, 1:2], accum_out=sm[:, 2:3]),
                 reads=[bpm, bsm], writes=[bex, bsm])
            p.op("dve", lambda e: e.reciprocal(sm[:, 3:4], sm[:, 2:3]), reads=[bsm], writes=[bsm])
            p.op("dve", lambda e: e.tensor_scalar(aff_dst, ex[:], sm[:, 3:4], None, ALU.mult), reads=[bex, bsm], writes=[baff])

        es1 = ExitStack()
        with es1:
            affT = c.sb([16, NT * 128], es1); baffT = Buf()
            mask = c.sb([16, 32 * 128], es1); bmask = Buf()
            hT1 = c.sb([128, 8, 128], es1); bhT1 = Buf()
            aff1 = c.sb([128, NE], es1); baff1 = Buf()
            bs = c.sb([16, 8], es1); bbs = Buf()
            diag = c.sb([16, 16], es1); bdiag = Buf()
            for t in range(NT):
                G, S, _ = mods[tile_kind(t)]
                i = t % 2
                p.dma("sp", xt[i][:], X[t * 128:(t + 1) * 128, :], reads=[bX[t]], writes=[bxt[i]])
                norm_mod_T(c, K, xt[i][:], bxt[i], G, S, hT1, bhT1)
                router(hT1, bhT1, aff1[:], baff1)
                p.op("pe", lambda e: e.transpose(pmisc[0:16, 128:256], aff1[:], K["ident"][:]),
                     reads=[baff1, K["bident"]], writes=[bpm])
                p.op("act", lambda e: e.copy(affT[:, t * 128:(t + 1) * 128], pmisc[0:16, 128:256]), reads=[bpm], writes=[baffT])
            for kind, lo_c, n_c, cap in (("x", 0, 32 * 128, CAP_X), ("c", 32 * 128, 2 * 128, CAP_C)):
                a = affT[:, lo_c:lo_c + n_c]
                m = mask[:, 0:n_c]
                lo, hi, mid, cnt, ge, d1, hh = (bs[:, j:j + 1] for j in range(7))
                p.op("dve", lambda e: e.memset(lo, 0.0), writes=[bbs])
                p.op("dve", lambda e: e.memset(hi, 1.0), writes=[bbs])
                for it in range(N_BISECT):
                    p.op("dve", lambda e: e.tensor_scalar(hh, hi, 0.5, None, ALU.mult), reads=[bbs], writes=[bbs])
                    p.op("dve", lambda e: e.scalar_tensor_tensor(mid, lo, 0.5, hh, ALU.mult, ALU.add), reads=[bbs], writes=[bbs])
                    p.op("dve", lambda e: e.tensor_scalar(m, a, mid, None, ALU.is_ge, ALU.add, accum_out=cnt),
                         reads=[baffT, bbs], writes=[bmask, bbs])
                    p.op("dve", lambda e: e.tensor_scalar(ge, cnt, cap - 0.5, None, ALU.is_ge), reads=[bbs], writes=[bbs])
                    p.op("dve", lambda e: e.tensor_sub(d1, mid, lo), reads=[bbs], writes=[bbs])
                    p.op("dve", lambda e: e.scalar_tensor_tensor(lo, d1, ge, lo, ALU.mult, ALU.add), reads=[bbs], writes=[bbs])
                    p.op("dve", lambda e: e.tensor_sub(d1, hi, mid), reads=[bbs], writes=[bbs])
                    p.op("dve", lambda e: e.scalar_tensor_tensor(hi, d1, ge, mid, ALU.mult, ALU.add), reads=[bbs], writes=[bbs])
                p.op("dve", lambda e: e.tensor_scalar(diag[:], K["ident"][0:16, 0:16], lo, None, ALU.mult),
                     reads=[bbs, K["bident"]], writes=[bdiag])
                p.op("pe", lambda e: e.matmul(pmisc[:, 256:256 + NE], ones16[:], diag[:], start=True, stop=True),
                     reads=[bones, bdiag], writes=[bpm])
                p.op("act", lambda e: e.copy(thr_bc[kind][:], pmisc[:, 256:256 + NE]), reads=[bpm], writes=[bthr[kind]])
            p.barrier()

        wg_sb = c.sb([128, 8, D], es0); bwg = Buf()
        wu_sb = c.sb([128, 8, D], es0); bwu = Buf()
        wd_sb = c.sb([128, 8, D], es0); bwd = Buf()
        hT = c.sb([128, 8, 512], es0); bhT = [Buf() for _ in range(4)]
        hid = c.sb([128, 8, 512], es0); bhid = [Buf() for _ in range(8)]
        acc = [c.sb([128, D], es0) for _ in range(4)]; bacc = [Buf() for _ in range(4)]
        gate = c.sb([128, 4, NE], es0); bgate = [Buf() for _ in range(4)]
        aff2 = c.sb([128, NE], es0); baff2 = Buf()
        msk2 = c.sb([128, NE], es0); bmsk2 = Buf()
        sg = [c.sb([128, 512], es0) for _ in range(2)]; bsg = [Buf() for _ in range(2)]
        pg = [c.ps([128, 512], es0) for _ in range(2)]; bpg = [PBuf() for _ in range(2)]
        pu = [c.ps([128, 512], es0) for _ in range(2)]; bpu = [PBuf() for _ in range(2)]
        py = [c.ps([128, 512], es0) for _ in range(2)]; bpy = [PBuf() for _ in range(2)]
        passes = [list(range(4 * q, 4 * q + 4)) for q in range(8)] + [[32, 33]]
        for tiles in passes:
            nt = len(tiles)
            N = nt * 128
            for k, t in enumerate(tiles):
                kind = tile_kind(t)
                G, S, _ = mods[kind]
                i = k % 2
                p.dma("pool", xt[i][:], X[t * 128:(t + 1) * 128, :], reads=[bX[t]], writes=[bxt[i]])
                norm_mod_T(c, K, xt[i][:], bxt[i], G, S, hT[:, :, k * 128:(k + 1) * 128], bhT[k])
                router(hT[:, :, k * 128:(k + 1) * 128], bhT[k], aff2[:], baff2)
                p.op("dve", lambda e: e.tensor_tensor(msk2[:], aff2[:], thr_bc[kind][:], ALU.is_ge),
                     reads=[baff2, bthr[kind]], writes=[bmsk2])
                p.op("dve", lambda e: e.tensor_mul(gate[:, k, :], msk2[:], aff2[:]), reads=[bmsk2, baff2], writes=[bgate[k]])
            for ex_i in range(NEH):
                p.dma("sp", wg_sb[:], wg[ex_i * D:(ex_i + 1) * D, :].rearrange("(c p) f -> p c f", p=128), reads=[bw], writes=[bwg])
                p.dma("pool", wu_sb[:], wu[ex_i * D:(ex_i + 1) * D, :].rearrange("(c p) f -> p c f", p=128), reads=[bw], writes=[bwu])
                p.dma("sp", wd_sb[:], wd[ex_i * D:(ex_i + 1) * D, :].rearrange("(c p) f -> p c f", p=128), reads=[bw], writes=[bwd])
                for fc in range(8):
                    j = fc % 2
                    for ch in range(8):
                        p.op("pe", lambda e: e.matmul(pg[j][:, 0:N], wg_sb[:, ch, fc * 128:(fc + 1) * 128], hT[:, ch, 0:N],
                                                      start=(ch == 0), stop=(ch == 7)),
                             reads=[bwg] + bhT[:nt], writes=[bpg[j]])
                    for ch in range(8):
                        p.op("pe", lambda e: e.matmul(pu[j][:, 0:N], wu_sb[:, ch, fc * 128:(fc + 1) * 128], hT[:, ch, 0:N],
                                                      start=(ch == 0), stop=(ch == 7)),
                             reads=[bwu] + bhT[:nt], writes=[bpu[j]])
                    p.op("act", lambda e: e.activation(sg[j][:, 0:N], pg[j][:, 0:N], AF.Silu), reads=[bpg[j]], writes=[bsg[j]])
                    p.op("dve", lambda e: e.tensor_mul(hid[:, fc, 0:N], sg[j][:, 0:N], pu[j][:, 0:N]),
                         reads=[bsg[j], bpu[j]], writes=[bhid[fc]])
                for k in range(nt):
                    for half in range(2):
                        for fc in range(8):
                            p.op("pe", lambda e: e.matmul(py[half][:], hid[:, fc, k * 128:(k + 1) * 128],
                                                          wd_sb[:, fc, half * 512:(half + 1) * 512],
                                                          start=(fc == 0), stop=(fc == 7)),
                                 reads=[bwd] + bhid, writes=[bpy[half]])
                        a_ap = acc[k][:, half * 512:(half + 1) * 512]
                        if ex_i == 0:
                            p.op("dve", lambda e: e.tensor_scalar(a_ap, py[half][:], gate[:, k, ex_i:ex_i + 1], None, ALU.mult),
                                 reads=[bpy[half], bgate[k]], writes=[bacc[k]])
                        else:
                            p.op("dve", lambda e: e.scalar_tensor_tensor(a_ap, py[half][:], gate[:, k, ex_i:ex_i + 1], a_ap, ALU.mult, ALU.add),
                                 reads=[bpy[half], bgate[k], bacc[k]], writes=[bacc[k]])
            for k, t in enumerate(tiles):
                p.dma("pool", P[t * 128:(t + 1) * 128, :], acc[k][:], reads=[bacc[k]], writes=[bP[t]])
        p.barrier()


def declare_moe_weights(c, li):
    wr = c.din(f"wr{li}", [D, NE])
    wg, b1 = gather_w(c, f"wg{li}", NEH * D, D)
    wu, b2 = gather_w(c, f"wu{li}", NEH * D, D)
    wd, b3 = gather_w(c, f"wd{li}", NEH * D, D)
    bw = Buf()
    for b in (b1, b2, b3):
        c.p._wait("sp", b.w)
    c.p.op("pool", lambda e: e.memset(c_dummy(c)[:], 0.0), reads=[b1, b2, b3], writes=[bw])
    return wr, wg, wu, wd, bw


def c_dummy(c):
    if not hasattr(c, "_dummy"):
        c._dummy = c.sb([128, 1])
    return c._dummy


NVEC = 17


def bc_tile(c, es, row, q="pool"):
    t = c.sb([128, D], es); b = Buf()
    c.p.dma(q, t[:], row.partition_broadcast(128), reads=list(getattr(c, "vec_reads", [])), writes=[b])
    return (t, b)


def mods_from_vec(c, es, vec, base, grow):
    p = c.p
    g = bc_tile(c, es, vec[grow:grow + 1, :])
    mods = {}
    for k, kind in enumerate("xc"):
        Sh = bc_tile(c, es, vec[6 * k + base:6 * k + base + 1, :])
        Sc = bc_tile(c, es, vec[6 * k + base + 1:6 * k + base + 2, :])
        p.op("dve", lambda e: e.scalar_tensor_tensor(Sc[0][:], Sc[0][:], 1.0, g[0][:], ALU.add, ALU.mult),
             reads=[g[1], Sc[1]], writes=[Sc[1]])
        mods[kind] = (Sc, Sh, None)
    return mods


def emit_residual_in(c, K, xprev, pa, pb, vec, X, bX, xo, ssa=None, ssb=None):
    p = c.p
    es = ExitStack()
    bo = []
    with es:
        gts = {"x": bc_tile(c, es, vec[15:16, :]), "c": bc_tile(c, es, vec[16:17, :])}
        xt = [c.sb([128, D], es) for _ in range(2)]; bxt = [Buf(), Buf()]
        ga = [c.sb([128, D], es) for _ in range(2)]; bga = [Buf(), Buf()]
        gb = [c.sb([128, D], es) for _ in range(2)]; bgb = [Buf(), Buf()]
        sst = c.sb([128, 8], es); bsst = Buf()
        for t in range(NT):
            i = t % 2
            Ga = gts[tile_kind(t)]
            sl = slice(t * 128, (t + 1) * 128)
            rr = list(getattr(c, "res_reads", []))
            p.dma("sp", xt[i][:], xprev[sl, :], reads=rr, writes=[bxt[i]])
            tp = getattr(c, "res_tile_aps", None)
            pa_t, pb_t = tp(t) if tp is not None else (pa[sl, :], pb[sl, :])
            p.dma("sp", ga[i][:], pa_t, reads=rr, writes=[bga[i]])
            p.dma("sp", gb[i][:], pb_t, reads=rr, writes=[bgb[i]])
            p.op("pool", lambda e: e.tensor_add(ga[i][:], ga[i][:], gb[i][:]), reads=[bga[i], bgb[i]], writes=[bga[i]])
            if ssa is not None:
                p.dma("sp", sst[:, 0:1], ssa[sl, :], reads=rr, writes=[bsst])
                p.dma("sp", sst[:, 1:2], ssb[sl, :], reads=rr, writes=[bsst])
                p.op("dve", lambda e: e.tensor_add(sst[:, 2:3], sst[:, 0:1], sst[:, 1:2]), reads=[bsst], writes=[bsst])
                p.op("act", lambda e: e.activation(sst[:, 3:4], sst[:, 2:3], AF.Sqrt, bias=K["eps"][:, 0:1], scale=1.0 / 2048), reads=[bsst], writes=[bsst])
                p.op("dve", lambda e: e.reciprocal(sst[:, 4:5], sst[:, 3:4]), reads=[bsst], writes=[bsst])
                p.op("dve", lambda e: e.scalar_tensor_tensor(ga[i][:], ga[i][:], sst[:, 4:5], Ga[0][:], ALU.mult, ALU.mult),
                     reads=[bga[i], Ga[1], bsst], writes=[bga[i]])
            else:
                p.op("dve", lambda e: e.tensor_mul(ga[i][:], ga[i][:], Ga[0][:]), reads=[bga[i], Ga[1]], writes=[bga[i]])
            p.op("dve", lambda e: e.tensor_add(xt[i][:], xt[i][:], ga[i][:]), reads=[bga[i], bxt[i]], writes=[bxt[i]])
            p.dma("pool", X[sl, :], xt[i][:], reads=[bxt[i]], writes=[bX[t]])
            if xo is not None:
                b = Buf(); bo.append(b)
                p.dma("pool", xo[sl, :], xt[i][:], reads=[bxt[i]], writes=[b])
        p.barrier()
    return bo


MLA_SCALE = 96.0 ** -0.5


def emit_mla(c, K, X, bX, mods, w_in, w_uqn, w_uqr, w_uqs, w_ukn, w_ukv, w_out, gq, gkv, cosT, sinT, cos_tm, sin_tm, P, bP):
    p = c.p
    nc = c.nc
    QnT = dram(c, [4, 128, NTOK]); KnT = dram(c, [4, 128, NTOK]); QrT = dram(c, [4, 64, NTOK])
    KrT2 = dram(c, [64, NTOK]); V = dram(c, [4, NT, 128, 130]); OT = dram(c, [4, 128, NTOK])
    bQ = [Buf() for _ in range(9)]; bKV = [Buf() for _ in range(9)]; bOT = [Buf() for _ in range(4)]
    groups = [list(range(4 * q, 4 * q + 4)) for q in range(8)] + [[32, 33]]
    esA = ExitStack()
    with esA:
        win = c.sb([128, 8, 1088], esA); bwin = Buf()
        p.dma("sp", win[:], w_in.rearrange("(c p) f -> p c f", p=128), writes=[bwin])
        wqn = c.sb([128, 6, 512], esA); wqr = c.sb([128, 6, 256], esA); wqs = c.sb([128, 6, 256], esA)
        wkn = c.sb([128, 2, 512], esA); wkv = c.sb([128, 2, 512], esA); bwq = Buf()
        for dst, src in ((wqn, w_uqn), (wqr, w_uqr), (wqs, w_uqs), (wkn, w_ukn), (wkv, w_ukv)):
            p.dma("sp", dst[:], src.rearrange("(c p) f -> p c f", p=128), writes=[bwq])
        gqt = c.sb([128, 768], esA); gkt = c.sb([128, 256], esA); bg = Buf()
        p.dma("pool", gqt[:], gq.partition_broadcast(128), writes=[bg])
        p.dma("pool", gkt[:], gkv.partition_broadcast(128), writes=[bg])
        cT = c.sb([64, 512], esA); sT = c.sb([64, 512], esA); bcs = Buf()
        xt = [c.sb([128, D], esA) for _ in range(2)]; bxt = [Buf(), Buf()]
        hT = c.sb([128, 8, 128], esA); bhT = Buf()
        pp = [c.ps([128, 512], esA) for _ in range(3)]; bpp = [PBuf() for _ in range(3)]
        pj = c.sb([128, 1088], esA); bpj = Buf()
        cqn = c.sb([128, 768], esA); ckn = c.sb([128, 256], esA); bcn = Buf()
        kr2 = c.sb([128, 64], esA); bkr2 = Buf()
        cst = c.sb([128, 32], esA); snt = c.sb([128, 32], esA); bcst = Buf()
        tmp32 = c.sb([128, 32], esA); btmp = Buf()
        cqT = c.sb([128, 6, 512], esA); bcqT = [Buf() for _ in range(4)]
        ckT = c.sb([128, 2, 512], esA); bckT = [Buf() for _ in range(4)]
        krT = c.sb([64, 512], esA); bkrT = [Buf() for _ in range(4)]
        vt = c.sb([128, 4, 130], esA); bvt = Buf()
        p.op("dve", lambda e: e.memset(vt[:], 1.0), writes=[bvt])
        outA = [c.sb([128, 512], esA) for _ in range(2)]; boutA = [Buf(), Buf()]
        outB = [c.sb([64, 512], esA) for _ in range(2)]; boutB = [Buf(), Buf()]
        tmpB = c.sb([64, 512], esA); btmpB = Buf()
        s4 = c.sb([128, 8], esA); bs4 = Buf()
        nA = 0
        for gi, tiles in enumerate(groups):
            N = len(tiles) * 128
            t0 = tiles[0] * 128
            for k, t in enumerate(tiles):
                kind = tile_kind(t)
                G, S, _ = mods[kind]
                i = t % 2
                ks = slice(k * 128, (k + 1) * 128)
                p.dma("pool", xt[i][:], X[t * 128:(t + 1) * 128, :], reads=[bX[t]], writes=[bxt[i]])
                norm_mod_T(c, K, xt[i][:], bxt[i], G, S, hT, bhT)
                for gidx, (n0, nn) in enumerate(((0, 512), (512, 512), (1024, 64))):
                    for ch in range(8):
                        p.op("pe", lambda e: e.matmul(pp[gidx][:, 0:nn], hT[:, ch, :], win[:, ch, n0:n0 + nn], start=(ch == 0), stop=(ch == 7)),
                             reads=[bhT, bwin], writes=[bpp[gidx]])
                    p.op("act" if gidx != 1 else "dve",
                         (lambda e: e.copy(pj[:, n0:n0 + nn], pp[gidx][:, 0:nn])) if gidx != 1 else (lambda e: e.tensor_copy(pj[:, n0:n0 + nn], pp[gidx][:, 0:nn])),
                         reads=[bpp[gidx]], writes=[bpj])
                for (lo_, n_, gt_, dst_, col) in ((0, 768, gqt, cqn, 0), (768, 256, gkt, ckn, 4)):
                    p.op("act", lambda e: e.activation(K["junk"][:, 0:n_], pj[:, lo_:lo_ + n_], AF.Square, accum_out=s4[:, col:col + 1]),
                         reads=[bpj], writes=[K["bjunk"], bs4])
                    p.op("act", lambda e: e.activation(s4[:, col + 1:col + 2], s4[:, col:col + 1], AF.Sqrt, bias=K["eps"][:, 0:1], scale=1.0 / n_),
                         reads=[bs4], writes=[bs4])
                    p.op("dve", lambda e: e.reciprocal(s4[:, col + 2:col + 3], s4[:, col + 1:col + 2]), reads=[bs4], writes=[bs4])
                    p.op("dve", lambda e: e.scalar_tensor_tensor(dst_[:], pj[:, lo_:lo_ + n_], s4[:, col + 2:col + 3], gt_[:], ALU.mult, ALU.mult),
                         reads=[bpj, bs4, bg], writes=[bcn])
                if kind == "x":
                    p.dma("pool", cst[:], cos_tm[t * 128:(t + 1) * 128, :], writes=[bcst])
                    p.dma("pool", snt[:], sin_tm[t * 128:(t + 1) * 128, :], writes=[bcst])
                    p.op("dve", lambda e: e.tensor_mul(kr2[:, 0:32], pj[:, 1024:1056], cst[:]), reads=[bpj, bcst], writes=[bkr2])
                    p.op("dve", lambda e: e.tensor_mul(tmp32[:], pj[:, 1056:1088], snt[:]), reads=[bpj, bcst], writes=[btmp])
                    p.op("dve", lambda e: e.tensor_add(kr2[:, 0:32], kr2[:, 0:32], tmp32[:]), reads=[bkr2, btmp], writes=[bkr2])
                else:
                    p.op("dve", lambda e: e.tensor_copy(kr2[:, 0:32], pj[:, 1024:1056]), reads=[bpj], writes=[bkr2])
                p.op("dve", lambda e: e.tensor_copy(kr2[:, 32:64], kr2[:, 0:32]), reads=[bkr2], writes=[bkr2])
                for blk, (src_, nchunk, dstT, bdst) in enumerate(((cqn, 6, cqT, bcqT), (ckn, 2, ckT, bckT))):
                    for c0 in range(0, nchunk, 4):
                        cn = min(4, nchunk - c0)
                        for cc in range(cn):
                            p.op("pe", lambda e: e.transpose(K["ptr"][:, cc * 128:(cc + 1) * 128], src_[:, (c0 + cc) * 128:(c0 + cc + 1) * 128], K["ident"][:]),
                                 reads=[bcn, K["bident"]], writes=[K["bptr"]])
                        p.op("act", lambda e: e.copy(dstT[:, c0:c0 + cn, ks], K["ptr"][:, 0:cn * 128].rearrange("p (c t) -> p c t", c=cn)),
                             reads=[K["bptr"]], writes=[bdst[k]])
                p.op("pe", lambda e: e.transpose(K["ptr"][0:64, 0:128], kr2[:], K["ident"][:]), reads=[bkr2, K["bident"]], writes=[K["bptr"]])
                p.op("act", lambda e: e.copy(krT[:, ks], K["ptr"][0:64, 0:128]), reads=[K["bptr"]], writes=[bkrT[k]])
                for ch in range(2):
                    p.op("pe", lambda e: e.matmul(pp[0][:, 0:512], ckT[:, ch, ks], wkv[:, ch, :], start=(ch == 0), stop=(ch == 1)),
                         reads=[bckT[k], bwq], writes=[bpp[0]])
                p.op("dve", lambda e: e.tensor_copy(vt[:].rearrange("p a (h d) -> p a h d", h=2)[:, :, :, 0:64],
                                                    pp[0][:, 0:512].rearrange("p (a h d) -> p a h d", a=4, h=2)),
                     reads=[bpp[0]], writes=[bvt])
                p.dma("sp", V[:, t].rearrange("a p f -> p a f"), vt[:], reads=[bvt], writes=[bKV[gi]])
            nt = len(tiles)
            p.dma("sp", KrT2[:, t0:t0 + N], krT[:, 0:N], reads=bkrT[:nt], writes=[bKV[gi]])
            if tiles[0] < 32:
                for hh in range(2):
                    p.dma("pool", cT[hh * 32:(hh + 1) * 32, 0:N], cosT[:, t0:t0 + N], writes=[bcs])
                    p.dma("pool", sT[hh * 32:(hh + 1) * 32, 0:N], sinT[:, t0:t0 + N], writes=[bcs])
            for pr in range(4):
                j = nA % 2
                nA += 1
                for ch in range(2):
                    p.op("pe", lambda e: e.matmul(pp[0][:, 0:N], wkn[:, ch, pr * 128:(pr + 1) * 128], ckT[:, ch, 0:N], start=(ch == 0), stop=(ch == 1)),
                         reads=bckT[:nt] + [bwq], writes=[bpp[0]])
                p.op("act", lambda e: e.copy(outA[j][:, 0:N], pp[0][:, 0:N]), reads=[bpp[0]], writes=[boutA[j]])
                p.dma("sp", KnT[pr][:, t0:t0 + N], outA[j][:, 0:N], reads=[boutA[j]], writes=[bKV[gi]])
                j = nA % 2
                nA += 1
                for ch in range(6):
                    p.op("pe", lambda e: e.matmul(pp[1][:, 0:N], wqn[:, ch, pr * 128:(pr + 1) * 128], cqT[:, ch, 0:N], start=(ch == 0), stop=(ch == 5)),
                         reads=bcqT[:nt] + [bwq], writes=[bpp[1]])
                p.op("dve", lambda e: e.tensor_copy(outA[j][:, 0:N], pp[1][:, 0:N]), reads=[bpp[1]], writes=[boutA[j]])
                p.dma("sp", QnT[pr][:, t0:t0 + N], outA[j][:, 0:N], reads=[boutA[j]], writes=[bQ[gi]])
                for ch in range(6):
                    p.op("pe", lambda e: e.matmul(pp[2][0:64, 0:N], wqr[:, ch, pr * 64:(pr + 1) * 64], cqT[:, ch, 0:N], start=(ch == 0), stop=(ch == 5)),
                         reads=bcqT[:nt] + [bwq], writes=[bpp[2]])
                jb = pr % 2
                if tiles[0] < 32:
                    p.op("dve", lambda e: e.tensor_mul(outB[jb][:, 0:N], pp[2][0:64, 0:N], cT[:, 0:N]), reads=[bpp[2], bcs], writes=[boutB[jb]])
                    for ch in range(6):
                        p.op("pe", lambda e: e.matmul(pp[2][0:64, 0:N], wqs[:, ch, pr * 64:(pr + 1) * 64], cqT[:, ch, 0:N], start=(ch == 0), stop=(ch == 5)),
                             reads=bcqT[:nt] + [bwq], writes=[bpp[2]])
                    p.op("dve", lambda e: e.tensor_mul(tmpB[:, 0:N], pp[2][0:64, 0:N], sT[:, 0:N]), reads=[bpp[2], bcs], writes=[btmpB])
                    p.op("pool", lambda e: e.tensor_add(outB[jb][:, 0:N], outB[jb][:, 0:N], tmpB[:, 0:N]), reads=[boutB[jb], btmpB], writes=[boutB[jb]])
                else:
                    p.op("dve", lambda e: e.tensor_copy(outB[jb][:, 0:N], pp[2][0:64, 0:N]), reads=[bpp[2]], writes=[boutB[jb]])
                p.dma("sp", QrT[pr][:, t0:t0 + N], outB[jb][:, 0:N], reads=[boutB[jb]], writes=[bQ[gi]])
        p.barrier()
    esB = ExitStack()
    with esB:
        qn = c.sb([128, NTOK], esB); kn = c.sb([128, NTOK], esB); qr = c.sb([64, NTOK], esB); kr = c.sb([64, NTOK], esB)
        vv = c.sb([128, NT, 130], esB)
        bqn, bkn, bqr, bkr, bvv = Buf(), Buf(), Buf(), Buf(), Buf()
        ones = c.sb([128, 64], esB); bon = Buf()
        p.op("dve", lambda e: e.memset(ones[:], 1.0), writes=[bon])
        pS = [c.ps([128, 512], esB) for _ in range(2)]; bpS = [PBuf(), PBuf()]
        pO = [c.ps([128, 512], esB) for _ in range(2)]; bpO = [PBuf(), PBuf()]
        pB = c.ps([128, 512], esB); bpB = PBuf()
        eS = [c.sb([128, 512], esB) for _ in range(2)]; beS = [Buf(), Buf()]
        rinv = c.sb([128, 512], esB); brinv = Buf()
        osb = [c.sb([64, 512], esB) for _ in range(2)]; bosb = [Buf(), Buf()]
        p.dma("sp", kr[:], KrT2, reads=bKV, writes=[bkr])
        nS = 0
        nO = 0
        for pr in range(4):
            p.dma("sp", qn[:], QnT[pr], reads=bQ, writes=[bqn])
            p.dma("sp", kn[:], KnT[pr], reads=bKV, writes=[bkn])
            p.dma("sp", qr[:], QrT[pr], reads=bQ, writes=[bqr])
            p.dma("sp", vv[:], V[pr].rearrange("t p f -> p t f"), reads=bKV, writes=[bvv])
            for hh in range(2):
                nb = hh * 64
                rb = hh * 32
                for gi, tiles in enumerate(groups):
                    N = len(tiles) * 128
                    q0 = tiles[0] * 128
                    keys = list(range(NT)) if tiles[0] < 32 else [32, 33]
                    jo = nO % 2
                    nO += 1
                    for ki, kt in enumerate(keys):
                        js = nS % 2
                        nS += 1
                        kk = slice(kt * 128, (kt + 1) * 128)
                        p.op("pe", lambda e: e.matmul(pS[js][:, 0:N], kn[nb:nb + 64, kk], qn[nb:nb + 64, q0:q0 + N], start=True, stop=False),
                             reads=[bkn, bqn], writes=[bpS[js]])
                        p.op("pe", lambda e: e.matmul(pS[js][:, 0:N], kr[rb:rb + 32, kk], qr[rb:rb + 32, q0:q0 + N], start=False, stop=True),
                             reads=[bkr, bqr], writes=[bpS[js]])
                        p.op("act", lambda e: e.activation(eS[js][:, 0:N], pS[js][:, 0:N], AF.Exp, scale=MLA_SCALE),
                             reads=[bpS[js]], writes=[beS[js]])
                        p.op("pe", lambda e: e.matmul(pO[jo][0:65, 0:N], vv[:, kt, hh * 65:(hh + 1) * 65], eS[js][:, 0:N],
                                                      start=(ki == 0), stop=(ki == len(keys) - 1)),
                             reads=[bvv, beS[js]], writes=[bpO[jo]])
                    p.op("dve", lambda e: e.reciprocal(rinv[64:65, 0:N], pO[jo][64:65, 0:N]), reads=[bpO[jo]], writes=[brinv])
                    p.op("pe", lambda e: e.matmul(pB[0:64, 0:N], ones[64:65, :], rinv[64:65, 0:N], start=True, stop=True),
                         reads=[bon, brinv], writes=[bpB])
                    p.op("act", lambda e: e.copy(osb[jo][:, 0:N], pB[0:64, 0:N]), reads=[bpB], writes=[bosb[jo]])
                    p.op("dve", lambda e: e.tensor_mul(osb[jo][:, 0:N], osb[jo][:, 0:N], pO[jo][0:64, 0:N]), reads=[bosb[jo], bpO[jo]], writes=[bosb[jo]])
                    p.dma("pool", OT[pr][hh * 64:(hh + 1) * 64, q0:q0 + N], osb[jo][:, 0:N], reads=[bosb[jo]], writes=[bOT[pr]])
        p.barrier()
    esC = ExitStack()
    with esC:
        wo = c.sb([128, 4, D], esC); bwo = Buf()
        p.dma("sp", wo[:], w_out.rearrange("(c p) f -> p c f", p=128), writes=[bwo])
        ot = c.sb([128, 4, NTOK], esC); bot = Buf()
        for pr in range(4):
            p.dma("sp", ot[:, pr, :], OT[pr], reads=[bOT[pr]], writes=[bot])
        pc = [c.ps([128, 512], esC) for _ in range(2)]; bpc = [PBuf(), PBuf()]
        ob = [c.sb([128, D], esC) for _ in range(2)]; bob = [Buf(), Buf()]
        for t in range(NT):
            i = t % 2
            for half in range(2):
                for pr in range(4):
                    p.op("pe", lambda e: e.matmul(pc[half][:], ot[:, pr, t * 128:(t + 1) * 128], wo[:, pr, half * 512:(half + 1) * 512],
                                                  start=(pr == 0), stop=(pr == 3)), reads=[bot, bwo], writes=[bpc[half]])
                p.op("act" if half else "dve",
                     (lambda e: e.copy(ob[i][:, half * 512:(half + 1) * 512], pc[half][:])) if half else
                     (lambda e: e.tensor_copy(ob[i][:, half * 512:(half + 1) * 512], pc[half][:])),
                     reads=[bpc[half]], writes=[bob[i]])
            p.dma("pool", P[t * 128:(t + 1) * 128, :], ob[i][:], reads=[bob[i]], writes=[bP[t]])
        p.barrier()


def rope_tables():
    t = np.arange(4096)
    rows = (t // 64).astype(np.float32)
    cols = (t % 64).astype(np.float32)
    inv = (10000.0 ** (-np.arange(8, dtype=np.float32) * 2.0 / 16)).astype(np.float32)
    ar = rows[:, None] * inv[None, :]
    ac = cols[:, None] * inv[None, :]
    cos = np.concatenate([np.cos(ar), np.cos(ar), np.cos(ac), np.cos(ac)], 1).astype(np.float32)
    sin = np.concatenate([-np.sin(ar), np.sin(ar), -np.sin(ac), np.sin(ac)], 1).astype(np.float32)
    return cos, sin, np.ascontiguousarray(cos.T), np.ascontiguousarray(sin.T)


ROPE_SWAP = list(range(8, 16)) + list(range(0, 8)) + list(range(24, 32)) + list(range(16, 24))


def build_stage(kind, stop=None, with_ss=False):
    nc = new_nc()
    es = ExitStack()
    with es:
        c = Ctx(nc, es); p = c.p
        K = common_consts(c, c.din("ident", [128, 128]))
        xprev = c.din("xprev", [NTOK, D]); pa = c.din("pa", [NTOK, D]); pb = c.din("pb", [NTOK, D])
        vec = c.din("vec", [NVEC, D])
        xo = c.dout("xo", [NTOK, D])
        X = dram(c, [NTOK, D]); bX = [Buf() for _ in range(NT)]
        ssa = c.din("ssa", [NTOK, 1]) if with_ss else None
        ssb = c.din("ssb", [NTOK, 1]) if with_ss else None
        bo = emit_residual_in(c, K, xprev, pa, pb, vec, X, bX, xo, ssa, ssb)
        if kind == "final":
            out = c.dout("p", [NTOK, D])
            es1 = ExitStack()
            with es1:
                fg = bc_tile(c, es1, vec[14:15, :])
                xt = [c.sb([128, D], es1) for _ in range(2)]; bxt = [Buf(), Buf()]
                ot = [c.sb([128, D], es1) for _ in range(2)]; bot = [Buf(), Buf()]
                for t in range(32):
                    i = t % 2
                    p.dma("sp", xt[i][:], X[t * 128:(t + 1) * 128, :], reads=[bX[t]], writes=[bxt[i]])
                    norm_mod_T(c, K, xt[i][:], bxt[i], fg, None, None, None)
                    p.op("act", lambda e: e.copy(ot[i][:], K["h"][:]), reads=[K["bh"]], writes=[bot[i]])
                    b = Buf(); bo.append(b)
                    p.dma("pool", out[t * 128:(t + 1) * 128, :], ot[i][:], reads=[bot[i]], writes=[b])
            p.finish(bo)
            return nc
        P = c.dout("p", [NTOK, D]); bP = [Buf() for _ in range(NT)]
        es1 = ExitStack()
        with es1:
            if kind == "moe":
                mods = mods_from_vec(c, es1, vec, 3, 13)
                wr = c.din("wr", [D, NE]); wg = c.din("wg", [NEH * D, D]); wu = c.din("wu", [NEH * D, D]); wd = c.din("wd", [NEH * D, D])
                emit_moe(c, K, X, bX, mods, wr, wg, wu, wd, Buf(), P, bP)
            elif kind == "mla":
                mods = mods_from_vec(c, es1, vec, 0, 12)
                a = dict(w_in=c.din("w_in", [D, 1088]), w_uqn=c.din("w_uqn", [768, 512]), w_uqr=c.din("w_uqr", [768, 256]),
                         w_uqs=c.din("w_uqs", [768, 256]), w_ukn=c.din("w_ukn", [256, 512]), w_ukv=c.din("w_ukv", [256, 512]),
                         w_out=c.din("w_out", [512, D]), gq=c.din("gq", [1, 768]), gkv=c.din("gkv", [1, 256]),
                         cosT=c.din("cosT", [32, 4096]), sinT=c.din("sinT", [32, 4096]),
                         cos_tm=c.din("cos_tm", [4096, 32]), sin_tm=c.din("sin_tm", [4096, 32]))
                emit_mla(c, K, X, bX, mods, P=P, bP=bP, **a)
            elif kind == "ssd":
                mods = mods_from_vec(c, es1, vec, 0, 12)
                a = dict(w_cv=c.din("w_cv", [D, 2048]), convp=c.din("convp", [16, 128, 4]), w_z=c.din("w_z", [D, D]), w_dt=c.din("w_dt", [D, 32]),
                         dtb=c.din("dtb", [1, 32]), alog=c.din("alog", [1, 32]), dvec=c.din("dvec", [1, D]), ng=c.din("ng", [1, D]),
                         w_out=c.din("w_out", [D, D]), triF=c.din("triF", [128, 128]), triB=c.din("triB", [128, 128]))
                SS = c.dout("ss", [NTOK, 1]); bSS = [Buf() for _ in range(NT)]
                emit_ssd(c, K, X, bX, mods, P=P, bP=bP, SS=SS, bSS=bSS, **a)
                bo = bo + bSS
            elif kind == "gdn":
                mods = mods_from_vec(c, es1, vec, 0, 12)
                a = dict(w_cv=c.din("w_cv", [D, 1536]), convp=c.din("convp", [12, 128, 4]), w_z=c.din("w_z", [D, 512]), w_bg=c.din("w_bg", [D, 16]),
                         dtb=c.din("dtb", [1, 8]), alog=c.din("alog", [1, 8]), ng=c.din("ng", [1, 128]), w_out=c.din("w_out", [512, D]),
                         masks=c.din("masks", [2, 7, 128, 128]))
                emit_gdn(c, K, X, bX, mods, P=P, bP=bP, stop=stop, **a)
            else:
                raise ValueError(kind)
        p.finish(bo + bP)
        print(kind, "ninst", p.ninst, "nsem", p.nsem)
    return nc


def mla_weights(z_w_in, z_uq, z_ukv, z_out, gq, gkv, h):
    heads = range(8 * h, 8 * h + 8)
    w_in = np.concatenate([z_w_in, z_w_in[:, 1024:1056][:, ROPE_SWAP]], 1)
    uqn = np.concatenate([z_uq[:, hd * 96:hd * 96 + 64] for hd in heads], 1)
    uqr = np.concatenate([z_uq[:, hd * 96 + 64:hd * 96 + 96] for hd in heads], 1)
    uqs = np.concatenate([z_uq[:, hd * 96 + 64:hd * 96 + 96][:, ROPE_SWAP] for hd in heads], 1)
    ukn = np.concatenate([z_ukv[:, hd * 128:hd * 128 + 64] for hd in heads], 1)
    ukv = np.concatenate([z_ukv[:, hd * 128 + 64:hd * 128 + 128] for hd in heads], 1)
    cos, sin, cosT, sinT = rope_tables()
    f = np.ascontiguousarray
    return dict(w_in=f(w_in), w_uqn=f(uqn), w_uqr=f(uqr), w_uqs=f(uqs), w_ukn=f(ukn), w_ukv=f(ukv),
                w_out=f(z_out[8 * h * 64:(8 * h + 8) * 64]), gq=f(gq[None, :]), gkv=f(gkv[None, :]),
                cosT=cosT, sinT=sinT, cos_tm=cos, sin_tm=sin)


def make_vec(mx_b, mc, n1g, n2g, fg, pgx, pgc):
    return np.ascontiguousarray(np.concatenate([mx_b, mc, n1g[None], n2g[None], fg[None], pgx[None], pgc[None]], 0).astype(np.float32))


def softplus_tile(c, out_ap, in_ap, tmp_a, tmp_b, rb, wb, bt):
    p = c.p
    p.op("act", lambda e: e.activation(tmp_a, in_ap, AF.Abs), reads=rb, writes=[bt])
    p.op("act", lambda e: e.activation(tmp_a, tmp_a, AF.Exp, scale=-1.0), reads=[bt], writes=[bt])
    p.op("act", lambda e: e.activation(tmp_b, tmp_a, AF.Ln, bias=1.0), reads=[bt], writes=[bt])
    p.op("dve", lambda e: e.scalar_tensor_tensor(out_ap, in_ap, 0.0, tmp_b, ALU.max, ALU.add), reads=rb + [bt], writes=wb)


def emit_ssd(c, K, X, bX, mods, w_cv, convp, w_z, w_dt, dtb, alog, dvec, ng, w_out, triF, triB, P, bP, SS, bSS):
    p = c.p
    HT = dram(c, [NT, 128, 8, 128]); bHT = [Buf() for _ in range(NT)]
    FT = dram(c, [16, 128, NTOK]); bFT = [Buf() for _ in range(16)]
    ZS = dram(c, [NTOK, D]); DT = dram(c, [NTOK, 32]); XTM = dram(c, [NTOK, D]); BTM = dram(c, [NTOK, 512]); YF = dram(c, [NTOK, D])
    bZS = [Buf() for _ in range(NT)]; bXB = [Buf() for _ in range(NT)]; bYF = [Buf() for _ in range(NT)]
    groups = [list(range(4 * q, 4 * q + 4)) for q in range(8)] + [[32, 33]]
    es = ExitStack()
    with es:
        wz = c.sb([128, 8, D], es); wdt = c.sb([128, 8, 32], es); bwz = Buf()
        p.dma("sp", wz[:], w_z.rearrange("(c p) f -> p c f", p=128), writes=[bwz])
        p.dma("sp", wdt[:], w_dt.rearrange("(c p) f -> p c f", p=128), writes=[bwz])
        dtb_t = c.sb([128, 32], es); bdb = Buf()
        p.dma("pool", dtb_t[:], dtb.partition_broadcast(128), writes=[bdb])
        xt = [c.sb([128, D], es) for _ in range(2)]; bxt = [Buf(), Buf()]
        hT = [c.sb([128, 8, 128], es) for _ in range(2)]; bhT = [Buf(), Buf()]
        pz = [c.ps([128, 512], es) for _ in range(2)]; bpz = [PBuf(), PBuf()]
        pd = c.ps([128, 512], es); bpd = PBuf()
        zs = [c.sb([128, D], es) for _ in range(2)]; bzs = [Buf(), Buf()]
        dr = c.sb([128, 32], es); ta = c.sb([128, 32], es); tb = c.sb([128, 32], es); do = [c.sb([128, 32], es) for _ in range(2)]
        bdr, btt, bdo = Buf(), Buf(), [Buf(), Buf()]
        for t in range(NT):
            i = t % 2
            G, S, _ = mods[tile_kind(t)]
            p.dma("pool", xt[i][:], X[t * 128:(t + 1) * 128, :], reads=[bX[t]], writes=[bxt[i]])
            norm_mod_T(c, K, xt[i][:], bxt[i], G, S, hT[i], bhT[i])
            p.dma("sp", HT[t], hT[i][:], reads=[bhT[i]], writes=[bHT[t]])
            for half in range(2):
                for ch in range(8):
                    p.op("pe", lambda e: e.matmul(pz[half][:], hT[i][:, ch, :], wz[:, ch, half * 512:(half + 1) * 512], start=(ch == 0), stop=(ch == 7)),
                         reads=[bhT[i], bwz], writes=[bpz[half]])
                p.op("act", lambda e: e.activation(zs[i][:, half * 512:(half + 1) * 512], pz[half][:], AF.Silu), reads=[bpz[half]], writes=[bzs[i]])
            p.dma("sp", ZS[t * 128:(t + 1) * 128, :], zs[i][:], reads=[bzs[i]], writes=[bZS[t]])
            for ch in range(8):
                p.op("pe", lambda e: e.matmul(pd[:, 0:32], hT[i][:, ch, :], wdt[:, ch, :], start=(ch == 0), stop=(ch == 7)),
                     reads=[bhT[i], bwz], writes=[bpd])
            p.op("dve", lambda e: e.tensor_add(dr[:], pd[:, 0:32], dtb_t[:]), reads=[bpd, bdb], writes=[bdr])
            softplus_tile(c, do[i][:], dr[:], ta[:], tb[:], [bdr], [bdo[i]], btt)
            p.dma("sp", DT[t * 128:(t + 1) * 128, :], do[i][:], reads=[bdo[i]], writes=[bZS[t]])
        p.barrier()
    es = ExitStack()
    with es:
        wc = c.sb([128, 8, 512], es); bwc = Buf()
        cp = c.sb([128, 16, 4], es); bcp = Buf()
        p.dma("pool", cp[:], convp.rearrange("k p f -> p k f"), writes=[bcp])
        hg = [c.sb([128, 4, 8, 128], es) for _ in range(2)]; bhg = [Buf(), Buf()]
        raw = [c.sb([128, NTOK], es) for _ in range(4)]; braw = [Buf() for _ in range(4)]
        cv = [c.sb([128, NTOK], es) for _ in range(2)]; bcv = [Buf(), Buf()]
        pr_ = [c.ps([128, 512], es) for _ in range(2)]; bpr = [PBuf(), PBuf()]
        n = 0
        for cb in range(4):
            p.dma("sp", wc[:], w_cv[:, cb * 512:(cb + 1) * 512].rearrange("(c p) f -> p c f", p=128), writes=[bwc])
            for gi, tiles in enumerate(groups):
                i = gi % 2
                nt = len(tiles)
                N = nt * 128
                t0 = tiles[0] * 128
                p.dma("pool", hg[i][:, 0:nt], HT[tiles[0]:tiles[0] + nt].rearrange("t p c k -> p t c k"), reads=[bHT[t] for t in tiles], writes=[bhg[i]])
                for cc in range(4):
                    j = n % 2
                    n += 1
                    for k in range(nt):
                        for ch in range(8):
                            p.op("pe", lambda e: e.matmul(pr_[j][:, k * 128:(k + 1) * 128], wc[:, ch, cc * 128:(cc + 1) * 128], hg[i][:, k, ch, :],
                                                          start=(ch == 0), stop=(ch == 7)), reads=[bwc, bhg[i]], writes=[bpr[j]])
                    p.op("act" if j else "dve",
                         (lambda e: e.copy(raw[cc][:, t0:t0 + N], pr_[j][:, 0:N])) if j else (lambda e: e.tensor_copy(raw[cc][:, t0:t0 + N], pr_[j][:, 0:N])),
                         reads=[bpr[j]], writes=[braw[cc]])
            for cc in range(4):
                k = cb * 4 + cc
                o = cv[cc % 2]; bo_ = bcv[cc % 2]; r = raw[cc]
                p.op("dve", lambda e: e.tensor_scalar(o[:], r[:], cp[:, k, 1:2], None, ALU.mult), reads=[braw[cc], bcp], writes=[bo_])
                for (a0, a1) in ((0, 4096), (4096, NTOK)):
                    p.op("dve", lambda e: e.scalar_tensor_tensor(o[:, a0 + 1:a1], r[:, a0:a1 - 1], cp[:, k, 0:1], o[:, a0 + 1:a1], ALU.mult, ALU.add),
                         reads=[braw[cc], bcp, bo_], writes=[bo_])
                    p.op("dve", lambda e: e.scalar_tensor_tensor(o[:, a0:a1 - 1], r[:, a0 + 1:a1], cp[:, k, 2:3], o[:, a0:a1 - 1], ALU.mult, ALU.add),
                         reads=[braw[cc], bcp, bo_], writes=[bo_])
                p.op("act", lambda e: e.activation(o[:], o[:], AF.Silu, bias=cp[:, k, 3:4]), reads=[bo_, bcp], writes=[bo_])
                p.dma("sp", FT[k], o[:], reads=[bo_], writes=[bFT[k]])
        p.barrier()
    es = ExitStack()
    with es:
        fx = [c.sb([128, 12, 128], es) for _ in range(2)]; bfx = [Buf(), Buf()]
        xo_ = [c.sb([128, 12, 128], es) for _ in range(2)]; bxo = [Buf(), Buf()]
        for t in range(NT):
            i = t % 2
            p.dma("sp", fx[i][:], FT[0:12, :, t * 128:(t + 1) * 128].rearrange("k p t -> p k t"), reads=bFT[0:12], writes=[bfx[i]])
            for q in range(3):
                for cc in range(4):
                    p.op("pe", lambda e: e.transpose(K["ptr"][:, cc * 128:(cc + 1) * 128], fx[i][:, q * 4 + cc, :], K["ident"][:]),
                         reads=[bfx[i], K["bident"]], writes=[K["bptr"]])
                p.op("act" if q % 2 else "dve",
                     (lambda e: e.copy(xo_[i][:, q * 4:(q + 1) * 4, :], K["ptr"][:].rearrange("p (c t) -> p c t", c=4))) if q % 2 else
                     (lambda e: e.tensor_copy(xo_[i][:, q * 4:(q + 1) * 4, :], K["ptr"][:].rearrange("p (c t) -> p c t", c=4))),
                     reads=[K["bptr"]], writes=[bxo[i]])
            p.dma("pool", XTM[t * 128:(t + 1) * 128, :], xo_[i][:, 0:8, :], reads=[bxo[i]], writes=[bXB[t]])
            p.dma("pool", BTM[t * 128:(t + 1) * 128, :], xo_[i][:, 8:12, :], reads=[bxo[i]], writes=[bXB[t]])
        p.barrier()
    es = ExitStack()
    with es:
        tri = {0: c.sb([128, 128], es), 1: c.sb([128, 128], es)}; btri = Buf()
        p.dma("pool", tri[0][:], triF, writes=[btri])
        p.dma("pool", tri[1][:], triB, writes=[btri])
        ones = c.sb([128, 128], es); bon = Buf()
        p.op("dve", lambda e: e.memset(ones[:], 1.0), writes=[bon])
        Abc = c.sb([128, 32], es); bA = Buf()
        p.dma("pool", Abc[:], alog.partition_broadcast(128), writes=[bA])
        p.op("act", lambda e: e.activation(Abc[:], Abc[:], AF.Exp), reads=[bA], writes=[bA])
        p.op("dve", lambda e: e.tensor_scalar(Abc[:], Abc[:], -1.0, None, ALU.mult), reads=[bA], writes=[bA])
        Dbc = c.sb([128, D], es); ngt = c.sb([128, D], es); bDn = Buf()
        p.dma("pool", Dbc[:], dvec.partition_broadcast(128), writes=[bDn])
        p.dma("pool", ngt[:], ng.partition_broadcast(128), writes=[bDn])
        wo = c.sb([128, 8, D], es); bwo = Buf()
        p.dma("sp", wo[:], w_out.rearrange("(c p) f -> p c f", p=128), writes=[bwo])
        xs = [c.sb([128, 16, 64], es) for _ in range(2)]; bts = [c.sb([128, 4, 128], es) for _ in range(2)]
        BT = [c.sb([128, 4, 128], es) for _ in range(2)]; CT = [c.sb([128, 4, 128], es) for _ in range(2)]
        dtt = [c.sb([128, 32], es) for _ in range(2)]
        bin_ = [Buf(), Buf()]
        hst = c.sb([128, 16, 64], es); bh = Buf()
        a_ = c.sb([128, 16], es); acs = c.sb([128, 16], es); nacs = c.sb([128, 16], es); etot = c.sb([128, 16], es); wgt = c.sb([128, 16], es)
        bsm = Buf()
        pA = c.ps([128, 512], es); bpA = PBuf()
        pCB = c.ps([128, 512], es); bpCB = PBuf()
        pST = c.ps([128, 512], es); bpST = PBuf()
        pRB = [c.ps([128, 512], es) for _ in range(2)]; bpRB = [PBuf(), PBuf()]
        pY = c.ps([128, 512], es); bpY = PBuf()
        pC = c.ps([128, 512], es); bpC = PBuf()
        CBm = c.sb([128, 128], es); bCBm = Buf()
        xh = c.sb([128, 4, 64], es); bxh = Buf()
        abc = [c.sb([128, 128], es) for _ in range(2)]; babc = [Buf(), Buf()]
        tmp = [c.sb([128, 128], es) for _ in range(2)]; btmp = [Buf(), Buf()]
        WT = [c.sb([128, 128], es) for _ in range(2)]; bWT = [Buf(), Buf()]
        Eb = [c.sb([128, 128], es) for _ in range(2)]; bEb = [Buf(), Buf()]
        LT = [c.sb([128, 128], es) for _ in range(2)]; bLT = [Buf(), Buf()]
        ysb = [c.sb([128, 16, 64], es) for _ in range(2)]; bys = [Buf(), Buf()]
        yfl = c.sb([128, D], es); zl = c.sb([128, D], es); bfl = Buf()
        ssb = c.sb([128, 2], es); bssb = Buf()
        ynT = c.sb([128, 8, 128], es); bynT = Buf()
        ob = c.sb([128, D], es); bob = Buf()
        nh = 0
        for d in range(2):
            order = [32, 33] + list(range(32)) if d == 0 else [33, 32] + list(range(31, -1, -1))
            p.op("dve", lambda e: e.memset(hst[:], 0.0), reads=[bh], writes=[bh])
            for vi, t in enumerate(order):
                i = vi % 2
                sl = slice(t * 128, (t + 1) * 128)
                p.dma("sp", xs[i][:], XTM[sl, :].rearrange("p (h d) -> p h d", h=16), reads=[bXB[t]], writes=[bin_[i]])
                p.dma("sp", bts[i][:], BTM[sl, :].rearrange("p (g n) -> p g n", g=4), reads=[bXB[t]], writes=[bin_[i]])
                p.dma("sp", BT[i][:], FT[8:12, :, sl].rearrange("g p t -> p g t"), reads=bFT[8:12], writes=[bin_[i]])
                p.dma("sp", CT[i][:], FT[12:16, :, sl].rearrange("g p t -> p g t"), reads=bFT[12:16], writes=[bin_[i]])
                p.dma("sp", dtt[i][:], DT[sl, :], reads=[bZS[t]], writes=[bin_[i]])
                dtd = dtt[i][:, d * 16:(d + 1) * 16]
                p.op("dve", lambda e: e.tensor_mul(a_[:], dtd, Abc[:, d * 16:(d + 1) * 16]), reads=[bin_[i], bA], writes=[bsm])
                p.op("pe", lambda e: e.matmul(pA[:, 0:16], tri[d][:], a_[:], start=True, stop=True), reads=[btri, bsm], writes=[bpA])
                p.op("pe", lambda e: e.matmul(pA[:, 16:32], ones[:], a_[:], start=True, stop=True), reads=[bon, bsm], writes=[bpA])
                p.op("act", lambda e: e.copy(acs[:], pA[:, 0:16]), reads=[bpA], writes=[bsm])
                p.op("dve", lambda e: e.tensor_scalar(nacs[:], pA[:, 0:16], -1.0, None, ALU.mult), reads=[bpA], writes=[bsm])
                p.op("act", lambda e: e.activation(etot[:], pA[:, 16:32], AF.Exp), reads=[bpA], writes=[bsm])
                p.op("dve", lambda e: e.tensor_tensor(wgt[:], pA[:, 16:32], acs[:], ALU.subtract), reads=[bpA, bsm], writes=[bsm])
                p.op("act", lambda e: e.activation(wgt[:], wgt[:], AF.Exp), reads=[bsm], writes=[bsm])
                p.op("dve", lambda e: e.tensor_mul(wgt[:], wgt[:], dtd), reads=[bsm, bin_[i]], writes=[bsm])
                yb = ysb[i]
                for g in range(4):
                    p.op("pe", lambda e: e.matmul(pCB[:, 0:128], BT[i][:, g, :], CT[i][:, g, :], start=True, stop=True), reads=[bin_[i]], writes=[bpCB])
                    p.op("dve", lambda e: e.tensor_mul(CBm[:], pCB[:, 0:128], tri[d][:]), reads=[bpCB, btri], writes=[bCBm])
                    for r in range(4):
                        hd = 4 * g + r
                        p.op("pool", lambda e: e.tensor_scalar(xh[:, r, :], xs[i][:, hd, :], wgt[:, hd:hd + 1], None, ALU.mult),
                             reads=[bin_[i], bsm], writes=[bxh])
                    p.op("pe", lambda e: e.matmul(pST[:, 0:256], bts[i][:, g, :], xh[:].rearrange("p r d -> p (r d)"), start=True, stop=True),
                         reads=[bin_[i], bxh], writes=[bpST])
                    for r in range(4):
                        hd = 4 * g + r
                        j = nh % 2
                        nh += 1
                        p.op("pool", lambda e: e.tensor_scalar(abc[j][:], ones[:], a_[:, hd:hd + 1], None, ALU.mult), reads=[bon, bsm], writes=[babc[j]])
                        p.op("pe", lambda e: e.matmul(pRB[j][:, 0:128], abc[j][:], tri[d][:], start=True, stop=True), reads=[babc[j], btri], writes=[bpRB[j]])
                        p.op("dve", lambda e: e.tensor_scalar(tmp[j][:], pRB[j][:, 0:128], nacs[:, hd:hd + 1], 0.0, ALU.add, ALU.min),
                             reads=[bpRB[j], bsm], writes=[btmp[j]])
                        p.op("act", lambda e: e.activation(tmp[j][:], tmp[j][:], AF.Exp), reads=[btmp[j]], writes=[btmp[j]])
                        p.op("dve", lambda e: e.scalar_tensor_tensor(WT[j][:], tmp[j][:], dtd[:, hd:hd + 1], CBm[:], ALU.mult, ALU.mult),
                             reads=[btmp[j], bin_[i], bCBm], writes=[bWT[j]])
                        p.op("act", lambda e: e.activation(Eb[j][:], pRB[j][:, 0:128], AF.Exp), reads=[bpRB[j]], writes=[bEb[j]])
                        p.op("pool", lambda e: e.tensor_mul(LT[j][:], CT[i][:, g, :], Eb[j][:]), reads=[bin_[i], bEb[j]], writes=[bLT[j]])
                        p.op("pe", lambda e: e.matmul(pY[:, 0:64], WT[j][:], xs[i][:, hd, :], start=True, stop=False), reads=[bWT[j], bin_[i]], writes=[bpY])
                        p.op("pe", lambda e: e.matmul(pY[:, 0:64], LT[j][:], hst[:, hd, :], start=False, stop=True), reads=[bLT[j], bh], writes=[bpY])
                        p.op("act", lambda e: e.copy(yb[:, hd, :], pY[:, 0:64]), reads=[bpY], writes=[bys[i]])
                    for r in range(4):
                        hd = 4 * g + r
                        p.op("dve", lambda e: e.scalar_tensor_tensor(hst[:, hd, :], hst[:, hd, :], etot[:, hd:hd + 1], pST[:, r * 64:(r + 1) * 64], ALU.mult, ALU.add),
                             reads=[bh, bsm, bpST], writes=[bh])
                ybf = yb[:].rearrange("p h d -> p (h d)")
                if d == 0:
                    p.dma("pool", YF[sl, :], ybf, reads=[bys[i]], writes=[bYF[t]])
                    continue
                p.dma("pool", yfl[:], YF[sl, :], reads=[bYF[t]], writes=[bfl])
                p.dma("pool", zl[:], ZS[sl, :], reads=[bZS[t]], writes=[bfl])
                p.op("dve", lambda e: e.tensor_add(ybf, ybf, yfl[:]), reads=[bys[i], bfl], writes=[bys[i]])
                p.op("pool", lambda e: e.tensor_mul(yfl[:], xs[i][:].rearrange("p h d -> p (h d)"), Dbc[:]), reads=[bin_[i], bDn, bfl], writes=[bfl])
                p.op("dve", lambda e: e.tensor_add(ybf, ybf, yfl[:]), reads=[bys[i], bfl], writes=[bys[i]])
                p.op("dve", lambda e: e.tensor_mul(ybf, ybf, zl[:]), reads=[bys[i], bfl], writes=[bys[i]])
                p.op("act", lambda e: e.activation(K["junk"][:], ybf, AF.Square, accum_out=ssb[:, 0:1]), reads=[bys[i]], writes=[K["bjunk"], bssb])
                p.dma("pool", SS[sl, :], ssb[:, 0:1], reads=[bssb], writes=[bSS[t]])
                p.op("dve", lambda e: e.tensor_mul(ybf, ybf, ngt[:]), reads=[bys[i], bDn], writes=[bys[i]])
                for q in range(2):
                    for cc in range(4):
                        ch = q * 4 + cc
                        p.op("pe", lambda e: e.transpose(K["ptr"][:, cc * 128:(cc + 1) * 128], ybf[:, ch * 128:(ch + 1) * 128], K["ident"][:]),
                             reads=[bys[i], K["bident"]], writes=[K["bptr"]])
                    p.op("act", lambda e: e.copy(ynT[:, q * 4:(q + 1) * 4, :], K["ptr"][:].rearrange("p (c t) -> p c t", c=4)), reads=[K["bptr"]], writes=[bynT])
                for half in range(2):
                    for ch in range(8):
                        p.op("pe", lambda e: e.matmul(pC[:], ynT[:, ch, :], wo[:, ch, half * 512:(half + 1) * 512], start=(ch == 0), stop=(ch == 7)),
                             reads=[bynT, bwo], writes=[bpC])
                    p.op("dve", lambda e: e.tensor_copy(ob[:, half * 512:(half + 1) * 512], pC[:]), reads=[bpC], writes=[bob])
                p.dma("pool", P[sl, :], ob[:], reads=[bob], writes=[bP[t]])
        p.barrier()


def ssd_weights(z, h):
    w_in = z["ssd_w_in"][0]
    f = np.ascontiguousarray
    cols = np.concatenate([2048 + h * 1024 + np.arange(1024), 4096 + h * 512 + np.arange(512), 5120 + h * 512 + np.arange(512)])
    cch = cols - 2048
    convp = np.stack([z["ssd_conv_w"][0][0, cch], z["ssd_conv_w"][0][1, cch], z["ssd_conv_w"][0][2, cch], z["ssd_conv_b"][0][cch]], 1)
    dtc = np.concatenate([6144 + 16 * h + np.arange(16), 6144 + 32 + 16 * h + np.arange(16)])
    hs = slice(16 * h, 16 * h + 16)
    tri = np.triu(np.ones((128, 128), np.float32))
    return dict(w_cv=f(w_in[:, cols]), convp=f(convp.reshape(16, 128, 4).astype(np.float32)), w_z=f(w_in[:, h * 1024:(h + 1) * 1024]),
                w_dt=f(w_in[:, dtc]), dtb=f(np.concatenate([z["ssd_dt_bias"][0][0, hs], z["ssd_dt_bias"][0][1, hs]])[None, :]),
                alog=f(np.concatenate([z["ssd_a_log"][0][0, hs], z["ssd_a_log"][0][1, hs]])[None, :]),
                dvec=f(np.repeat(z["ssd_d"][0][hs], 64)[None, :]), ng=f(z["ssd_norm_g"][0][h * 1024:(h + 1) * 1024][None, :]),
                w_out=f(z["ssd_w_out"][0][h * 1024:(h + 1) * 1024]), triF=tri, triB=f(tri.T))


def conv_fm(c, K, HT, bHT, w_cv, convp, nblk, FT, bFT):
    p = c.p
    groups = [list(range(4 * q, 4 * q + 4)) for q in range(8)] + [[32, 33]]
    es = ExitStack()
    with es:
        wc = c.sb([128, 8, 512], es); bwc = Buf()
        cp = c.sb([128, 4 * nblk, 4], es); bcp = Buf()
        p.dma("pool", cp[:], convp.rearrange("k p f -> p k f"), writes=[bcp])
        hg = [c.sb([128, 4, 8, 128], es) for _ in range(2)]; bhg = [Buf(), Buf()]
        raw = [c.sb([128, NTOK], es) for _ in range(4)]; braw = [Buf() for _ in range(4)]
        cv = [c.sb([128, NTOK], es) for _ in range(2)]; bcv = [Buf(), Buf()]
        pr_ = [c.ps([128, 512], es) for _ in range(2)]; bpr = [PBuf(), PBuf()]
        n = 0
        for cb in range(nblk):
            p.dma("sp", wc[:], w_cv[:, cb * 512:(cb + 1) * 512].rearrange("(c p) f -> p c f", p=128), writes=[bwc])
            for gi, tiles in enumerate(groups):
                i = gi % 2
                nt = len(tiles)
                N = nt * 128
                t0 = tiles[0] * 128
                p.dma("pool", hg[i][:, 0:nt], HT[tiles[0]:tiles[0] + nt].rearrange("t p c k -> p t c k"), reads=[bHT[t] for t in tiles], writes=[bhg[i]])
                for cc in range(4):
                    j = n % 2
                    n += 1
                    for k in range(nt):
                        for ch in range(8):
                            p.op("pe", lambda e: e.matmul(pr_[j][:, k * 128:(k + 1) * 128], wc[:, ch, cc * 128:(cc + 1) * 128], hg[i][:, k, ch, :],
                                                          start=(ch == 0), stop=(ch == 7)), reads=[bwc, bhg[i]], writes=[bpr[j]])
                    p.op("act" if j else "dve",
                         (lambda e: e.copy(raw[cc][:, t0:t0 + N], pr_[j][:, 0:N])) if j else (lambda e: e.tensor_copy(raw[cc][:, t0:t0 + N], pr_[j][:, 0:N])),
                         reads=[bpr[j]], writes=[braw[cc]])
            for cc in range(4):
                k = cb * 4 + cc
                o = cv[cc % 2]; bo_ = bcv[cc % 2]; r = raw[cc]
                p.op("dve", lambda e: e.tensor_scalar(o[:], r[:], cp[:, k, 1:2], None, ALU.mult), reads=[braw[cc], bcp], writes=[bo_])
                for (a0, a1) in ((0, 4096), (4096, NTOK)):
                    p.op("dve", lambda e: e.scalar_tensor_tensor(o[:, a0 + 1:a1], r[:, a0:a1 - 1], cp[:, k, 0:1], o[:, a0 + 1:a1], ALU.mult, ALU.add),
                         reads=[braw[cc], bcp, bo_], writes=[bo_])
                    p.op("dve", lambda e: e.scalar_tensor_tensor(o[:, a0:a1 - 1], r[:, a0 + 1:a1], cp[:, k, 2:3], o[:, a0:a1 - 1], ALU.mult, ALU.add),
                         reads=[braw[cc], bcp, bo_], writes=[bo_])
                p.op("act", lambda e: e.activation(o[:], o[:], AF.Silu, bias=cp[:, k, 3:4]), reads=[bo_, bcp], writes=[bo_])
                p.dma("sp", FT[k], o[:], reads=[bo_], writes=[bFT[k]])
        p.barrier()


def emit_gdn(c, K, X, bX, mods, w_cv, convp, w_z, w_bg, dtb, alog, ng, w_out, masks, P, bP, stop=None):
    p = c.p
    HT = dram(c, [NT, 128, 8, 128]); bHT = [Buf() for _ in range(NT)]
    FT = dram(c, [12, 128, NTOK]); bFT = [Buf() for _ in range(12)]
    ZS = dram(c, [NTOK, 512]); BG = dram(c, [NTOK, 16]); QKV = dram(c, [NTOK, 1536]); QKT = dram(c, [8, 128, NTOK]); OF = dram(c, [NTOK, 512])
    bZS = [Buf() for _ in range(NT)]; bQ = [Buf() for _ in range(NT)]; bOF = [Buf() for _ in range(NT)]
    es = ExitStack()
    with es:
        wz = c.sb([128, 8, 512], es); wbg = c.sb([128, 8, 16], es); bwz = Buf()
        p.dma("sp", wz[:], w_z.rearrange("(c p) f -> p c f", p=128), writes=[bwz])
        p.dma("sp", wbg[:], w_bg.rearrange("(c p) f -> p c f", p=128), writes=[bwz])
        dtb_t = c.sb([128, 8], es); na = c.sb([128, 8], es); bdb = Buf()
        p.dma("pool", dtb_t[:], dtb.partition_broadcast(128), writes=[bdb])
        p.dma("pool", na[:], alog.partition_broadcast(128), writes=[bdb])
        p.op("act", lambda e: e.activation(na[:], na[:], AF.Exp), reads=[bdb], writes=[bdb])
        p.op("dve", lambda e: e.tensor_scalar(na[:], na[:], -1.0, None, ALU.mult), reads=[bdb], writes=[bdb])
        xt = [c.sb([128, D], es) for _ in range(2)]; bxt = [Buf(), Buf()]
        hT = [c.sb([128, 8, 128], es) for _ in range(2)]; bhT = [Buf(), Buf()]
        pz = c.ps([128, 512], es); bpz = PBuf()
        pd = c.ps([128, 512], es); bpd = PBuf()
        zs = [c.sb([128, 512], es) for _ in range(2)]; bzs = [Buf(), Buf()]
        dr = c.sb([128, 8], es); ta = c.sb([128, 8], es); tb = c.sb([128, 8], es); bgo = [c.sb([128, 16], es) for _ in range(2)]
        bdr, btt, bbgo = Buf(), Buf(), [Buf(), Buf()]
        for t in range(NT):
            i = t % 2
            G, S, _ = mods[tile_kind(t)]
            p.dma("pool", xt[i][:], X[t * 128:(t + 1) * 128, :], reads=[bX[t]], writes=[bxt[i]])
            norm_mod_T(c, K, xt[i][:], bxt[i], G, S, hT[i], bhT[i])
            p.dma("sp", HT[t], hT[i][:], reads=[bhT[i]], writes=[bHT[t]])
            for ch in range(8):
                p.op("pe", lambda e: e.matmul(pz[:], hT[i][:, ch, :], wz[:, ch, :], start=(ch == 0), stop=(ch == 7)), reads=[bhT[i], bwz], writes=[bpz])
            p.op("act", lambda e: e.activation(zs[i][:], pz[:], AF.Silu), reads=[bpz], writes=[bzs[i]])
            p.dma("sp", ZS[t * 128:(t + 1) * 128, :], zs[i][:], reads=[bzs[i]], writes=[bZS[t]])
            for ch in range(8):
                p.op("pe", lambda e: e.matmul(pd[:, 0:16], hT[i][:, ch, :], wbg[:, ch, :], start=(ch == 0), stop=(ch == 7)), reads=[bhT[i], bwz], writes=[bpd])
            p.op("act", lambda e: e.activation(bgo[i][:, 0:8], pd[:, 0:8], AF.Sigmoid), reads=[bpd], writes=[bbgo[i]])
            p.op("dve", lambda e: e.tensor_add(dr[:], pd[:, 8:16], dtb_t[:]), reads=[bpd, bdb], writes=[bdr])
            softplus_tile(c, ta[:], dr[:], ta[:], tb[:], [bdr], [btt], btt)
            p.op("dve", lambda e: e.tensor_mul(bgo[i][:, 8:16], ta[:], na[:]), reads=[btt, bdb], writes=[bbgo[i]])
            p.dma("sp", BG[t * 128:(t + 1) * 128, :], bgo[i][:], reads=[bbgo[i]], writes=[bZS[t]])
        p.barrier()
    if stop == "A1":
        return
    conv_fm(c, K, HT, bHT, w_cv, convp, 3, FT, bFT)
    if stop == "A2":
        return
    es = ExitStack()
    with es:
        fx = [c.sb([128, 12, 128], es) for _ in range(2)]; bfx = [Buf(), Buf()]
        tm = [c.sb([128, 12, 128], es) for _ in range(2)]; btm = [Buf(), Buf()]
        qkT = [c.sb([128, 8, 128], es) for _ in range(2)]; bqkT = [Buf(), Buf()]
        s8 = c.sb([128, 16], es); bs8 = Buf()
        for t in range(NT):
            i = t % 2
            p.dma("sp", fx[i][:], FT[0:12, :, t * 128:(t + 1) * 128].rearrange("k p t -> p k t"), reads=bFT, writes=[bfx[i]])
            for q in range(3):
                for cc in range(4):
                    p.op("pe", lambda e: e.transpose(K["ptr"][:, cc * 128:(cc + 1) * 128], fx[i][:, q * 4 + cc, :], K["ident"][:]),
                         reads=[bfx[i], K["bident"]], writes=[K["bptr"]])
                p.op("act" if q % 2 else "dve",
                     (lambda e: e.copy(tm[i][:, q * 4:(q + 1) * 4, :], K["ptr"][:].rearrange("p (c t) -> p c t", c=4))) if q % 2 else
                     (lambda e: e.tensor_copy(tm[i][:, q * 4:(q + 1) * 4, :], K["ptr"][:].rearrange("p (c t) -> p c t", c=4))),
                     reads=[K["bptr"]], writes=[btm[i]])
            for hq in range(8):
                p.op("act", lambda e: e.activation(K["junk"][:, 0:128], tm[i][:, hq, :], AF.Square, accum_out=s8[:, hq:hq + 1]),
                     reads=[btm[i]], writes=[K["bjunk"], bs8])
            p.op("act", lambda e: e.activation(s8[:, 8:16], s8[:, 0:8], AF.Sqrt, bias=K["eps"][:, 0:1]), reads=[bs8], writes=[bs8])
            p.op("dve", lambda e: e.reciprocal(s8[:, 0:8], s8[:, 8:16]), reads=[bs8], writes=[bs8])
            for hq in range(8):
                sc2 = (128.0 ** -0.5) if hq < 4 else 1.0
                p.op("dve", lambda e: e.tensor_scalar(tm[i][:, hq, :], tm[i][:, hq, :], s8[:, hq:hq + 1], sc2, ALU.mult, ALU.mult),
                     reads=[btm[i], bs8], writes=[btm[i]])
            p.dma("pool", QKV[t * 128:(t + 1) * 128, :], tm[i][:].rearrange("p k d -> p (k d)"), reads=[btm[i]], writes=[bQ[t]])
            for q in range(2):
                for cc in range(4):
                    p.op("pe", lambda e: e.transpose(K["ptr"][:, cc * 128:(cc + 1) * 128], tm[i][:, q * 4 + cc, :], K["ident"][:]),
                         reads=[btm[i], K["bident"]], writes=[K["bptr"]])
                p.op("act", lambda e: e.copy(qkT[i][:, q * 4:(q + 1) * 4, :], K["ptr"][:].rearrange("p (c t) -> p c t", c=4)),
                     reads=[K["bptr"]], writes=[bqkT[i]])
            p.dma("pool", QKT[:, :, t * 128:(t + 1) * 128].rearrange("h p t -> p h t"), qkT[i][:], reads=[bqkT[i]], writes=[bQ[t]])
        p.barrier()
    if stop == "A3":
        return
    es = ExitStack()
    with es:
        mk = c.sb([128, 2, 7, 128], es); bmk = Buf()
        for dd in range(2):
            for mm_ in range(7):
                p.dma("sp", mk[:, dd, mm_, :], masks[dd, mm_], writes=[bmk])
        m4 = c.sb([128, 4, 4, 128], es); id4 = c.sb([128, 4, 128], es); bm4 = Buf()
        for hh in range(4):
            p.op("dve", lambda e: e.tensor_copy(m4[:, :, hh, :], mk[:, 0, 3:7, :]), reads=[bmk], writes=[bm4])
            p.op("dve", lambda e: e.tensor_copy(id4[:, hh, :], K["ident"][:]), reads=[K["bident"]], writes=[bm4])
        ones = c.sb([128, 128], es); bon = Buf()
        p.op("dve", lambda e: e.memset(ones[:], 1.0), writes=[bon])
        tri_t = [c.sb([128, 128], es) for _ in range(2)]
        ms_t = [c.sb([128, 128], es) for _ in range(2)]
        nm_t = [c.sb([128, 128], es) for _ in range(2)]
        for dd in range(2):
            p.op("dve", lambda e: e.tensor_copy(tri_t[dd][:], mk[:, dd, 0, :]), reads=[bmk], writes=[bmk])
            p.op("dve", lambda e: e.tensor_copy(ms_t[dd][:], mk[:, dd, 1, :]), reads=[bmk], writes=[bmk])
            p.op("dve", lambda e: e.tensor_copy(nm_t[dd][:], mk[:, dd, 2, :]), reads=[bmk], writes=[bmk])
        ngt = c.sb([128, 128], es); bng = Buf()
        p.dma("pool", ngt[:], ng.partition_broadcast(128), writes=[bng])
        wo = c.sb([128, 4, D], es); bwo = Buf()
        p.dma("sp", wo[:], w_out.rearrange("(c p) f -> p c f", p=128), writes=[bwo])
        qkv = [c.sb([128, 12, 128], es) for _ in range(2)]; qkt = [c.sb([128, 8, 128], es) for _ in range(2)]; bg_ = [c.sb([128, 16], es) for _ in range(2)]
        bin_ = [Buf(), Buf()]
        S = c.sb([128, 4, 128], es); bS = Buf()
        sm = c.sb([128, 32], es); bsm = Buf()
        sums = c.sb([128, 32], es); bsums = Buf()
        rbs = (c.sb([128, 4, 128], es), Buf())
        gc, egc, etot, kdw, bs_ = (sm[:, 4 * j:4 * j + 4] for j in range(5))
        banks = [(c.ps([128, 512], es), PBuf()) for _ in range(7)]
        bank_i = [0]

        def nb():
            b = banks[bank_i[0] % 7]
            bank_i[0] += 1
            return b

        def T3(name=None):
            return (c.sb([128, 4, 128], es), Buf())
        Kb, Vb, kd, abc, tA, tB, E1, E2, Eg, Lm, AT, qgT, LT, D0, D0T, ImD0T, O1T, O2T, O3T = (T3() for _ in range(19))
        D2, D2T, IpD2T, D4, D4T, IpD4T, IpD8, R1, R2, Xa, XaT, Xb, XbT, Gt, nwT, vn, osb = (T3() for _ in range(17))
        ofl = c.sb([128, 512], es); zl = c.sb([128, 512], es); bfl = Buf()
        s4 = c.sb([128, 12], es); bs4 = Buf()
        ynT = c.sb([128, 4, 128], es); bynT = Buf()
        ob = c.sb([128, D], es); bob = Buf()

        def f(tb_):
            return tb_[0][:].rearrange("p h d -> p (h d)")

        def mm4(dst, lhs, rhs, lhs_b, rhs_b):
            for hh in range(4):
                p.op("pe", lambda e: e.matmul(dst[0][:, hh * 128:(hh + 1) * 128], lhs[0][:, hh, :], rhs[0][:, hh, :], start=True, stop=True),
                     reads=[lhs_b, rhs_b], writes=[dst[1]])

        def ev(eng, dst, src_ps, add=None, sub_from=None):
            if sub_from is not None:
                p.op("dve", lambda e: e.tensor_sub(f(dst), f(sub_from), src_ps[0][:]), reads=[src_ps[1], sub_from[1]], writes=[dst[1]])
            elif add is not None:
                p.op("dve", lambda e: e.tensor_add(f(dst), src_ps[0][:], add), reads=[src_ps[1], bm4], writes=[dst[1]])
            elif eng == "act":
                p.op("act", lambda e: e.copy(f(dst), src_ps[0][:]), reads=[src_ps[1]], writes=[dst[1]])
            else:
                p.op("dve", lambda e: e.tensor_copy(f(dst), src_ps[0][:]), reads=[src_ps[1]], writes=[dst[1]])
        idf = id4[:].rearrange("p h d -> p (h d)")
        for d in range(2):
            order = [32, 33] + list(range(32)) if d == 0 else [33, 32] + list(range(31, -1, -1))
            if isinstance(stop, int):
                order = order[:stop]
            tri_d, MS_d, NM_d = mk[:, d, 0, :], mk[:, d, 1, :], mk[:, d, 2, :]
            p.op("dve", lambda e: e.memset(S[:], 0.0), reads=[bS], writes=[bS])
            if stop == ("B", 1):
                p.barrier()
                return
            for vi, t in enumerate(order):
                i = vi % 2
                sl = slice(t * 128, (t + 1) * 128)
                p.dma("sp", qkv[i][:], QKV[sl, :].rearrange("p (k d) -> p k d", k=12), reads=[bQ[t]], writes=[bin_[i]])
                for hq in range(8):
                    p.dma("sp", qkt[i][:, hq, :], QKT[hq, :, sl], reads=[bQ[t]], writes=[bin_[i]])
                p.dma("sp", bg_[i][:], BG[sl, :], reads=[bZS[t]], writes=[bin_[i]])
                beta = bg_[i][:, d * 4:(d + 1) * 4]
                g = bg_[i][:, 8 + d * 4:8 + (d + 1) * 4]
                qv = (qkv[i], bin_[i]); kT = qkt[i]
                pa_ = nb()
                gcol = slice(8 + d * 4, 8 + (d + 1) * 4)
                tcol = slice(16 + 8 + d * 4, 16 + 8 + (d + 1) * 4)
                p.op("pe", lambda e: e.matmul(pa_[0][:, 0:16], tri_t[d][:], bg_[i][:, 0:16], start=True, stop=True), reads=[bmk, bin_[i]], writes=[pa_[1]])
                p.op("pe", lambda e: e.matmul(pa_[0][:, 16:32], ones[:], bg_[i][:, 0:16], start=True, stop=True), reads=[bon, bin_[i]], writes=[pa_[1]])
                p.op("act", lambda e: e.copy(sums[:], pa_[0][:, 0:32]), reads=[pa_[1]], writes=[bsums])
                p.op("dve", lambda e: e.tensor_copy(gc, sums[:, gcol]), reads=[bsums], writes=[bsm])
                p.op("act", lambda e: e.activation(egc, sums[:, gcol], AF.Exp), reads=[bsums], writes=[bsm])
                p.op("act", lambda e: e.activation(etot, sums[:, tcol], AF.Exp), reads=[bsums], writes=[bsm])
                p.op("dve", lambda e: e.tensor_tensor(kdw, sums[:, tcol], gc, ALU.subtract), reads=[bsums, bsm], writes=[bsm])
                p.op("act", lambda e: e.activation(kdw, kdw, AF.Exp), reads=[bsm], writes=[bsm])
                p.op("dve", lambda e: e.tensor_mul(bs_, beta, egc), reads=[bin_[i], bsm], writes=[bsm])
                prb, pkk, pqk = nb(), nb(), nb()
                for hh in range(4):
                    p.op("pool", lambda e: e.tensor_scalar(Kb[0][:, hh, :], qkv[i][:, 4 + hh, :], bs_[:, hh:hh + 1], None, ALU.mult), reads=[bin_[i], bsm], writes=[Kb[1]])
                    p.op("pool", lambda e: e.tensor_scalar(Vb[0][:, hh, :], qkv[i][:, 8 + hh, :], beta[:, hh:hh + 1], None, ALU.mult), reads=[bin_[i]], writes=[Vb[1]])
                    p.op("pool", lambda e: e.tensor_scalar(kd[0][:, hh, :], qkv[i][:, 4 + hh, :], kdw[:, hh:hh + 1], None, ALU.mult), reads=[bin_[i], bsm], writes=[kd[1]])
                    p.op("pool", lambda e: e.tensor_scalar(abc[0][:, hh, :], ones[:], g[:, hh:hh + 1], None, ALU.mult), reads=[bon, bin_[i]], writes=[abc[1]])
                    p.op("pe", lambda e: e.matmul(prb[0][:, hh * 128:(hh + 1) * 128], abc[0][:, hh, :], tri_t[d][:], start=True, stop=True), reads=[abc[1], bmk], writes=[prb[1]])
                    p.op("pe", lambda e: e.matmul(pkk[0][:, hh * 128:(hh + 1) * 128], kT[:, 4 + hh, :], kT[:, 4 + hh, :], start=True, stop=True), reads=[bin_[i]], writes=[pkk[1]])
                    p.op("pe", lambda e: e.matmul(pqk[0][:, hh * 128:(hh + 1) * 128], kT[:, 4 + hh, :], kT[:, hh, :], start=True, stop=True), reads=[bin_[i]], writes=[pqk[1]])
                    if stop == ("B", 2):
                        p.barrier()
                        return
                p.op("act", lambda e: e.copy(rbs[0][:].rearrange("p h d -> p (h d)"), prb[0][:]), reads=[prb[1]], writes=[rbs[1]])
                for hh in range(4):
                    p.op("dve", lambda e: e.scalar_tensor_tensor(tA[0][:, hh, :], rbs[0][:, hh, :], gc[:, hh:hh + 1], ms_t[d][:], ALU.subtract, ALU.max),
                         reads=[rbs[1], bsm, bmk], writes=[tA[1]])
                    p.op("dve", lambda e: e.scalar_tensor_tensor(tB[0][:, hh, :], rbs[0][:, hh, :], gc[:, hh:hh + 1], nm_t[d][:], ALU.subtract, ALU.min),
                         reads=[rbs[1], bsm, bmk], writes=[tB[1]])
                p.op("act", lambda e: e.activation(f(E1), f(tA), AF.Exp, scale=-1.0), reads=[tA[1]], writes=[E1[1]])
                p.op("act", lambda e: e.activation(f(E2), f(tB), AF.Exp), reads=[tB[1]], writes=[E2[1]])
                p.op("act", lambda e: e.activation(f(Eg), rbs[0][:].rearrange("p h d -> p (h d)"), AF.Exp), reads=[rbs[1]], writes=[Eg[1]])
                for hh in range(4):
                    p.op("dve", lambda e: e.scalar_tensor_tensor(Lm[0][:, hh, :], pkk[0][:, hh * 128:(hh + 1) * 128], beta[:, hh:hh + 1], E1[0][:, hh, :], ALU.mult, ALU.mult),
                         reads=[pkk[1], bin_[i], E1[1]], writes=[Lm[1]])
                p.op("dve", lambda e: e.tensor_mul(f(AT), pqk[0][:], f(E2)), reads=[pqk[1], E2[1]], writes=[AT[1]])
                p.op("pool", lambda e: e.tensor_mul(f(qgT), qkt[i][:, 0:4, :].rearrange("p h d -> p (h d)"), f(Eg)), reads=[bin_[i], Eg[1]], writes=[qgT[1]])
                if stop == ("B", 3):
                    p.barrier()
                    return
                plt = nb()
                for hh in range(4):
                    p.op("pe", lambda e: e.transpose(plt[0][:, hh * 128:(hh + 1) * 128], Lm[0][:, hh, :], K["ident"][:]), reads=[Lm[1], K["bident"]], writes=[plt[1]])
                ev("act", LT, plt)
                p.op("dve", lambda e: e.tensor_mul(f(D0), f(Lm), m4[:, 0].rearrange("p h d -> p (h d)")), reads=[Lm[1], bm4], writes=[D0[1]])
                p.op("pool", lambda e: e.tensor_mul(f(D0T), f(LT), m4[:, 0].rearrange("p h d -> p (h d)")), reads=[LT[1], bm4], writes=[D0T[1]])
                p.op("pool", lambda e: e.tensor_mul(f(O1T), f(LT), m4[:, 1].rearrange("p h d -> p (h d)")), reads=[LT[1], bm4], writes=[O1T[1]])
                p.op("dve", lambda e: e.tensor_mul(f(O2T), f(LT), m4[:, 2].rearrange("p h d -> p (h d)")), reads=[LT[1], bm4], writes=[O2T[1]])
                p.op("pool", lambda e: e.tensor_mul(f(O3T), f(LT), m4[:, 3].rearrange("p h d -> p (h d)")), reads=[LT[1], bm4], writes=[O3T[1]])
                p.op("pool", lambda e: e.tensor_sub(f(ImD0T), idf, f(D0T)), reads=[D0T[1], bm4], writes=[ImD0T[1]])
                if stop == ("B", 4):
                    p.barrier()
                    return
                b1 = nb(); mm4(b1, D0T, D0, D0T[1], D0[1]); ev("act", D2, b1)
                b2 = nb(); mm4(b2, D0, D0T, D0[1], D0T[1]); ev("act", D2T, b2); ev("dve", IpD2T, b2, add=idf)
                b3 = nb(); mm4(b3, D2T, D2, D2T[1], D2[1]); ev("act", D4, b3)
                b4 = nb(); mm4(b4, D2, D2T, D2[1], D2T[1]); ev("act", D4T, b4); ev("dve", IpD4T, b4, add=idf)
                b5 = nb(); mm4(b5, D4T, D4, D4T[1], D4[1]); ev("dve", IpD8, b5, add=idf)
                b6 = nb(); mm4(b6, IpD4T, IpD8, IpD4T[1], IpD8[1]); ev("act", R1, b6)
                b7 = nb(); mm4(b7, IpD2T, R1, IpD2T[1], R1[1]); ev("dve", R2, b7)
                b8 = nb(); mm4(b8, ImD0T, R2, ImD0T[1], R2[1]); ev("act", Xa, b8)
                b9 = nb()
                for hh in range(4):
                    p.op("pe", lambda e: e.transpose(b9[0][:, hh * 128:(hh + 1) * 128], Xa[0][:, hh, :], K["ident"][:]), reads=[Xa[1], K["bident"]], writes=[b9[1]])
                ev("dve", XaT, b9)
                if stop == ("B", 5):
                    p.barrier()
                    return
                Xc, XcT, Xn, XnT = Xa, XaT, Xb, XbT
                for lvl, OT_ in enumerate((O1T, O2T, O3T)):
                    bg1 = nb(); mm4(bg1, OT_, Xc, OT_[1], Xc[1]); ev("act", Gt, bg1)
                    if lvl < 2:
                        bp1 = nb(); mm4(bp1, XcT, Gt, XcT[1], Gt[1]); ev("dve", Xn, bp1, sub_from=Xc)
                    bp2 = nb(); mm4(bp2, Gt, XcT, Gt[1], XcT[1]); ev("dve", XnT, bp2, sub_from=XcT)
                    Xc, XcT, Xn, XnT = Xn, XnT, Xc, XcT
                TT = XcT
                if stop == ("B", 6):
                    p.barrier()
                    return
                bw = nb(); mm4(bw, Kb, TT, Kb[1], TT[1])
                p.op("dve", lambda e: e.tensor_scalar(f(nwT), bw[0][:], -1.0, None, ALU.mult), reads=[bw[1]], writes=[nwT[1]])
                bv = nb()
                for hh in range(4):
                    p.op("pe", lambda e: e.matmul(bv[0][:, hh * 128:(hh + 1) * 128], TT[0][:, hh, :], Vb[0][:, hh, :], start=True, stop=False), reads=[TT[1], Vb[1]], writes=[bv[1]])
                    p.op("pe", lambda e: e.matmul(bv[0][:, hh * 128:(hh + 1) * 128], nwT[0][:, hh, :], S[:, hh, :], start=False, stop=True), reads=[nwT[1], bS], writes=[bv[1]])
                ev("act", vn, bv)
                bo_ = nb()
                for hh in range(4):
                    p.op("pe", lambda e: e.matmul(bo_[0][:, hh * 128:(hh + 1) * 128], qgT[0][:, hh, :], S[:, hh, :], start=True, stop=False), reads=[qgT[1], bS], writes=[bo_[1]])
                    p.op("pe", lambda e: e.matmul(bo_[0][:, hh * 128:(hh + 1) * 128], AT[0][:, hh, :], vn[0][:, hh, :], start=False, stop=True), reads=[AT[1], vn[1]], writes=[bo_[1]])
                ev("dve", osb, bo_)
                bsn = nb(); mm4(bsn, kd, vn, kd[1], vn[1])
                for hh in range(4):
                    p.op("dve", lambda e: e.scalar_tensor_tensor(S[:, hh, :], S[:, hh, :], etot[:, hh:hh + 1], bsn[0][:, hh * 128:(hh + 1) * 128], ALU.mult, ALU.add),
                         reads=[bS, bsm, bsn[1]], writes=[bS])
                if stop == ("B", 7):
                    p.barrier()
                    return
                if d == 0:
                    p.dma("pool", OF[sl, :], f(osb), reads=[osb[1]], writes=[bOF[t]])
                    continue
                p.dma("pool", ofl[:], OF[sl, :], reads=[bOF[t]], writes=[bfl])
                p.dma("pool", zl[:], ZS[sl, :], reads=[bZS[t]], writes=[bfl])
                p.op("dve", lambda e: e.tensor_add(f(osb), f(osb), ofl[:]), reads=[osb[1], bfl], writes=[osb[1]])
                for hh in range(4):
                    p.op("act", lambda e: e.activation(K["junk"][:, 0:128], osb[0][:, hh, :], AF.Square, accum_out=s4[:, hh:hh + 1]), reads=[osb[1]], writes=[K["bjunk"], bs4])
                p.op("act", lambda e: e.activation(s4[:, 4:8], s4[:, 0:4], AF.Sqrt, bias=K["eps"][:, 0:1], scale=1.0 / 128), reads=[bs4], writes=[bs4])
                p.op("dve", lambda e: e.reciprocal(s4[:, 8:12], s4[:, 4:8]), reads=[bs4], writes=[bs4])
                for hh in range(4):
                    p.op("dve", lambda e: e.scalar_tensor_tensor(osb[0][:, hh, :], osb[0][:, hh, :], s4[:, 8 + hh:9 + hh], ngt[:], ALU.mult, ALU.mult),
                         reads=[osb[1], bs4, bng], writes=[osb[1]])
                p.op("dve", lambda e: e.tensor_mul(f(osb), f(osb), zl[:]), reads=[osb[1], bfl], writes=[osb[1]])
                for hh in range(4):
                    p.op("pe", lambda e: e.transpose(K["ptr"][:, hh * 128:(hh + 1) * 128], osb[0][:, hh, :], K["ident"][:]), reads=[osb[1], K["bident"]], writes=[K["bptr"]])
                p.op("act", lambda e: e.copy(ynT[:].rearrange("p h d -> p (h d)"), K["ptr"][:]), reads=[K["bptr"]], writes=[bynT])
                for half in range(2):
                    pc_ = nb()
                    for ch in range(4):
                        p.op("pe", lambda e: e.matmul(pc_[0][:], ynT[:, ch, :], wo[:, ch, half * 512:(half + 1) * 512], start=(ch == 0), stop=(ch == 3)),
                             reads=[bynT, bwo], writes=[pc_[1]])
                    p.op("dve", lambda e: e.tensor_copy(ob[:, half * 512:(half + 1) * 512], pc_[0][:]), reads=[pc_[1]], writes=[bob])
                p.dma("pool", P[sl, :], ob[:], reads=[bob], writes=[bP[t]])
        p.barrier()


def gdn_masks():
    i = np.arange(128)[:, None]; j = np.arange(128)[None, :]
    blk = lambda n: (i // n == j // n)
    mk16 = blk(16).astype(np.float32)
    mo1 = (blk(32) & ~blk(16)).astype(np.float32)
    mo2 = (blk(64) & ~blk(32)).astype(np.float32)
    mo3 = (~blk(64)).astype(np.float32)
    out = np.zeros((2, 7, 128, 128), np.float32)
    for d in range(2):
        before = (j < i) if d == 0 else (j > i)
        tri = (i <= j) if d == 0 else (i >= j)
        out[d, 0] = tri
        out[d, 1] = np.where(before, 0.0, 1e4)
        out[d, 2] = np.where(tri, 0.0, -1e4)
        out[d, 3], out[d, 4], out[d, 5], out[d, 6] = mk16, mo1, mo2, mo3
    return out


def gdn_weights(z, jl, h):
    w_in = z["gdn_w_in"][jl]
    f = np.ascontiguousarray
    cols = np.concatenate([h * 512 + np.arange(512), 1024 + h * 512 + np.arange(512), 2048 + h * 512 + np.arange(512)])
    cw = z["gdn_conv_w"][jl]
    convp = np.stack([cw[0, cols], cw[1, cols], cw[2, cols], np.zeros(1536, np.float32)], 1)
    hs = 4 * h + np.arange(4)
    bgc = np.concatenate([4096 + hs, 4096 + 8 + hs, 4112 + hs, 4112 + 8 + hs])
    return dict(w_cv=f(w_in[:, cols]), convp=f(convp.reshape(12, 128, 4).astype(np.float32)), w_z=f(w_in[:, 3072 + h * 512:3072 + (h + 1) * 512]),
                w_bg=f(w_in[:, bgc]), dtb=f(np.concatenate([z["gdn_dt_bias"][jl][0, hs], z["gdn_dt_bias"][jl][1, hs]])[None, :]),
                alog=f(np.concatenate([z["gdn_a_log"][jl][0, hs], z["gdn_a_log"][jl][1, hs]])[None, :]),
                ng=f(z["gdn_norm_g"][jl][None, :]), w_out=f(z["gdn_w_out"][jl][h * 512:(h + 1) * 512]), masks=gdn_masks())


def build_ada():
    nc = new_nc()
    es = ExitStack()
    with es:
        c = Ctx(nc, es); p = c.p
        K = common_consts(c, c.din("ident", [128, 128]))
        cvec = c.din("cvec", [5, D]); aw = c.din("aw", [D, 3072]); ab = c.din("ab", [1, 3072])
        out = c.dout("m", [5, 3072])
        craw = c.sb([40, 128]); bcraw = Buf()
        p.dma("pool", craw[:], cvec.rearrange("k (c p) -> (k c) p", p=128), writes=[bcraw])
        sc = c.sb([128, 8, 5]); bsc = Buf()
        p.op("pe", lambda e: e.transpose(K["ptr"][:, 0:40], craw[:], K["ident"][0:40, 0:40]), reads=[bcraw, K["bident"]], writes=[K["bptr"]])
        p.op("act", lambda e: e.activation(sc[:].rearrange("p c k -> p k c"), K["ptr"][:, 0:40].rearrange("p (k c) -> p k c", k=5), AF.Silu),
             reads=[K["bptr"]], writes=[bsc])
        abt = c.sb([5, 3072]); babt = Buf()
        p.dma("pool", abt[:], ab.partition_broadcast(5), writes=[babt])
        wt = [c.sb([128, 8, 512]) for _ in range(2)]; bwt = [Buf(), Buf()]
        pa = [c.ps([128, 512]) for _ in range(2)]; bpa = [PBuf(), PBuf()]
        orow = c.sb([5, 3072]); borow = Buf()
        for g in range(6):
            j = g % 2
            p.dma("sp", wt[j][:], aw[:, g * 512:(g + 1) * 512].rearrange("(c p) f -> p c f", p=128), writes=[bwt[j]])
            for ch in range(8):
                p.op("pe", lambda e: e.matmul(pa[j][0:5, :], sc[:, ch, :], wt[j][:, ch, :], start=(ch == 0), stop=(ch == 7)), reads=[bsc, bwt[j]], writes=[bpa[j]])
            p.op("dve", lambda e: e.tensor_add(orow[:, g * 512:(g + 1) * 512], pa[j][0:5, :], abt[:, g * 512:(g + 1) * 512]), reads=[bpa[j], babt], writes=[borow])
        bo = Buf()
        p.dma("pool", out, orow[:], reads=[borow], writes=[bo])
        p.finish([bo])
    return nc


_STAGES = {}


def _stage(kind, **kw):
    key = (kind, tuple(sorted(kw.items())))
    if key not in _STAGES:
        _STAGES[key] = build_ada() if kind == "ada" else build_stage(kind, **kw)
    return _STAGES[key]


def kernel_unfused(**z):
    z = {k: np.asarray(v) for k, v in z.items()}
    f = np.ascontiguousarray
    cores = list(range(8))
    ident = np.eye(128, dtype=np.float32)
    cvec = f(np.concatenate([z["c"], z["c_ctx"][None]], 0).astype(np.float32))
    ins = [dict(ident=ident, cvec=cvec, aw=f(z["ada_w"][k // 2][:, (k % 2) * 3072:(k % 2 + 1) * 3072]),
                ab=f(z["ada_b"][k // 2][None, (k % 2) * 3072:(k % 2 + 1) * 3072])) for k in cores]
    r = run_bass_kernel_spmd(_stage("ada"), ins, core_ids=cores).results
    mods = np.stack([np.concatenate([r[2 * i]["m"], r[2 * i + 1]["m"]], 1) for i in range(4)]).reshape(4, 5, 6, D)
    zero_v = np.zeros(D, np.float32)

    def vec_for(li, b, pgx, pgc):
        return make_vec(mods[li, b], mods[li, 4], z["norm1_g"][li], z["norm2_g"][li], z["final_norm_g"], pgx, pgc)

    xprev = [f(np.concatenate([z["x"][b], z["ctx"][b]], 0)) for b in range(4)]
    part = [np.zeros((NTOK, D), np.float32) for _ in cores]
    ss = None
    pg = [(zero_v, zero_v) for _ in range(4)]
    plan = []
    for li in range(4):
        plan.append((("gdn", "ssd", "mla")[li % 3], li))
        plan.append(("moe", li))
    plan.append(("final", 3))
    for kind, li in plan:
        with_ss = ss is not None
        nc = _stage(kind, with_ss=True) if with_ss else _stage(kind)
        ins = []
        for k in cores:
            b, h = divmod(k, 2)
            d = dict(ident=ident, xprev=xprev[b], pa=part[2 * b], pb=part[2 * b + 1], vec=vec_for(li, b, *pg[b]))
            if with_ss:
                d.update(ssa=ss[2 * b], ssb=ss[2 * b + 1])
            if kind == "gdn":
                d.update(gdn_weights(z, li // 3, h))
            elif kind == "ssd":
                d.update(ssd_weights(z, h))
            elif kind == "mla":
                d.update(mla_weights(z["mla_w_in"][0], z["mla_w_uq"][0], z["mla_w_ukv"][0], z["mla_w_out"][0],
                                     z["mla_q_norm_g"][0], z["mla_kv_norm_g"][0], h))
            elif kind == "moe":
                perm = list(range(8 * h, 8 * h + 8)) + list(range(8 * (1 - h), 8 * (1 - h) + 8))
                d.update(wr=f(z["router_w"][li][:, perm]), wg=f(z["moe_w_gate"][li][8 * h:8 * h + 8].reshape(8 * D, D)),
                         wu=f(z["moe_w_up"][li][8 * h:8 * h + 8].reshape(8 * D, D)), wd=f(z["moe_w_down"][li][8 * h:8 * h + 8].reshape(8 * D, D)))
            ins.append(d)
        r = run_bass_kernel_spmd(nc, ins, core_ids=cores).results
        if kind == "final":
            return f(np.stack([r[2 * b]["p"][:4096] for b in range(4)]).astype(np.float32))
        xprev = [r[2 * b]["xo"] for b in range(4)]
        part = [r[k]["p"] for k in cores]
        ss = [r[k]["ss"] for k in cores] if kind == "ssd" else None
        gi = 5 if kind == "moe" else 2
        pg = [(mods[li, b, gi], mods[li, 4, gi]) for b in range(4)]


def decl_weights(c, kind, pre):
    d = lambda n, sh: c.din(pre + n, sh)
    if kind == "moe":
        return dict(wr=d("wr", [D, NE]), wg=d("wg", [NEH * D, D]), wu=d("wu", [NEH * D, D]), wd=d("wd", [NEH * D, D]))
    if kind == "mla":
        return dict(w_in=d("w_in", [D, 1088]), w_uqn=d("w_uqn", [768, 512]), w_uqr=d("w_uqr", [768, 256]), w_uqs=d("w_uqs", [768, 256]),
                    w_ukn=d("w_ukn", [256, 512]), w_ukv=d("w_ukv", [256, 512]), w_out=d("w_out", [512, D]), gq=d("gq", [1, 768]), gkv=d("gkv", [1, 256]),
                    cosT=d("cosT", [32, 4096]), sinT=d("sinT", [32, 4096]), cos_tm=d("cos_tm", [4096, 32]), sin_tm=d("sin_tm", [4096, 32]))
    if kind == "ssd":
        return dict(w_cv=d("w_cv", [D, 2048]), convp=d("convp", [16, 128, 4]), w_z=d("w_z", [D, D]), w_dt=d("w_dt", [D, 32]), dtb=d("dtb", [1, 32]),
                    alog=d("alog", [1, 32]), dvec=d("dvec", [1, D]), ng=d("ng", [1, D]), w_out=d("w_out", [D, D]), triF=d("triF", [128, 128]), triB=d("triB", [128, 128]))
    if kind == "gdn":
        return dict(w_cv=d("w_cv", [D, 1536]), convp=d("convp", [12, 128, 4]), w_z=d("w_z", [D, 512]), w_bg=d("w_bg", [D, 16]), dtb=d("dtb", [1, 8]),
                    alog=d("alog", [1, 8]), ng=d("ng", [1, 128]), w_out=d("w_out", [512, D]), masks=d("masks", [2, 7, 128, 128]))
    raise ValueError(kind)


FUSED_PLAN = [("gdn", 0), ("moe", 0), ("ssd", 1), ("moe", 1), ("mla", 2), ("moe", 2), ("gdn", 3), ("moe", 3)]


def build_fused():
    nc = new_nc()
    es = ExitStack()
    with es:
        c = Ctx(nc, es); p = c.p
        K = common_consts(c, c.din("ident", [128, 128]))
        xin = c.din("xin", [NTOK, D]); cvec = c.din("cvec", [2, D])
        ada_w = c.din("ada_w", [4 * D, 6 * D]); ada_b = c.din("ada_b", [4, 6 * D]); gains = c.din("gains", [9, D])
        out = c.dout("out", [4096, D])
        vecs, bv = emit_ada(c, K, cvec, ada_w, Buf(), ada_b)
        Xc = dram(c, [NTOK, D]); bXc = [Buf() for _ in range(NT)]
        for t in range(NT):
            p.dma("sp", Xc[t * 128:(t + 1) * 128, :], xin[t * 128:(t + 1) * 128, :], writes=[bXc[t]])
        prev = None

        def make_vec(li):
            vs = dram(c, [NVEC, D]); bvs = Buf()
            rows = [vecs[li, 0:1, j * D:(j + 1) * D] for j in range(6)] + [vecs[li, 1:2, j * D:(j + 1) * D] for j in range(6)]
            rows += [gains[li:li + 1, :], gains[4 + li:5 + li, :], gains[8:9, :]]
            if prev is not None:
                pli, gi = prev[8], prev[9]
                rows += [vecs[pli, 0:1, gi * D:(gi + 1) * D], vecs[pli, 1:2, gi * D:(gi + 1) * D]]
            else:
                rows += [gains[8:9, :], gains[8:9, :]]
            for r_, src in enumerate(rows):
                p.dma("sp", vs[r_:r_ + 1, :], src, reads=[bv], writes=[bvs])
            return vs, bvs

        def fold(li):
            nonlocal Xc, bXc
            vs, bvs = make_vec(li)
            c.vec_reads = [bvs]
            if prev is not None:
                Pa, bPa, Pb, bPb, SSa, bSSa, SSb, bSSb = prev[:8]
                Xn = dram(c, [NTOK, D]); bXn = [Buf() for _ in range(NT)]
                c.res_reads = list(bXc) + list(bPa) + list(bPb) + (list(bSSa) + list(bSSb) if SSa is not None else [])
                emit_residual_in(c, K, Xc, Pa, Pb, vs, Xn, bXn, None, SSa, SSb)
                c.res_reads = []
                Xc, bXc = Xn, bXn
            return vs

        for kind, li in FUSED_PLAN:
            vs = fold(li)
            Ps = []
            for h in range(2):
                w = decl_weights(c, kind, f"L{li}h{h}_")
                P = dram(c, [NTOK, D]); bP = [Buf() for _ in range(NT)]
                SS = bSS = None
                es1 = ExitStack()
                with es1:
                    if kind == "moe":
                        mods = mods_from_vec(c, es1, vs, 3, 13)
                        emit_moe(c, K, Xc, bXc, mods, w["wr"], w["wg"], w["wu"], w["wd"], Buf(), P, bP)
                    else:
                        mods = mods_from_vec(c, es1, vs, 0, 12)
                        if kind == "ssd":
                            SS = dram(c, [NTOK, 1]); bSS = [Buf() for _ in range(NT)]
                            emit_ssd(c, K, Xc, bXc, mods, P=P, bP=bP, SS=SS, bSS=bSS, **w)
                        elif kind == "mla":
                            emit_mla(c, K, Xc, bXc, mods, P=P, bP=bP, **w)
                        else:
                            emit_gdn(c, K, Xc, bXc, mods, P=P, bP=bP, **w)
                    p.barrier()
                Ps.append((P, bP, SS, bSS))
            prev = (Ps[0][0], Ps[0][1], Ps[1][0], Ps[1][1], Ps[0][2], Ps[0][3], Ps[1][2], Ps[1][3], li, 5 if kind == "moe" else 2)
        fold(3)
        es1 = ExitStack()
        bo = []
        with es1:
            fg = bc_tile(c, es1, gains[8:9, :])
            xt = [c.sb([128, D], es1) for _ in range(2)]; bxt = [Buf(), Buf()]
            ot = [c.sb([128, D], es1) for _ in range(2)]; bot = [Buf(), Buf()]
            for t in range(32):
                i = t % 2
                p.dma("sp", xt[i][:], Xc[t * 128:(t + 1) * 128, :], reads=[bXc[t]], writes=[bxt[i]])
                norm_mod_T(c, K, xt[i][:], bxt[i], fg, None, None, None)
                p.op("act", lambda e: e.copy(ot[i][:], K["h"][:]), reads=[K["bh"]], writes=[bot[i]])
                b = Buf(); bo.append(b)
                p.dma("pool", out[t * 128:(t + 1) * 128, :], ot[i][:], reads=[bot[i]], writes=[b])
        p.finish(bo)
        print("fused ninst", p.ninst, "nsem", p.nsem)
    return nc


def fused_inputs(z, b):
    f = np.ascontiguousarray
    d = dict(ident=np.eye(128, dtype=np.float32), xin=f(np.concatenate([z["x"][b], z["ctx"][b]], 0)),
             cvec=f(np.stack([z["c"][b], z["c_ctx"]]).astype(np.float32)), ada_w=f(z["ada_w"].reshape(4 * D, 6 * D)), ada_b=f(z["ada_b"]),
             gains=f(np.concatenate([z["norm1_g"], z["norm2_g"], z["final_norm_g"][None]], 0).astype(np.float32)))
    for kind, li in FUSED_PLAN:
        for h in range(2):
            pre = f"L{li}h{h}_"
            if kind == "gdn":
                w = gdn_weights(z, li // 3, h)
            elif kind == "ssd":
                w = ssd_weights(z, h)
            elif kind == "mla":
                w = mla_weights(z["mla_w_in"][0], z["mla_w_uq"][0], z["mla_w_ukv"][0], z["mla_w_out"][0], z["mla_q_norm_g"][0], z["mla_kv_norm_g"][0], h)
            else:
                perm = list(range(8 * h, 8 * h + 8)) + list(range(8 * (1 - h), 8 * (1 - h) + 8))
                w = dict(wr=f(z["router_w"][li][:, perm]), wg=f(z["moe_w_gate"][li][8 * h:8 * h + 8].reshape(8 * D, D)),
                         wu=f(z["moe_w_up"][li][8 * h:8 * h + 8].reshape(8 * D, D)), wd=f(z["moe_w_down"][li][8 * h:8 * h + 8].reshape(8 * D, D)))
            d.update({pre + k: v for k, v in w.items()})
    return d


def kernel_fused_dup(**z):
    z = {k: np.asarray(v) for k, v in z.items()}
    nc = _stage_fused()
    per_sample = [fused_inputs(z, b) for b in range(4)]
    ins = [per_sample[k // 2] for k in range(8)]
    r = run_bass_kernel_spmd(nc, ins, core_ids=list(range(8))).results
    return np.ascontiguousarray(np.stack([r[2 * b]["out"] for b in range(4)]).astype(np.float32))


def _stage_fused():
    if "fused" not in _STAGES:
        _STAGES["fused"] = build_fused()
    return _STAGES["fused"]


CH_ROWS = 256
NCHUNK = NTOK // CH_ROWS


def build_fused_pair():
    nc = new_nc()
    es = ExitStack()
    with es:
        c = Ctx(nc, es); p = c.p
        K = common_consts(c, c.din("ident", [128, 128]))
        xin = c.din("xin", [NTOK, D]); cvec = c.din("cvec", [2, D])
        ada_w = c.din("ada_w", [4 * D, 6 * D]); ada_b = c.din("ada_b", [4, 6 * D]); gains = c.din("gains", [9, D])
        out = c.dout("out", [4096, D])
        vecs, bv = emit_ada(c, K, cvec, ada_w, Buf(), ada_b)
        Xc = dram(c, [NTOK, D]); bXc = [Buf() for _ in range(NT)]
        for t in range(NT):
            p.dma("sp", Xc[t * 128:(t + 1) * 128, :], xin[t * 128:(t + 1) * 128, :], writes=[bXc[t]])
        prev = None

        def make_vec(li):
            vs = dram(c, [NVEC, D]); bvs = Buf()
            rows = [vecs[li, 0:1, j * D:(j + 1) * D] for j in range(6)] + [vecs[li, 1:2, j * D:(j + 1) * D] for j in range(6)]
            rows += [gains[li:li + 1, :], gains[4 + li:5 + li, :], gains[8:9, :]]
            if prev is not None:
                pli, gi = prev[4], prev[5]
                rows += [vecs[pli, 0:1, gi * D:(gi + 1) * D], vecs[pli, 1:2, gi * D:(gi + 1) * D]]
            else:
                rows += [gains[8:9, :], gains[8:9, :]]
            for r_, src in enumerate(rows):
                p.dma("sp", vs[r_:r_ + 1, :], src, reads=[bv], writes=[bvs])
            return vs, bvs

        def fold(li):
            nonlocal Xc, bXc
            vs, bvs = make_vec(li)
            c.vec_reads = [bvs]
            if prev is not None:
                G, bG, GSS, bGSS = prev[:4]
                Xn = dram(c, [NTOK, D]); bXn = [Buf() for _ in range(NT)]
                c.res_reads = list(bXc) + list(bG) + ([bGSS] if GSS is not None else [])

                def tile_aps(t):
                    j, off = divmod(t * 128, CH_ROWS)
                    return G[j][off:off + 128, :], G[j][CH_ROWS + off:CH_ROWS + off + 128, :]
                c.res_tile_aps = tile_aps
                ssa = GSS[0:NTOK, :] if GSS is not None else None
                ssb = GSS[NTOK:2 * NTOK, :] if GSS is not None else None
                emit_residual_in(c, K, Xc, None, None, vs, Xn, bXn, None, ssa, ssb)
                c.res_reads = []
                c.res_tile_aps = None
                Xc, bXc = Xn, bXn
            return vs

        for kind, li in FUSED_PLAN:
            vs = fold(li)
            w = decl_weights(c, kind, f"L{li}_")
            P = dram(c, [NTOK, D]); bP = [Buf() for _ in range(NT)]
            SS = bSS = None
            es1 = ExitStack()
            with es1:
                if kind == "moe":
                    mods = mods_from_vec(c, es1, vs, 3, 13)
                    emit_moe(c, K, Xc, bXc, mods, w["wr"], w["wg"], w["wu"], w["wd"], Buf(), P, bP)
                else:
                    mods = mods_from_vec(c, es1, vs, 0, 12)
                    if kind == "ssd":
                        SS = dram(c, [NTOK, 1]); bSS = [Buf() for _ in range(NT)]
                        emit_ssd(c, K, Xc, bXc, mods, P=P, bP=bP, SS=SS, bSS=bSS, **w)
                    elif kind == "mla":
                        emit_mla(c, K, Xc, bXc, mods, P=P, bP=bP, **w)
                    else:
                        emit_gdn(c, K, Xc, bXc, mods, P=P, bP=bP, **w)
                p.barrier()
            G, bG = [], []
            for j in range(NCHUNK):
                src = dram(c, [CH_ROWS, D]); bs = Buf()
                p.dma("sp", src, P[j * CH_ROWS:(j + 1) * CH_ROWS, :], reads=[bP[2 * j], bP[2 * j + 1]], writes=[bs])
                g = dram(c, [2 * CH_ROWS, D]); bg = Buf()
                allgather(c, src, g, GRP_PAIR, [bs], [bg])
                G.append(g); bG.append(bg)
            GSS = bGSS = None
            if SS is not None:
                GSS = dram(c, [2 * NTOK, 1]); bGSS = Buf()
                allgather(c, SS, GSS, GRP_PAIR, list(bSS), [bGSS])
            prev = (G, bG, GSS, bGSS, li, 5 if kind == "moe" else 2)
        fold(3)
        es1 = ExitStack()
        bo = []
        with es1:
            fg = bc_tile(c, es1, gains[8:9, :])
            xt = [c.sb([128, D], es1) for _ in range(2)]; bxt = [Buf(), Buf()]
            ot = [c.sb([128, D], es1) for _ in range(2)]; bot = [Buf(), Buf()]
            for t in range(32):
                i = t % 2
                p.dma("sp", xt[i][:], Xc[t * 128:(t + 1) * 128, :], reads=[bXc[t]], writes=[bxt[i]])
                norm_mod_T(c, K, xt[i][:], bxt[i], fg, None, None, None)
                p.op("act", lambda e: e.copy(ot[i][:], K["h"][:]), reads=[K["bh"]], writes=[bot[i]])
                b = Buf(); bo.append(b)
                p.dma("pool", out[t * 128:(t + 1) * 128, :], ot[i][:], reads=[bot[i]], writes=[b])
        p.finish(bo)
        print("fused-pair ninst", p.ninst, "nsem", p.nsem)
    return nc


def fused_pair_inputs(z, b, h):
    f = np.ascontiguousarray
    d = dict(ident=np.eye(128, dtype=np.float32), xin=f(np.concatenate([z["x"][b], z["ctx"][b]], 0)),
             cvec=f(np.stack([z["c"][b], z["c_ctx"]]).astype(np.float32)), ada_w=f(z["ada_w"].reshape(4 * D, 6 * D)), ada_b=f(z["ada_b"]),
             gains=f(np.concatenate([z["norm1_g"], z["norm2_g"], z["final_norm_g"][None]], 0).astype(np.float32)))
    for kind, li in FUSED_PLAN:
        pre = f"L{li}_"
        if kind == "gdn":
            w = gdn_weights(z, li // 3, h)
        elif kind == "ssd":
            w = ssd_weights(z, h)
        elif kind == "mla":
            w = mla_weights(z["mla_w_in"][0], z["mla_w_uq"][0], z["mla_w_ukv"][0], z["mla_w_out"][0], z["mla_q_norm_g"][0], z["mla_kv_norm_g"][0], h)
        else:
            perm = list(range(8 * h, 8 * h + 8)) + list(range(8 * (1 - h), 8 * (1 - h) + 8))
            w = dict(wr=f(z["router_w"][li][:, perm]), wg=f(z["moe_w_gate"][li][8 * h:8 * h + 8].reshape(8 * D, D)),
                     wu=f(z["moe_w_up"][li][8 * h:8 * h + 8].reshape(8 * D, D)), wd=f(z["moe_w_down"][li][8 * h:8 * h + 8].reshape(8 * D, D)))
        d.update({pre + k: v for k, v in w.items()})
    return d


def kernel(**z):
    z = {k: np.asarray(v) for k, v in z.items()}
    if "fused_pair" not in _STAGES:
        _STAGES["fused_pair"] = build_fused_pair()
    ins = [fused_pair_inputs(z, k // 2, k % 2) for k in range(8)]
    r = run_bass_kernel_spmd(_STAGES["fused_pair"], ins, core_ids=list(range(8))).results
    return np.ascontiguousarray(np.stack([r[2 * b]["out"] for b in range(4)]).astype(np.float32))
```

```python
import numpy as np
from contextlib import ExitStack
import concourse.bass as bass
import concourse.mybir as mybir
from concourse.bass_utils import run_bass_kernel_spmd

F32 = mybir.dt.float32
AF = mybir.ActivationFunctionType
ALU = mybir.AluOpType
AX = mybir.AxisListType

SEM_EPOCH = 20000
N_DMA_RING = 8
D = 1024
NE = 16


class Buf:
    __slots__ = ("name", "w", "r", "excl")

    def __init__(self, name=""):
        self.name = name
        self.w = None
        self.r = {}
        self.excl = False


def PBuf():
    b = Buf()
    b.excl = True
    return b


class Prog:
    def __init__(self, nc, es):
        self.nc = nc
        self.es = es
        self.eng = {"pe": nc.tensor, "act": nc.scalar, "dve": nc.vector, "pool": nc.gpsimd, "sp": nc.sync}
        self.sem = {}
        self.cnt = {}
        self.semown = {}
        self.seen = {e: {} for e in self.eng}
        self.nsem = 0
        self.ninst = 0
        self.allsems = []
        for e in self.eng:
            self._new_sem(e)
        self.dring = {}
        self.dpos = {}

    def _mk(self, tag):
        self.nsem += 1
        return self.es.enter_context(self.nc.semaphore(f"{tag}{self.nsem}"))

    def _new_sem(self, e):
        s = self._mk("s" + e)
        self.sem[e] = s
        self.cnt[e] = 0
        self.semown[id(s)] = e

    def _wait(self, e, tok):
        s, v = tok
        if e == "pe" and self.semown.get(id(s)) == "pe":
            return
        if self.seen[e].get(id(s), 0) >= v:
            return
        self.eng[e].wait_ge(s, v)
        self.ninst += 1
        self.seen[e][id(s)] = v

    def _deps(self, e, reads, writes):
        for b in reads:
            if b.w is not None:
                self._wait(e, b.w)
            if b.excl:
                for tok in list(b.r.values()):
                    if self.semown.get(id(tok[0])) != e:
                        self._wait(e, tok)
        for b in writes:
            if b.w is not None:
                self._wait(e, b.w)
            for tok in list(b.r.values()):
                self._wait(e, tok)

    def _mark(self, tok, reads, writes):
        s, v = tok
        for b in reads:
            b.r[id(s)] = tok
        for b in writes:
            b.w = tok
            b.r = {}

    def op(self, e, fn, reads=(), writes=()):
        self._deps(e, reads, writes)
        ins = fn(self.eng[e])
        if self.cnt[e] >= SEM_EPOCH:
            self._new_sem(e)
        self.cnt[e] += 1
        ins.then_inc(self.sem[e], 1)
        self.ninst += 1
        self._mark((self.sem[e], self.cnt[e]), reads, writes)
        return ins

    def dma(self, q, out, in_, reads=(), writes=(), **kw):
        self._deps(q, reads, writes)
        if q not in self.dring:
            self.dring[q] = [[self._mk("d" + q), 0] for _ in range(N_DMA_RING)]
            self.dpos[q] = 0
        slot = self.dring[q][self.dpos[q] % N_DMA_RING]
        self.dpos[q] += 1
        if slot[1] > 0:
            self._wait(q, (slot[0], slot[1]))
        slot[1] += 16
        self.eng[q].dma_start(out=out, in_=in_, **kw).then_inc(slot[0], 16)
        self.ninst += 1
        self._mark((slot[0], slot[1]), reads, writes)

    def barrier(self):
        toks = [(self.sem[f], self.cnt[f]) for f in self.eng if self.cnt[f] > 0]
        for q in self.dring:
            toks += [(s, v) for s, v in self.dring[q] if v > 0]
        for e in self.eng:
            for t in toks:
                if self.semown.get(id(t[0])) == e:
                    continue
                self._wait(e, t)

    def finish(self, bufs, e="sp"):
        for b in bufs:
            if b.w is not None:
                self._wait(e, b.w)


class Ctx:
    def __init__(self, nc, es):
        self.nc = nc
        self.es = es
        self.p = Prog(nc, es)
        self.n = 0

    def sb(self, shape, es=None, dt=F32):
        self.n += 1
        return (es or self.es).enter_context(self.nc.sbuf_tensor(f"sb{self.n}", list(shape), dt))

    def ps(self, shape, es=None):
        self.n += 1
        return (es or self.es).enter_context(self.nc.psum_tensor(f"ps{self.n}", list(shape), F32))

    def din(self, name, shape):
        return self.nc.dram_tensor(name, list(shape), F32, kind="ExternalInput").ap()

    def dout(self, name, shape):
        return self.nc.dram_tensor(name, list(shape), F32, kind="ExternalOutput").ap()


def load_bc(c, q, dst, vec_row, buf):
    c.p.dma(q, dst, vec_row.partition_broadcast(128), writes=[buf])


def norm_mod_T(c, K, xt, xb, G, S, hT_dst, hb):
    p = c.p
    p.op("act", lambda e: e.activation(K["junk"][:], xt, AF.Square, accum_out=K["ss"][:, 0:1]),
         reads=[xb], writes=[K["bjunk"], K["bss"]])
    p.op("act", lambda e: e.activation(K["ss"][:, 1:2], K["ss"][:, 0:1], AF.Sqrt, bias=K["eps"][:, 0:1], scale=1.0 / D),
         reads=[K["bss"]], writes=[K["bss"]])
    p.op("dve", lambda e: e.reciprocal(K["ss"][:, 2:3], K["ss"][:, 1:2]), reads=[K["bss"]], writes=[K["bss"]])
    if G is not None:
        p.op("dve", lambda e: e.scalar_tensor_tensor(K["h"][:], xt, K["ss"][:, 2:3], G[0][:], ALU.mult, ALU.mult),
             reads=[xb, K["bss"], G[1]], writes=[K["bh"]])
    else:
        p.op("dve", lambda e: e.tensor_scalar(K["h"][:], xt, K["ss"][:, 2:3], None, ALU.mult),
             reads=[xb, K["bss"]], writes=[K["bh"]])
    if S is not None:
        p.op("pool", lambda e: e.tensor_add(K["h"][:], K["h"][:], S[0][:]), reads=[K["bh"], S[1]], writes=[K["bh"]])
    if hT_dst is None:
        return
    for half in range(2):
        for cc in range(4):
            ch = half * 4 + cc
            p.op("pe", lambda e: e.transpose(K["ptr"][:, cc * 128:(cc + 1) * 128], K["h"][:, ch * 128:(ch + 1) * 128], K["ident"][:]),
                 reads=[K["bh"], K["bident"]], writes=[K["bptr"]])
        p.op("act", lambda e: e.copy(hT_dst[:, half * 4:(half + 1) * 4, :], K["ptr"][:].rearrange("p (c t) -> p c t", c=4)),
             reads=[K["bptr"]], writes=[hb])


def common_consts(c, ident_d):
    K = {}
    K["ident"] = c.sb([128, 128]); K["bident"] = Buf()
    c.p.dma("pool", K["ident"][:], ident_d, writes=[K["bident"]])
    K["junk"] = c.sb([128, D]); K["bjunk"] = Buf()
    K["h"] = c.sb([128, D]); K["bh"] = Buf()
    K["ss"] = c.sb([128, 4]); K["bss"] = Buf()
    K["eps"] = c.sb([128, 1]); K["beps"] = Buf()
    c.p.op("dve", lambda e: e.memset(K["eps"][:], 1e-6), writes=[K["beps"], K["bss"]])
    K["ptr"] = c.ps([128, 512]); K["bptr"] = PBuf()
    return K


GRP_PAIR = [[0, 1], [2, 3], [4, 5], [6, 7]]
GRP_QUAD = [[0, 2, 4, 6], [1, 3, 5, 7]]
GRP_ALL = [list(range(8))]
NTOK = 4352
NT = 34


def new_nc():
    return bass.Bass("TRN2", target_bir_lowering=False, num_devices=8)


def allgather(c, src, dst, groups, reads, writes):
    p = c.p
    p._deps("pool", reads, writes)
    if not hasattr(c, "_cc_sem"):
        c._cc_sem = p._mk("cc")
        c._cc_cnt = 0
    if c._cc_cnt > 0:
        p._wait("pool", (c._cc_sem, c._cc_cnt))
    c._cc_cnt += 1
    c.nc.gpsimd.collective_compute("AllGather", ALU.bypass, replica_groups=groups, ins=[src.opt()], outs=[dst.opt()]).then_inc(c._cc_sem)
    p.ninst += 1
    p._mark((c._cc_sem, c._cc_cnt), reads, writes)


def dram(c, shape):
    c.n += 1
    return c.nc.dram_tensor(f"dr{c.n}", list(shape), F32).ap()


def gather_w(c, name, rows, cols, groups=GRP_QUAD):
    g = len(groups[0])
    ext = c.din(name, [rows // g, cols])
    src = dram(c, [rows // g, cols])
    full = dram(c, [rows, cols])
    bs, bf = Buf(), Buf()
    c.p.dma("sp", src, ext, writes=[bs])
    allgather(c, src, full, groups, [bs], [bf])
    return full, bf


def emit_ada(c, K, cvec, ada_w_full, baw, ada_b):
    p = c.p
    vecs = dram(c, [4, 2, 6 * D])
    bv = Buf()
    es = ExitStack()
    with es:
        sc = c.sb([128, 8, 2], es); bsc = Buf()
        craw = c.sb([16, 128], es); bcraw = Buf()
        p.dma("pool", craw[:], cvec.rearrange("k (c p) -> (k c) p", p=128), writes=[bcraw])
        p.op("pe", lambda e: e.transpose(K["ptr"][:, 0:16], craw[:], K["ident"][0:16, 0:16]), reads=[bcraw, K["bident"]], writes=[K["bptr"]])
        p.op("act", lambda e: e.activation(sc[:].rearrange("p c k -> p k c"), K["ptr"][:, 0:16].rearrange("p (k c) -> p k c", k=2), AF.Silu),
             reads=[K["bptr"]], writes=[bsc])
        ab = c.sb([2, 6 * D], es); bab = Buf()
        orow = c.sb([2, 6 * D], es); borow = Buf()
        wt = [c.sb([128, 8, 512], es) for _ in range(2)]; bwt = [Buf(), Buf()]
        pa = [c.ps([128, 512], es) for _ in range(2)]; bpa = [PBuf(), PBuf()]
        n = 0
        for i in range(4):
            for k in range(2):
                p.dma("pool", ab[k:k + 1, :], ada_b[i:i + 1, :], writes=[bab])
            for g in range(12):
                j = n % 2
                n += 1
                p.dma("sp", wt[j][:], ada_w_full[i * D:(i + 1) * D, g * 512:(g + 1) * 512].rearrange("(c p) f -> p c f", p=128),
                      reads=[baw], writes=[bwt[j]])
                for ch in range(8):
                    p.op("pe", lambda e: e.matmul(pa[j][0:2, :], sc[:, ch, :], wt[j][:, ch, :], start=(ch == 0), stop=(ch == 7)),
                         reads=[bsc, bwt[j]], writes=[bpa[j]])
                p.op("dve", lambda e: e.tensor_add(orow[:, g * 512:(g + 1) * 512], pa[j][0:2, :], ab[:, g * 512:(g + 1) * 512]),
                     reads=[bpa[j], bab], writes=[borow])
            p.dma("pool", vecs[i], orow[:], reads=[borow], writes=[bv])
        p.barrier()
    return vecs, bv


def load_mods(c, es, vecs, bv, i, base, norm_g_row):
    p = c.p
    g2 = c.sb([128, D], es); bg2 = Buf()
    load_bc(c, "pool", g2[:], norm_g_row, bg2)
    mods = {}
    for k, kind in enumerate("xc"):
        t = []
        for j in range(3):
            tt = c.sb([128, D], es); bt = Buf()
            p.dma("pool", tt[:], vecs[i, k:k + 1, (base + j) * D:(base + j + 1) * D].partition_broadcast(128), reads=[bv], writes=[bt])
            t.append((tt, bt))
        Sh, Sc, Ga = t
        p.op("dve", lambda e: e.scalar_tensor_tensor(Sc[0][:], Sc[0][:], 1.0, g2[:], ALU.add, ALU.mult),
             reads=[bg2, Sc[1]], writes=[Sc[1]])
        mods[kind] = (Sc, Sh, Ga)
    return mods


def tile_kind(t):
    return "x" if t < 32 else "c"


def emit_residual(c, K, X, bX, P, bP, mods):
    p = c.p
    G = dram(c, [2 * NTOK, D]); bG = Buf()
    allgather(c, P, G, GRP_PAIR, bP, [bG])
    es = ExitStack()
    with es:
        xt = [c.sb([128, D], es) for _ in range(2)]; bxt = [Buf(), Buf()]
        ga = [c.sb([128, D], es) for _ in range(2)]; bga = [Buf(), Buf()]
        gb = [c.sb([128, D], es) for _ in range(2)]; bgb = [Buf(), Buf()]
        for t in range(NT):
            i = t % 2
            Ga = mods[tile_kind(t)][2]
            p.dma("sp", xt[i][:], X[t * 128:(t + 1) * 128, :], reads=[bX[t]], writes=[bxt[i]])
            p.dma("sp", ga[i][:], G[t * 128:(t + 1) * 128, :], reads=[bG], writes=[bga[i]])
            p.dma("sp", gb[i][:], G[NTOK + t * 128:NTOK + (t + 1) * 128, :], reads=[bG], writes=[bgb[i]])
            p.op("pool", lambda e: e.tensor_add(ga[i][:], ga[i][:], gb[i][:]), reads=[bga[i], bgb[i]], writes=[bga[i]])
            if ssa is not None:
                p.dma("sp", sst[:, 0:1], ssa[sl, :], reads=rr, writes=[bsst])
                p.dma("sp", sst[:, 1:2], ssb[sl, :], reads=rr, writes=[bsst])
                p.op("dve", lambda e: e.tensor_add(sst[:, 2:3], sst[:, 0:1], sst[:, 1:2]), reads=[bsst], writes=[bsst])
                p.op("act", lambda e: e.activation(sst[:, 3:4], sst[:, 2:3], AF.Sqrt, bias=K["eps"][:, 0:1], scale=1.0 / 2048), reads=[bsst], writes=[bsst])
                p.op("dve", lambda e: e.reciprocal(sst[:, 4:5], sst[:, 3:4]), reads=[bsst], writes=[bsst])
                p.op("dve", lambda e: e.scalar_tensor_tensor(ga[i][:], ga[i][:], sst[:, 4:5], Ga[0][:], ALU.mult, ALU.mult),
                     reads=[bga[i], Ga[1], bsst], writes=[bga[i]])
            else:
                p.op("dve", lambda e: e.tensor_mul(ga[i][:], ga[i][:], Ga[0][:]), reads=[bga[i], Ga[1]], writes=[bga[i]])
            p.op("dve", lambda e: e.tensor_add(xt[i][:], xt[i][:], ga[i][:]), reads=[bga[i], bxt[i]], writes=[bxt[i]])
            p.dma("pool", X[t * 128:(t + 1) * 128, :], xt[i][:], reads=[bxt[i]], writes=[bX[t]])
        p.barrier()


CAP_X = 512
CAP_C = 32
N_BISECT = 36
NEH = 8


def emit_moe(c, K, X, bX, mods, wr, wg, wu, wd, bw, P, bP):
    p = c.p
    es0 = ExitStack()
    with es0:
        wr_sb = c.sb([128, 8, NE], es0); bwr = Buf()
        p.dma("pool", wr_sb[:], wr.rearrange("(c p) e -> p c e", p=128), writes=[bwr])
        ones16 = c.sb([16, 128], es0); bones = Buf()
        p.op("dve", lambda e: e.memset(ones16[:], 1.0), writes=[bones])
        xt = [c.sb([128, D], es0) for _ in range(2)]
        bxt = [Buf() for _ in range(2)]
        pmisc = c.ps([128, 512], es0); bpm = PBuf()
        sm = c.sb([128, 8], es0); bsm = Buf()
        ex = c.sb([128, NE], es0); bex = Buf()
        thr_bc = {k: c.sb([128, NE], es0) for k in "xc"}
        bthr = {k: Buf() for k in "xc"}

        def router(hT_src, hb, aff_dst, baff):
            for ch in range(8):
                p.op("pe", lambda e: e.matmul(pmisc[:, 0:NE], hT_src[:, ch, :], wr_sb[:, ch, :], start=(ch == 0), stop=(ch == 7)),
                     reads=[hb, bwr], writes=[bpm])
            p.op("dve", lambda e: e.reduce_max(sm[:, 0:1], pmisc[:, 0:NE], AX.X), reads=[bpm], writes=[bsm])
            p.op("dve", lambda e: e.tensor_scalar(sm[:, 1:2], sm[:, 0:1], -1.0, None, ALU.mult), reads=[bsm], writes=[bsm])
            p.op("act", lambda e: e.activation(ex[:], pmisc[:, 0:NE], AF.Exp, bias=sm[:, 1:2], accum_out=sm[:, 2:3]),
                 reads=[bpm, bsm], writes=[bex, bsm])
            p.op("dve", lambda e: e.reciprocal(sm[:, 3:4], sm[:, 2:3]), reads=[bsm], writes=[bsm])
            p.op("dve", lambda e: e.tensor_scalar(aff_dst, ex[:], sm[:, 3:4], None, ALU.mult), reads=[bex, bsm], writes=[baff])

        es1 = ExitStack()
        with es1:
            affT = c.sb([16, NT * 128], es1); baffT = Buf()
            mask = c.sb([16, 32 * 128], es1); bmask = Buf()
            hT1 = c.sb([128, 8, 128], es1); bhT1 = Buf()
            aff1 = c.sb([128, NE], es1); baff1 = Buf()
            bs = c.sb([16, 8], es1); bbs = Buf()
            diag = c.sb([16, 16], es1); bdiag = Buf()
            for t in range(NT):
                G, S, _ = mods[tile_kind(t)]
                i = t % 2
                p.dma("sp", xt[i][:], X[t * 128:(t + 1) * 128, :], reads=[bX[t]], writes=[bxt[i]])
                norm_mod_T(c, K, xt[i][:], bxt[i], G, S, hT1, bhT1)
                router(hT1, bhT1, aff1[:], baff1)
                p.op("pe", lambda e: e.transpose(pmisc[0:16, 128:256], aff1[:], K["ident"][:]),
                     reads=[baff1, K["bident"]], writes=[bpm])
                p.op("act", lambda e: e.copy(affT[:, t * 128:(t + 1) * 128], pmisc[0:16, 128:256]), reads=[bpm], writes=[baffT])
            for kind, lo_c, n_c, cap in (("x", 0, 32 * 128, CAP_X), ("c", 32 * 128, 2 * 128, CAP_C)):
                a = affT[:, lo_c:lo_c + n_c]
                m = mask[:, 0:n_c]
                lo, hi, mid, cnt, ge, d1, hh = (bs[:, j:j + 1] for j in range(7))
                p.op("dve", lambda e: e.memset(lo, 0.0), writes=[bbs])
                p.op("dve", lambda e: e.memset(hi, 1.0), writes=[bbs])
                for it in range(N_BISECT):
                    p.op("dve", lambda e: e.tensor_scalar(hh, hi, 0.5, None, ALU.mult), reads=[bbs], writes=[bbs])
                    p.op("dve", lambda e: e.scalar_tensor_tensor(mid, lo, 0.5, hh, ALU.mult, ALU.add), reads=[bbs], writes=[bbs])
                    p.op("dve", lambda e: e.tensor_scalar(m, a, mid, None, ALU.is_ge, ALU.add, accum_out=cnt),
                         reads=[baffT, bbs], writes=[bmask, bbs])
                    p.op("dve", lambda e: e.tensor_scalar(ge, cnt, cap - 0.5, None, ALU.is_ge), reads=[bbs], writes=[bbs])
                    p.op("dve", lambda e: e.tensor_sub(d1, mid, lo), reads=[bbs], writes=[bbs])
                    p.op("dve", lambda e: e.scalar_tensor_tensor(lo, d1, ge, lo, ALU.mult, ALU.add), reads=[bbs], writes=[bbs])
                    p.op("dve", lambda e: e.tensor_sub(d1, hi, mid), reads=[bbs], writes=[bbs])
                    p.op("dve", lambda e: e.scalar_tensor_tensor(hi, d1, ge, mid, ALU.mult, ALU.add), reads=[bbs], writes=[bbs])
                p.op("dve", lambda e: e.tensor_scalar(diag[:], K["ident"][0:16, 0:16], lo, None, ALU.mult),
                     reads=[bbs, K["bident"]], writes=[bdiag])
                p.op("pe", lambda e: e.matmul(pmisc[:, 256:256 + NE], ones16[:], diag[:], start=True, stop=True),
                     reads=[bones, bdiag], writes=[bpm])
                p.op("act", lambda e: e.copy(thr_bc[kind][:], pmisc[:, 256:256 + NE]), reads=[bpm], writes=[bthr[kind]])
            p.barrier()

        wg_sb = c.sb([128, 8, D], es0); bwg = Buf()
        wu_sb = c.sb([128, 8, D], es0); bwu = Buf()
        wd_sb = c.sb([128, 8, D], es0); bwd = Buf()
        hT = c.sb([128, 8, 512], es0); bhT = [Buf() for _ in range(4)]
        hid = c.sb([128, 8, 512], es0); bhid = [Buf() for _ in range(8)]
        acc = [c.sb([128, D], es0) for _ in range(4)]; bacc = [Buf() for _ in range(4)]
        gate = c.sb([128, 4, NE], es0); bgate = [Buf() for _ in range(4)]
        aff2 = c.sb([128, NE], es0); baff2 = Buf()
        msk2 = c.sb([128, NE], es0); bmsk2 = Buf()
        sg = [c.sb([128, 512], es0) for _ in range(2)]; bsg = [Buf() for _ in range(2)]
        pg = [c.ps([128, 512], es0) for _ in range(2)]; bpg = [PBuf() for _ in range(2)]
        pu = [c.ps([128, 512], es0) for _ in range(2)]; bpu = [PBuf() for _ in range(2)]
        py = [c.ps([128, 512], es0) for _ in range(2)]; bpy = [PBuf() for _ in range(2)]
        passes = [list(range(4 * q, 4 * q + 4)) for q in range(8)] + [[32, 33]]
        for tiles in passes:
            nt = len(tiles)
            N = nt * 128
            for k, t in enumerate(tiles):
                kind = tile_kind(t)
                G, S, _ = mods[kind]
                i = k % 2
                p.dma("pool", xt[i][:], X[t * 128:(t + 1) * 128, :], reads=[bX[t]], writes=[bxt[i]])
                norm_mod_T(c, K, xt[i][:], bxt[i], G, S, hT[:, :, k * 128:(k + 1) * 128], bhT[k])
                router(hT[:, :, k * 128:(k + 1) * 128], bhT[k], aff2[:], baff2)
                p.op("dve", lambda e: e.tensor_tensor(msk2[:], aff2[:], thr_bc[kind][:], ALU.is_ge),
                     reads=[baff2, bthr[kind]], writes=[bmsk2])
                p.op("dve", lambda e: e.tensor_mul(gate[:, k, :], msk2[:], aff2[:]), reads=[bmsk2, baff2], writes=[bgate[k]])
            for ex_i in range(NEH):
                p.dma("sp", wg_sb[:], wg[ex_i * D:(ex_i + 1) * D, :].rearrange("(c p) f -> p c f", p=128), reads=[bw], writes=[bwg])
                p.dma("pool", wu_sb[:], wu[ex_i * D:(ex_i + 1) * D, :].rearrange("(c p) f -> p c f", p=128), reads=[bw], writes=[bwu])
                p.dma("sp", wd_sb[:], wd[ex_i * D:(ex_i + 1) * D, :].rearrange("(c p) f -> p c f", p=128), reads=[bw], writes=[bwd])
                for fc in range(8):
                    j = fc % 2
                    for ch in range(8):
                        p.op("pe", lambda e: e.matmul(pg[j][:, 0:N], wg_sb[:, ch, fc * 128:(fc + 1) * 128], hT[:, ch, 0:N],
                                                      start=(ch == 0), stop=(ch == 7)),
                             reads=[bwg] + bhT[:nt], writes=[bpg[j]])
                    for ch in range(8):
                        p.op("pe", lambda e: e.matmul(pu[j][:, 0:N], wu_sb[:, ch, fc * 128:(fc + 1) * 128], hT[:, ch, 0:N],
                                                      start=(ch == 0), stop=(ch == 7)),
                             reads=[bwu] + bhT[:nt], writes=[bpu[j]])
                    p.op("act", lambda e: e.activation(sg[j][:, 0:N], pg[j][:, 0:N], AF.Silu), reads=[bpg[j]], writes=[bsg[j]])
                    p.op("dve", lambda e: e.tensor_mul(hid[:, fc, 0:N], sg[j][:, 0:N], pu[j][:, 0:N]),
                         reads=[bsg[j], bpu[j]], writes=[bhid[fc]])
                for k in range(nt):
                    for half in range(2):
                        for fc in range(8):
                            p.op("pe", lambda e: e.matmul(py[half][:], hid[:, fc, k * 128:(k + 1) * 128],
                                                          wd_sb[:, fc, half * 512:(half + 1) * 512],
                                                          start=(fc == 0), stop=(fc == 7)),
                                 reads=[bwd] + bhid, writes=[bpy[half]])
                        a_ap = acc[k][:, half * 512:(half + 1) * 512]
                        if ex_i == 0:
                            p.op("dve", lambda e: e.tensor_scalar(a_ap, py[half][:], gate[:, k, ex_i:ex_i + 1], None, ALU.mult),
                                 reads=[bpy[half], bgate[k]], writes=[bacc[k]])
                        else:
                            p.op("dve", lambda e: e.scalar_tensor_tensor(a_ap, py[half][:], gate[:, k, ex_i:ex_i + 1], a_ap, ALU.mult, ALU.add),
                                 reads=[bpy[half], bgate[k], bacc[k]], writes=[bacc[k]])
            for k, t in enumerate(tiles):
                p.dma("pool", P[t * 128:(t + 1) * 128, :], acc[k][:], reads=[bacc[k]], writes=[bP[t]])
        p.barrier()


def declare_moe_weights(c, li):
    wr = c.din(f"wr{li}", [D, NE])
    wg, b1 = gather_w(c, f"wg{li}", NEH * D, D)
    wu, b2 = gather_w(c, f"wu{li}", NEH * D, D)
    wd, b3 = gather_w(c, f"wd{li}", NEH * D, D)
    bw = Buf()
    for b in (b1, b2, b3):
        c.p._wait("sp", b.w)
    c.p.op("pool", lambda e: e.memset(c_dummy(c)[:], 0.0), reads=[b1, b2, b3], writes=[bw])
    return wr, wg, wu, wd, bw


def c_dummy(c):
    if not hasattr(c, "_dummy"):
        c._dummy = c.sb([128, 1])
    return c._dummy


NVEC = 17


def bc_tile(c, es, row, q="pool"):
    t = c.sb([128, D], es); b = Buf()
    c.p.dma(q, t[:], row.partition_broadcast(128), reads=list(getattr(c, "vec_reads", [])), writes=[b])
    return (t, b)


def mods_from_vec(c, es, vec, base, grow):
    p = c.p
    g = bc_tile(c, es, vec[grow:grow + 1, :])
    mods = {}
    for k, kind in enumerate("xc"):
        Sh = bc_tile(c, es, vec[6 * k + base:6 * k + base + 1, :])
        Sc = bc_tile(c, es, vec[6 * k + base + 1:6 * k + base + 2, :])
        p.op("dve", lambda e: e.scalar_tensor_tensor(Sc[0][:], Sc[0][:], 1.0, g[0][:], ALU.add, ALU.mult),
             reads=[g[1], Sc[1]], writes=[Sc[1]])
        mods[kind] = (Sc, Sh, None)
    return mods


def emit_residual_in(c, K, xprev, pa, pb, vec, X, bX, xo, ssa=None, ssb=None):
    p = c.p
    es = ExitStack()
    bo = []
    with es:
        gts = {"x": bc_tile(c, es, vec[15:16, :]), "c": bc_tile(c, es, vec[16:17, :])}
        xt = [c.sb([128, D], es) for _ in range(2)]; bxt = [Buf(), Buf()]
        ga = [c.sb([128, D], es) for _ in range(2)]; bga = [Buf(), Buf()]
        gb = [c.sb([128, D], es) for _ in range(2)]; bgb = [Buf(), Buf()]
        sst = c.sb([128, 8], es); bsst = Buf()
        for t in range(NT):
            i = t % 2
            Ga = gts[tile_kind(t)]
            sl = slice(t * 128, (t + 1) * 128)
            rr = list(getattr(c, "res_reads", []))
            p.dma("sp", xt[i][:], xprev[sl, :], reads=rr, writes=[bxt[i]])
            tp = getattr(c, "res_tile_aps", None)
            pa_t, pb_t = tp(t) if tp is not None else (pa[sl, :], pb[sl, :])
            p.dma("sp", ga[i][:], pa_t, reads=rr, writes=[bga[i]])
            p.dma("sp", gb[i][:], pb_t, reads=rr, writes=[bgb[i]])
            p.op("pool", lambda e: e.tensor_add(ga[i][:], ga[i][:], gb[i][:]), reads=[bga[i], bgb[i]], writes=[bga[i]])
            if ssa is not None:
                p.dma("sp", sst[:, 0:1], ssa[sl, :], reads=rr, writes=[bsst])
                p.dma("sp", sst[:, 1:2], ssb[sl, :], reads=rr, writes=[bsst])
                p.op("dve", lambda e: e.tensor_add(sst[:, 2:3], sst[:, 0:1], sst[:, 1:2]), reads=[bsst], writes=[bsst])
                p.op("act", lambda e: e.activation(sst[:, 3:4], sst[:, 2:3], AF.Sqrt, bias=K["eps"][:, 0:1], scale=1.0 / 2048), reads=[bsst], writes=[bsst])
                p.op("dve", lambda e: e.reciprocal(sst[:, 4:5], sst[:, 3:4]), reads=[bsst], writes=[bsst])
                p.op("dve", lambda e: e.scalar_tensor_tensor(ga[i][:], ga[i][:], sst[:, 4:5], Ga[0][:], ALU.mult, ALU.mult),
                     reads=[bga[i], Ga[1], bsst], writes=[bga[i]])
            else:
                p.op("dve", lambda e: e.tensor_mul(ga[i][:], ga[i][:], Ga[0][:]), reads=[bga[i], Ga[1]], writes=[bga[i]])
            p.op("dve", lambda e: e.tensor_add(xt[i][:], xt[i][:], ga[i][:]), reads=[bga[i], bxt[i]], writes=[bxt[i]])
            p.dma("pool", X[sl, :], xt[i][:], reads=[bxt[i]], writes=[bX[t]])
            if xo is not None:
                b = Buf(); bo.append(b)
                p.dma("pool", xo[sl, :], xt[i][:], reads=[bxt[i]], writes=[b])
        p.barrier()
    return bo


MLA_SCALE = 96.0 ** -0.5


def emit_mla(c, K, X, bX, mods, w_in, w_uqn, w_uqr, w_uqs, w_ukn, w_ukv, w_out, gq, gkv, cosT, sinT, cos_tm, sin_tm, P, bP):
    p = c.p
    nc = c.nc
    QnT = dram(c, [4, 128, NTOK]); KnT = dram(c, [4, 128, NTOK]); QrT = dram(c, [4, 64, NTOK])
    KrT2 = dram(c, [64, NTOK]); V = dram(c, [4, NT, 128, 130]); OT = dram(c, [4, 128, NTOK])
    bQ = [Buf() for _ in range(9)]; bKV = [Buf() for _ in range(9)]; bOT = [Buf() for _ in range(4)]
    groups = [list(range(4 * q, 4 * q + 4)) for q in range(8)] + [[32, 33]]
    esA = ExitStack()
    with esA:
        win = c.sb([128, 8, 1088], esA); bwin = Buf()
        p.dma("sp", win[:], w_in.rearrange("(c p) f -> p c f", p=128), writes=[bwin])
        wqn = c.sb([128, 6, 512], esA); wqr = c.sb([128, 6, 256], esA); wqs = c.sb([128, 6, 256], esA)
        wkn = c.sb([128, 2, 512], esA); wkv = c.sb([128, 2, 512], esA); bwq = Buf()
        for dst, src in ((wqn, w_uqn), (wqr, w_uqr), (wqs, w_uqs), (wkn, w_ukn), (wkv, w_ukv)):
            p.dma("sp", dst[:], src.rearrange("(c p) f -> p c f", p=128), writes=[bwq])
        gqt = c.sb([128, 768], esA); gkt = c.sb([128, 256], esA); bg = Buf()
        p.dma("pool", gqt[:], gq.partition_broadcast(128), writes=[bg])
        p.dma("pool", gkt[:], gkv.partition_broadcast(128), writes=[bg])
        cT = c.sb([64, 512], esA); sT = c.sb([64, 512], esA); bcs = Buf()
        xt = [c.sb([128, D], esA) for _ in range(2)]; bxt = [Buf(), Buf()]
        hT = c.sb([128, 8, 128], esA); bhT = Buf()
        pp = [c.ps([128, 512], esA) for _ in range(3)]; bpp = [PBuf() for _ in range(3)]
        pj = c.sb([128, 1088], esA); bpj = Buf()
        cqn = c.sb([128, 768], esA); ckn = c.sb([128, 256], esA); bcn = Buf()
        kr2 = c.sb([128, 64], esA); bkr2 = Buf()
        cst = c.sb([128, 32], esA); snt = c.sb([128, 32], esA); bcst = Buf()
        tmp32 = c.sb([128, 32], esA); btmp = Buf()
        cqT = c.sb([128, 6, 512], esA); bcqT = [Buf() for _ in range(4)]
        ckT = c.sb([128, 2, 512], esA); bckT = [Buf() for _ in range(4)]
        krT = c.sb([64, 512], esA); bkrT = [Buf() for _ in range(4)]
        vt = c.sb([128, 4, 130], esA); bvt = Buf()
        p.op("dve", lambda e: e.memset(vt[:], 1.0), writes=[bvt])
        outA = [c.sb([128, 512], esA) for _ in range(2)]; boutA = [Buf(), Buf()]
        outB = [c.sb([64, 512], esA) for _ in range(2)]; boutB = [Buf(), Buf()]
        tmpB = c.sb([64, 512], esA); btmpB = Buf()
        s4 = c.sb([128, 8], esA); bs4 = Buf()
        nA = 0
        for gi, tiles in enumerate(groups):
            N = len(tiles) * 128
            t0 = tiles[0] * 128
            for k, t in enumerate(tiles):
                kind = tile_kind(t)
                G, S, _ = mods[kind]
                i = t % 2
                ks = slice(k * 128, (k + 1) * 128)
                p.dma("pool", xt[i][:], X[t * 128:(t + 1) * 128, :], reads=[bX[t]], writes=[bxt[i]])
                norm_mod_T(c, K, xt[i][:], bxt[i], G, S, hT, bhT)
                for gidx, (n0, nn) in enumerate(((0, 512), (512, 512), (1024, 64))):
                    for ch in range(8):
                        p.op("pe", lambda e: e.matmul(pp[gidx][:, 0:nn], hT[:, ch, :], win[:, ch, n0:n0 + nn], start=(ch == 0), stop=(ch == 7)),
                             reads=[bhT, bwin], writes=[bpp[gidx]])
                    p.op("act" if gidx != 1 else "dve",
                         (lambda e: e.copy(pj[:, n0:n0 + nn], pp[gidx][:, 0:nn])) if gidx != 1 else (lambda e: e.tensor_copy(pj[:, n0:n0 + nn], pp[gidx][:, 0:nn])),
                         reads=[bpp[gidx]], writes=[bpj])
                for (lo_, n_, gt_, dst_, col) in ((0, 768, gqt, cqn, 0), (768, 256, gkt, ckn, 4)):
                    p.op("act", lambda e: e.activation(K["junk"][:, 0:n_], pj[:, lo_:lo_ + n_], AF.Square, accum_out=s4[:, col:col + 1]),
                         reads=[bpj], writes=[K["bjunk"], bs4])
                    p.op("act", lambda e: e.activation(s4[:, col + 1:col + 2], s4[:, col:col + 1], AF.Sqrt, bias=K["eps"][:, 0:1], scale=1.0 / n_),
                         reads=[bs4], writes=[bs4])
                    p.op("dve", lambda e: e.reciprocal(s4[:, col + 2:col + 3], s4[:, col + 1:col + 2]), reads=[bs4], writes=[bs4])
                    p.op("dve", lambda e: e.scalar_tensor_tensor(dst_[:], pj[:, lo_:lo_ + n_], s4[:, col + 2:col + 3], gt_[:], ALU.mult, ALU.mult),
                         reads=[bpj, bs4, bg], writes=[bcn])
                if kind == "x":
                    p.dma("pool", cst[:], cos_tm[t * 128:(t + 1) * 128, :], writes=[bcst])
                    p.dma("pool", snt[:], sin_tm[t * 128:(t + 1) * 128, :], writes=[bcst])
                    p.op("dve", lambda e: e.tensor_mul(kr2[:, 0:32], pj[:, 1024:1056], cst[:]), reads=[bpj, bcst], writes=[bkr2])
                    p.op("dve", lambda e: e.tensor_mul(tmp32[:], pj[:, 1056:1088], snt[:]), reads=[bpj, bcst], writes=[btmp])
                    p.op("dve", lambda e: e.tensor_add(kr2[:, 0:32], kr2[:, 0:32], tmp32[:]), reads=[bkr2, btmp], writes=[bkr2])
                else:
                    p.op("dve", lambda e: e.tensor_copy(kr2[:, 0:32], pj[:, 1024:1056]), reads=[bpj], writes=[bkr2])
                p.op("dve", lambda e: e.tensor_copy(kr2[:, 32:64], kr2[:, 0:32]), reads=[bkr2], writes=[bkr2])
                for blk, (src_, nchunk, dstT, bdst) in enumerate(((cqn, 6, cqT, bcqT), (ckn, 2, ckT, bckT))):
                    for c0 in range(0, nchunk, 4):
                        cn = min(4, nchunk - c0)
                        for cc in range(cn):
                            p.op("pe", lambda e: e.transpose(K["ptr"][:, cc * 128:(cc + 1) * 128], src_[:, (c0 + cc) * 128:(c0 + cc + 1) * 128], K["ident"][:]),
                                 reads=[bcn, K["bident"]], writes=[K["bptr"]])
                        p.op("act", lambda e: e.copy(dstT[:, c0:c0 + cn, ks], K["ptr"][:, 0:cn * 128].rearrange("p (c t) -> p c t", c=cn)),
                             reads=[K["bptr"]], writes=[bdst[k]])
                p.op("pe", lambda e: e.transpose(K["ptr"][0:64, 0:128], kr2[:], K["ident"][:]), reads=[bkr2, K["bident"]], writes=[K["bptr"]])
                p.op("act", lambda e: e.copy(krT[:, ks], K["ptr"][0:64, 0:128]), reads=[K["bptr"]], writes=[bkrT[k]])
                for ch in range(2):
                    p.op("pe", lambda e: e.matmul(pp[0][:, 0:512], ckT[:, ch, ks], wkv[:, ch, :], start=(ch == 0), stop=(ch == 1)),
                         reads=[bckT[k], bwq], writes=[bpp[0]])
                p.op("dve", lambda e: e.tensor_copy(vt[:].rearrange("p a (h d) -> p a h d", h=2)[:, :, :, 0:64],
                                                    pp[0][:, 0:512].rearrange("p (a h d) -> p a h d", a=4, h=2)),
                     reads=[bpp[0]], writes=[bvt])
                p.dma("sp", V[:, t].rearrange("a p f -> p a f"), vt[:], reads=[bvt], writes=[bKV[gi]])
            nt = len(tiles)
            p.dma("sp", KrT2[:, t0:t0 + N], krT[:, 0:N], reads=bkrT[:nt], writes=[bKV[gi]])
            if tiles[0] < 32:
                for hh in range(2):
                    p.dma("pool", cT[hh * 32:(hh + 1) * 32, 0:N], cosT[:, t0:t0 + N], writes=[bcs])
                    p.dma("pool", sT[hh * 32:(hh + 1) * 32, 0:N], sinT[:, t0:t0 + N], writes=[bcs])
            for pr in range(4):
                j = nA % 2
                nA += 1
                for ch in range(2):
                    p.op("pe", lambda e: e.matmul(pp[0][:, 0:N], wkn[:, ch, pr * 128:(pr + 1) * 128], ckT[:, ch, 0:N], start=(ch == 0), stop=(ch == 1)),
                         reads=bckT[:nt] + [bwq], writes=[bpp[0]])
                p.op("act", lambda e: e.copy(outA[j][:, 0:N], pp[0][:, 0:N]), reads=[bpp[0]], writes=[boutA[j]])
                p.dma("sp", KnT[pr][:, t0:t0 + N], outA[j][:, 0:N], reads=[boutA[j]], writes=[bKV[gi]])
                j = nA % 2
                nA += 1
                for ch in range(6):
                    p.op("pe", lambda e: e.matmul(pp[1][:, 0:N], wqn[:, ch, pr * 128:(pr + 1) * 128], cqT[:, ch, 0:N], start=(ch == 0), stop=(ch == 5)),
                         reads=bcqT[:nt] + [bwq], writes=[bpp[1]])
                p.op("dve", lambda e: e.tensor_copy(outA[j][:, 0:N], pp[1][:, 0:N]), reads=[bpp[1]], writes=[boutA[j]])
                p.dma("sp", QnT[pr][:, t0:t0 + N], outA[j][:, 0:N], reads=[boutA[j]], writes=[bQ[gi]])
                for ch in range(6):
                    p.op("pe", lambda e: e.matmul(pp[2][0:64, 0:N], wqr[:, ch, pr * 64:(pr + 1) * 64], cqT[:, ch, 0:N], start=(ch == 0), stop=(ch == 5)),
                         reads=bcqT[:nt] + [bwq], writes=[bpp[2]])
                jb = pr % 2
                if tiles[0] < 32:
                    p.op("dve", lambda e: e.tensor_mul(outB[jb][:, 0:N], pp[2][0:64, 0:N], cT[:, 0:N]), reads=[bpp[2], bcs], writes=[boutB[jb]])
                    for ch in range(6):
                        p.op("pe", lambda e: e.matmul(pp[2][0:64, 0:N], wqs[:, ch, pr * 64:(pr + 1) * 64], cqT[:, ch, 0:N], start=(ch == 0), stop=(ch == 5)),
                             reads=bcqT[:nt] + [bwq], writes=[bpp[2]])
                    p.op("dve", lambda e: e.tensor_mul(tmpB[:, 0:N], pp[2][0:64, 0:N], sT[:, 0:N]), reads=[bpp[2], bcs], writes=[btmpB])
                    p.op("pool", lambda e: e.tensor_add(outB[jb][:, 0:N], outB[jb][:, 0:N], tmpB[:, 0:N]), reads=[boutB[jb], btmpB], writes=[boutB[jb]])
                else:
                    p.op("dve", lambda e: e.tensor_copy(outB[jb][:, 0:N], pp[2][0:64, 0:N]), reads=[bpp[2]], writes=[boutB[jb]])
                p.dma("sp", QrT[pr][:, t0:t0 + N], outB[jb][:, 0:N], reads=[boutB[jb]], writes=[bQ[gi]])
        p.barrier()
    esB = ExitStack()
    with esB:
        qn = c.sb([128, NTOK], esB); kn = c.sb([128, NTOK], esB); qr = c.sb([64, NTOK], esB); kr = c.sb([64, NTOK], esB)
        vv = c.sb([128, NT, 130], esB)
        bqn, bkn, bqr, bkr, bvv = Buf(), Buf(), Buf(), Buf(), Buf()
        ones = c.sb([128, 64], esB); bon = Buf()
        p.op("dve", lambda e: e.memset(ones[:], 1.0), writes=[bon])
        pS = [c.ps([128, 512], esB) for _ in range(2)]; bpS = [PBuf(), PBuf()]
        pO = [c.ps([128, 512], esB) for _ in range(2)]; bpO = [PBuf(), PBuf()]
        pB = c.ps([128, 512], esB); bpB = PBuf()
        eS = [c.sb([128, 512], esB) for _ in range(2)]; beS = [Buf(), Buf()]
        rinv = c.sb([128, 512], esB); brinv = Buf()
        osb = [c.sb([64, 512], esB) for _ in range(2)]; bosb = [Buf(), Buf()]
        p.dma("sp", kr[:], KrT2, reads=bKV, writes=[bkr])
        nS = 0
        nO = 0
        for pr in range(4):
            p.dma("sp", qn[:], QnT[pr], reads=bQ, writes=[bqn])
            p.dma("sp", kn[:], KnT[pr], reads=bKV, writes=[bkn])
            p.dma("sp", qr[:], QrT[pr], reads=bQ, writes=[bqr])
            p.dma("sp", vv[:], V[pr].rearrange("t p f -> p t f"), reads=bKV, writes=[bvv])
            for hh in range(2):
                nb = hh * 64
                rb = hh * 32
                for gi, tiles in enumerate(groups):
                    N = len(tiles) * 128
                    q0 = tiles[0] * 128
                    keys = list(range(NT)) if tiles[0] < 32 else [32, 33]
                    jo = nO % 2
                    nO += 1
                    for ki, kt in enumerate(keys):
                        js = nS % 2
                        nS += 1
                        kk = slice(kt * 128, (kt + 1) * 128)
                        p.op("pe", lambda e: e.matmul(pS[js][:, 0:N], kn[nb:nb + 64, kk], qn[nb:nb + 64, q0:q0 + N], start=True, stop=False),
                             reads=[bkn, bqn], writes=[bpS[js]])
                        p.op("pe", lambda e: e.matmul(pS[js][:, 0:N], kr[rb:rb + 32, kk], qr[rb:rb + 32, q0:q0 + N], start=False, stop=True),
                             reads=[bkr, bqr], writes=[bpS[js]])
                        p.op("act", lambda e: e.activation(eS[js][:, 0:N], pS[js][:, 0:N], AF.Exp, scale=MLA_SCALE),
                             reads=[bpS[js]], writes=[beS[js]])
                        p.op("pe", lambda e: e.matmul(pO[jo][0:65, 0:N], vv[:, kt, hh * 65:(hh + 1) * 65], eS[js][:, 0:N],
                                                      start=(ki == 0), stop=(ki == len(keys) - 1)),
                             reads=[bvv, beS[js]], writes=[bpO[jo]])
                    p.op("dve", lambda e: e.reciprocal(rinv[64:65, 0:N], pO[jo][64:65, 0:N]), reads=[bpO[jo]], writes=[brinv])
                    p.op("pe", lambda e: e.matmul(pB[0:64, 0:N], ones[64:65, :], rinv[64:65, 0:N], start=True, stop=True),
                         reads=[bon, brinv], writes=[bpB])
                    p.op("act", lambda e: e.copy(osb[jo][:, 0:N], pB[0:64, 0:N]), reads=[bpB], writes=[bosb[jo]])
                    p.op("dve", lambda e: e.tensor_mul(osb[jo][:, 0:N], osb[jo][:, 0:N], pO[jo][0:64, 0:N]), reads=[bosb[jo], bpO[jo]], writes=[bosb[jo]])
                    p.dma("pool", OT[pr][hh * 64:(hh + 1) * 64, q0:q0 + N], osb[jo][:, 0:N], reads=[bosb[jo]], writes=[bOT[pr]])
        p.barrier()
    esC = ExitStack()
    with esC:
        wo = c.sb([128, 4, D], esC); bwo = Buf()
        p.dma("sp", wo[:], w_out.rearrange("(c p) f -> p c f", p=128), writes=[bwo])
        ot = c.sb([128, 4, NTOK], esC); bot = Buf()
        for pr in range(4):
            p.dma("sp", ot[:, pr, :], OT[pr], reads=[bOT[pr]], writes=[bot])
        pc = [c.ps([128, 512], esC) for _ in range(2)]; bpc = [PBuf(), PBuf()]
        ob = [c.sb([128, D], esC) for _ in range(2)]; bob = [Buf(), Buf()]
        for t in range(NT):
            i = t % 2
            for half in range(2):
                for pr in range(4):
                    p.op("pe", lambda e: e.matmul(pc[half][:], ot[:, pr, t * 128:(t + 1) * 128], wo[:, pr, half * 512:(half + 1) * 512],
                                                  start=(pr == 0), stop=(pr == 3)), reads=[bot, bwo], writes=[bpc[half]])
                p.op("act" if half else "dve",
                     (lambda e: e.copy(ob[i][:, half * 512:(half + 1) * 512], pc[half][:])) if half else
                     (lambda e: e.tensor_copy(ob[i][:, half * 512:(half + 1) * 512], pc[half][:])),
                     reads=[bpc[half]], writes=[bob[i]])
            p.dma("pool", P[t * 128:(t + 1) * 128, :], ob[i][:], reads=[bob[i]], writes=[bP[t]])
        p.barrier()


def rope_tables():
    t = np.arange(4096)
    rows = (t // 64).astype(np.float32)
    cols = (t % 64).astype(np.float32)
    inv = (10000.0 ** (-np.arange(8, dtype=np.float32) * 2.0 / 16)).astype(np.float32)
    ar = rows[:, None] * inv[None, :]
    ac = cols[:, None] * inv[None, :]
    cos = np.concatenate([np.cos(ar), np.cos(ar), np.cos(ac), np.cos(ac)], 1).astype(np.float32)
    sin = np.concatenate([-np.sin(ar), np.sin(ar), -np.sin(ac), np.sin(ac)], 1).astype(np.float32)
    return cos, sin, np.ascontiguousarray(cos.T), np.ascontiguousarray(sin.T)


ROPE_SWAP = list(range(8, 16)) + list(range(0, 8)) + list(range(24, 32)) + list(range(16, 24))


def build_stage(kind, stop=None, with_ss=False):
    nc = new_nc()
    es = ExitStack()
    with es:
        c = Ctx(nc, es); p = c.p
        K = common_consts(c, c.din("ident", [128, 128]))
        xprev = c.din("xprev", [NTOK, D]); pa = c.din("pa", [NTOK, D]); pb = c.din("pb", [NTOK, D])
        vec = c.din("vec", [NVEC, D])
        xo = c.dout("xo", [NTOK, D])
        X = dram(c, [NTOK, D]); bX = [Buf() for _ in range(NT)]
        ssa = c.din("ssa", [NTOK, 1]) if with_ss else None
        ssb = c.din("ssb", [NTOK, 1]) if with_ss else None
        bo = emit_residual_in(c, K, xprev, pa, pb, vec, X, bX, xo, ssa, ssb)
        if kind == "final":
            out = c.dout("p", [NTOK, D])
            es1 = ExitStack()
            with es1:
                fg = bc_tile(c, es1, vec[14:15, :])
                xt = [c.sb([128, D], es1) for _ in range(2)]; bxt = [Buf(), Buf()]
                ot = [c.sb([128, D], es1) for _ in range(2)]; bot = [Buf(), Buf()]
                for t in range(32):
                    i = t % 2
                    p.dma("sp", xt[i][:], X[t * 128:(t + 1) * 128, :], reads=[bX[t]], writes=[bxt[i]])
                    norm_mod_T(c, K, xt[i][:], bxt[i], fg, None, None, None)
                    p.op("act", lambda e: e.copy(ot[i][:], K["h"][:]), reads=[K["bh"]], writes=[bot[i]])
                    b = Buf(); bo.append(b)
                    p.dma("pool", out[t * 128:(t + 1) * 128, :], ot[i][:], reads=[bot[i]], writes=[b])
            p.finish(bo)
            return nc
        P = c.dout("p", [NTOK, D]); bP = [Buf() for _ in range(NT)]
        es1 = ExitStack()
        with es1:
            if kind == "moe":
                mods = mods_from_vec(c, es1, vec, 3, 13)
                wr = c.din("wr", [D, NE]); wg = c.din("wg", [NEH * D, D]); wu = c.din("wu", [NEH * D, D]); wd = c.din("wd", [NEH * D, D])
                emit_moe(c, K, X, bX, mods, wr, wg, wu, wd, Buf(), P, bP)
            elif kind == "mla":
                mods = mods_from_vec(c, es1, vec, 0, 12)
                a = dict(w_in=c.din("w_in", [D, 1088]), w_uqn=c.din("w_uqn", [768, 512]), w_uqr=c.din("w_uqr", [768, 256]),
                         w_uqs=c.din("w_uqs", [768, 256]), w_ukn=c.din("w_ukn", [256, 512]), w_ukv=c.din("w_ukv", [256, 512]),
                         w_out=c.din("w_out", [512, D]), gq=c.din("gq", [1, 768]), gkv=c.din("gkv", [1, 256]),
                         cosT=c.din("cosT", [32, 4096]), sinT=c.din("sinT", [32, 4096]),
                         cos_tm=c.din("cos_tm", [4096, 32]), sin_tm=c.din("sin_tm", [4096, 32]))
                emit_mla(c, K, X, bX, mods, P=P, bP=bP, **a)
            elif kind == "ssd":
                mods = mods_from_vec(c, es1, vec, 0, 12)
                a = dict(w_cv=c.din("w_cv", [D, 2048]), convp=c.din("convp", [16, 128, 4]), w_z=c.din("w_z", [D, D]), w_dt=c.din("w_dt", [D, 32]),
                         dtb=c.din("dtb", [1, 32]), alog=c.din("alog", [1, 32]), dvec=c.din("dvec", [1, D]), ng=c.din("ng", [1, D]),
                         w_out=c.din("w_out", [D, D]), triF=c.din("triF", [128, 128]), triB=c.din("triB", [128, 128]))
                SS = c.dout("ss", [NTOK, 1]); bSS = [Buf() for _ in range(NT)]
                emit_ssd(c, K, X, bX, mods, P=P, bP=bP, SS=SS, bSS=bSS, **a)
                bo = bo + bSS
            elif kind == "gdn":
                mods = mods_from_vec(c, es1, vec, 0, 12)
                a = dict(w_cv=c.din("w_cv", [D, 1536]), convp=c.din("convp", [12, 128, 4]), w_z=c.din("w_z", [D, 512]), w_bg=c.din("w_bg", [D, 16]),
                         dtb=c.din("dtb", [1, 8]), alog=c.din("alog", [1, 8]), ng=c.din("ng", [1, 128]), w_out=c.din("w_out", [512, D]),
                         masks=c.din("masks", [2, 7, 128, 128]))
                emit_gdn(c, K, X, bX, mods, P=P, bP=bP, stop=stop, **a)
            else:
                raise ValueError(kind)
        p.finish(bo + bP)
        print(kind, "ninst", p.ninst, "nsem", p.nsem)
    return nc


def mla_weights(z_w_in, z_uq, z_ukv, z_out, gq, gkv, h):
    heads = range(8 * h, 8 * h + 8)
    w_in = np.concatenate([z_w_in, z_w_in[:, 1024:1056][:, ROPE_SWAP]], 1)
    uqn = np.concatenate([z_uq[:, hd * 96:hd * 96 + 64] for hd in heads], 1)
    uqr = np.concatenate([z_uq[:, hd * 96 + 64:hd * 96 + 96] for hd in heads], 1)
    uqs = np.concatenate([z_uq[:, hd * 96 + 64:hd * 96 + 96][:, ROPE_SWAP] for hd in heads], 1)
    ukn = np.concatenate([z_ukv[:, hd * 128:hd * 128 + 64] for hd in heads], 1)
    ukv = np.concatenate([z_ukv[:, hd * 128 + 64:hd * 128 + 128] for hd in heads], 1)
    cos, sin, cosT, sinT = rope_tables()
    f = np.ascontiguousarray
    return dict(w_in=f(w_in), w_uqn=f(uqn), w_uqr=f(uqr), w_uqs=f(uqs), w_ukn=f(ukn), w_ukv=f(ukv),
                w_out=f(z_out[8 * h * 64:(8 * h + 8) * 64]), gq=f(gq[None, :]), gkv=f(gkv[None, :]),
                cosT=cosT, sinT=sinT, cos_tm=cos, sin_tm=sin)


def make_vec(mx_b, mc, n1g, n2g, fg, pgx, pgc):
    return np.ascontiguousarray(np.concatenate([mx_b, mc, n1g[None], n2g[None], fg[None], pgx[None], pgc[None]], 0).astype(np.float32))


def softplus_tile(c, out_ap, in_ap, tmp_a, tmp_b, rb, wb, bt):
    p = c.p
    p.op("act", lambda e: e.activation(tmp_a, in_ap, AF.Abs), reads=rb, writes=[bt])
    p.op("act", lambda e: e.activation(tmp_a, tmp_a, AF.Exp, scale=-1.0), reads=[bt], writes=[bt])
    p.op("act", lambda e: e.activation(tmp_b, tmp_a, AF.Ln, bias=1.0), reads=[bt], writes=[bt])
    p.op("dve", lambda e: e.scalar_tensor_tensor(out_ap, in_ap, 0.0, tmp_b, ALU.max, ALU.add), reads=rb + [bt], writes=wb)


def emit_ssd(c, K, X, bX, mods, w_cv, convp, w_z, w_dt, dtb, alog, dvec, ng, w_out, triF, triB, P, bP, SS, bSS):
    p = c.p
    HT = dram(c, [NT, 128, 8, 128]); bHT = [Buf() for _ in range(NT)]
    FT = dram(c, [16, 128, NTOK]); bFT = [Buf() for _ in range(16)]
    ZS = dram(c, [NTOK, D]); DT = dram(c, [NTOK, 32]); XTM = dram(c, [NTOK, D]); BTM = dram(c, [NTOK, 512]); YF = dram(c, [NTOK, D])
    bZS = [Buf() for _ in range(NT)]; bXB = [Buf() for _ in range(NT)]; bYF = [Buf() for _ in range(NT)]
    groups = [list(range(4 * q, 4 * q + 4)) for q in range(8)] + [[32, 33]]
    es = ExitStack()
    with es:
        wz = c.sb([128, 8, D], es); wdt = c.sb([128, 8, 32], es); bwz = Buf()
        p.dma("sp", wz[:], w_z.rearrange("(c p) f -> p c f", p=128), writes=[bwz])
        p.dma("sp", wdt[:], w_dt.rearrange("(c p) f -> p c f", p=128), writes=[bwz])
        dtb_t = c.sb([128, 32], es); bdb = Buf()
        p.dma("pool", dtb_t[:], dtb.partition_broadcast(128), writes=[bdb])
        xt = [c.sb([128, D], es) for _ in range(2)]; bxt = [Buf(), Buf()]
        hT = [c.sb([128, 8, 128], es) for _ in range(2)]; bhT = [Buf(), Buf()]
        pz = [c.ps([128, 512], es) for _ in range(2)]; bpz = [PBuf(), PBuf()]
        pd = c.ps([128, 512], es); bpd = PBuf()
        zs = [c.sb([128, D], es) for _ in range(2)]; bzs = [Buf(), Buf()]
        dr = c.sb([128, 32], es); ta = c.sb([128, 32], es); tb = c.sb([128, 32], es); do = [c.sb([128, 32], es) for _ in range(2)]
        bdr, btt, bdo = Buf(), Buf(), [Buf(), Buf()]
        for t in range(NT):
            i = t % 2
            G, S, _ = mods[tile_kind(t)]
            p.dma("pool", xt[i][:], X[t * 128:(t + 1) * 128, :], reads=[bX[t]], writes=[bxt[i]])
            norm_mod_T(c, K, xt[i][:], bxt[i], G, S, hT[i], bhT[i])
            p.dma("sp", HT[t], hT[i][:], reads=[bhT[i]], writes=[bHT[t]])
            for half in range(2):
                for ch in range(8):
                    p.op("pe", lambda e: e.matmul(pz[half][:], hT[i][:, ch, :], wz[:, ch, half * 512:(half + 1) * 512], start=(ch == 0), stop=(ch == 7)),
                         reads=[bhT[i], bwz], writes=[bpz[half]])
                p.op("act", lambda e: e.activation(zs[i][:, half * 512:(half + 1) * 512], pz[half][:], AF.Silu), reads=[bpz[half]], writes=[bzs[i]])
            p.dma("sp", ZS[t * 128:(t + 1) * 128, :], zs[i][:], reads=[bzs[i]], writes=[bZS[t]])
            for ch in range(8):
                p.op("pe", lambda e: e.matmul(pd[:, 0:32], hT[i][:, ch, :], wdt[:, ch, :], start=(ch == 0), stop=(ch == 7)),
                     reads=[bhT[i], bwz], writes=[bpd])
            p.op("dve", lambda e: e.tensor_add(dr[:], pd[:, 0:32], dtb_t[:]), reads=[bpd, bdb], writes=[bdr])
            softplus_tile(c, do[i][:], dr[:], ta[:], tb[:], [bdr], [bdo[i]], btt)
            p.dma("sp", DT[t * 128:(t + 1) * 128, :], do[i][:], reads=[bdo[i]], writes=[bZS[t]])
        p.barrier()
    es = ExitStack()
    with es:
        wc = c.sb([128, 8, 512], es); bwc = Buf()
        cp = c.sb([128, 16, 4], es); bcp = Buf()
        p.dma("pool", cp[:], convp.rearrange("k p f -> p k f"), writes=[bcp])
        hg = [c.sb([128, 4, 8, 128], es) for _ in range(2)]; bhg = [Buf(), Buf()]
        raw = [c.sb([128, NTOK], es) for _ in range(4)]; braw = [Buf() for _ in range(4)]
        cv = [c.sb([128, NTOK], es) for _ in range(2)]; bcv = [Buf(), Buf()]
        pr_ = [c.ps([128, 512], es) for _ in range(2)]; bpr = [PBuf(), PBuf()]
        n = 0
        for cb in range(4):
            p.dma("sp", wc[:], w_cv[:, cb * 512:(cb + 1) * 512].rearrange("(c p) f -> p c f", p=128), writes=[bwc])
            for gi, tiles in enumerate(groups):
                i = gi % 2
                nt = len(tiles)
                N = nt * 128
                t0 = tiles[0] * 128
                p.dma("pool", hg[i][:, 0:nt], HT[tiles[0]:tiles[0] + nt].rearrange("t p c k -> p t c k"), reads=[bHT[t] for t in tiles], writes=[bhg[i]])
                for cc in range(4):
                    j = n % 2
                    n += 1
                    for k in range(nt):
                        for ch in range(8):
                            p.op("pe", lambda e: e.matmul(pr_[j][:, k * 128:(k + 1) * 128], wc[:, ch, cc * 128:(cc + 1) * 128], hg[i][:, k, ch, :],
                                                          start=(ch == 0), stop=(ch == 7)), reads=[bwc, bhg[i]], writes=[bpr[j]])
                    p.op("act" if j else "dve",
                         (lambda e: e.copy(raw[cc][:, t0:t0 + N], pr_[j][:, 0:N])) if j else (lambda e: e.tensor_copy(raw[cc][:, t0:t0 + N], pr_[j][:, 0:N])),
                         reads=[bpr[j]], writes=[braw[cc]])
            for cc in range(4):
                k = cb * 4 + cc
                o = cv[cc % 2]; bo_ = bcv[cc % 2]; r = raw[cc]
                p.op("dve", lambda e: e.tensor_scalar(o[:], r[:], cp[:, k, 1:2], None, ALU.mult), reads=[braw[cc], bcp], writes=[bo_])
                for (a0, a1) in ((0, 4096), (4096, NTOK)):
                    p.op("dve", lambda e: e.scalar_tensor_tensor(o[:, a0 + 1:a1], r[:, a0:a1 - 1], cp[:, k, 0:1], o[:, a0 + 1:a1], ALU.mult, ALU.add),
                         reads=[braw[cc], bcp, bo_], writes=[bo_])
                    p.op("dve", lambda e: e.scalar_tensor_tensor(o[:, a0:a1 - 1], r[:, a0 + 1:a1], cp[:, k, 2:3], o[:, a0:a1 - 1], ALU.mult, ALU.add),
                         reads=[braw[cc], bcp, bo_], writes=[bo_])
                p.op("act", lambda e: e.activation(o[:], o[:], AF.Silu, bias=cp[:, k, 3:4]), reads=[bo_, bcp], writes=[bo_])
                p.dma("sp", FT[k], o[:], reads=[bo_], writes=[bFT[k]])
        p.barrier()
    es = ExitStack()
    with es:
        fx = [c.sb([128, 12, 128], es) for _ in range(2)]; bfx = [Buf(), Buf()]
        xo_ = [c.sb([128, 12, 128], es) for _ in range(2)]; bxo = [Buf(), Buf()]
        for t in range(NT):
            i = t % 2
            p.dma("sp", fx[i][:], FT[0:12, :, t * 128:(t + 1) * 128].rearrange("k p t -> p k t"), reads=bFT[0:12], writes=[bfx[i]])
            for q in range(3):
                for cc in range(4):
                    p.op("pe", lambda e: e.transpose(K["ptr"][:, cc * 128:(cc + 1) * 128], fx[i][:, q * 4 + cc, :], K["ident"][:]),
                         reads=[bfx[i], K["bident"]], writes=[K["bptr"]])
                p.op("act" if q % 2 else "dve",
                     (lambda e: e.copy(xo_[i][:, q * 4:(q + 1) * 4, :], K["ptr"][:].rearrange("p (c t) -> p c t", c=4))) if q % 2 else
                     (lambda e: e.tensor_copy(xo_[i][:, q * 4:(q + 1) * 4, :], K["ptr"][:].rearrange("p (c t) -> p c t", c=4))),
                     reads=[K["bptr"]], writes=[bxo[i]])
            p.dma("pool", XTM[t * 128:(t + 1) * 128, :], xo_[i][:, 0:8, :], reads=[bxo[i]], writes=[bXB[t]])
            p.dma("pool", BTM[t * 128:(t + 1) * 128, :], xo_[i][:, 8:12, :], reads=[bxo[i]], writes=[bXB[t]])
        p.barrier()
    es = ExitStack()
    with es:
        tri = {0: c.sb([128, 128], es), 1: c.sb([128, 128], es)}; btri = Buf()
        p.dma("pool", tri[0][:], triF, writes=[btri])
        p.dma("pool", tri[1][:], triB, writes=[btri])
        ones = c.sb([128, 128], es); bon = Buf()
        p.op("dve", lambda e: e.memset(ones[:], 1.0), writes=[bon])
        Abc = c.sb([128, 32], es); bA = Buf()
        p.dma("pool", Abc[:], alog.partition_broadcast(128), writes=[bA])
        p.op("act", lambda e: e.activation(Abc[:], Abc[:], AF.Exp), reads=[bA], writes=[bA])
        p.op("dve", lambda e: e.tensor_scalar(Abc[:], Abc[:], -1.0, None, ALU.mult), reads=[bA], writes=[bA])
        Dbc = c.sb([128, D], es); ngt = c.sb([128, D], es); bDn = Buf()
        p.dma("pool", Dbc[:], dvec.partition_broadcast(128), writes=[bDn])
        p.dma("pool", ngt[:], ng.partition_broadcast(128), writes=[bDn])
        wo = c.sb([128, 8, D], es); bwo = Buf()
        p.dma("sp", wo[:], w_out.rearrange("(c p) f -> p c f", p=128), writes=[bwo])
        xs = [c.sb([128, 16, 64], es) for _ in range(2)]; bts = [c.sb([128, 4, 128], es) for _ in range(2)]
        BT = [c.sb([128, 4, 128], es) for _ in range(2)]; CT = [c.sb([128, 4, 128], es) for _ in range(2)]
        dtt = [c.sb([128, 32], es) for _ in range(2)]
        bin_ = [Buf(), Buf()]
        hst = c.sb([128, 16, 64], es); bh = Buf()
        a_ = c.sb([128, 16], es); acs = c.sb([128, 16], es); nacs = c.sb([128, 16], es); etot = c.sb([128, 16], es); wgt = c.sb([128, 16], es)
        bsm = Buf()
        pA = c.ps([128, 512], es); bpA = PBuf()
        pCB = c.ps([128, 512], es); bpCB = PBuf()
        pST = c.ps([128, 512], es); bpST = PBuf()
        pRB = [c.ps([128, 512], es) for _ in range(2)]; bpRB = [PBuf(), PBuf()]
        pY = c.ps([128, 512], es); bpY = PBuf()
        pC = c.ps([128, 512], es); bpC = PBuf()
        CBm = c.sb([128, 128], es); bCBm = Buf()
        xh = c.sb([128, 4, 64], es); bxh = Buf()
        abc = [c.sb([128, 128], es) for _ in range(2)]; babc = [Buf(), Buf()]
        tmp = [c.sb([128, 128], es) for _ in range(2)]; btmp = [Buf(), Buf()]
        WT = [c.sb([128, 128], es) for _ in range(2)]; bWT = [Buf(), Buf()]
        Eb = [c.sb([128, 128], es) for _ in range(2)]; bEb = [Buf(), Buf()]
        LT = [c.sb([128, 128], es) for _ in range(2)]; bLT = [Buf(), Buf()]
        ysb = [c.sb([128, 16, 64], es) for _ in range(2)]; bys = [Buf(), Buf()]
        yfl = c.sb([128, D], es); zl = c.sb([128, D], es); bfl = Buf()
        ssb = c.sb([128, 2], es); bssb = Buf()
        ynT = c.sb([128, 8, 128], es); bynT = Buf()
        ob = c.sb([128, D], es); bob = Buf()
        nh = 0
        for d in range(2):
            order = [32, 33] + list(range(32)) if d == 0 else [33, 32] + list(range(31, -1, -1))
            p.op("dve", lambda e: e.memset(hst[:], 0.0), reads=[bh], writes=[bh])
            for vi, t in enumerate(order):
                i = vi % 2
                sl = slice(t * 128, (t + 1) * 128)
                p.dma("sp", xs[i][:], XTM[sl, :].rearrange("p (h d) -> p h d", h=16), reads=[bXB[t]], writes=[bin_[i]])
                p.dma("sp", bts[i][:], BTM[sl, :].rearrange("p (g n) -> p g n", g=4), reads=[bXB[t]], writes=[bin_[i]])
                p.dma("sp", BT[i][:], FT[8:12, :, sl].rearrange("g p t -> p g t"), reads=bFT[8:12], writes=[bin_[i]])
                p.dma("sp", CT[i][:], FT[12:16, :, sl].rearrange("g p t -> p g t"), reads=bFT[12:16], writes=[bin_[i]])
                p.dma("sp", dtt[i][:], DT[sl, :], reads=[bZS[t]], writes=[bin_[i]])
                dtd = dtt[i][:, d * 16:(d + 1) * 16]
                p.op("dve", lambda e: e.tensor_mul(a_[:], dtd, Abc[:, d * 16:(d + 1) * 16]), reads=[bin_[i], bA], writes=[bsm])
                p.op("pe", lambda e: e.matmul(pA[:, 0:16], tri[d][:], a_[:], start=True, stop=True), reads=[btri, bsm], writes=[bpA])
                p.op("pe", lambda e: e.matmul(pA[:, 16:32], ones[:], a_[:], start=True, stop=True), reads=[bon, bsm], writes=[bpA])
                p.op("act", lambda e: e.copy(acs[:], pA[:, 0:16]), reads=[bpA], writes=[bsm])
                p.op("dve", lambda e: e.tensor_scalar(nacs[:], pA[:, 0:16], -1.0, None, ALU.mult), reads=[bpA], writes=[bsm])
                p.op("act", lambda e: e.activation(etot[:], pA[:, 16:32], AF.Exp), reads=[bpA], writes=[bsm])
                p.op("dve", lambda e: e.tensor_tensor(wgt[:], pA[:, 16:32], acs[:], ALU.subtract), reads=[bpA, bsm], writes=[bsm])
                p.op("act", lambda e: e.activation(wgt[:], wgt[:], AF.Exp), reads=[bsm], writes=[bsm])
                p.op("dve", lambda e: e.tensor_mul(wgt[:], wgt[:], dtd), reads=[bsm, bin_[i]], writes=[bsm])
                yb = ysb[i]
                for g in range(4):
                    p.op("pe", lambda e: e.matmul(pCB[:, 0:128], BT[i][:, g, :], CT[i][:, g, :], start=True, stop=True), reads=[bin_[i]], writes=[bpCB])
                    p.op("dve", lambda e: e.tensor_mul(CBm[:], pCB[:, 0:128], tri[d][:]), reads=[bpCB, btri], writes=[bCBm])
                    for r in range(4):
                        hd = 4 * g + r
                        p.op("pool", lambda e: e.tensor_scalar(xh[:, r, :], xs[i][:, hd, :], wgt[:, hd:hd + 1], 1.0, ALU.mult, ALU.mult),
                             reads=[bin_[i], bsm], writes=[bxh])
                    p.op("pe", lambda e: e.matmul(pST[:, 0:256], bts[i][:, g, :], xh[:].rearrange("p r d -> p (r d)"), start=True, stop=True),
                         reads=[bin_[i], bxh], writes=[bpST])
                    for r in range(4):
                        hd = 4 * g + r
                        j = nh % 2
                        nh += 1
                        p.op("pool", lambda e: e.tensor_scalar(abc[j][:], ones[:], a_[:, hd:hd + 1], 1.0, ALU.mult, ALU.mult), reads=[bon, bsm], writes=[babc[j]])
                        p.op("pe", lambda e: e.matmul(pRB[j][:, 0:128], abc[j][:], tri[d][:], start=True, stop=True), reads=[babc[j], btri], writes=[bpRB[j]])
                        p.op("dve", lambda e: e.tensor_scalar(tmp[j][:], pRB[j][:, 0:128], nacs[:, hd:hd + 1], 0.0, ALU.add, ALU.min),
                             reads=[bpRB[j], bsm], writes=[btmp[j]])
                        p.op("act", lambda e: e.activation(tmp[j][:], tmp[j][:], AF.Exp), reads=[btmp[j]], writes=[btmp[j]])
                        p.op("dve", lambda e: e.scalar_tensor_tensor(WT[j][:], tmp[j][:], dtd[:, hd:hd + 1], CBm[:], ALU.mult, ALU.mult),
                             reads=[btmp[j], bin_[i], bCBm], writes=[bWT[j]])
                        p.op("act", lambda e: e.activation(Eb[j][:], pRB[j][:, 0:128], AF.Exp), reads=[bpRB[j]], writes=[bEb[j]])
                        p.op("pool", lambda e: e.tensor_mul(LT[j][:], CT[i][:, g, :], Eb[j][:]), reads=[bin_[i], bEb[j]], writes=[bLT[j]])
                        p.op("pe", lambda e: e.matmul(pY[:, 0:64], WT[j][:], xs[i][:, hd, :], start=True, stop=False), reads=[bWT[j], bin_[i]], writes=[bpY])
                        p.op("pe", lambda e: e.matmul(pY[:, 0:64], LT[j][:], hst[:, hd, :], start=False, stop=True), reads=[bLT[j], bh], writes=[bpY])
                        p.op("act", lambda e: e.copy(yb[:, hd, :], pY[:, 0:64]), reads=[bpY], writes=[bys[i]])
                    for r in range(4):
                        hd = 4 * g + r
                        p.op("dve", lambda e: e.scalar_tensor_tensor(hst[:, hd, :], hst[:, hd, :], etot[:, hd:hd + 1], pST[:, r * 64:(r + 1) * 64], ALU.mult, ALU.add),
                             reads=[bh, bsm, bpST], writes=[bh])
                ybf = yb[:].rearrange("p h d -> p (h d)")
                if d == 0:
                    p.dma("pool", YF[sl, :], ybf, reads=[bys[i]], writes=[bYF[t]])
                    continue
                p.dma("pool", yfl[:], YF[sl, :], reads=[bYF[t]], writes=[bfl])
                p.dma("pool", zl[:], ZS[sl, :], reads=[bZS[t]], writes=[bfl])
                p.op("dve", lambda e: e.tensor_add(ybf, ybf, yfl[:]), reads=[bys[i], bfl], writes=[bys[i]])
                p.op("pool", lambda e: e.tensor_mul(yfl[:], xs[i][:].rearrange("p h d -> p (h d)"), Dbc[:]), reads=[bin_[i], bDn, bfl], writes=[bfl])
                p.op("dve", lambda e: e.tensor_add(ybf, ybf, yfl[:]), reads=[bys[i], bfl], writes=[bys[i]])
                p.op("dve", lambda e: e.tensor_mul(ybf, ybf, zl[:]), reads=[bys[i], bfl], writes=[bys[i]])
                p.op("act", lambda e: e.activation(K["junk"][:], ybf, AF.Square, accum_out=ssb[:, 0:1]), reads=[bys[i]], writes=[K["bjunk"], bssb])
                p.dma("pool", SS[sl, :], ssb[:, 0:1], reads=[bssb], writes=[bSS[t]])
                p.op("dve", lambda e: e.tensor_mul(ybf, ybf, ngt[:]), reads=[bys[i], bDn], writes=[bys[i]])
                for q in range(2):
                    for cc in range(4):
                        ch = q * 4 + cc
                        p.op("pe", lambda e: e.transpose(K["ptr"][:, cc * 128:(cc + 1) * 128], ybf[:, ch * 128:(ch + 1) * 128], K["ident"][:]),
                             reads=[bys[i], K["bident"]], writes=[K["bptr"]])
                    p.op("act", lambda e: e.copy(ynT[:, q * 4:(q + 1) * 4, :], K["ptr"][:].rearrange("p (c t) -> p c t", c=4)), reads=[K["bptr"]], writes=[bynT])
                for half in range(2):
                    for ch in range(8):
                        p.op("pe", lambda e: e.matmul(pC[:], ynT[:, ch, :], wo[:, ch, half * 512:(half + 1) * 512], start=(ch == 0), stop=(ch == 7)),
                             reads=[bynT, bwo], writes=[bpC])
                    p.op("dve", lambda e: e.tensor_copy(ob[:, half * 512:(half + 1) * 512], pC[:]), reads=[bpC], writes=[bob])
                p.dma("pool", P[sl, :], ob[:], reads=[bob], writes=[bP[t]])
        p.barrier()


def ssd_weights(z, h):
    w_in = z["ssd_w_in"][0]
    f = np.ascontiguousarray
    cols = np.concatenate([2048 + h * 1024 + np.arange(1024), 4096 + h * 512 + np.arange(512), 5120 + h * 512 + np.arange(512)])
    cch = cols - 2048
    convp = np.stack([z["ssd_conv_w"][0][0, cch], z["ssd_conv_w"][0][1, cch], z["ssd_conv_w"][0][2, cch], z["ssd_conv_b"][0][cch]], 1)
    dtc = np.concatenate([6144 + 16 * h + np.arange(16), 6144 + 32 + 16 * h + np.arange(16)])
    hs = slice(16 * h, 16 * h + 16)
    tri = np.triu(np.ones((128, 128), np.float32))
    return dict(w_cv=f(w_in[:, cols]), convp=f(convp.reshape(16, 128, 4).astype(np.float32)), w_z=f(w_in[:, h * 1024:(h + 1) * 1024]),
                w_dt=f(w_in[:, dtc]), dtb=f(np.concatenate([z["ssd_dt_bias"][0][0, hs], z["ssd_dt_bias"][0][1, hs]])[None, :]),
                alog=f(np.concatenate([z["ssd_a_log"][0][0, hs], z["ssd_a_log"][0][1, hs]])[None, :]),
                dvec=f(np.repeat(z["ssd_d"][0][hs], 64)[None, :]), ng=f(z["ssd_norm_g"][0][h * 1024:(h + 1) * 1024][None, :]),
                w_out=f(z["ssd_w_out"][0][h * 1024:(h + 1) * 1024]), triF=tri, triB=f(tri.T))


def conv_fm(c, K, HT, bHT, w_cv, convp, nblk, FT, bFT):
    p = c.p
    groups = [list(range(4 * q, 4 * q + 4)) for q in range(8)] + [[32, 33]]
    es = ExitStack()
    with es:
        wc = c.sb([128, 8, 512], es); bwc = Buf()
        cp = c.sb([128, 4 * nblk, 4], es); bcp = Buf()
        p.dma("pool", cp[:], convp.rearrange("k p f -> p k f"), writes=[bcp])
        hg = [c.sb([128, 4, 8, 128], es) for _ in range(2)]; bhg = [Buf(), Buf()]
        raw = [c.sb([128, NTOK], es) for _ in range(4)]; braw = [Buf() for _ in range(4)]
        cv = [c.sb([128, NTOK], es) for _ in range(2)]; bcv = [Buf(), Buf()]
        pr_ = [c.ps([128, 512], es) for _ in range(2)]; bpr = [PBuf(), PBuf()]
        n = 0
        for cb in range(nblk):
            p.dma("sp", wc[:], w_cv[:, cb * 512:(cb + 1) * 512].rearrange("(c p) f -> p c f", p=128), writes=[bwc])
            for gi, tiles in enumerate(groups):
                i = gi % 2
                nt = len(tiles)
                N = nt * 128
                t0 = tiles[0] * 128
                p.dma("pool", hg[i][:, 0:nt], HT[tiles[0]:tiles[0] + nt].rearrange("t p c k -> p t c k"), reads=[bHT[t] for t in tiles], writes=[bhg[i]])
                for cc in range(4):
                    j = n % 2
                    n += 1
                    for k in range(nt):
                        for ch in range(8):
                            p.op("pe", lambda e: e.matmul(pr_[j][:, k * 128:(k + 1) * 128], wc[:, ch, cc * 128:(cc + 1) * 128], hg[i][:, k, ch, :],
                                                          start=(ch == 0), stop=(ch == 7)), reads=[bwc, bhg[i]], writes=[bpr[j]])
                    p.op("act" if j else "dve",
                         (lambda e: e.copy(raw[cc][:, t0:t0 + N], pr_[j][:, 0:N])) if j else (lambda e: e.tensor_copy(raw[cc][:, t0:t0 + N], pr_[j][:, 0:N])),
                         reads=[bpr[j]], writes=[braw[cc]])
            for cc in range(4):
                k = cb * 4 + cc
                o = cv[cc % 2]; bo_ = bcv[cc % 2]; r = raw[cc]
                p.op("dve", lambda e: e.tensor_scalar(o[:], r[:], cp[:, k, 1:2], None, ALU.mult), reads=[braw[cc], bcp], writes=[bo_])
                for (a0, a1) in ((0, 4096), (4096, NTOK)):
                    p.op("dve", lambda e: e.scalar_tensor_tensor(o[:, a0 + 1:a1], r[:, a0:a1 - 1], cp[:, k, 0:1], o[:, a0 + 1:a1], ALU.mult, ALU.add),
                         reads=[braw[cc], bcp, bo_], writes=[bo_])
                    p.op("dve", lambda e: e.scalar_tensor_tensor(o[:, a0:a1 - 1], r[:, a0 + 1:a1], cp[:, k, 2:3], o[:, a0:a1 - 1], ALU.mult, ALU.add),
                         reads=[braw[cc], bcp, bo_], writes=[bo_])
                p.op("act", lambda e: e.activation(o[:], o[:], AF.Silu, bias=cp[:, k, 3:4]), reads=[bo_, bcp], writes=[bo_])
                p.dma("sp", FT[k], o[:], reads=[bo_], writes=[bFT[k]])
        p.barrier()


def emit_gdn(c, K, X, bX, mods, w_cv, convp, w_z, w_bg, dtb, alog, ng, w_out, masks, P, bP, stop=None):
    p = c.p
    HT = dram(c, [NT, 128, 8, 128]); bHT = [Buf() for _ in range(NT)]
    FT = dram(c, [12, 128, NTOK]); bFT = [Buf() for _ in range(12)]
    ZS = dram(c, [NTOK, 512]); BG = dram(c, [NTOK, 16]); QKV = dram(c, [NTOK, 1536]); QKT = dram(c, [8, 128, NTOK]); OF = dram(c, [NTOK, 512])
    bZS = [Buf() for _ in range(NT)]; bQ = [Buf() for _ in range(NT)]; bOF = [Buf() for _ in range(NT)]
    es = ExitStack()
    with es:
        wz = c.sb([128, 8, 512], es); wbg = c.sb([128, 8, 16], es); bwz = Buf()
        p.dma("sp", wz[:], w_z.rearrange("(c p) f -> p c f", p=128), writes=[bwz])
        p.dma("sp", wbg[:], w_bg.rearrange("(c p) f -> p c f", p=128), writes=[bwz])
        dtb_t = c.sb([128, 8], es); na = c.sb([128, 8], es); bdb = Buf()
        p.dma("pool", dtb_t[:], dtb.partition_broadcast(128), writes=[bdb])
        p.dma("pool", na[:], alog.partition_broadcast(128), writes=[bdb])
        p.op("act", lambda e: e.activation(na[:], na[:], AF.Exp), reads=[bdb], writes=[bdb])
        p.op("dve", lambda e: e.tensor_scalar(na[:], na[:], -1.0, None, ALU.mult), reads=[bdb], writes=[bdb])
        xt = [c.sb([128, D], es) for _ in range(2)]; bxt = [Buf(), Buf()]
        hT = [c.sb([128, 8, 128], es) for _ in range(2)]; bhT = [Buf(), Buf()]
        pz = c.ps([128, 512], es); bpz = PBuf()
        pd = c.ps([128, 512], es); bpd = PBuf()
        zs = [c.sb([128, 512], es) for _ in range(2)]; bzs = [Buf(), Buf()]
        dr = c.sb([128, 8], es); ta = c.sb([128, 8], es); tb = c.sb([128, 8], es); bgo = [c.sb([128, 16], es) for _ in range(2)]
        bdr, btt, bbgo = Buf(), Buf(), [Buf(), Buf()]
        for t in range(NT):
            i = t % 2
            G, S, _ = mods[tile_kind(t)]
            p.dma("pool", xt[i][:], X[t * 128:(t + 1) * 128, :], reads=[bX[t]], writes=[bxt[i]])
            norm_mod_T(c, K, xt[i][:], bxt[i], G, S, hT[i], bhT[i])
            p.dma("sp", HT[t], hT[i][:], reads=[bhT[i]], writes=[bHT[t]])
            for ch in range(8):
                p.op("pe", lambda e: e.matmul(pz[:], hT[i][:, ch, :], wz[:, ch, :], start=(ch == 0), stop=(ch == 7)), reads=[bhT[i], bwz], writes=[bpz])
            p.op("act", lambda e: e.activation(zs[i][:], pz[:], AF.Silu), reads=[bpz], writes=[bzs[i]])
            p.dma("sp", ZS[t * 128:(t + 1) * 128, :], zs[i][:], reads=[bzs[i]], writes=[bZS[t]])
            for ch in range(8):
                p.op("pe", lambda e: e.matmul(pd[:, 0:16], hT[i][:, ch, :], wbg[:, ch, :], start=(ch == 0), stop=(ch == 7)), reads=[bhT[i], bwz], writes=[bpd])
            p.op("act", lambda e: e.activation(bgo[i][:, 0:8], pd[:, 0:8], AF.Sigmoid), reads=[bpd], writes=[bbgo[i]])
            p.op("dve", lambda e: e.tensor_add(dr[:], pd[:, 8:16], dtb_t[:]), reads=[bpd, bdb], writes=[bdr])
            softplus_tile(c, ta[:], dr[:], ta[:], tb[:], [bdr], [btt], btt)
            p.op("dve", lambda e: e.tensor_mul(bgo[i][:, 8:16], ta[:], na[:]), reads=[btt, bdb], writes=[bbgo[i]])
            p.dma("sp", BG[t * 128:(t + 1) * 128, :], bgo[i][:], reads=[bbgo[i]], writes=[bZS[t]])
        p.barrier()
    if stop == "A1":
        return
    conv_fm(c, K, HT, bHT, w_cv, convp, 3, FT, bFT)
    if stop == "A2":
        return
    es = ExitStack()
    with es:
        fx = [c.sb([128, 12, 128], es) for _ in range(2)]; bfx = [Buf(), Buf()]
        tm = [c.sb([128, 12, 128], es) for _ in range(2)]; btm = [Buf(), Buf()]
        qkT = [c.sb([128, 8, 128], es) for _ in range(2)]; bqkT = [Buf(), Buf()]
        s8 = c.sb([128, 16], es); bs8 = Buf()
        for t in range(NT):
            i = t % 2
            p.dma("sp", fx[i][:], FT[0:12, :, t * 128:(t + 1) * 128].rearrange("k p t -> p k t"), reads=bFT, writes=[bfx[i]])
            for q in range(3):
                for cc in range(4):
                    p.op("pe", lambda e: e.transpose(K["ptr"][:, cc * 128:(cc + 1) * 128], fx[i][:, q * 4 + cc, :], K["ident"][:]),
                         reads=[bfx[i], K["bident"]], writes=[K["bptr"]])
                p.op("act" if q % 2 else "dve",
                     (lambda e: e.copy(tm[i][:, q * 4:(q + 1) * 4, :], K["ptr"][:].rearrange("p (c t) -> p c t", c=4))) if q % 2 else
                     (lambda e: e.tensor_copy(tm[i][:, q * 4:(q + 1) * 4, :], K["ptr"][:].rearrange("p (c t) -> p c t", c=4))),
                     reads=[K["bptr"]], writes=[btm[i]])
            for hq in range(8):
                p.op("act", lambda e: e.activation(K["junk"][:, 0:128], tm[i][:, hq, :], AF.Square, accum_out=s8[:, hq:hq + 1]),
                     reads=[btm[i]], writes=[K["bjunk"], bs8])
            p.op("act", lambda e: e.activation(s8[:, 8:16], s8[:, 0:8], AF.Sqrt, bias=K["eps"][:, 0:1]), reads=[bs8], writes=[bs8])
            p.op("dve", lambda e: e.reciprocal(s8[:, 0:8], s8[:, 8:16]), reads=[bs8], writes=[bs8])
            for hq in range(8):
                sc2 = (128.0 ** -0.5) if hq < 4 else 1.0
                p.op("dve", lambda e: e.tensor_scalar(tm[i][:, hq, :], tm[i][:, hq, :], s8[:, hq:hq + 1], sc2, ALU.mult, ALU.mult),
                     reads=[btm[i], bs8], writes=[btm[i]])
            p.dma("pool", QKV[t * 128:(t + 1) * 128, :], tm[i][:].rearrange("p k d -> p (k d)"), reads=[btm[i]], writes=[bQ[t]])
            for q in range(2):
                for cc in range(4):
                    p.op("pe", lambda e: e.transpose(K["ptr"][:, cc * 128:(cc + 1) * 128], tm[i][:, q * 4 + cc, :], K["ident"][:]),
                         reads=[btm[i], K["bident"]], writes=[K["bptr"]])
                p.op("act", lambda e: e.copy(qkT[i][:, q * 4:(q + 1) * 4, :], K["ptr"][:].rearrange("p (c t) -> p c t", c=4)),
                     reads=[K["bptr"]], writes=[bqkT[i]])
            p.dma("pool", QKT[:, :, t * 128:(t + 1) * 128].rearrange("h p t -> p h t"), qkT[i][:], reads=[bqkT[i]], writes=[bQ[t]])
        p.barrier()
    if stop == "A3":
        return
    es = ExitStack()
    with es:
        mk = c.sb([128, 2, 7, 128], es); bmk = Buf()
        for dd in range(2):
            for mm_ in range(7):
                p.dma("sp", mk[:, dd, mm_, :], masks[dd, mm_], writes=[bmk])
        m4 = c.sb([128, 4, 4, 128], es); id4 = c.sb([128, 4, 128], es); bm4 = Buf()
        for hh in range(4):
            p.op("dve", lambda e: e.tensor_copy(m4[:, :, hh, :], mk[:, 0, 3:7, :]), reads=[bmk], writes=[bm4])
            p.op("dve", lambda e: e.tensor_copy(id4[:, hh, :], K["ident"][:]), reads=[K["bident"]], writes=[bm4])
        ones = c.sb([128, 128], es); bon = Buf()
        p.op("dve", lambda e: e.memset(ones[:], 1.0), writes=[bon])
        tri_t = [c.sb([128, 128], es) for _ in range(2)]
        ms_t = [c.sb([128, 128], es) for _ in range(2)]
        nm_t = [c.sb([128, 128], es) for _ in range(2)]
        for dd in range(2):
            p.op("dve", lambda e: e.tensor_copy(tri_t[dd][:], mk[:, dd, 0, :]), reads=[bmk], writes=[bmk])
            p.op("dve", lambda e: e.tensor_copy(ms_t[dd][:], mk[:, dd, 1, :]), reads=[bmk], writes=[bmk])
            p.op("dve", lambda e: e.tensor_copy(nm_t[dd][:], mk[:, dd, 2, :]), reads=[bmk], writes=[bmk])
        ngt = c.sb([128, 128], es); bng = Buf()
        p.dma("pool", ngt[:], ng.partition_broadcast(128), writes=[bng])
        wo = c.sb([128, 4, D], es); bwo = Buf()
        p.dma("sp", wo[:], w_out.rearrange("(c p) f -> p c f", p=128), writes=[bwo])
        qkv = [c.sb([128, 12, 128], es) for _ in range(2)]; qkt = [c.sb([128, 8, 128], es) for _ in range(2)]; bg_ = [c.sb([128, 16], es) for _ in range(2)]
        bin_ = [Buf(), Buf()]
        S = c.sb([128, 4, 128], es); bS = Buf()
        sm = c.sb([128, 32], es); bsm = Buf()
        sums = c.sb([128, 32], es); bsums = Buf()
        rbs = (c.sb([128, 4, 128], es), Buf())
        gc, egc, etot, kdw, bs_ = (sm[:, 4 * j:4 * j + 4] for j in range(5))
        banks = [(c.ps([128, 512], es), PBuf()) for _ in range(7)]
        bank_i = [0]

        def nb():
            b = banks[bank_i[0] % 7]
            bank_i[0] += 1
            return b

        def T3(name=None):
            return (c.sb([128, 4, 128], es), Buf())
        Kb, Vb, kd, abc, tA, tB, E1, E2, Eg, Lm, AT, qgT, LT, D0, D0T, ImD0T, O1T, O2T, O3T = (T3() for _ in range(19))
        D2, D2T, IpD2T, D4, D4T, IpD4T, IpD8, R1, R2, Xa, XaT, Xb, XbT, Gt, nwT, vn, osb = (T3() for _ in range(17))
        ofl = c.sb([128, 512], es); zl = c.sb([128, 512], es); bfl = Buf()
        s4 = c.sb([128, 12], es); bs4 = Buf()
        ynT = c.sb([128, 4, 128], es); bynT = Buf()
        ob = c.sb([128, D], es); bob = Buf()

        def f(tb_):
            return tb_[0][:].rearrange("p h d -> p (h d)")

        def mm4(dst, lhs, rhs, lhs_b, rhs_b):
            for hh in range(4):
                p.op("pe", lambda e: e.matmul(dst[0][:, hh * 128:(hh + 1) * 128], lhs[0][:, hh, :], rhs[0][:, hh, :], start=True, stop=True),
                     reads=[lhs_b, rhs_b], writes=[dst[1]])

        def ev(eng, dst, src_ps, add=None, sub_from=None):
            if sub_from is not None:
                p.op("dve", lambda e: e.tensor_sub(f(dst), f(sub_from), src_ps[0][:]), reads=[src_ps[1], sub_from[1]], writes=[dst[1]])
            elif add is not None:
                p.op("dve", lambda e: e.tensor_add(f(dst), src_ps[0][:], add), reads=[src_ps[1], bm4], writes=[dst[1]])
            elif eng == "act":
                p.op("act", lambda e: e.copy(f(dst), src_ps[0][:]), reads=[src_ps[1]], writes=[dst[1]])
            else:
                p.op("dve", lambda e: e.tensor_copy(f(dst), src_ps[0][:]), reads=[src_ps[1]], writes=[dst[1]])
        idf = id4[:].rearrange("p h d -> p (h d)")
        for d in range(2):
            order = [32, 33] + list(range(32)) if d == 0 else [33, 32] + list(range(31, -1, -1))
            if isinstance(stop, int):
                order = order[:stop]
            tri_d, MS_d, NM_d = mk[:, d, 0, :], mk[:, d, 1, :], mk[:, d, 2, :]
            p.op("dve", lambda e: e.memset(S[:], 0.0), reads=[bS], writes=[bS])
            if stop == ("B", 1):
                p.barrier()
                return
            for vi, t in enumerate(order):
                i = vi % 2
                sl = slice(t * 128, (t + 1) * 128)
                p.dma("sp", qkv[i][:], QKV[sl, :].rearrange("p (k d) -> p k d", k=12), reads=[bQ[t]], writes=[bin_[i]])
                for hq in range(8):
                    p.dma("sp", qkt[i][:, hq, :], QKT[hq, :, sl], reads=[bQ[t]], writes=[bin_[i]])
                p.dma("sp", bg_[i][:], BG[sl, :], reads=[bZS[t]], writes=[bin_[i]])
                beta = bg_[i][:, d * 4:(d + 1) * 4]
                g = bg_[i][:, 8 + d * 4:8 + (d + 1) * 4]
                qv = (qkv[i], bin_[i]); kT = qkt[i]
                pa_ = nb()
                gcol = slice(8 + d * 4, 8 + (d + 1) * 4)
                tcol = slice(16 + 8 + d * 4, 16 + 8 + (d + 1) * 4)
                p.op("pe", lambda e: e.matmul(pa_[0][:, 0:16], tri_t[d][:], bg_[i][:, 0:16], start=True, stop=True), reads=[bmk, bin_[i]], writes=[pa_[1]])
                p.op("pe", lambda e: e.matmul(pa_[0][:, 16:32], ones[:], bg_[i][:, 0:16], start=True, stop=True), reads=[bon, bin_[i]], writes=[pa_[1]])
                p.op("act", lambda e: e.copy(sums[:], pa_[0][:, 0:32]), reads=[pa_[1]], writes=[bsums])
                p.op("dve", lambda e: e.tensor_copy(gc, sums[:, gcol]), reads=[bsums], writes=[bsm])
                p.op("act", lambda e: e.activation(egc, sums[:, gcol], AF.Exp), reads=[bsums], writes=[bsm])
                p.op("act", lambda e: e.activation(etot, sums[:, tcol], AF.Exp), reads=[bsums], writes=[bsm])
                p.op("dve", lambda e: e.tensor_tensor(kdw, sums[:, tcol], gc, ALU.subtract), reads=[bsums, bsm], writes=[bsm])
                p.op("act", lambda e: e.activation(kdw, kdw, AF.Exp), reads=[bsm], writes=[bsm])
                p.op("dve", lambda e: e.tensor_mul(bs_, beta, egc), reads=[bin_[i], bsm], writes=[bsm])
                prb, pkk, pqk = nb(), nb(), nb()
                for hh in range(4):
                    p.op("pool", lambda e: e.tensor_scalar(Kb[0][:, hh, :], qkv[i][:, 4 + hh, :], bs_[:, hh:hh + 1], 1.0, ALU.mult, ALU.mult), reads=[bin_[i], bsm], writes=[Kb[1]])
                    p.op("pool", lambda e: e.tensor_scalar(Vb[0][:, hh, :], qkv[i][:, 8 + hh, :], beta[:, hh:hh + 1], 1.0, ALU.mult, ALU.mult), reads=[bin_[i]], writes=[Vb[1]])
                    p.op("pool", lambda e: e.tensor_scalar(kd[0][:, hh, :], qkv[i][:, 4 + hh, :], kdw[:, hh:hh + 1], 1.0, ALU.mult, ALU.mult), reads=[bin_[i], bsm], writes=[kd[1]])
                    p.op("pool", lambda e: e.tensor_scalar(abc[0][:, hh, :], ones[:], g[:, hh:hh + 1], 1.0, ALU.mult, ALU.mult), reads=[bon, bin_[i]], writes=[abc[1]])
                    p.op("pe", lambda e: e.matmul(prb[0][:, hh * 128:(hh + 1) * 128], abc[0][:, hh, :], tri_t[d][:], start=True, stop=True), reads=[abc[1], bmk], writes=[prb[1]])
                    p.op("pe", lambda e: e.matmul(pkk[0][:, hh * 128:(hh + 1) * 128], kT[:, 4 + hh, :], kT[:, 4 + hh, :], start=True, stop=True), reads=[bin_[i]], writes=[pkk[1]])
                    p.op("pe", lambda e: e.matmul(pqk[0][:, hh * 128:(hh + 1) * 128], kT[:, 4 + hh, :], kT[:, hh, :], start=True, stop=True), reads=[bin_[i]], writes=[pqk[1]])
                    if stop == ("B", 2):
                        p.barrier()
                        return
                p.op("act", lambda e: e.copy(rbs[0][:].rearrange("p h d -> p (h d)"), prb[0][:]), reads=[prb[1]], writes=[rbs[1]])
                for hh in range(4):
                    p.op("dve", lambda e: e.scalar_tensor_tensor(tA[0][:, hh, :], rbs[0][:, hh, :], gc[:, hh:hh + 1], ms_t[d][:], ALU.subtract, ALU.max),
                         reads=[rbs[1], bsm, bmk], writes=[tA[1]])
                    p.op("dve", lambda e: e.scalar_tensor_tensor(tB[0][:, hh, :], rbs[0][:, hh, :], gc[:, hh:hh + 1], nm_t[d][:], ALU.subtract, ALU.min),
                         reads=[rbs[1], bsm, bmk], writes=[tB[1]])
                p.op("act", lambda e: e.activation(f(E1), f(tA), AF.Exp, scale=-1.0), reads=[tA[1]], writes=[E1[1]])
                p.op("act", lambda e: e.activation(f(E2), f(tB), AF.Exp), reads=[tB[1]], writes=[E2[1]])
                p.op("act", lambda e: e.activation(f(Eg), rbs[0][:].rearrange("p h d -> p (h d)"), AF.Exp), reads=[rbs[1]], writes=[Eg[1]])
                for hh in range(4):
                    p.op("dve", lambda e: e.scalar_tensor_tensor(Lm[0][:, hh, :], pkk[0][:, hh * 128:(hh + 1) * 128], beta[:, hh:hh + 1], E1[0][:, hh, :], ALU.mult, ALU.mult),
                         reads=[pkk[1], bin_[i], E1[1]], writes=[Lm[1]])
                p.op("dve", lambda e: e.tensor_mul(f(AT), pqk[0][:], f(E2)), reads=[pqk[1], E2[1]], writes=[AT[1]])
                p.op("pool", lambda e: e.tensor_mul(f(qgT), qkt[i][:, 0:4, :].rearrange("p h d -> p (h d)"), f(Eg)), reads=[bin_[i], Eg[1]], writes=[qgT[1]])
                if stop == ("B", 3):
                    p.barrier()
                    return
                plt = nb()
                for hh in range(4):
                    p.op("pe", lambda e: e.transpose(plt[0][:, hh * 128:(hh + 1) * 128], Lm[0][:, hh, :], K["ident"][:]), reads=[Lm[1], K["bident"]], writes=[plt[1]])
                ev("act", LT, plt)
                p.op("dve", lambda e: e.tensor_mul(f(D0), f(Lm), m4[:, 0].rearrange("p h d -> p (h d)")), reads=[Lm[1], bm4], writes=[D0[1]])
                p.op("pool", lambda e: e.tensor_mul(f(D0T), f(LT), m4[:, 0].rearrange("p h d -> p (h d)")), reads=[LT[1], bm4], writes=[D0T[1]])
                p.op("pool", lambda e: e.tensor_mul(f(O1T), f(LT), m4[:, 1].rearrange("p h d -> p (h d)")), reads=[LT[1], bm4], writes=[O1T[1]])
                p.op("dve", lambda e: e.tensor_mul(f(O2T), f(LT), m4[:, 2].rearrange("p h d -> p (h d)")), reads=[LT[1], bm4], writes=[O2T[1]])
                p.op("pool", lambda e: e.tensor_mul(f(O3T), f(LT), m4[:, 3].rearrange("p h d -> p (h d)")), reads=[LT[1], bm4], writes=[O3T[1]])
                p.op("pool", lambda e: e.tensor_sub(f(ImD0T), idf, f(D0T)), reads=[D0T[1], bm4], writes=[ImD0T[1]])
                if stop == ("B", 4):
                    p.barrier()
                    return
                b1 = nb(); mm4(b1, D0T, D0, D0T[1], D0[1]); ev("act", D2, b1)
                b2 = nb(); mm4(b2, D0, D0T, D0[1], D0T[1]); ev("act", D2T, b2); ev("dve", IpD2T, b2, add=idf)
                b3 = nb(); mm4(b3, D2T, D2, D2T[1], D2[1]); ev("act", D4, b3)
                b4 = nb(); mm4(b4, D2, D2T, D2[1], D2T[1]); ev("act", D4T, b4); ev("dve", IpD4T, b4, add=idf)
                b5 = nb(); mm4(b5, D4T, D4, D4T[1], D4[1]); ev("dve", IpD8, b5, add=idf)
                b6 = nb(); mm4(b6, IpD4T, IpD8, IpD4T[1], IpD8[1]); ev("act", R1, b6)
                b7 = nb(); mm4(b7, IpD2T, R1, IpD2T[1], R1[1]); ev("dve", R2, b7)
                b8 = nb(); mm4(b8, ImD0T, R2, ImD0T[1], R2[1]); ev("act", Xa, b8)
                b9 = nb()
                for hh in range(4):
                    p.op("pe", lambda e: e.transpose(b9[0][:, hh * 128:(hh + 1) * 128], Xa[0][:, hh, :], K["ident"][:]), reads=[Xa[1], K["bident"]], writes=[b9[1]])
                ev("dve", XaT, b9)
                if stop == ("B", 5):
                    p.barrier()
                    return
                Xc, XcT, Xn, XnT = Xa, XaT, Xb, XbT
                for lvl, OT_ in enumerate((O1T, O2T, O3T)):
                    bg1 = nb(); mm4(bg1, OT_, Xc, OT_[1], Xc[1]); ev("act", Gt, bg1)
                    if lvl < 2:
                        bp1 = nb(); mm4(bp1, XcT, Gt, XcT[1], Gt[1]); ev("dve", Xn, bp1, sub_from=Xc)
                    bp2 = nb(); mm4(bp2, Gt, XcT, Gt[1], XcT[1]); ev("dve", XnT, bp2, sub_from=XcT)
                    Xc, XcT, Xn, XnT = Xn, XnT, Xc, XcT
                TT = XcT
                if stop == ("B", 6):
                    p.barrier()
                    return
                bw = nb(); mm4(bw, Kb, TT, Kb[1], TT[1])
                p.op("dve", lambda e: e.tensor_scalar(f(nwT), bw[0][:], -1.0, None, ALU.mult), reads=[bw[1]], writes=[nwT[1]])
                bv = nb()
                for hh in range(4):
                    p.op("pe", lambda e: e.matmul(bv[0][:, hh * 128:(hh + 1) * 128], TT[0][:, hh, :], Vb[0][:, hh, :], start=True, stop=False), reads=[TT[1], Vb[1]], writes=[bv[1]])
                    p.op("pe", lambda e: e.matmul(bv[0][:, hh * 128:(hh + 1) * 128], nwT[0][:, hh, :], S[:, hh, :], start=False, stop=True), reads=[nwT[1], bS], writes=[bv[1]])
                ev("act", vn, bv)
                bo_ = nb()
                for hh in range(4):
                    p.op("pe", lambda e: e.matmul(bo_[0][:, hh * 128:(hh + 1) * 128], qgT[0][:, hh, :], S[:, hh, :], start=True, stop=False), reads=[qgT[1], bS], writes=[bo_[1]])
                    p.op("pe", lambda e: e.matmul(bo_[0][:, hh * 128:(hh + 1) * 128], AT[0][:, hh, :], vn[0][:, hh, :], start=False, stop=True), reads=[AT[1], vn[1]], writes=[bo_[1]])
                ev("dve", osb, bo_)
                bsn = nb(); mm4(bsn, kd, vn, kd[1], vn[1])
                for hh in range(4):
                    p.op("dve", lambda e: e.scalar_tensor_tensor(S[:, hh, :], S[:, hh, :], etot[:, hh:hh + 1], bsn[0][:, hh * 128:(hh + 1) * 128], ALU.mult, ALU.add),
                         reads=[bS, bsm, bsn[1]], writes=[bS])
                if stop == ("B", 7):
                    p.barrier()
                    return
                if d == 0:
                    p.dma("pool", OF[sl, :], f(osb), reads=[osb[1]], writes=[bOF[t]])
                    continue
                p.dma("pool", ofl[:], OF[sl, :], reads=[bOF[t]], writes=[bfl])
                p.dma("pool", zl[:], ZS[sl, :], reads=[bZS[t]], writes=[bfl])
                p.op("dve", lambda e: e.tensor_add(f(osb), f(osb), ofl[:]), reads=[osb[1], bfl], writes=[osb[1]])
                for hh in range(4):
                    p.op("act", lambda e: e.activation(K["junk"][:, 0:128], osb[0][:, hh, :], AF.Square, accum_out=s4[:, hh:hh + 1]), reads=[osb[1]], writes=[K["bjunk"], bs4])
                p.op("act", lambda e: e.activation(s4[:, 4:8], s4[:, 0:4], AF.Sqrt, bias=K["eps"][:, 0:1], scale=1.0 / 128), reads=[bs4], writes=[bs4])
                p.op("dve", lambda e: e.reciprocal(s4[:, 8:12], s4[:, 4:8]), reads=[bs4], writes=[bs4])
                for hh in range(4):
                    p.op("dve", lambda e: e.scalar_tensor_tensor(osb[0][:, hh, :], osb[0][:, hh, :], s4[:, 8 + hh:9 + hh], ngt[:], ALU.mult, ALU.mult),
                         reads=[osb[1], bs4, bng], writes=[osb[1]])
                p.op("dve", lambda e: e.tensor_mul(f(osb), f(osb), zl[:]), reads=[osb[1], bfl], writes=[osb[1]])
                for hh in range(4):
                    p.op("pe", lambda e: e.transpose(K["ptr"][:, hh * 128:(hh + 1) * 128], osb[0][:, hh, :], K["ident"][:]), reads=[osb[1], K["bident"]], writes=[K["bptr"]])
                p.op("act", lambda e: e.copy(ynT[:].rearrange("p h d -> p (h d)"), K["ptr"][:]), reads=[K["bptr"]], writes=[bynT])
                for half in range(2):
                    pc_ = nb()
                    for ch in range(4):
                        p.op("pe", lambda e: e.matmul(pc_[0][:], ynT[:, ch, :], wo[:, ch, half * 512:(half + 1) * 512], start=(ch == 0), stop=(ch == 3)),
                             reads=[bynT, bwo], writes=[pc_[1]])
                    p.op("dve", lambda e: e.tensor_copy(ob[:, half * 512:(half + 1) * 512], pc_[0][:]), reads=[pc_[1]], writes=[bob])
                p.dma("pool", P[sl, :], ob[:], reads=[bob], writes=[bP[t]])
        p.barrier()


def gdn_masks():
    i = np.arange(128)[:, None]; j = np.arange(128)[None, :]
    blk = lambda n: (i // n == j // n)
    mk16 = blk(16).astype(np.float32)
    mo1 = (blk(32) & ~blk(16)).astype(np.float32)
    mo2 = (blk(64) & ~blk(32)).astype(np.float32)
    mo3 = (~blk(64)).astype(np.float32)
    out = np.zeros((2, 7, 128, 128), np.float32)
    for d in range(2):
        before = (j < i) if d == 0 else (j > i)
        tri = (i <= j) if d == 0 else (i >= j)
        out[d, 0] = tri
        out[d, 1] = np.where(before, 0.0, 1e4)
        out[d, 2] = np.where(tri, 0.0, -1e4)
        out[d, 3], out[d, 4], out[d, 5], out[d, 6] = mk16, mo1, mo2, mo3
    return out


def gdn_weights(z, jl, h):
    w_in = z["gdn_w_in"][jl]
    f = np.ascontiguousarray
    cols = np.concatenate([h * 512 + np.arange(512), 1024 + h * 512 + np.arange(512), 2048 + h * 512 + np.arange(512)])
    cw = z["gdn_conv_w"][jl]
    convp = np.stack([cw[0, cols], cw[1, cols], cw[2, cols], np.zeros(1536, np.float32)], 1)
    hs = 4 * h + np.arange(4)
    bgc = np.concatenate([4096 + hs, 4096 + 8 + hs, 4112 + hs, 4112 + 8 + hs])
    return dict(w_cv=f(w_in[:, cols]), convp=f(convp.reshape(12, 128, 4).astype(np.float32)), w_z=f(w_in[:, 3072 + h * 512:3072 + (h + 1) * 512]),
                w_bg=f(w_in[:, bgc]), dtb=f(np.concatenate([z["gdn_dt_bias"][jl][0, hs], z["gdn_dt_bias"][jl][1, hs]])[None, :]),
                alog=f(np.concatenate([z["gdn_a_log"][jl][0, hs], z["gdn_a_log"][jl][1, hs]])[None, :]),
                ng=f(z["gdn_norm_g"][jl][None, :]), w_out=f(z["gdn_w_out"][jl][h * 512:(h + 1) * 512]), masks=gdn_masks())


def build_ada():
    nc = new_nc()
    es = ExitStack()
    with es:
        c = Ctx(nc, es); p = c.p
        K = common_consts(c, c.din("ident", [128, 128]))
        cvec = c.din("cvec", [5, D]); aw = c.din("aw", [D, 3072]); ab = c.din("ab", [1, 3072])
        out = c.dout("m", [5, 3072])
        craw = c.sb([40, 128]); bcraw = Buf()
        p.dma("pool", craw[:], cvec.rearrange("k (c p) -> (k c) p", p=128), writes=[bcraw])
        sc = c.sb([128, 8, 5]); bsc = Buf()
        p.op("pe", lambda e: e.transpose(K["ptr"][:, 0:40], craw[:], K["ident"][0:40, 0:40]), reads=[bcraw, K["bident"]], writes=[K["bptr"]])
        p.op("act", lambda e: e.activation(sc[:].rearrange("p c k -> p k c"), K["ptr"][:, 0:40].rearrange("p (k c) -> p k c", k=5), AF.Silu),
             reads=[K["bptr"]], writes=[bsc])
        abt = c.sb([5, 3072]); babt = Buf()
        p.dma("pool", abt[:], ab.partition_broadcast(5), writes=[babt])
        wt = [c.sb([128, 8, 512]) for _ in range(2)]; bwt = [Buf(), Buf()]
        pa = [c.ps([128, 512]) for _ in range(2)]; bpa = [PBuf(), PBuf()]
        orow = c.sb([5, 3072]); borow = Buf()
        for g in range(6):
            j = g % 2
            p.dma("sp", wt[j][:], aw[:, g * 512:(g + 1) * 512].rearrange("(c p) f -> p c f", p=128), writes=[bwt[j]])
            for ch in range(8):
                p.op("pe", lambda e: e.matmul(pa[j][0:5, :], sc[:, ch, :], wt[j][:, ch, :], start=(ch == 0), stop=(ch == 7)), reads=[bsc, bwt[j]], writes=[bpa[j]])
            p.op("dve", lambda e: e.tensor_add(orow[:, g * 512:(g + 1) * 512], pa[j][0:5, :], abt[:, g * 512:(g + 1) * 512]), reads=[bpa[j], babt], writes=[borow])
        bo = Buf()
        p.dma("pool", out, orow[:], reads=[borow], writes=[bo])
        p.finish([bo])
    return nc


_STAGES = {}


def _stage(kind, **kw):
    key = (kind, tuple(sorted(kw.items())))
    if key not in _STAGES:
        _STAGES[key] = build_ada() if kind == "ada" else build_stage(kind, **kw)
    return _STAGES[key]


def kernel_unfused(**z):
    z = {k: np.asarray(v) for k, v in z.items()}
    f = np.ascontiguousarray
    cores = list(range(8))
    ident = np.eye(128, dtype=np.float32)
    cvec = f(np.concatenate([z["c"], z["c_ctx"][None]], 0).astype(np.float32))
    ins = [dict(ident=ident, cvec=cvec, aw=f(z["ada_w"][k // 2][:, (k % 2) * 3072:(k % 2 + 1) * 3072]),
                ab=f(z["ada_b"][k // 2][None, (k % 2) * 3072:(k % 2 + 1) * 3072])) for k in cores]
    r = run_bass_kernel_spmd(_stage("ada"), ins, core_ids=cores).results
    mods = np.stack([np.concatenate([r[2 * i]["m"], r[2 * i + 1]["m"]], 1) for i in range(4)]).reshape(4, 5, 6, D)
    zero_v = np.zeros(D, np.float32)

    def vec_for(li, b, pgx, pgc):
        return make_vec(mods[li, b], mods[li, 4], z["norm1_g"][li], z["norm2_g"][li], z["final_norm_g"], pgx, pgc)

    xprev = [f(np.concatenate([z["x"][b], z["ctx"][b]], 0)) for b in range(4)]
    part = [np.zeros((NTOK, D), np.float32) for _ in cores]
    ss = None
    pg = [(zero_v, zero_v) for _ in range(4)]
    plan = []
    for li in range(4):
        plan.append((("gdn", "ssd", "mla")[li % 3], li))
        plan.append(("moe", li))
    plan.append(("final", 3))
    for kind, li in plan:
        with_ss = ss is not None
        nc = _stage(kind, with_ss=True) if with_ss else _stage(kind)
        ins = []
        for k in cores:
            b, h = divmod(k, 2)
            d = dict(ident=ident, xprev=xprev[b], pa=part[2 * b], pb=part[2 * b + 1], vec=vec_for(li, b, *pg[b]))
            if with_ss:
                d.update(ssa=ss[2 * b], ssb=ss[2 * b + 1])
            if kind == "gdn":
                d.update(gdn_weights(z, li // 3, h))
            elif kind == "ssd":
                d.update(ssd_weights(z, h))
            elif kind == "mla":
                d.update(mla_weights(z["mla_w_in"][0], z["mla_w_uq"][0], z["mla_w_ukv"][0], z["mla_w_out"][0],
                                     z["mla_q_norm_g"][0], z["mla_kv_norm_g"][0], h))
            elif kind == "moe":
                perm = list(range(8 * h, 8 * h + 8)) + list(range(8 * (1 - h), 8 * (1 - h) + 8))
                d.update(wr=f(z["router_w"][li][:, perm]), wg=f(z["moe_w_gate"][li][8 * h:8 * h + 8].reshape(8 * D, D)),
                         wu=f(z["moe_w_up"][li][8 * h:8 * h + 8].reshape(8 * D, D)), wd=f(z["moe_w_down"][li][8 * h:8 * h + 8].reshape(8 * D, D)))
            ins.append(d)
        r = run_bass_kernel_spmd(nc, ins, core_ids=cores).results
        if kind == "final":
            return f(np.stack([r[2 * b]["p"][:4096] for b in range(4)]).astype(np.float32))
        xprev = [r[2 * b]["xo"] for b in range(4)]
        part = [r[k]["p"] for k in cores]
        ss = [r[k]["ss"] for k in cores] if kind == "ssd" else None
        gi = 5 if kind == "moe" else 2
        pg = [(mods[li, b, gi], mods[li, 4, gi]) for b in range(4)]


def decl_weights(c, kind, pre):
    d = lambda n, sh: c.din(pre + n, sh)
    if kind == "moe":
        return dict(wr=d("wr", [D, NE]), wg=d("wg", [NEH * D, D]), wu=d("wu", [NEH * D, D]), wd=d("wd", [NEH * D, D]))
    if kind == "mla":
        return dict(w_in=d("w_in", [D, 1088]), w_uqn=d("w_uqn", [768, 512]), w_uqr=d("w_uqr", [768, 256]), w_uqs=d("w_uqs", [768, 256]),
                    w_ukn=d("w_ukn", [256, 512]), w_ukv=d("w_ukv", [256, 512]), w_out=d("w_out", [512, D]), gq=d("gq", [1, 768]), gkv=d("gkv", [1, 256]),
                    cosT=d("cosT", [32, 4096]), sinT=d("sinT", [32, 4096]), cos_tm=d("cos_tm", [4096, 32]), sin_tm=d("sin_tm", [4096, 32]))
    if kind == "ssd":
        return dict(w_cv=d("w_cv", [D, 2048]), convp=d("convp", [16, 128, 4]), w_z=d("w_z", [D, D]), w_dt=d("w_dt", [D, 32]), dtb=d("dtb", [1, 32]),
                    alog=d("alog", [1, 32]), dvec=d("dvec", [1, D]), ng=d("ng", [1, D]), w_out=d("w_out", [D, D]), triF=d("triF", [128, 128]), triB=d("triB", [128, 128]))
    if kind == "gdn":
        return dict(w_cv=d("w_cv", [D, 1536]), convp=d("convp", [12, 128, 4]), w_z=d("w_z", [D, 512]), w_bg=d("w_bg", [D, 16]), dtb=d("dtb", [1, 8]),
                    alog=d("alog", [1, 8]), ng=d("ng", [1, 128]), w_out=d("w_out", [512, D]), masks=d("masks", [2, 7, 128, 128]))
    raise ValueError(kind)


FUSED_PLAN = [("gdn", 0), ("moe", 0), ("ssd", 1), ("moe", 1), ("mla", 2), ("moe", 2), ("gdn", 3), ("moe", 3)]


def build_fused():
    nc = new_nc()
    es = ExitStack()
    with es:
        c = Ctx(nc, es); p = c.p
        K = common_consts(c, c.din("ident", [128, 128]))
        xin = c.din("xin", [NTOK, D]); cvec = c.din("cvec", [2, D])
        ada_w = c.din("ada_w", [4 * D, 6 * D]); ada_b = c.din("ada_b", [4, 6 * D]); gains = c.din("gains", [9, D])
        out = c.dout("out", [4096, D])
        vecs, bv = emit_ada(c, K, cvec, ada_w, Buf(), ada_b)
        Xc = dram(c, [NTOK, D]); bXc = [Buf() for _ in range(NT)]
        for t in range(NT):
            p.dma("sp", Xc[t * 128:(t + 1) * 128, :], xin[t * 128:(t + 1) * 128, :], writes=[bXc[t]])
        prev = None

        def make_vec(li):
            vs = dram(c, [NVEC, D]); bvs = Buf()
            rows = [vecs[li, 0:1, j * D:(j + 1) * D] for j in range(6)] + [vecs[li, 1:2, j * D:(j + 1) * D] for j in range(6)]
            rows += [gains[li:li + 1, :], gains[4 + li:5 + li, :], gains[8:9, :]]
            if prev is not None:
                pli, gi = prev[8], prev[9]
                rows += [vecs[pli, 0:1, gi * D:(gi + 1) * D], vecs[pli, 1:2, gi * D:(gi + 1) * D]]
            else:
                rows += [gains[8:9, :], gains[8:9, :]]
            for r_, src in enumerate(rows):
                p.dma("sp", vs[r_:r_ + 1, :], src, reads=[bv], writes=[bvs])
            return vs, bvs

        def fold(li):
            nonlocal Xc, bXc
            vs, bvs = make_vec(li)
            c.vec_reads = [bvs]
            if prev is not None:
                Pa, bPa, Pb, bPb, SSa, bSSa, SSb, bSSb = prev[:8]
                Xn = dram(c, [NTOK, D]); bXn = [Buf() for _ in range(NT)]
                c.res_reads = list(bXc) + list(bPa) + list(bPb) + (list(bSSa) + list(bSSb) if SSa is not None else [])
                emit_residual_in(c, K, Xc, Pa, Pb, vs, Xn, bXn, None, SSa, SSb)
                c.res_reads = []
                Xc, bXc = Xn, bXn
            return vs

        for kind, li in FUSED_PLAN:
            vs = fold(li)
            Ps = []
            for h in range(2):
                w = decl_weights(c, kind, f"L{li}h{h}_")
                P = dram(c, [NTOK, D]); bP = [Buf() for _ in range(NT)]
                SS = bSS = None
                es1 = ExitStack()
                with es1:
                    if kind == "moe":
                        mods = mods_from_vec(c, es1, vs, 3, 13)
                        emit_moe(c, K, Xc, bXc, mods, w["wr"], w["wg"], w["wu"], w["wd"], Buf(), P, bP)
                    else:
                        mods = mods_from_vec(c, es1, vs, 0, 12)
                        if kind == "ssd":
                            SS = dram(c, [NTOK, 1]); bSS = [Buf() for _ in range(NT)]
                            emit_ssd(c, K, Xc, bXc, mods, P=P, bP=bP, SS=SS, bSS=bSS, **w)
                        elif kind == "mla":
                            emit_mla(c, K, Xc, bXc, mods, P=P, bP=bP, **w)
                        else:
                            emit_gdn(c, K, Xc, bXc, mods, P=P, bP=bP, **w)
                    p.barrier()
                Ps.append((P, bP, SS, bSS))
            prev = (Ps[0][0], Ps[0][1], Ps[1][0], Ps[1][1], Ps[0][2], Ps[0][3], Ps[1][2], Ps[1][3], li, 5 if kind == "moe" else 2)
        fold(3)
        es1 = ExitStack()
        bo = []
        with es1:
            fg = bc_tile(c, es1, gains[8:9, :])
            xt = [c.sb([128, D], es1) for _ in range(2)]; bxt = [Buf(), Buf()]
            ot = [c.sb([128, D], es1) for _ in range(2)]; bot = [Buf(), Buf()]
            for t in range(32):
                i = t % 2
                p.dma("sp", xt[i][:], Xc[t * 128:(t + 1) * 128, :], reads=[bXc[t]], writes=[bxt[i]])
                norm_mod_T(c, K, xt[i][:], bxt[i], fg, None, None, None)
                p.op("act", lambda e: e.copy(ot[i][:], K["h"][:]), reads=[K["bh"]], writes=[bot[i]])
                b = Buf(); bo.append(b)
                p.dma("pool", out[t * 128:(t + 1) * 128, :], ot[i][:], reads=[bot[i]], writes=[b])
        p.finish(bo)
        print("fused ninst", p.ninst, "nsem", p.nsem)
    return nc


def fused_inputs(z, b):
    f = np.ascontiguousarray
    d = dict(ident=np.eye(128, dtype=np.float32), xin=f(np.concatenate([z["x"][b], z["ctx"][b]], 0)),
             cvec=f(np.stack([z["c"][b], z["c_ctx"]]).astype(np.float32)), ada_w=f(z["ada_w"].reshape(4 * D, 6 * D)), ada_b=f(z["ada_b"]),
             gains=f(np.concatenate([z["norm1_g"], z["norm2_g"], z["final_norm_g"][None]], 0).astype(np.float32)))
    for kind, li in FUSED_PLAN:
        for h in range(2):
            pre = f"L{li}h{h}_"
            if kind == "gdn":
                w = gdn_weights(z, li // 3, h)
            elif kind == "ssd":
                w = ssd_weights(z, h)
            elif kind == "mla":
                w = mla_weights(z["mla_w_in"][0], z["mla_w_uq"][0], z["mla_w_ukv"][0], z["mla_w_out"][0], z["mla_q_norm_g"][0], z["mla_kv_norm_g"][0], h)
            else:
                perm = list(range(8 * h, 8 * h + 8)) + list(range(8 * (1 - h), 8 * (1 - h) + 8))
                w = dict(wr=f(z["router_w"][li][:, perm]), wg=f(z["moe_w_gate"][li][8 * h:8 * h + 8].reshape(8 * D, D)),
                         wu=f(z["moe_w_up"][li][8 * h:8 * h + 8].reshape(8 * D, D)), wd=f(z["moe_w_down"][li][8 * h:8 * h + 8].reshape(8 * D, D)))
            d.update({pre + k: v for k, v in w.items()})
    return d


def kernel_fused_dup(**z):
    z = {k: np.asarray(v) for k, v in z.items()}
    nc = _stage_fused()
    per_sample = [fused_inputs(z, b) for b in range(4)]
    ins = [per_sample[k // 2] for k in range(8)]
    r = run_bass_kernel_spmd(nc, ins, core_ids=list(range(8))).results
    return np.ascontiguousarray(np.stack([r[2 * b]["out"] for b in range(4)]).astype(np.float32))


def _stage_fused():
    if "fused" not in _STAGES:
        _STAGES["fused"] = build_fused()
    return _STAGES["fused"]


CH_ROWS = 256
NCHUNK = NTOK // CH_ROWS


def build_fused_pair():
    nc = new_nc()
    es = ExitStack()
    with es:
        c = Ctx(nc, es); p = c.p
        K = common_consts(c, c.din("ident", [128, 128]))
        xin = c.din("xin", [NTOK, D]); cvec = c.din("cvec", [2, D])
        ada_w = c.din("ada_w", [4 * D, 6 * D]); ada_b = c.din("ada_b", [4, 6 * D]); gains = c.din("gains", [9, D])
        out = c.dout("out", [4096, D])
        vecs, bv = emit_ada(c, K, cvec, ada_w, Buf(), ada_b)
        Xc = dram(c, [NTOK, D]); bXc = [Buf() for _ in range(NT)]
        for t in range(NT):
            p.dma("sp", Xc[t * 128:(t + 1) * 128, :], xin[t * 128:(t + 1) * 128, :], writes=[bXc[t]])
        prev = None

        def make_vec(li):
            vs = dram(c, [NVEC, D]); bvs = Buf()
            rows = [vecs[li, 0:1, j * D:(j + 1) * D] for j in range(6)] + [vecs[li, 1:2, j * D:(j + 1) * D] for j in range(6)]
            rows += [gains[li:li + 1, :], gains[4 + li:5 + li, :], gains[8:9, :]]
            if prev is not None:
                pli, gi = prev[4], prev[5]
                rows += [vecs[pli, 0:1, gi * D:(gi + 1) * D], vecs[pli, 1:2, gi * D:(gi + 1) * D]]
            else:
                rows += [gains[8:9, :], gains[8:9, :]]
            for r_, src in enumerate(rows):
                p.dma("sp", vs[r_:r_ + 1, :], src, reads=[bv], writes=[bvs])
            return vs, bvs

        def fold(li):
            nonlocal Xc, bXc
            vs, bvs = make_vec(li)
            c.vec_reads = [bvs]
            if prev is not None:
                G, bG, GSS, bGSS = prev[:4]
                Xn = dram(c, [NTOK, D]); bXn = [Buf() for _ in range(NT)]
                c.res_reads = list(bXc) + list(bG) + ([bGSS] if GSS is not None else [])

                def tile_aps(t):
                    j, off = divmod(t * 128, CH_ROWS)
                    return G[j][off:off + 128, :], G[j][CH_ROWS + off:CH_ROWS + off + 128, :]
                c.res_tile_aps = tile_aps
                ssa = GSS[0:NTOK, :] if GSS is not None else None
                ssb = GSS[NTOK:2 * NTOK, :] if GSS is not None else None
                emit_residual_in(c, K, Xc, None, None, vs, Xn, bXn, None, ssa, ssb)
                c.res_reads = []
                c.res_tile_aps = None
                Xc, bXc = Xn, bXn
            return vs

        for kind, li in FUSED_PLAN:
            vs = fold(li)
            w = decl_weights(c, kind, f"L{li}_")
            P = dram(c, [NTOK, D]); bP = [Buf() for _ in range(NT)]
            SS = bSS = None
            es1 = ExitStack()
            with es1:
                if kind == "moe":
                    mods = mods_from_vec(c, es1, vs, 3, 13)
                    emit_moe(c, K, Xc, bXc, mods, w["wr"], w["wg"], w["wu"], w["wd"], Buf(), P, bP)
                else:
                    mods = mods_from_vec(c, es1, vs, 0, 12)
                    if kind == "ssd":
                        SS = dram(c, [NTOK, 1]); bSS = [Buf() for _ in range(NT)]
                        emit_ssd(c, K, Xc, bXc, mods, P=P, bP=bP, SS=SS, bSS=bSS, **w)
                    elif kind == "mla":
                        emit_mla(c, K, Xc, bXc, mods, P=P, bP=bP, **w)
                    else:
                        emit_gdn(c, K, Xc, bXc, mods, P=P, bP=bP, **w)
                p.barrier()
            G, bG = [], []
            for j in range(NCHUNK):
                src = dram(c, [CH_ROWS, D]); bs = Buf()
                p.dma("sp", src, P[j * CH_ROWS:(j + 1) * CH_ROWS, :], reads=[bP[2 * j], bP[2 * j + 1]], writes=[bs])
                g = dram(c, [2 * CH_ROWS, D]); bg = Buf()
                allgather(c, src, g, GRP_PAIR, [bs], [bg])
                G.append(g); bG.append(bg)
            GSS = bGSS = None
            if SS is not None:
                GSS = dram(c, [2 * NTOK, 1]); bGSS = Buf()
                allgather(c, SS, GSS, GRP_PAIR, list(bSS), [bGSS])
            prev = (G, bG, GSS, bGSS, li, 5 if kind == "moe" else 2)
        fold(3)
        es1 = ExitStack()
        bo = []
        with es1:
            fg = bc_tile(c, es1, gains[8:9, :])
            xt = [c.sb([128, D], es1) for _ in range(2)]; bxt = [Buf(), Buf()]
            ot = [c.sb([128, D], es1) for _ in range(2)]; bot = [Buf(), Buf()]
            for t in range(32):
                i = t % 2
                p.dma("sp", xt[i][:], Xc[t * 128:(t + 1) * 128, :], reads=[bXc[t]], writes=[bxt[i]])
                norm_mod_T(c, K, xt[i][:], bxt[i], fg, None, None, None)
                p.op("act", lambda e: e.copy(ot[i][:], K["h"][:]), reads=[K["bh"]], writes=[bot[i]])
                b = Buf(); bo.append(b)
                p.dma("pool", out[t * 128:(t + 1) * 128, :], ot[i][:], reads=[bot[i]], writes=[b])
        p.finish(bo)
        print("fused-pair ninst", p.ninst, "nsem", p.nsem)
    return nc


def fused_pair_inputs(z, b, h):
    f = np.ascontiguousarray
    d = dict(ident=np.eye(128, dtype=np.float32), xin=f(np.concatenate([z["x"][b], z["ctx"][b]], 0)),
             cvec=f(np.stack([z["c"][b], z["c_ctx"]]).astype(np.float32)), ada_w=f(z["ada_w"].reshape(4 * D, 6 * D)), ada_b=f(z["ada_b"]),
             gains=f(np.concatenate([z["norm1_g"], z["norm2_g"], z["final_norm_g"][None]], 0).astype(np.float32)))
    for kind, li in FUSED_PLAN:
        pre = f"L{li}_"
        if kind == "gdn":
            w = gdn_weights(z, li // 3, h)
        elif kind == "ssd":
            w = ssd_weights(z, h)
        elif kind == "mla":
            w = mla_weights(z["mla_w_in"][0], z["mla_w_uq"][0], z["mla_w_ukv"][0], z["mla_w_out"][0], z["mla_q_norm_g"][0], z["mla_kv_norm_g"][0], h)
        else:
            perm = list(range(8 * h, 8 * h + 8)) + list(range(8 * (1 - h), 8 * (1 - h) + 8))
            w = dict(wr=f(z["router_w"][li][:, perm]), wg=f(z["moe_w_gate"][li][8 * h:8 * h + 8].reshape(8 * D, D)),
                     wu=f(z["moe_w_up"][li][8 * h:8 * h + 8].reshape(8 * D, D)), wd=f(z["moe_w_down"][li][8 * h:8 * h + 8].reshape(8 * D, D)))
        d.update({pre + k: v for k, v in w.items()})
    return d


def kernel(**z):
    z = {k: np.asarray(v) for k, v in z.items()}
    if "fused_pair" not in _STAGES:
        _STAGES["fused_pair"] = build_fused_pair()
    ins = [fused_pair_inputs(z, k // 2, k % 2) for k in range(8)]
    r = run_bass_kernel_spmd(_STAGES["fused_pair"], ins, core_ids=list(range(8))).results
    return np.ascontiguousarray(np.stack([r[2 * b]["out"] for b in range(4)]).astype(np.float32))
```

```python
import numpy as np
from contextlib import ExitStack
import concourse.bass as bass
import concourse.mybir as mybir
from concourse.bass_utils import run_bass_kernel_spmd

F32 = mybir.dt.float32
AF = mybir.ActivationFunctionType
ALU = mybir.AluOpType
AX = mybir.AxisListType

SEM_EPOCH = 20000
N_DMA_RING = 8
D = 1024
NE = 16


class Buf:
    __slots__ = ("name", "w", "r", "excl")

    def __init__(self, name=""):
        self.name = name
        self.w = None
        self.r = {}
        self.excl = False


def PBuf():
    b = Buf()
    b.excl = True
    return b


class Prog:
    def __init__(self, nc, es):
        self.nc = nc
        self.es = es
        self.eng = {"pe": nc.tensor, "act": nc.scalar, "dve": nc.vector, "pool": nc.gpsimd, "sp": nc.sync}
        self.sem = {}
        self.cnt = {}
        self.semown = {}
        self.seen = {e: {} for e in self.eng}
        self.nsem = 0
        self.ninst = 0
        self.allsems = []
        for e in self.eng:
            self._new_sem(e)
        self.dring = {}
        self.dpos = {}

    def _mk(self, tag):
        self.nsem += 1
        return self.es.enter_context(self.nc.semaphore(f"{tag}{self.nsem}"))

    def _new_sem(self, e):
        s = self._mk("s" + e)
        self.sem[e] = s
        self.cnt[e] = 0
        self.semown[id(s)] = e

    def _wait(self, e, tok):
        s, v = tok
        if e == "pe" and self.semown.get(id(s)) == "pe":
            return
        if self.seen[e].get(id(s), 0) >= v:
            return
        self.eng[e].wait_ge(s, v)
        self.ninst += 1
        self.seen[e][id(s)] = v

    def _deps(self, e, reads, writes):
        for b in reads:
            if b.w is not None:
                self._wait(e, b.w)
            if b.excl:
                for tok in list(b.r.values()):
                    if self.semown.get(id(tok[0])) != e:
                        self._wait(e, tok)
        for b in writes:
            if b.w is not None:
                self._wait(e, b.w)
            for tok in list(b.r.values()):
                self._wait(e, tok)

    def _mark(self, tok, reads, writes):
        s, v = tok
        for b in reads:
            b.r[id(s)] = tok
        for b in writes:
            b.w = tok
            b.r = {}

    def op(self, e, fn, reads=(), writes=()):
        self._deps(e, reads, writes)
        ins = fn(self.eng[e])
        if self.cnt[e] >= SEM_EPOCH:
            self._new_sem(e)
        self.cnt[e] += 1
        ins.then_inc(self.sem[e], 1)
        self.ninst += 1
        self._mark((self.sem[e], self.cnt[e]), reads, writes)
        return ins

    def dma(self, q, out, in_, reads=(), writes=(), **kw):
        self._deps(q, reads, writes)
        if q not in self.dring:
            self.dring[q] = [[self._mk("d" + q), 0] for _ in range(N_DMA_RING)]
            self.dpos[q] = 0
        slot = self.dring[q][self.dpos[q] % N_DMA_RING]
        self.dpos[q] += 1
        if slot[1] > 0:
            self._wait(q, (slot[0], slot[1]))
        slot[1] += 16
        self.eng[q].dma_start(out=out, in_=in_, **kw).then_inc(slot[0], 16)
        self.ninst += 1
        self._mark((slot[0], slot[1]), reads, writes)

    def barrier(self):
        toks = [(self.sem[f], self.cnt[f]) for f in self.eng if self.cnt[f] > 0]
        for q in self.dring:
            toks += [(s, v) for s, v in self.dring[q] if v > 0]
        for e in self.eng:
            for t in toks:
                if self.semown.get(id(t[0])) == e:
                    continue
                self._wait(e, t)

    def finish(self, bufs, e="sp"):
        for b in bufs:
            if b.w is not None:
                self._wait(e, b.w)


class Ctx:
    def __init__(self, nc, es):
        self.nc = nc
        self.es = es
        self.p = Prog(nc, es)
        self.n = 0

    def sb(self, shape, es=None, dt=F32):
        self.n += 1
        return (es or self.es).enter_context(self.nc.sbuf_tensor(f"sb{self.n}", list(shape), dt))

    def ps(self, shape, es=None):
        self.n += 1
        return (es or self.es).enter_context(self.nc.psum_tensor(f"ps{self.n}", list(shape), F32))

    def din(self, name, shape):
        return self.nc.dram_tensor(name, list(shape), F32, kind="ExternalInput").ap()

    def dout(self, name, shape):
        return self.nc.dram_tensor(name, list(shape), F32, kind="ExternalOutput").ap()


def load_bc(c, q, dst, vec_row, buf):
    c.p.dma(q, dst, vec_row.partition_broadcast(128), writes=[buf])


def norm_mod_T(c, K, xt, xb, G, S, hT_dst, hb):
    p = c.p
    p.op("act", lambda e: e.activation(K["junk"][:], xt, AF.Square, accum_out=K["ss"][:, 0:1]),
         reads=[xb], writes=[K["bjunk"], K["bss"]])
    p.op("act", lambda e: e.activation(K["ss"][:, 1:2], K["ss"][:, 0:1], AF.Sqrt, bias=K["eps"][:, 0:1], scale=1.0 / D),
         reads=[K["bss"]], writes=[K["bss"]])
    p.op("dve", lambda e: e.reciprocal(K["ss"][:, 2:3], K["ss"][:, 1:2]), reads=[K["bss"]], writes=[K["bss"]])
    if G is not None:
        p.op("dve", lambda e: e.scalar_tensor_tensor(K["h"][:], xt, K["ss"][:, 2:3], G[0][:], ALU.mult, ALU.mult),
             reads=[xb, K["bss"], G[1]], writes=[K["bh"]])
    else:
        p.op("dve", lambda e: e.tensor_scalar(K["h"][:], xt, K["ss"][:, 2:3], None, ALU.mult),
             reads=[xb, K["bss"]], writes=[K["bh"]])
    if S is not None:
        p.op("pool", lambda e: e.tensor_add(K["h"][:], K["h"][:], S[0][:]), reads=[K["bh"], S[1]], writes=[K["bh"]])
    if hT_dst is None:
        return
    for half in range(2):
        for cc in range(4):
            ch = half * 4 + cc
            p.op("pe", lambda e: e.transpose(K["ptr"][:, cc * 128:(cc + 1) * 128], K["h"][:, ch * 128:(ch + 1) * 128], K["ident"][:]),
                 reads=[K["bh"], K["bident"]], writes=[K["bptr"]])
        p.op("act", lambda e: e.copy(hT_dst[:, half * 4:(half + 1) * 4, :], K["ptr"][:].rearrange("p (c t) -> p c t", c=4)),
             reads=[K["bptr"]], writes=[hb])


def common_consts(c, ident_d):
    K = {}
    K["ident"] = c.sb([128, 128]); K["bident"] = Buf()
    c.p.dma("pool", K["ident"][:], ident_d, writes=[K["bident"]])
    K["junk"] = c.sb([128, D]); K["bjunk"] = Buf()
    K["h"] = c.sb([128, D]); K["bh"] = Buf()
    K["ss"] = c.sb([128, 4]); K["bss"] = Buf()
    K["eps"] = c.sb([128, 1]); K["beps"] = Buf()
    c.p.op("dve", lambda e: e.memset(K["eps"][:], 1e-6), writes=[K["beps"], K["bss"]])
    K["ptr"] = c.ps([128, 512]); K["bptr"] = PBuf()
    return K


GRP_PAIR = [[0, 1], [2, 3], [4, 5], [6, 7]]
GRP_QUAD = [[0, 2, 4, 6], [1, 3, 5, 7]]
GRP_ALL = [list(range(8))]
NTOK = 4352
NT = 34


def new_nc():
    return bass.Bass("TRN2", target_bir_lowering=False, num_devices=8)


def allgather(c, src, dst, groups, reads, writes):
    p = c.p
    p._deps("pool", reads, writes)
    if not hasattr(c, "_cc_sem"):
        c._cc_sem = p._mk("cc")
        c._cc_cnt = 0
    if c._cc_cnt > 0:
        p._wait("pool", (c._cc_sem, c._cc_cnt))
    c._cc_cnt += 1
    c.nc.gpsimd.collective_compute("AllGather", ALU.bypass, replica_groups=groups, ins=[src.opt()], outs=[dst.opt()]).then_inc(c._cc_sem)
    p.ninst += 1
    p._mark((c._cc_sem, c._cc_cnt), reads, writes)


def dram(c, shape):
    c.n += 1
    return c.nc.dram_tensor(f"dr{c.n}", list(shape), F32).ap()


def gather_w(c, name, rows, cols, groups=GRP_QUAD):
    g = len(groups[0])
    ext = c.din(name, [rows // g, cols])
    src = dram(c, [rows // g, cols])
    full = dram(c, [rows, cols])
    bs, bf = Buf(), Buf()
    c.p.dma("sp", src, ext, writes=[bs])
    allgather(c, src, full, groups, [bs], [bf])
    return full, bf


def emit_ada(c, K, cvec, ada_w_full, baw, ada_b):
    p = c.p
    vecs = dram(c, [4, 2, 6 * D])
    bv = Buf()
    es = ExitStack()
    with es:
        sc = c.sb([128, 8, 2], es); bsc = Buf()
        craw = c.sb([16, 128], es); bcraw = Buf()
        p.dma("pool", craw[:], cvec.rearrange("k (c p) -> (k c) p", p=128), writes=[bcraw])
        p.op("pe", lambda e: e.transpose(K["ptr"][:, 0:16], craw[:], K["ident"][0:16, 0:16]), reads=[bcraw, K["bident"]], writes=[K["bptr"]])
        p.op("act", lambda e: e.activation(sc[:].rearrange("p c k -> p k c"), K["ptr"][:, 0:16].rearrange("p (k c) -> p k c", k=2), AF.Silu),
             reads=[K["bptr"]], writes=[bsc])
        ab = c.sb([2, 6 * D], es); bab = Buf()
        orow = c.sb([2, 6 * D], es); borow = Buf()
        wt = [c.sb([128, 8, 512], es) for _ in range(2)]; bwt = [Buf(), Buf()]
        pa = [c.ps([128, 512], es) for _ in range(2)]; bpa = [PBuf(), PBuf()]
        n = 0
        for i in range(4):
            for k in range(2):
                p.dma("pool", ab[k:k + 1, :], ada_b[i:i + 1, :], writes=[bab])
            for g in range(12):
                j = n % 2
                n += 1
                p.dma("sp", wt[j][:], ada_w_full[i * D:(i + 1) * D, g * 512:(g + 1) * 512].rearrange("(c p) f -> p c f", p=128),
                      reads=[baw], writes=[bwt[j]])
                for ch in range(8):
                    p.op("pe", lambda e: e.matmul(pa[j][0:2, :], sc[:, ch, :], wt[j][:, ch, :], start=(ch == 0), stop=(ch == 7)),
                         reads=[bsc, bwt[j]], writes=[bpa[j]])
                p.op("dve", lambda e: e.tensor_add(orow[:, g * 512:(g + 1) * 512], pa[j][0:2, :], ab[:, g * 512:(g + 1) * 512]),
                     reads=[bpa[j], bab], writes=[borow])
            p.dma("pool", vecs[i], orow[:], reads=[borow], writes=[bv])
        p.barrier()
    return vecs, bv


def load_mods(c, es, vecs, bv, i, base, norm_g_row):
    p = c.p
    g2 = c.sb([128, D], es); bg2 = Buf()
    load_bc(c, "pool", g2[:], norm_g_row, bg2)
    mods = {}
    for k, kind in enumerate("xc"):
        t = []
        for j in range(3):
            tt = c.sb([128, D], es); bt = Buf()
            p.dma("pool", tt[:], vecs[i, k:k + 1, (base + j) * D:(base + j + 1) * D].partition_broadcast(128), reads=[bv], writes=[bt])
            t.append((tt, bt))
        Sh, Sc, Ga = t
        p.op("dve", lambda e: e.scalar_tensor_tensor(Sc[0][:], Sc[0][:], 1.0, g2[:], ALU.add, ALU.mult),
             reads=[bg2, Sc[1]], writes=[Sc[1]])
        mods[kind] = (Sc, Sh, Ga)
    return mods


def tile_kind(t):
    return "x" if t < 32 else "c"


def emit_residual(c, K, X, bX, P, bP, mods):
    p = c.p
    G = dram(c, [2 * NTOK, D]); bG = Buf()
    allgather(c, P, G, GRP_PAIR, bP, [bG])
    es = ExitStack()
    with es:
        xt = [c.sb([128, D], es) for _ in range(2)]; bxt = [Buf(), Buf()]
        ga = [c.sb([128, D], es) for _ in range(2)]; bga = [Buf(), Buf()]
        gb = [c.sb([128, D], es) for _ in range(2)]; bgb = [Buf(), Buf()]
        for t in range(NT):
            i = t % 2
            Ga = mods[tile_kind(t)][2]
            p.dma("sp", xt[i][:], X[t * 128:(t + 1) * 128, :], reads=[bX[t]], writes=[bxt[i]])
            p.dma("sp", ga[i][:], G[t * 128:(t + 1) * 128, :], reads=[bG], writes=[bga[i]])
            p.dma("sp", gb[i][:], G[NTOK + t * 128:NTOK + (t + 1) * 128, :], reads=[bG], writes=[bgb[i]])
            p.op("pool", lambda e: e.tensor_add(ga[i][:], ga[i][:], gb[i][:]), reads=[bga[i], bgb[i]], writes=[bga[i]])
            if ssa is not None:
                p.dma("sp", sst[:, 0:1], ssa[sl, :], reads=rr, writes=[bsst])
                p.dma("sp", sst[:, 1:2], ssb[sl, :], reads=rr, writes=[bsst])
                p.op("dve", lambda e: e.tensor_add(sst[:, 2:3], sst[:, 0:1], sst[:, 1:2]), reads=[bsst], writes=[bsst])
                p.op("act", lambda e: e.activation(sst[:, 3:4], sst[:, 2:3], AF.Sqrt, bias=K["eps"][:, 0:1], scale=1.0 / 2048), reads=[bsst], writes=[bsst])
                p.op("dve", lambda e: e.reciprocal(sst[:, 4:5], sst[:, 3:4]), reads=[bsst], writes=[bsst])
                p.op("dve", lambda e: e.scalar_tensor_tensor(ga[i][:], ga[i][:], sst[:, 4:5], Ga[0][:], ALU.mult, ALU.mult),
                     reads=[bga[i], Ga[1], bsst], writes=[bga[i]])
            else:
                p.op("dve", lambda e: e.tensor_mul(ga[i][:], ga[i][:], Ga[0][:]), reads=[bga[i], Ga[1]], writes=[bga[i]])
            p.op("dve", lambda e: e.tensor_add(xt[i][:], xt[i][:], ga[i][:]), reads=[bga[i], bxt[i]], writes=[bxt[i]])
            p.dma("pool", X[t * 128:(t + 1) * 128, :], xt[i][:], reads=[bxt[i]], writes=[bX[t]])
        p.barrier()


CAP_X = 512
CAP_C = 32
N_BISECT = 36
NEH = 8


def emit_moe(c, K, X, bX, mods, wr, wg, wu, wd, bw, P, bP, skip_ctx=False):
    p = c.p
    es0 = ExitStack()
    with es0:
        wr_sb = c.sb([128, 8, NE], es0); bwr = Buf()
        p.dma("pool", wr_sb[:], wr.rearrange("(c p) e -> p c e", p=128), writes=[bwr])
        ones16 = c.sb([16, 128], es0); bones = Buf()
        p.op("dve", lambda e: e.memset(ones16[:], 1.0), writes=[bones])
        xt = [c.sb([128, D], es0) for _ in range(2)]
        bxt = [Buf() for _ in range(2)]
        pmisc = c.ps([128, 512], es0); bpm = PBuf()
        sm = c.sb([128, 8], es0); bsm = Buf()
        ex = c.sb([128, NE], es0); bex = Buf()
        thr_bc = {k: c.sb([128, NE], es0) for k in "xc"}
        bthr = {k: Buf() for k in "xc"}

        def router(hT_src, hb, aff_dst, baff):
            for ch in range(8):
                p.op("pe", lambda e: e.matmul(pmisc[:, 0:NE], hT_src[:, ch, :], wr_sb[:, ch, :], start=(ch == 0), stop=(ch == 7)),
                     reads=[hb, bwr], writes=[bpm])
            p.op("dve", lambda e: e.reduce_max(sm[:, 0:1], pmisc[:, 0:NE], AX.X), reads=[bpm], writes=[bsm])
            p.op("dve", lambda e: e.tensor_scalar(sm[:, 1:2], sm[:, 0:1], -1.0, None, ALU.mult), reads=[bsm], writes=[bsm])
            p.op("act", lambda e: e.activation(ex[:], pmisc[:, 0:NE], AF.Exp, bias=sm[:, 1:2], accum_out=sm[:, 2:3]),
                 reads=[bpm, bsm], writes=[bex, bsm])
            p.op("dve", lambda e: e.reciprocal(sm[:, 3:4], sm[:, 2:3]), reads=[bsm], writes=[bsm])
            p.op("dve", lambda e: e.tensor_scalar(aff_dst, ex[:], sm[:, 3:4], None, ALU.mult), reads=[bex, bsm], writes=[baff])

        es1 = ExitStack()
        with es1:
            affT = c.sb([16, NT * 128], es1); baffT = Buf()
            mask = c.sb([16, 32 * 128], es1); bmask = Buf()
            hT1 = c.sb([128, 8, 128], es1); bhT1 = Buf()
            aff1 = c.sb([128, NE], es1); baff1 = Buf()
            bs = c.sb([16, 8], es1); bbs = Buf()
            diag = c.sb([16, 16], es1); bdiag = Buf()
            for t in range(NT):
                G, S, _ = mods[tile_kind(t)]
                i = t % 2
                p.dma("sp", xt[i][:], X[t * 128:(t + 1) * 128, :], reads=[bX[t]], writes=[bxt[i]])
                norm_mod_T(c, K, xt[i][:], bxt[i], G, S, hT1, bhT1)
                router(hT1, bhT1, aff1[:], baff1)
                p.op("pe", lambda e: e.transpose(pmisc[0:16, 128:256], aff1[:], K["ident"][:]),
                     reads=[baff1, K["bident"]], writes=[bpm])
                p.op("act", lambda e: e.copy(affT[:, t * 128:(t + 1) * 128], pmisc[0:16, 128:256]), reads=[bpm], writes=[baffT])
            for kind, lo_c, n_c, cap in (("x", 0, 32 * 128, CAP_X), ("c", 32 * 128, 2 * 128, CAP_C)):
                a = affT[:, lo_c:lo_c + n_c]
                m = mask[:, 0:n_c]
                lo, hi, mid, cnt, ge, d1, hh = (bs[:, j:j + 1] for j in range(7))
                p.op("dve", lambda e: e.memset(lo, 0.0), writes=[bbs])
                p.op("dve", lambda e: e.memset(hi, 1.0), writes=[bbs])
                for it in range(N_BISECT):
                    p.op("dve", lambda e: e.tensor_scalar(hh, hi, 0.5, None, ALU.mult), reads=[bbs], writes=[bbs])
                    p.op("dve", lambda e: e.scalar_tensor_tensor(mid, lo, 0.5, hh, ALU.mult, ALU.add), reads=[bbs], writes=[bbs])
                    p.op("dve", lambda e: e.tensor_scalar(m, a, mid, None, ALU.is_ge, ALU.add, accum_out=cnt),
                         reads=[baffT, bbs], writes=[bmask, bbs])
                    p.op("dve", lambda e: e.tensor_scalar(ge, cnt, cap - 0.5, None, ALU.is_ge), reads=[bbs], writes=[bbs])
                    p.op("dve", lambda e: e.tensor_sub(d1, mid, lo), reads=[bbs], writes=[bbs])
                    p.op("dve", lambda e: e.scalar_tensor_tensor(lo, d1, ge, lo, ALU.mult, ALU.add), reads=[bbs], writes=[bbs])
                    p.op("dve", lambda e: e.tensor_sub(d1, hi, mid), reads=[bbs], writes=[bbs])
                    p.op("dve", lambda e: e.scalar_tensor_tensor(hi, d1, ge, mid, ALU.mult, ALU.add), reads=[bbs], writes=[bbs])
                p.op("dve", lambda e: e.tensor_scalar(diag[:], K["ident"][0:16, 0:16], lo, None, ALU.mult),
                     reads=[bbs, K["bident"]], writes=[bdiag])
                p.op("pe", lambda e: e.matmul(pmisc[:, 256:256 + NE], ones16[:], diag[:], start=True, stop=True),
                     reads=[bones, bdiag], writes=[bpm])
                p.op("act", lambda e: e.copy(thr_bc[kind][:], pmisc[:, 256:256 + NE]), reads=[bpm], writes=[bthr[kind]])
            p.barrier()

        wg_sb = c.sb([128, 8, D], es0); bwg = Buf()
        wu_sb = c.sb([128, 8, D], es0); bwu = Buf()
        wd_sb = c.sb([128, 8, D], es0); bwd = Buf()
        hT = c.sb([128, 8, 512], es0); bhT = [Buf() for _ in range(4)]
        hid = c.sb([128, 8, 512], es0); bhid = [Buf() for _ in range(8)]
        acc = [c.sb([128, D], es0) for _ in range(4)]; bacc = [Buf() for _ in range(4)]
        gate = c.sb([128, 4, NE], es0); bgate = [Buf() for _ in range(4)]
        aff2 = c.sb([128, NE], es0); baff2 = Buf()
        msk2 = c.sb([128, NE], es0); bmsk2 = Buf()
        sg = [c.sb([128, 512], es0) for _ in range(2)]; bsg = [Buf() for _ in range(2)]
        pg = [c.ps([128, 512], es0) for _ in range(2)]; bpg = [PBuf() for _ in range(2)]
        pu = [c.ps([128, 512], es0) for _ in range(2)]; bpu = [PBuf() for _ in range(2)]
        py = [c.ps([128, 512], es0) for _ in range(2)]; bpy = [PBuf() for _ in range(2)]
        passes = [list(range(4 * q, 4 * q + 4)) for q in range(8)] + [[32, 33]]
        for tiles in passes:
            nt = len(tiles)
            N = nt * 128
            if skip_ctx and tiles[0] >= 32:
                for k, t in enumerate(tiles):
                    p.op("dve", lambda e: e.memset(acc[k][:], 0.0), reads=[bacc[k]], writes=[bacc[k]])
                    p.dma("pool", P[t * 128:(t + 1) * 128, :], acc[k][:], reads=[bacc[k]], writes=[bP[t]])
                continue
            for k, t in enumerate(tiles):
                kind = tile_kind(t)
                G, S, _ = mods[kind]
                i = k % 2
                p.dma("pool", xt[i][:], X[t * 128:(t + 1) * 128, :], reads=[bX[t]], writes=[bxt[i]])
                norm_mod_T(c, K, xt[i][:], bxt[i], G, S, hT[:, :, k * 128:(k + 1) * 128], bhT[k])
                router(hT[:, :, k * 128:(k + 1) * 128], bhT[k], aff2[:], baff2)
                p.op("dve", lambda e: e.tensor_tensor(msk2[:], aff2[:], thr_bc[kind][:], ALU.is_ge),
                     reads=[baff2, bthr[kind]], writes=[bmsk2])
                p.op("dve", lambda e: e.tensor_mul(gate[:, k, :], msk2[:], aff2[:]), reads=[bmsk2, baff2], writes=[bgate[k]])
            for ex_i in range(NEH):
                p.dma("sp", wg_sb[:], wg[ex_i * D:(ex_i + 1) * D, :].rearrange("(c p) f -> p c f", p=128), reads=[bw], writes=[bwg])
                p.dma("pool", wu_sb[:], wu[ex_i * D:(ex_i + 1) * D, :].rearrange("(c p) f -> p c f", p=128), reads=[bw], writes=[bwu])
                p.dma("sp", wd_sb[:], wd[ex_i * D:(ex_i + 1) * D, :].rearrange("(c p) f -> p c f", p=128), reads=[bw], writes=[bwd])
                for fc in range(8):
                    j = fc % 2
                    for ch in range(8):
                        p.op("pe", lambda e: e.matmul(pg[j][:, 0:N], wg_sb[:, ch, fc * 128:(fc + 1) * 128], hT[:, ch, 0:N],
                                                      start=(ch == 0), stop=(ch == 7)),
                             reads=[bwg] + bhT[:nt], writes=[bpg[j]])
                    for ch in range(8):
                        p.op("pe", lambda e: e.matmul(pu[j][:, 0:N], wu_sb[:, ch, fc * 128:(fc + 1) * 128], hT[:, ch, 0:N],
                                                      start=(ch == 0), stop=(ch == 7)),
                             reads=[bwu] + bhT[:nt], writes=[bpu[j]])
                    p.op("act", lambda e: e.activation(sg[j][:, 0:N], pg[j][:, 0:N], AF.Silu), reads=[bpg[j]], writes=[bsg[j]])
                    p.op("dve", lambda e: e.tensor_mul(hid[:, fc, 0:N], sg[j][:, 0:N], pu[j][:, 0:N]),
                         reads=[bsg[j], bpu[j]], writes=[bhid[fc]])
                for k in range(nt):
                    for half in range(2):
                        for fc in range(8):
                            p.op("pe", lambda e: e.matmul(py[half][:], hid[:, fc, k * 128:(k + 1) * 128],
                                                          wd_sb[:, fc, half * 512:(half + 1) * 512],
                                                          start=(fc == 0), stop=(fc == 7)),
                                 reads=[bwd] + bhid, writes=[bpy[half]])
                        a_ap = acc[k][:, half * 512:(half + 1) * 512]
                        if ex_i == 0:
                            p.op("dve", lambda e: e.tensor_scalar(a_ap, py[half][:], gate[:, k, ex_i:ex_i + 1], None, ALU.mult),
                                 reads=[bpy[half], bgate[k]], writes=[bacc[k]])
                        else:
                            p.op("dve", lambda e: e.scalar_tensor_tensor(a_ap, py[half][:], gate[:, k, ex_i:ex_i + 1], a_ap, ALU.mult, ALU.add),
                                 reads=[bpy[half], bgate[k], bacc[k]], writes=[bacc[k]])
            for k, t in enumerate(tiles):
                p.dma("pool", P[t * 128:(t + 1) * 128, :], acc[k][:], reads=[bacc[k]], writes=[bP[t]])
        p.barrier()


def declare_moe_weights(c, li):
    wr = c.din(f"wr{li}", [D, NE])
    wg, b1 = gather_w(c, f"wg{li}", NEH * D, D)
    wu, b2 = gather_w(c, f"wu{li}", NEH * D, D)
    wd, b3 = gather_w(c, f"wd{li}", NEH * D, D)
    bw = Buf()
    for b in (b1, b2, b3):
        c.p._wait("sp", b.w)
    c.p.op("pool", lambda e: e.memset(c_dummy(c)[:], 0.0), reads=[b1, b2, b3], writes=[bw])
    return wr, wg, wu, wd, bw


def c_dummy(c):
    if not hasattr(c, "_dummy"):
        c._dummy = c.sb([128, 1])
    return c._dummy


NVEC = 17


def bc_tile(c, es, row, q="pool"):
    t = c.sb([128, D], es); b = Buf()
    c.p.dma(q, t[:], row.partition_broadcast(128), reads=list(getattr(c, "vec_reads", [])), writes=[b])
    return (t, b)


def mods_from_vec(c, es, vec, base, grow):
    p = c.p
    g = bc_tile(c, es, vec[grow:grow + 1, :])
    mods = {}
    for k, kind in enumerate("xc"):
        Sh = bc_tile(c, es, vec[6 * k + base:6 * k + base + 1, :])
        Sc = bc_tile(c, es, vec[6 * k + base + 1:6 * k + base + 2, :])
        p.op("dve", lambda e: e.scalar_tensor_tensor(Sc[0][:], Sc[0][:], 1.0, g[0][:], ALU.add, ALU.mult),
             reads=[g[1], Sc[1]], writes=[Sc[1]])
        mods[kind] = (Sc, Sh, None)
    return mods


def emit_residual_in(c, K, xprev, pa, pb, vec, X, bX, xo, ssa=None, ssb=None):
    p = c.p
    es = ExitStack()
    bo = []
    with es:
        gts = {"x": bc_tile(c, es, vec[15:16, :]), "c": bc_tile(c, es, vec[16:17, :])}
        xt = [c.sb([128, D], es) for _ in range(2)]; bxt = [Buf(), Buf()]
        ga = [c.sb([128, D], es) for _ in range(2)]; bga = [Buf(), Buf()]
        gb = [c.sb([128, D], es) for _ in range(2)]; bgb = [Buf(), Buf()]
        sst = c.sb([128, 8], es); bsst = Buf()
        for t in range(NT):
            i = t % 2
            Ga = gts[tile_kind(t)]
            sl = slice(t * 128, (t + 1) * 128)
            rr = list(getattr(c, "res_reads", []))
            p.dma("sp", xt[i][:], xprev[sl, :], reads=rr, writes=[bxt[i]])
            tp = getattr(c, "res_tile_aps", None)
            pa_t, pb_t = tp(t) if tp is not None else (pa[sl, :], pb[sl, :])
            p.dma("sp", ga[i][:], pa_t, reads=rr, writes=[bga[i]])
            p.dma("sp", gb[i][:], pb_t, reads=rr, writes=[bgb[i]])
            p.op("pool", lambda e: e.tensor_add(ga[i][:], ga[i][:], gb[i][:]), reads=[bga[i], bgb[i]], writes=[bga[i]])
            if ssa is not None:
                p.dma("sp", sst[:, 0:1], ssa[sl, :], reads=rr, writes=[bsst])
                p.dma("sp", sst[:, 1:2], ssb[sl, :], reads=rr, writes=[bsst])
                p.op("dve", lambda e: e.tensor_add(sst[:, 2:3], sst[:, 0:1], sst[:, 1:2]), reads=[bsst], writes=[bsst])
                p.op("act", lambda e: e.activation(sst[:, 3:4], sst[:, 2:3], AF.Sqrt, bias=K["eps"][:, 0:1], scale=1.0 / 2048), reads=[bsst], writes=[bsst])
                p.op("dve", lambda e: e.reciprocal(sst[:, 4:5], sst[:, 3:4]), reads=[bsst], writes=[bsst])
                p.op("dve", lambda e: e.scalar_tensor_tensor(ga[i][:], ga[i][:], sst[:, 4:5], Ga[0][:], ALU.mult, ALU.mult),
                     reads=[bga[i], Ga[1], bsst], writes=[bga[i]])
            else:
                p.op("dve", lambda e: e.tensor_mul(ga[i][:], ga[i][:], Ga[0][:]), reads=[bga[i], Ga[1]], writes=[bga[i]])
            p.op("dve", lambda e: e.tensor_add(xt[i][:], xt[i][:], ga[i][:]), reads=[bga[i], bxt[i]], writes=[bxt[i]])
            p.dma("pool", X[sl, :], xt[i][:], reads=[bxt[i]], writes=[bX[t]])
            if xo is not None:
                b = Buf(); bo.append(b)
                p.dma("pool", xo[sl, :], xt[i][:], reads=[bxt[i]], writes=[b])
        p.barrier()
    return bo


MLA_SCALE = 96.0 ** -0.5


def emit_mla(c, K, X, bX, mods, w_in, w_uqn, w_uqr, w_uqs, w_ukn, w_ukv, w_out, gq, gkv, cosT, sinT, cos_tm, sin_tm, P, bP):
    p = c.p
    nc = c.nc
    QnT = dram(c, [4, 128, NTOK]); KnT = dram(c, [4, 128, NTOK]); QrT = dram(c, [4, 64, NTOK])
    KrT2 = dram(c, [64, NTOK]); V = dram(c, [4, NT, 128, 130]); OT = dram(c, [4, 128, NTOK])
    bQ = [Buf() for _ in range(9)]; bKV = [Buf() for _ in range(9)]; bOT = [Buf() for _ in range(4)]
    groups = [list(range(4 * q, 4 * q + 4)) for q in range(8)] + [[32, 33]]
    esA = ExitStack()
    with esA:
        win = c.sb([128, 8, 1088], esA); bwin = Buf()
        p.dma("sp", win[:], w_in.rearrange("(c p) f -> p c f", p=128), writes=[bwin])
        wqn = c.sb([128, 6, 512], esA); wqr = c.sb([128, 6, 256], esA); wqs = c.sb([128, 6, 256], esA)
        wkn = c.sb([128, 2, 512], esA); wkv = c.sb([128, 2, 512], esA); bwq = Buf()
        for dst, src in ((wqn, w_uqn), (wqr, w_uqr), (wqs, w_uqs), (wkn, w_ukn), (wkv, w_ukv)):
            p.dma("sp", dst[:], src.rearrange("(c p) f -> p c f", p=128), writes=[bwq])
        gqt = c.sb([128, 768], esA); gkt = c.sb([128, 256], esA); bg = Buf()
        p.dma("pool", gqt[:], gq.partition_broadcast(128), writes=[bg])
        p.dma("pool", gkt[:], gkv.partition_broadcast(128), writes=[bg])
        cT = c.sb([64, 512], esA); sT = c.sb([64, 512], esA); bcs = Buf()
        xt = [c.sb([128, D], esA) for _ in range(2)]; bxt = [Buf(), Buf()]
        hT = c.sb([128, 8, 128], esA); bhT = Buf()
        pp = [c.ps([128, 512], esA) for _ in range(3)]; bpp = [PBuf() for _ in range(3)]
        pj = c.sb([128, 1088], esA); bpj = Buf()
        cqn = c.sb([128, 768], esA); ckn = c.sb([128, 256], esA); bcn = Buf()
        kr2 = c.sb([128, 64], esA); bkr2 = Buf()
        cst = c.sb([128, 32], esA); snt = c.sb([128, 32], esA); bcst = Buf()
        tmp32 = c.sb([128, 32], esA); btmp = Buf()
        cqT = c.sb([128, 6, 512], esA); bcqT = [Buf() for _ in range(4)]
        ckT = c.sb([128, 2, 512], esA); bckT = [Buf() for _ in range(4)]
        krT = c.sb([64, 512], esA); bkrT = [Buf() for _ in range(4)]
        vt = c.sb([128, 4, 130], esA); bvt = Buf()
        p.op("dve", lambda e: e.memset(vt[:], 1.0), writes=[bvt])
        outA = [c.sb([128, 512], esA) for _ in range(2)]; boutA = [Buf(), Buf()]
        outB = [c.sb([64, 512], esA) for _ in range(2)]; boutB = [Buf(), Buf()]
        tmpB = c.sb([64, 512], esA); btmpB = Buf()
        s4 = c.sb([128, 8], esA); bs4 = Buf()
        nA = 0
        for gi, tiles in enumerate(groups):
            N = len(tiles) * 128
            t0 = tiles[0] * 128
            for k, t in enumerate(tiles):
                kind = tile_kind(t)
                G, S, _ = mods[kind]
                i = t % 2
                ks = slice(k * 128, (k + 1) * 128)
                p.dma("pool", xt[i][:], X[t * 128:(t + 1) * 128, :], reads=[bX[t]], writes=[bxt[i]])
                norm_mod_T(c, K, xt[i][:], bxt[i], G, S, hT, bhT)
                for gidx, (n0, nn) in enumerate(((0, 512), (512, 512), (1024, 64))):
                    for ch in range(8):
                        p.op("pe", lambda e: e.matmul(pp[gidx][:, 0:nn], hT[:, ch, :], win[:, ch, n0:n0 + nn], start=(ch == 0), stop=(ch == 7)),
                             reads=[bhT, bwin], writes=[bpp[gidx]])
                    p.op("act" if gidx != 1 else "dve",
                         (lambda e: e.copy(pj[:, n0:n0 + nn], pp[gidx][:, 0:nn])) if gidx != 1 else (lambda e: e.tensor_copy(pj[:, n0:n0 + nn], pp[gidx][:, 0:nn])),
                         reads=[bpp[gidx]], writes=[bpj])
                for (lo_, n_, gt_, dst_, col) in ((0, 768, gqt, cqn, 0), (768, 256, gkt, ckn, 4)):
                    p.op("act", lambda e: e.activation(K["junk"][:, 0:n_], pj[:, lo_:lo_ + n_], AF.Square, accum_out=s4[:, col:col + 1]),
                         reads=[bpj], writes=[K["bjunk"], bs4])
                    p.op("act", lambda e: e.activation(s4[:, col + 1:col + 2], s4[:, col:col + 1], AF.Sqrt, bias=K["eps"][:, 0:1], scale=1.0 / n_),
                         reads=[bs4], writes=[bs4])
                    p.op("dve", lambda e: e.reciprocal(s4[:, col + 2:col + 3], s4[:, col + 1:col + 2]), reads=[bs4], writes=[bs4])
                    p.op("dve", lambda e: e.scalar_tensor_tensor(dst_[:], pj[:, lo_:lo_ + n_], s4[:, col + 2:col + 3], gt_[:], ALU.mult, ALU.mult),
                         reads=[bpj, bs4, bg], writes=[bcn])
                if kind == "x":
                    p.dma("pool", cst[:], cos_tm[t * 128:(t + 1) * 128, :], writes=[bcst])
                    p.dma("pool", snt[:], sin_tm[t * 128:(t + 1) * 128, :], writes=[bcst])
                    p.op("dve", lambda e: e.tensor_mul(kr2[:, 0:32], pj[:, 1024:1056], cst[:]), reads=[bpj, bcst], writes=[bkr2])
                    p.op("dve", lambda e: e.tensor_mul(tmp32[:], pj[:, 1056:1088], snt[:]), reads=[bpj, bcst], writes=[btmp])
                    p.op("dve", lambda e: e.tensor_add(kr2[:, 0:32], kr2[:, 0:32], tmp32[:]), reads=[bkr2, btmp], writes=[bkr2])
                else:
                    p.op("dve", lambda e: e.tensor_copy(kr2[:, 0:32], pj[:, 1024:1056]), reads=[bpj], writes=[bkr2])
                p.op("dve", lambda e: e.tensor_copy(kr2[:, 32:64], kr2[:, 0:32]), reads=[bkr2], writes=[bkr2])
                for blk, (src_, nchunk, dstT, bdst) in enumerate(((cqn, 6, cqT, bcqT), (ckn, 2, ckT, bckT))):
                    for c0 in range(0, nchunk, 4):
                        cn = min(4, nchunk - c0)
                        for cc in range(cn):
                            p.op("pe", lambda e: e.transpose(K["ptr"][:, cc * 128:(cc + 1) * 128], src_[:, (c0 + cc) * 128:(c0 + cc + 1) * 128], K["ident"][:]),
                                 reads=[bcn, K["bident"]], writes=[K["bptr"]])
                        p.op("act", lambda e: e.copy(dstT[:, c0:c0 + cn, ks], K["ptr"][:, 0:cn * 128].rearrange("p (c t) -> p c t", c=cn)),
                             reads=[K["bptr"]], writes=[bdst[k]])
                p.op("pe", lambda e: e.transpose(K["ptr"][0:64, 0:128], kr2[:], K["ident"][:]), reads=[bkr2, K["bident"]], writes=[K["bptr"]])
                p.op("act", lambda e: e.copy(krT[:, ks], K["ptr"][0:64, 0:128]), reads=[K["bptr"]], writes=[bkrT[k]])
                for ch in range(2):
                    p.op("pe", lambda e: e.matmul(pp[0][:, 0:512], ckT[:, ch, ks], wkv[:, ch, :], start=(ch == 0), stop=(ch == 1)),
                         reads=[bckT[k], bwq], writes=[bpp[0]])
                p.op("dve", lambda e: e.tensor_copy(vt[:].rearrange("p a (h d) -> p a h d", h=2)[:, :, :, 0:64],
                                                    pp[0][:, 0:512].rearrange("p (a h d) -> p a h d", a=4, h=2)),
                     reads=[bpp[0]], writes=[bvt])
                p.dma("sp", V[:, t].rearrange("a p f -> p a f"), vt[:], reads=[bvt], writes=[bKV[gi]])
            nt = len(tiles)
            p.dma("sp", KrT2[:, t0:t0 + N], krT[:, 0:N], reads=bkrT[:nt], writes=[bKV[gi]])
            if tiles[0] < 32:
                for hh in range(2):
                    p.dma("pool", cT[hh * 32:(hh + 1) * 32, 0:N], cosT[:, t0:t0 + N], writes=[bcs])
                    p.dma("pool", sT[hh * 32:(hh + 1) * 32, 0:N], sinT[:, t0:t0 + N], writes=[bcs])
            for pr in range(4):
                j = nA % 2
                nA += 1
                for ch in range(2):
                    p.op("pe", lambda e: e.matmul(pp[0][:, 0:N], wkn[:, ch, pr * 128:(pr + 1) * 128], ckT[:, ch, 0:N], start=(ch == 0), stop=(ch == 1)),
                         reads=bckT[:nt] + [bwq], writes=[bpp[0]])
                p.op("act", lambda e: e.copy(outA[j][:, 0:N], pp[0][:, 0:N]), reads=[bpp[0]], writes=[boutA[j]])
                p.dma("sp", KnT[pr][:, t0:t0 + N], outA[j][:, 0:N], reads=[boutA[j]], writes=[bKV[gi]])
                j = nA % 2
                nA += 1
                for ch in range(6):
                    p.op("pe", lambda e: e.matmul(pp[1][:, 0:N], wqn[:, ch, pr * 128:(pr + 1) * 128], cqT[:, ch, 0:N], start=(ch == 0), stop=(ch == 5)),
                         reads=bcqT[:nt] + [bwq], writes=[bpp[1]])
                p.op("dve", lambda e: e.tensor_copy(outA[j][:, 0:N], pp[1][:, 0:N]), reads=[bpp[1]], writes=[boutA[j]])
                p.dma("sp", QnT[pr][:, t0:t0 + N], outA[j][:, 0:N], reads=[boutA[j]], writes=[bQ[gi]])
                for ch in range(6):
                    p.op("pe", lambda e: e.matmul(pp[2][0:64, 0:N], wqr[:, ch, pr * 64:(pr + 1) * 64], cqT[:, ch, 0:N], start=(ch == 0), stop=(ch == 5)),
                         reads=bcqT[:nt] + [bwq], writes=[bpp[2]])
                jb = pr % 2
                if tiles[0] < 32:
                    p.op("dve", lambda e: e.tensor_mul(outB[jb][:, 0:N], pp[2][0:64, 0:N], cT[:, 0:N]), reads=[bpp[2], bcs], writes=[boutB[jb]])
                    for ch in range(6):
                        p.op("pe", lambda e: e.matmul(pp[2][0:64, 0:N], wqs[:, ch, pr * 64:(pr + 1) * 64], cqT[:, ch, 0:N], start=(ch == 0), stop=(ch == 5)),
                             reads=bcqT[:nt] + [bwq], writes=[bpp[2]])
                    p.op("dve", lambda e: e.tensor_mul(tmpB[:, 0:N], pp[2][0:64, 0:N], sT[:, 0:N]), reads=[bpp[2], bcs], writes=[btmpB])
                    p.op("pool", lambda e: e.tensor_add(outB[jb][:, 0:N], outB[jb][:, 0:N], tmpB[:, 0:N]), reads=[boutB[jb], btmpB], writes=[boutB[jb]])
                else:
                    p.op("dve", lambda e: e.tensor_copy(outB[jb][:, 0:N], pp[2][0:64, 0:N]), reads=[bpp[2]], writes=[boutB[jb]])
                p.dma("sp", QrT[pr][:, t0:t0 + N], outB[jb][:, 0:N], reads=[boutB[jb]], writes=[bQ[gi]])
        p.barrier()
    esB = ExitStack()
    with esB:
        qn = c.sb([128, NTOK], esB); kn = c.sb([128, NTOK], esB); qr = c.sb([64, NTOK], esB); kr = c.sb([64, NTOK], esB)
        vv = c.sb([128, NT, 130], esB)
        bqn, bkn, bqr, bkr, bvv = Buf(), Buf(), Buf(), Buf(), Buf()
        ones = c.sb([128, 64], esB); bon = Buf()
        p.op("dve", lambda e: e.memset(ones[:], 1.0), writes=[bon])
        pS = [c.ps([128, 512], esB) for _ in range(2)]; bpS = [PBuf(), PBuf()]
        pO = [c.ps([128, 512], esB) for _ in range(2)]; bpO = [PBuf(), PBuf()]
        pB = c.ps([128, 512], esB); bpB = PBuf()
        eS = [c.sb([128, 512], esB) for _ in range(2)]; beS = [Buf(), Buf()]
        rinv = c.sb([128, 512], esB); brinv = Buf()
        osb = [c.sb([64, 512], esB) for _ in range(2)]; bosb = [Buf(), Buf()]
        p.dma("sp", kr[:], KrT2, reads=bKV, writes=[bkr])
        nS = 0
        nO = 0
        for pr in range(4):
            p.dma("sp", qn[:], QnT[pr], reads=bQ, writes=[bqn])
            p.dma("sp", kn[:], KnT[pr], reads=bKV, writes=[bkn])
            p.dma("sp", qr[:], QrT[pr], reads=bQ, writes=[bqr])
            p.dma("sp", vv[:], V[pr].rearrange("t p f -> p t f"), reads=bKV, writes=[bvv])
            for hh in range(2):
                nb = hh * 64
                rb = hh * 32
                for gi, tiles in enumerate(groups):
                    N = len(tiles) * 128
                    q0 = tiles[0] * 128
                    keys = list(range(NT)) if tiles[0] < 32 else [32, 33]
                    jo = nO % 2
                    nO += 1
                    for ki, kt in enumerate(keys):
                        js = nS % 2
                        nS += 1
                        kk = slice(kt * 128, (kt + 1) * 128)
                        p.op("pe", lambda e: e.matmul(pS[js][:, 0:N], kn[nb:nb + 64, kk], qn[nb:nb + 64, q0:q0 + N], start=True, stop=False),
                             reads=[bkn, bqn], writes=[bpS[js]])
                        p.op("pe", lambda e: e.matmul(pS[js][:, 0:N], kr[rb:rb + 32, kk], qr[rb:rb + 32, q0:q0 + N], start=False, stop=True),
                             reads=[bkr, bqr], writes=[bpS[js]])
                        p.op("act", lambda e: e.activation(eS[js][:, 0:N], pS[js][:, 0:N], AF.Exp, scale=MLA_SCALE),
                             reads=[bpS[js]], writes=[beS[js]])
                        p.op("pe", lambda e: e.matmul(pO[jo][0:65, 0:N], vv[:, kt, hh * 65:(hh + 1) * 65], eS[js][:, 0:N],
                                                      start=(ki == 0), stop=(ki == len(keys) - 1)),
                             reads=[bvv, beS[js]], writes=[bpO[jo]])
                    p.op("dve", lambda e: e.reciprocal(rinv[64:65, 0:N], pO[jo][64:65, 0:N]), reads=[bpO[jo]], writes=[brinv])
                    p.op("pe", lambda e: e.matmul(pB[0:64, 0:N], ones[64:65, :], rinv[64:65, 0:N], start=True, stop=True),
                         reads=[bon, brinv], writes=[bpB])
                    p.op("act", lambda e: e.copy(osb[jo][:, 0:N], pB[0:64, 0:N]), reads=[bpB], writes=[bosb[jo]])
                    p.op("dve", lambda e: e.tensor_mul(osb[jo][:, 0:N], osb[jo][:, 0:N], pO[jo][0:64, 0:N]), reads=[bosb[jo], bpO[jo]], writes=[bosb[jo]])
                    p.dma("pool", OT[pr][hh * 64:(hh + 1) * 64, q0:q0 + N], osb[jo][:, 0:N], reads=[bosb[jo]], writes=[bOT[pr]])
        p.barrier()
    esC = ExitStack()
    with esC:
        wo = c.sb([128, 4, D], esC); bwo = Buf()
        p.dma("sp", wo[:], w_out.rearrange("(c p) f -> p c f", p=128), writes=[bwo])
        ot = c.sb([128, 4, NTOK], esC); bot = Buf()
        for pr in range(4):
            p.dma("sp", ot[:, pr, :], OT[pr], reads=[bOT[pr]], writes=[bot])
        pc = [c.ps([128, 512], esC) for _ in range(2)]; bpc = [PBuf(), PBuf()]
        ob = [c.sb([128, D], esC) for _ in range(2)]; bob = [Buf(), Buf()]
        for t in range(NT):
            i = t % 2
            for half in range(2):
                for pr in range(4):
                    p.op("pe", lambda e: e.matmul(pc[half][:], ot[:, pr, t * 128:(t + 1) * 128], wo[:, pr, half * 512:(half + 1) * 512],
                                                  start=(pr == 0), stop=(pr == 3)), reads=[bot, bwo], writes=[bpc[half]])
                p.op("act" if half else "dve",
                     (lambda e: e.copy(ob[i][:, half * 512:(half + 1) * 512], pc[half][:])) if half else
                     (lambda e: e.tensor_copy(ob[i][:, half * 512:(half + 1) * 512], pc[half][:])),
                     reads=[bpc[half]], writes=[bob[i]])
            p.dma("pool", P[t * 128:(t + 1) * 128, :], ob[i][:], reads=[bob[i]], writes=[bP[t]])
        p.barrier()


def rope_tables():
    t = np.arange(4096)
    rows = (t // 64).astype(np.float32)
    cols = (t % 64).astype(np.float32)
    inv = (10000.0 ** (-np.arange(8, dtype=np.float32) * 2.0 / 16)).astype(np.float32)
    ar = rows[:, None] * inv[None, :]
    ac = cols[:, None] * inv[None, :]
    cos = np.concatenate([np.cos(ar), np.cos(ar), np.cos(ac), np.cos(ac)], 1).astype(np.float32)
    sin = np.concatenate([-np.sin(ar), np.sin(ar), -np.sin(ac), np.sin(ac)], 1).astype(np.float32)
    return cos, sin, np.ascontiguousarray(cos.T), np.ascontiguousarray(sin.T)


ROPE_SWAP = list(range(8, 16)) + list(range(0, 8)) + list(range(24, 32)) + list(range(16, 24))


def build_stage(kind, stop=None, with_ss=False):
    nc = new_nc()
    es = ExitStack()
    with es:
        c = Ctx(nc, es); p = c.p
        K = common_consts(c, c.din("ident", [128, 128]))
        xprev = c.din("xprev", [NTOK, D]); pa = c.din("pa", [NTOK, D]); pb = c.din("pb", [NTOK, D])
        vec = c.din("vec", [NVEC, D])
        xo = c.dout("xo", [NTOK, D])
        X = dram(c, [NTOK, D]); bX = [Buf() for _ in range(NT)]
        ssa = c.din("ssa", [NTOK, 1]) if with_ss else None
        ssb = c.din("ssb", [NTOK, 1]) if with_ss else None
        bo = emit_residual_in(c, K, xprev, pa, pb, vec, X, bX, xo, ssa, ssb)
        if kind == "final":
            out = c.dout("p", [NTOK, D])
            es1 = ExitStack()
            with es1:
                fg = bc_tile(c, es1, vec[14:15, :])
                xt = [c.sb([128, D], es1) for _ in range(2)]; bxt = [Buf(), Buf()]
                ot = [c.sb([128, D], es1) for _ in range(2)]; bot = [Buf(), Buf()]
                for t in range(32):
                    i = t % 2
                    p.dma("sp", xt[i][:], X[t * 128:(t + 1) * 128, :], reads=[bX[t]], writes=[bxt[i]])
                    norm_mod_T(c, K, xt[i][:], bxt[i], fg, None, None, None)
                    p.op("act", lambda e: e.copy(ot[i][:], K["h"][:]), reads=[K["bh"]], writes=[bot[i]])
                    b = Buf(); bo.append(b)
                    p.dma("pool", out[t * 128:(t + 1) * 128, :], ot[i][:], reads=[bot[i]], writes=[b])
            p.finish(bo)
            return nc
        P = c.dout("p", [NTOK, D]); bP = [Buf() for _ in range(NT)]
        es1 = ExitStack()
        with es1:
            if kind == "moe":
                mods = mods_from_vec(c, es1, vec, 3, 13)
                wr = c.din("wr", [D, NE]); wg = c.din("wg", [NEH * D, D]); wu = c.din("wu", [NEH * D, D]); wd = c.din("wd", [NEH * D, D])
                emit_moe(c, K, X, bX, mods, wr, wg, wu, wd, Buf(), P, bP)
            elif kind == "mla":
                mods = mods_from_vec(c, es1, vec, 0, 12)
                a = dict(w_in=c.din("w_in", [D, 1088]), w_uqn=c.din("w_uqn", [768, 512]), w_uqr=c.din("w_uqr", [768, 256]),
                         w_uqs=c.din("w_uqs", [768, 256]), w_ukn=c.din("w_ukn", [256, 512]), w_ukv=c.din("w_ukv", [256, 512]),
                         w_out=c.din("w_out", [512, D]), gq=c.din("gq", [1, 768]), gkv=c.din("gkv", [1, 256]),
                         cosT=c.din("cosT", [32, 4096]), sinT=c.din("sinT", [32, 4096]),
                         cos_tm=c.din("cos_tm", [4096, 32]), sin_tm=c.din("sin_tm", [4096, 32]))
                emit_mla(c, K, X, bX, mods, P=P, bP=bP, **a)
            elif kind == "ssd":
                mods = mods_from_vec(c, es1, vec, 0, 12)
                a = dict(w_cv=c.din("w_cv", [D, 2048]), convp=c.din("convp", [16, 128, 4]), w_z=c.din("w_z", [D, D]), w_dt=c.din("w_dt", [D, 32]),
                         dtb=c.din("dtb", [1, 32]), alog=c.din("alog", [1, 32]), dvec=c.din("dvec", [1, D]), ng=c.din("ng", [1, D]),
                         w_out=c.din("w_out", [D, D]), triF=c.din("triF", [128, 128]), triB=c.din("triB", [128, 128]))
                SS = c.dout("ss", [NTOK, 1]); bSS = [Buf() for _ in range(NT)]
                emit_ssd(c, K, X, bX, mods, P=P, bP=bP, SS=SS, bSS=bSS, **a)
                bo = bo + bSS
            elif kind == "gdn":
                mods = mods_from_vec(c, es1, vec, 0, 12)
                a = dict(w_cv=c.din("w_cv", [D, 1536]), convp=c.din("convp", [12, 128, 4]), w_z=c.din("w_z", [D, 512]), w_bg=c.din("w_bg", [D, 16]),
                         dtb=c.din("dtb", [1, 8]), alog=c.din("alog", [1, 8]), ng=c.din("ng", [1, 128]), w_out=c.din("w_out", [512, D]),
                         masks=c.din("masks", [2, 7, 128, 128]))
                emit_gdn(c, K, X, bX, mods, P=P, bP=bP, stop=stop, **a)
            else:
                raise ValueError(kind)
        p.finish(bo + bP)
        print(kind, "ninst", p.ninst, "nsem", p.nsem)
    return nc


def mla_weights(z_w_in, z_uq, z_ukv, z_out, gq, gkv, h):
    heads = range(8 * h, 8 * h + 8)
    w_in = np.concatenate([z_w_in, z_w_in[:, 1024:1056][:, ROPE_SWAP]], 1)
    uqn = np.concatenate([z_uq[:, hd * 96:hd * 96 + 64] for hd in heads], 1)
    uqr = np.concatenate([z_uq[:, hd * 96 + 64:hd * 96 + 96] for hd in heads], 1)
    uqs = np.concatenate([z_uq[:, hd * 96 + 64:hd * 96 + 96][:, ROPE_SWAP] for hd in heads], 1)
    ukn = np.concatenate([z_ukv[:, hd * 128:hd * 128 + 64] for hd in heads], 1)
    ukv = np.concatenate([z_ukv[:, hd * 128 + 64:hd * 128 + 128] for hd in heads], 1)
    cos, sin, cosT, sinT = rope_tables()
    f = np.ascontiguousarray
    return dict(w_in=f(w_in), w_uqn=f(uqn), w_uqr=f(uqr), w_uqs=f(uqs), w_ukn=f(ukn), w_ukv=f(ukv),
                w_out=f(z_out[8 * h * 64:(8 * h + 8) * 64]), gq=f(gq[None, :]), gkv=f(gkv[None, :]),
                cosT=cosT, sinT=sinT, cos_tm=cos, sin_tm=sin)


def make_vec(mx_b, mc, n1g, n2g, fg, pgx, pgc):
    return np.ascontiguousarray(np.concatenate([mx_b, mc, n1g[None], n2g[None], fg[None], pgx[None], pgc[None]], 0).astype(np.float32))


def softplus_tile(c, out_ap, in_ap, tmp_a, tmp_b, rb, wb, bt):
    p = c.p
    p.op("act", lambda e: e.activation(tmp_a, in_ap, AF.Abs), reads=rb, writes=[bt])
    p.op("act", lambda e: e.activation(tmp_a, tmp_a, AF.Exp, scale=-1.0), reads=[bt], writes=[bt])
    p.op("act", lambda e: e.activation(tmp_b, tmp_a, AF.Ln, bias=1.0), reads=[bt], writes=[bt])
    p.op("dve", lambda e: e.scalar_tensor_tensor(out_ap, in_ap, 0.0, tmp_b, ALU.max, ALU.add), reads=rb + [bt], writes=wb)


def emit_ssd(c, K, X, bX, mods, w_cv, convp, w_z, w_dt, dtb, alog, dvec, ng, w_out, triF, triB, P, bP, SS, bSS):
    p = c.p
    HT = dram(c, [NT, 128, 8, 128]); bHT = [Buf() for _ in range(NT)]
    FT = dram(c, [16, 128, NTOK]); bFT = [Buf() for _ in range(16)]
    ZS = dram(c, [NTOK, D]); DT = dram(c, [NTOK, 32]); XTM = dram(c, [NTOK, D]); BTM = dram(c, [NTOK, 512]); YF = dram(c, [NTOK, D])
    bZS = [Buf() for _ in range(NT)]; bXB = [Buf() for _ in range(NT)]; bYF = [Buf() for _ in range(NT)]
    groups = [list(range(4 * q, 4 * q + 4)) for q in range(8)] + [[32, 33]]
    es = ExitStack()
    with es:
        wz = c.sb([128, 8, D], es); wdt = c.sb([128, 8, 32], es); bwz = Buf()
        p.dma("sp", wz[:], w_z.rearrange("(c p) f -> p c f", p=128), writes=[bwz])
        p.dma("sp", wdt[:], w_dt.rearrange("(c p) f -> p c f", p=128), writes=[bwz])
        dtb_t = c.sb([128, 32], es); bdb = Buf()
        p.dma("pool", dtb_t[:], dtb.partition_broadcast(128), writes=[bdb])
        xt = [c.sb([128, D], es) for _ in range(2)]; bxt = [Buf(), Buf()]
        hT = [c.sb([128, 8, 128], es) for _ in range(2)]; bhT = [Buf(), Buf()]
        pz = [c.ps([128, 512], es) for _ in range(2)]; bpz = [PBuf(), PBuf()]
        pd = c.ps([128, 512], es); bpd = PBuf()
        zs = [c.sb([128, D], es) for _ in range(2)]; bzs = [Buf(), Buf()]
        dr = c.sb([128, 32], es); ta = c.sb([128, 32], es); tb = c.sb([128, 32], es); do = [c.sb([128, 32], es) for _ in range(2)]
        bdr, btt, bdo = Buf(), Buf(), [Buf(), Buf()]
        for t in range(NT):
            i = t % 2
            G, S, _ = mods[tile_kind(t)]
            p.dma("pool", xt[i][:], X[t * 128:(t + 1) * 128, :], reads=[bX[t]], writes=[bxt[i]])
            norm_mod_T(c, K, xt[i][:], bxt[i], G, S, hT[i], bhT[i])
            p.dma("sp", HT[t], hT[i][:], reads=[bhT[i]], writes=[bHT[t]])
            for half in range(2):
                for ch in range(8):
                    p.op("pe", lambda e: e.matmul(pz[half][:], hT[i][:, ch, :], wz[:, ch, half * 512:(half + 1) * 512], start=(ch == 0), stop=(ch == 7)),
                         reads=[bhT[i], bwz], writes=[bpz[half]])
                p.op("act", lambda e: e.activation(zs[i][:, half * 512:(half + 1) * 512], pz[half][:], AF.Silu), reads=[bpz[half]], writes=[bzs[i]])
            p.dma("sp", ZS[t * 128:(t + 1) * 128, :], zs[i][:], reads=[bzs[i]], writes=[bZS[t]])
            for ch in range(8):
                p.op("pe", lambda e: e.matmul(pd[:, 0:32], hT[i][:, ch, :], wdt[:, ch, :], start=(ch == 0), stop=(ch == 7)),
                     reads=[bhT[i], bwz], writes=[bpd])
            p.op("dve", lambda e: e.tensor_add(dr[:], pd[:, 0:32], dtb_t[:]), reads=[bpd, bdb], writes=[bdr])
            softplus_tile(c, do[i][:], dr[:], ta[:], tb[:], [bdr], [bdo[i]], btt)
            p.dma("sp", DT[t * 128:(t + 1) * 128, :], do[i][:], reads=[bdo[i]], writes=[bZS[t]])
        p.barrier()
    es = ExitStack()
    with es:
        wc = c.sb([128, 8, 512], es); bwc = Buf()
        cp = c.sb([128, 16, 4], es); bcp = Buf()
        p.dma("pool", cp[:], convp.rearrange("k p f -> p k f"), writes=[bcp])
        hg = [c.sb([128, 4, 8, 128], es) for _ in range(2)]; bhg = [Buf(), Buf()]
        raw = [c.sb([128, NTOK], es) for _ in range(4)]; braw = [Buf() for _ in range(4)]
        cv = [c.sb([128, NTOK], es) for _ in range(2)]; bcv = [Buf(), Buf()]
        pr_ = [c.ps([128, 512], es) for _ in range(2)]; bpr = [PBuf(), PBuf()]
        n = 0
        for cb in range(4):
            p.dma("sp", wc[:], w_cv[:, cb * 512:(cb + 1) * 512].rearrange("(c p) f -> p c f", p=128), writes=[bwc])
            for gi, tiles in enumerate(groups):
                i = gi % 2
                nt = len(tiles)
                N = nt * 128
                t0 = tiles[0] * 128
                p.dma("pool", hg[i][:, 0:nt], HT[tiles[0]:tiles[0] + nt].rearrange("t p c k -> p t c k"), reads=[bHT[t] for t in tiles], writes=[bhg[i]])
                for cc in range(4):
                    j = n % 2
                    n += 1
                    for k in range(nt):
                        for ch in range(8):
                            p.op("pe", lambda e: e.matmul(pr_[j][:, k * 128:(k + 1) * 128], wc[:, ch, cc * 128:(cc + 1) * 128], hg[i][:, k, ch, :],
                                                          start=(ch == 0), stop=(ch == 7)), reads=[bwc, bhg[i]], writes=[bpr[j]])
                    p.op("act" if j else "dve",
                         (lambda e: e.copy(raw[cc][:, t0:t0 + N], pr_[j][:, 0:N])) if j else (lambda e: e.tensor_copy(raw[cc][:, t0:t0 + N], pr_[j][:, 0:N])),
                         reads=[bpr[j]], writes=[braw[cc]])
            for cc in range(4):
                k = cb * 4 + cc
                o = cv[cc % 2]; bo_ = bcv[cc % 2]; r = raw[cc]
                p.op("dve", lambda e: e.tensor_scalar(o[:], r[:], cp[:, k, 1:2], None, ALU.mult), reads=[braw[cc], bcp], writes=[bo_])
                for (a0, a1) in ((0, 4096), (4096, NTOK)):
                    p.op("dve", lambda e: e.scalar_tensor_tensor(o[:, a0 + 1:a1], r[:, a0:a1 - 1], cp[:, k, 0:1], o[:, a0 + 1:a1], ALU.mult, ALU.add),
                         reads=[braw[cc], bcp, bo_], writes=[bo_])
                    p.op("dve", lambda e: e.scalar_tensor_tensor(o[:, a0:a1 - 1], r[:, a0 + 1:a1], cp[:, k, 2:3], o[:, a0:a1 - 1], ALU.mult, ALU.add),
                         reads=[braw[cc], bcp, bo_], writes=[bo_])
                p.op("act", lambda e: e.activation(o[:], o[:], AF.Silu, bias=cp[:, k, 3:4]), reads=[bo_, bcp], writes=[bo_])
                p.dma("sp", FT[k], o[:], reads=[bo_], writes=[bFT[k]])
        p.barrier()
    es = ExitStack()
    with es:
        fx = [c.sb([128, 12, 128], es) for _ in range(2)]; bfx = [Buf(), Buf()]
        xo_ = [c.sb([128, 12, 128], es) for _ in range(2)]; bxo = [Buf(), Buf()]
        for t in range(NT):
            i = t % 2
            p.dma("sp", fx[i][:], FT[0:12, :, t * 128:(t + 1) * 128].rearrange("k p t -> p k t"), reads=bFT[0:12], writes=[bfx[i]])
            for q in range(3):
                for cc in range(4):
                    p.op("pe", lambda e: e.transpose(K["ptr"][:, cc * 128:(cc + 1) * 128], fx[i][:, q * 4 + cc, :], K["ident"][:]),
                         reads=[bfx[i], K["bident"]], writes=[K["bptr"]])
                p.op("act" if q % 2 else "dve",
                     (lambda e: e.copy(xo_[i][:, q * 4:(q + 1) * 4, :], K["ptr"][:].rearrange("p (c t) -> p c t", c=4))) if q % 2 else
                     (lambda e: e.tensor_copy(xo_[i][:, q * 4:(q + 1) * 4, :], K["ptr"][:].rearrange("p (c t) -> p c t", c=4))),
                     reads=[K["bptr"]], writes=[bxo[i]])
            p.dma("pool", XTM[t * 128:(t + 1) * 128, :], xo_[i][:, 0:8, :], reads=[bxo[i]], writes=[bXB[t]])
            p.dma("pool", BTM[t * 128:(t + 1) * 128, :], xo_[i][:, 8:12, :], reads=[bxo[i]], writes=[bXB[t]])
        p.barrier()
    es = ExitStack()
    with es:
        tri = {0: c.sb([128, 128], es), 1: c.sb([128, 128], es)}; btri = Buf()
        p.dma("pool", tri[0][:], triF, writes=[btri])
        p.dma("pool", tri[1][:], triB, writes=[btri])
        ones = c.sb([128, 128], es); bon = Buf()
        p.op("dve", lambda e: e.memset(ones[:], 1.0), writes=[bon])
        Abc = c.sb([128, 32], es); bA = Buf()
        p.dma("pool", Abc[:], alog.partition_broadcast(128), writes=[bA])
        p.op("act", lambda e: e.activation(Abc[:], Abc[:], AF.Exp), reads=[bA], writes=[bA])
        p.op("dve", lambda e: e.tensor_scalar(Abc[:], Abc[:], -1.0, None, ALU.mult), reads=[bA], writes=[bA])
        Dbc = c.sb([128, D], es); ngt = c.sb([128, D], es); bDn = Buf()
        p.dma("pool", Dbc[:], dvec.partition_broadcast(128), writes=[bDn])
        p.dma("pool", ngt[:], ng.partition_broadcast(128), writes=[bDn])
        wo = c.sb([128, 8, D], es); bwo = Buf()
        p.dma("sp", wo[:], w_out.rearrange("(c p) f -> p c f", p=128), writes=[bwo])
        xs = [c.sb([128, 16, 64], es) for _ in range(2)]; bts = [c.sb([128, 4, 128], es) for _ in range(2)]
        BT = [c.sb([128, 4, 128], es) for _ in range(2)]; CT = [c.sb([128, 4, 128], es) for _ in range(2)]
        dtt = [c.sb([128, 32], es) for _ in range(2)]
        bin_ = [Buf(), Buf()]
        hst = c.sb([128, 16, 64], es); bh = Buf()
        a_ = c.sb([128, 16], es); acs = c.sb([128, 16], es); nacs = c.sb([128, 16], es); etot = c.sb([128, 16], es); wgt = c.sb([128, 16], es)
        bsm = Buf()
        pA = c.ps([128, 512], es); bpA = PBuf()
        pCB = c.ps([128, 512], es); bpCB = PBuf()
        pST = c.ps([128, 512], es); bpST = PBuf()
        pRB = [c.ps([128, 512], es) for _ in range(2)]; bpRB = [PBuf(), PBuf()]
        pY = c.ps([128, 512], es); bpY = PBuf()
        pC = c.ps([128, 512], es); bpC = PBuf()
        CBm = c.sb([128, 128], es); bCBm = Buf()
        xh = c.sb([128, 4, 64], es); bxh = Buf()
        abc = [c.sb([128, 128], es) for _ in range(2)]; babc = [Buf(), Buf()]
        tmp = [c.sb([128, 128], es) for _ in range(2)]; btmp = [Buf(), Buf()]
        WT = [c.sb([128, 128], es) for _ in range(2)]; bWT = [Buf(), Buf()]
        Eb = [c.sb([128, 128], es) for _ in range(2)]; bEb = [Buf(), Buf()]
        LT = [c.sb([128, 128], es) for _ in range(2)]; bLT = [Buf(), Buf()]
        ysb = [c.sb([128, 16, 64], es) for _ in range(2)]; bys = [Buf(), Buf()]
        yfl = c.sb([128, D], es); zl = c.sb([128, D], es); bfl = Buf()
        ssb = c.sb([128, 2], es); bssb = Buf()
        ynT = c.sb([128, 8, 128], es); bynT = Buf()
        ob = c.sb([128, D], es); bob = Buf()
        nh = 0
        for d in range(2):
            order = [32, 33] + list(range(32)) if d == 0 else [33, 32] + list(range(31, -1, -1))
            p.op("dve", lambda e: e.memset(hst[:], 0.0), reads=[bh], writes=[bh])
            for vi, t in enumerate(order):
                i = vi % 2
                sl = slice(t * 128, (t + 1) * 128)
                p.dma("sp", xs[i][:], XTM[sl, :].rearrange("p (h d) -> p h d", h=16), reads=[bXB[t]], writes=[bin_[i]])
                p.dma("sp", bts[i][:], BTM[sl, :].rearrange("p (g n) -> p g n", g=4), reads=[bXB[t]], writes=[bin_[i]])
                p.dma("sp", BT[i][:], FT[8:12, :, sl].rearrange("g p t -> p g t"), reads=bFT[8:12], writes=[bin_[i]])
                p.dma("sp", CT[i][:], FT[12:16, :, sl].rearrange("g p t -> p g t"), reads=bFT[12:16], writes=[bin_[i]])
                p.dma("sp", dtt[i][:], DT[sl, :], reads=[bZS[t]], writes=[bin_[i]])
                dtd = dtt[i][:, d * 16:(d + 1) * 16]
                p.op("dve", lambda e: e.tensor_mul(a_[:], dtd, Abc[:, d * 16:(d + 1) * 16]), reads=[bin_[i], bA], writes=[bsm])
                p.op("pe", lambda e: e.matmul(pA[:, 0:16], tri[d][:], a_[:], start=True, stop=True), reads=[btri, bsm], writes=[bpA])
                p.op("pe", lambda e: e.matmul(pA[:, 16:32], ones[:], a_[:], start=True, stop=True), reads=[bon, bsm], writes=[bpA])
                p.op("act", lambda e: e.copy(acs[:], pA[:, 0:16]), reads=[bpA], writes=[bsm])
                p.op("dve", lambda e: e.tensor_scalar(nacs[:], pA[:, 0:16], -1.0, None, ALU.mult), reads=[bpA], writes=[bsm])
                p.op("act", lambda e: e.activation(etot[:], pA[:, 16:32], AF.Exp), reads=[bpA], writes=[bsm])
                p.op("dve", lambda e: e.tensor_tensor(wgt[:], pA[:, 16:32], acs[:], ALU.subtract), reads=[bpA, bsm], writes=[bsm])
                p.op("act", lambda e: e.activation(wgt[:], wgt[:], AF.Exp), reads=[bsm], writes=[bsm])
                p.op("dve", lambda e: e.tensor_mul(wgt[:], wgt[:], dtd), reads=[bsm, bin_[i]], writes=[bsm])
                yb = ysb[i]
                for g in range(4):
                    p.op("pe", lambda e: e.matmul(pCB[:, 0:128], BT[i][:, g, :], CT[i][:, g, :], start=True, stop=True), reads=[bin_[i]], writes=[bpCB])
                    p.op("dve", lambda e: e.tensor_mul(CBm[:], pCB[:, 0:128], tri[d][:]), reads=[bpCB, btri], writes=[bCBm])
                    for r in range(4):
                        hd = 4 * g + r
                        p.op("pool", lambda e: e.tensor_scalar(xh[:, r, :], xs[i][:, hd, :], wgt[:, hd:hd + 1], 1.0, ALU.mult, ALU.mult),
                             reads=[bin_[i], bsm], writes=[bxh])
                    p.op("pe", lambda e: e.matmul(pST[:, 0:256], bts[i][:, g, :], xh[:].rearrange("p r d -> p (r d)"), start=True, stop=True),
                         reads=[bin_[i], bxh], writes=[bpST])
                    for r in range(4):
                        hd = 4 * g + r
                        j = nh % 2
                        nh += 1
                        p.op("pool", lambda e: e.tensor_scalar(abc[j][:], ones[:], a_[:, hd:hd + 1], 1.0, ALU.mult, ALU.mult), reads=[bon, bsm], writes=[babc[j]])
                        p.op("pe", lambda e: e.matmul(pRB[j][:, 0:128], abc[j][:], tri[d][:], start=True, stop=True), reads=[babc[j], btri], writes=[bpRB[j]])
                        p.op("dve", lambda e: e.tensor_scalar(tmp[j][:], pRB[j][:, 0:128], nacs[:, hd:hd + 1], 0.0, ALU.add, ALU.min),
                             reads=[bpRB[j], bsm], writes=[btmp[j]])
                        p.op("act", lambda e: e.activation(tmp[j][:], tmp[j][:], AF.Exp), reads=[btmp[j]], writes=[btmp[j]])
                        p.op("dve", lambda e: e.scalar_tensor_tensor(WT[j][:], tmp[j][:], dtd[:, hd:hd + 1], CBm[:], ALU.mult, ALU.mult),
                             reads=[btmp[j], bin_[i], bCBm], writes=[bWT[j]])
                        p.op("act", lambda e: e.activation(Eb[j][:], pRB[j][:, 0:128], AF.Exp), reads=[bpRB[j]], writes=[bEb[j]])
                        p.op("pool", lambda e: e.tensor_mul(LT[j][:], CT[i][:, g, :], Eb[j][:]), reads=[bin_[i], bEb[j]], writes=[bLT[j]])
                        p.op("pe", lambda e: e.matmul(pY[:, 0:64], WT[j][:], xs[i][:, hd, :], start=True, stop=False), reads=[bWT[j], bin_[i]], writes=[bpY])
                        p.op("pe", lambda e: e.matmul(pY[:, 0:64], LT[j][:], hst[:, hd, :], start=False, stop=True), reads=[bLT[j], bh], writes=[bpY])
                        p.op("act", lambda e: e.copy(yb[:, hd, :], pY[:, 0:64]), reads=[bpY], writes=[bys[i]])
                    for r in range(4):
                        hd = 4 * g + r
                        p.op("dve", lambda e: e.scalar_tensor_tensor(hst[:, hd, :], hst[:, hd, :], etot[:, hd:hd + 1], pST[:, r * 64:(r + 1) * 64], ALU.mult, ALU.add),
                             reads=[bh, bsm, bpST], writes=[bh])
                ybf = yb[:].rearrange("p h d -> p (h d)")
                if d == 0:
                    p.dma("pool", YF[sl, :], ybf, reads=[bys[i]], writes=[bYF[t]])
                    continue
                p.dma("pool", yfl[:], YF[sl, :], reads=[bYF[t]], writes=[bfl])
                p.dma("pool", zl[:], ZS[sl, :], reads=[bZS[t]], writes=[bfl])
                p.op("dve", lambda e: e.tensor_add(ybf, ybf, yfl[:]), reads=[bys[i], bfl], writes=[bys[i]])
                p.op("pool", lambda e: e.tensor_mul(yfl[:], xs[i][:].rearrange("p h d -> p (h d)"), Dbc[:]), reads=[bin_[i], bDn, bfl], writes=[bfl])
                p.op("dve", lambda e: e.tensor_add(ybf, ybf, yfl[:]), reads=[bys[i], bfl], writes=[bys[i]])
                p.op("dve", lambda e: e.tensor_mul(ybf, ybf, zl[:]), reads=[bys[i], bfl], writes=[bys[i]])
                p.op("act", lambda e: e.activation(K["junk"][:], ybf, AF.Square, accum_out=ssb[:, 0:1]), reads=[bys[i]], writes=[K["bjunk"], bssb])
                p.dma("pool", SS[sl, :], ssb[:, 0:1], reads=[bssb], writes=[bSS[t]])
                p.op("dve", lambda e: e.tensor_mul(ybf, ybf, ngt[:]), reads=[bys[i], bDn], writes=[bys[i]])
                for q in range(2):
                    for cc in range(4):
                        ch = q * 4 + cc
                        p.op("pe", lambda e: e.transpose(K["ptr"][:, cc * 128:(cc + 1) * 128], ybf[:, ch * 128:(ch + 1) * 128], K["ident"][:]),
                             reads=[bys[i], K["bident"]], writes=[K["bptr"]])
                    p.op("act", lambda e: e.copy(ynT[:, q * 4:(q + 1) * 4, :], K["ptr"][:].rearrange("p (c t) -> p c t", c=4)), reads=[K["bptr"]], writes=[bynT])
                for half in range(2):
                    for ch in range(8):
                        p.op("pe", lambda e: e.matmul(pC[:], ynT[:, ch, :], wo[:, ch, half * 512:(half + 1) * 512], start=(ch == 0), stop=(ch == 7)),
                             reads=[bynT, bwo], writes=[bpC])
                    p.op("dve", lambda e: e.tensor_copy(ob[:, half * 512:(half + 1) * 512], pC[:]), reads=[bpC], writes=[bob])
                p.dma("pool", P[sl, :], ob[:], reads=[bob], writes=[bP[t]])
        p.barrier()


def ssd_weights(z, h):
    w_in = z["ssd_w_in"][0]
    f = np.ascontiguousarray
    cols = np.concatenate([2048 + h * 1024 + np.arange(1024), 4096 + h * 512 + np.arange(512), 5120 + h * 512 + np.arange(512)])
    cch = cols - 2048
    convp = np.stack([z["ssd_conv_w"][0][0, cch], z["ssd_conv_w"][0][1, cch], z["ssd_conv_w"][0][2, cch], z["ssd_conv_b"][0][cch]], 1)
    dtc = np.concatenate([6144 + 16 * h + np.arange(16), 6144 + 32 + 16 * h + np.arange(16)])
    hs = slice(16 * h, 16 * h + 16)
    tri = np.triu(np.ones((128, 128), np.float32))
    return dict(w_cv=f(w_in[:, cols]), convp=f(convp.reshape(16, 128, 4).astype(np.float32)), w_z=f(w_in[:, h * 1024:(h + 1) * 1024]),
                w_dt=f(w_in[:, dtc]), dtb=f(np.concatenate([z["ssd_dt_bias"][0][0, hs], z["ssd_dt_bias"][0][1, hs]])[None, :]),
                alog=f(np.concatenate([z["ssd_a_log"][0][0, hs], z["ssd_a_log"][0][1, hs]])[None, :]),
                dvec=f(np.repeat(z["ssd_d"][0][hs], 64)[None, :]), ng=f(z["ssd_norm_g"][0][h * 1024:(h + 1) * 1024][None, :]),
                w_out=f(z["ssd_w_out"][0][h * 1024:(h + 1) * 1024]), triF=tri, triB=f(tri.T))


def conv_fm(c, K, HT, bHT, w_cv, convp, nblk, FT, bFT):
    p = c.p
    groups = [list(range(4 * q, 4 * q + 4)) for q in range(8)] + [[32, 33]]
    es = ExitStack()
    with es:
        wc = c.sb([128, 8, 512], es); bwc = Buf()
        cp = c.sb([128, 4 * nblk, 4], es); bcp = Buf()
        p.dma("pool", cp[:], convp.rearrange("k p f -> p k f"), writes=[bcp])
        hg = [c.sb([128, 4, 8, 128], es) for _ in range(2)]; bhg = [Buf(), Buf()]
        raw = [c.sb([128, NTOK], es) for _ in range(4)]; braw = [Buf() for _ in range(4)]
        cv = [c.sb([128, NTOK], es) for _ in range(2)]; bcv = [Buf(), Buf()]
        pr_ = [c.ps([128, 512], es) for _ in range(2)]; bpr = [PBuf(), PBuf()]
        n = 0
        for cb in range(nblk):
            p.dma("sp", wc[:], w_cv[:, cb * 512:(cb + 1) * 512].rearrange("(c p) f -> p c f", p=128), writes=[bwc])
            for gi, tiles in enumerate(groups):
                i = gi % 2
                nt = len(tiles)
                N = nt * 128
                t0 = tiles[0] * 128
                p.dma("pool", hg[i][:, 0:nt], HT[tiles[0]:tiles[0] + nt].rearrange("t p c k -> p t c k"), reads=[bHT[t] for t in tiles], writes=[bhg[i]])
                for cc in range(4):
                    j = n % 2
                    n += 1
                    for k in range(nt):
                        for ch in range(8):
                            p.op("pe", lambda e: e.matmul(pr_[j][:, k * 128:(k + 1) * 128], wc[:, ch, cc * 128:(cc + 1) * 128], hg[i][:, k, ch, :],
                                                          start=(ch == 0), stop=(ch == 7)), reads=[bwc, bhg[i]], writes=[bpr[j]])
                    p.op("act" if j else "dve",
                         (lambda e: e.copy(raw[cc][:, t0:t0 + N], pr_[j][:, 0:N])) if j else (lambda e: e.tensor_copy(raw[cc][:, t0:t0 + N], pr_[j][:, 0:N])),
                         reads=[bpr[j]], writes=[braw[cc]])
            for cc in range(4):
                k = cb * 4 + cc
                o = cv[cc % 2]; bo_ = bcv[cc % 2]; r = raw[cc]
                p.op("dve", lambda e: e.tensor_scalar(o[:], r[:], cp[:, k, 1:2], None, ALU.mult), reads=[braw[cc], bcp], writes=[bo_])
                for (a0, a1) in ((0, 4096), (4096, NTOK)):
                    p.op("dve", lambda e: e.scalar_tensor_tensor(o[:, a0 + 1:a1], r[:, a0:a1 - 1], cp[:, k, 0:1], o[:, a0 + 1:a1], ALU.mult, ALU.add),
                         reads=[braw[cc], bcp, bo_], writes=[bo_])
                    p.op("dve", lambda e: e.scalar_tensor_tensor(o[:, a0:a1 - 1], r[:, a0 + 1:a1], cp[:, k, 2:3], o[:, a0:a1 - 1], ALU.mult, ALU.add),
                         reads=[braw[cc], bcp, bo_], writes=[bo_])
                p.op("act", lambda e: e.activation(o[:], o[:], AF.Silu, bias=cp[:, k, 3:4]), reads=[bo_, bcp], writes=[bo_])
                p.dma("sp", FT[k], o[:], reads=[bo_], writes=[bFT[k]])
        p.barrier()


def emit_gdn(c, K, X, bX, mods, w_cv, convp, w_z, w_bg, dtb, alog, ng, w_out, masks, P, bP, stop=None):
    p = c.p
    HT = dram(c, [NT, 128, 8, 128]); bHT = [Buf() for _ in range(NT)]
    FT = dram(c, [12, 128, NTOK]); bFT = [Buf() for _ in range(12)]
    ZS = dram(c, [NTOK, 512]); BG = dram(c, [NTOK, 16]); QKV = dram(c, [NTOK, 1536]); QKT = dram(c, [8, 128, NTOK]); OF = dram(c, [NTOK, 512])
    bZS = [Buf() for _ in range(NT)]; bQ = [Buf() for _ in range(NT)]; bOF = [Buf() for _ in range(NT)]
    es = ExitStack()
    with es:
        wz = c.sb([128, 8, 512], es); wbg = c.sb([128, 8, 16], es); bwz = Buf()
        p.dma("sp", wz[:], w_z.rearrange("(c p) f -> p c f", p=128), writes=[bwz])
        p.dma("sp", wbg[:], w_bg.rearrange("(c p) f -> p c f", p=128), writes=[bwz])
        dtb_t = c.sb([128, 8], es); na = c.sb([128, 8], es); bdb = Buf()
        p.dma("pool", dtb_t[:], dtb.partition_broadcast(128), writes=[bdb])
        p.dma("pool", na[:], alog.partition_broadcast(128), writes=[bdb])
        p.op("act", lambda e: e.activation(na[:], na[:], AF.Exp), reads=[bdb], writes=[bdb])
        p.op("dve", lambda e: e.tensor_scalar(na[:], na[:], -1.0, None, ALU.mult), reads=[bdb], writes=[bdb])
        xt = [c.sb([128, D], es) for _ in range(2)]; bxt = [Buf(), Buf()]
        hT = [c.sb([128, 8, 128], es) for _ in range(2)]; bhT = [Buf(), Buf()]
        pz = c.ps([128, 512], es); bpz = PBuf()
        pd = c.ps([128, 512], es); bpd = PBuf()
        zs = [c.sb([128, 512], es) for _ in range(2)]; bzs = [Buf(), Buf()]
        dr = c.sb([128, 8], es); ta = c.sb([128, 8], es); tb = c.sb([128, 8], es); bgo = [c.sb([128, 16], es) for _ in range(2)]
        bdr, btt, bbgo = Buf(), Buf(), [Buf(), Buf()]
        for t in range(NT):
            i = t % 2
            G, S, _ = mods[tile_kind(t)]
            p.dma("pool", xt[i][:], X[t * 128:(t + 1) * 128, :], reads=[bX[t]], writes=[bxt[i]])
            norm_mod_T(c, K, xt[i][:], bxt[i], G, S, hT[i], bhT[i])
            p.dma("sp", HT[t], hT[i][:], reads=[bhT[i]], writes=[bHT[t]])
            for ch in range(8):
                p.op("pe", lambda e: e.matmul(pz[:], hT[i][:, ch, :], wz[:, ch, :], start=(ch == 0), stop=(ch == 7)), reads=[bhT[i], bwz], writes=[bpz])
            p.op("act", lambda e: e.activation(zs[i][:], pz[:], AF.Silu), reads=[bpz], writes=[bzs[i]])
            p.dma("sp", ZS[t * 128:(t + 1) * 128, :], zs[i][:], reads=[bzs[i]], writes=[bZS[t]])
            for ch in range(8):
                p.op("pe", lambda e: e.matmul(pd[:, 0:16], hT[i][:, ch, :], wbg[:, ch, :], start=(ch == 0), stop=(ch == 7)), reads=[bhT[i], bwz], writes=[bpd])
            p.op("act", lambda e: e.activation(bgo[i][:, 0:8], pd[:, 0:8], AF.Sigmoid), reads=[bpd], writes=[bbgo[i]])
            p.op("dve", lambda e: e.tensor_add(dr[:], pd[:, 8:16], dtb_t[:]), reads=[bpd, bdb], writes=[bdr])
            softplus_tile(c, ta[:], dr[:], ta[:], tb[:], [bdr], [btt], btt)
            p.op("dve", lambda e: e.tensor_mul(bgo[i][:, 8:16], ta[:], na[:]), reads=[btt, bdb], writes=[bbgo[i]])
            p.dma("sp", BG[t * 128:(t + 1) * 128, :], bgo[i][:], reads=[bbgo[i]], writes=[bZS[t]])
        p.barrier()
    if stop == "A1":
        return
    conv_fm(c, K, HT, bHT, w_cv, convp, 3, FT, bFT)
    if stop == "A2":
        return
    es = ExitStack()
    with es:
        fx = [c.sb([128, 12, 128], es) for _ in range(2)]; bfx = [Buf(), Buf()]
        tm = [c.sb([128, 12, 128], es) for _ in range(2)]; btm = [Buf(), Buf()]
        qkT = [c.sb([128, 8, 128], es) for _ in range(2)]; bqkT = [Buf(), Buf()]
        s8 = c.sb([128, 16], es); bs8 = Buf()
        for t in range(NT):
            i = t % 2
            p.dma("sp", fx[i][:], FT[0:12, :, t * 128:(t + 1) * 128].rearrange("k p t -> p k t"), reads=bFT, writes=[bfx[i]])
            for q in range(3):
                for cc in range(4):
                    p.op("pe", lambda e: e.transpose(K["ptr"][:, cc * 128:(cc + 1) * 128], fx[i][:, q * 4 + cc, :], K["ident"][:]),
                         reads=[bfx[i], K["bident"]], writes=[K["bptr"]])
                p.op("act" if q % 2 else "dve",
                     (lambda e: e.copy(tm[i][:, q * 4:(q + 1) * 4, :], K["ptr"][:].rearrange("p (c t) -> p c t", c=4))) if q % 2 else
                     (lambda e: e.tensor_copy(tm[i][:, q * 4:(q + 1) * 4, :], K["ptr"][:].rearrange("p (c t) -> p c t", c=4))),
                     reads=[K["bptr"]], writes=[btm[i]])
            for hq in range(8):
                p.op("act", lambda e: e.activation(K["junk"][:, 0:128], tm[i][:, hq, :], AF.Square, accum_out=s8[:, hq:hq + 1]),
                     reads=[btm[i]], writes=[K["bjunk"], bs8])
            p.op("act", lambda e: e.activation(s8[:, 8:16], s8[:, 0:8], AF.Sqrt, bias=K["eps"][:, 0:1]), reads=[bs8], writes=[bs8])
            p.op("dve", lambda e: e.reciprocal(s8[:, 0:8], s8[:, 8:16]), reads=[bs8], writes=[bs8])
            for hq in range(8):
                sc2 = (128.0 ** -0.5) if hq < 4 else 1.0
                p.op("dve", lambda e: e.tensor_scalar(tm[i][:, hq, :], tm[i][:, hq, :], s8[:, hq:hq + 1], sc2, ALU.mult, ALU.mult),
                     reads=[btm[i], bs8], writes=[btm[i]])
            p.dma("pool", QKV[t * 128:(t + 1) * 128, :], tm[i][:].rearrange("p k d -> p (k d)"), reads=[btm[i]], writes=[bQ[t]])
            for q in range(2):
                for cc in range(4):
                    p.op("pe", lambda e: e.transpose(K["ptr"][:, cc * 128:(cc + 1) * 128], tm[i][:, q * 4 + cc, :], K["ident"][:]),
                         reads=[btm[i], K["bident"]], writes=[K["bptr"]])
                p.op("act", lambda e: e.copy(qkT[i][:, q * 4:(q + 1) * 4, :], K["ptr"][:].rearrange("p (c t) -> p c t", c=4)),
                     reads=[K["bptr"]], writes=[bqkT[i]])
            p.dma("pool", QKT[:, :, t * 128:(t + 1) * 128].rearrange("h p t -> p h t"), qkT[i][:], reads=[bqkT[i]], writes=[bQ[t]])
        p.barrier()
    if stop == "A3":
        return
    es = ExitStack()
    with es:
        mk = c.sb([128, 2, 7, 128], es); bmk = Buf()
        for dd in range(2):
            for mm_ in range(7):
                p.dma("sp", mk[:, dd, mm_, :], masks[dd, mm_], writes=[bmk])
        m4 = c.sb([128, 4, 4, 128], es); id4 = c.sb([128, 4, 128], es); bm4 = Buf()
        for hh in range(4):
            p.op("dve", lambda e: e.tensor_copy(m4[:, :, hh, :], mk[:, 0, 3:7, :]), reads=[bmk], writes=[bm4])
            p.op("dve", lambda e: e.tensor_copy(id4[:, hh, :], K["ident"][:]), reads=[K["bident"]], writes=[bm4])
        ones = c.sb([128, 128], es); bon = Buf()
        p.op("dve", lambda e: e.memset(ones[:], 1.0), writes=[bon])
        tri_t = [c.sb([128, 128], es) for _ in range(2)]
        ms_t = [c.sb([128, 128], es) for _ in range(2)]
        nm_t = [c.sb([128, 128], es) for _ in range(2)]
        for dd in range(2):
            p.op("dve", lambda e: e.tensor_copy(tri_t[dd][:], mk[:, dd, 0, :]), reads=[bmk], writes=[bmk])
            p.op("dve", lambda e: e.tensor_copy(ms_t[dd][:], mk[:, dd, 1, :]), reads=[bmk], writes=[bmk])
            p.op("dve", lambda e: e.tensor_copy(nm_t[dd][:], mk[:, dd, 2, :]), reads=[bmk], writes=[bmk])
        ngt = c.sb([128, 128], es); bng = Buf()
        p.dma("pool", ngt[:], ng.partition_broadcast(128), writes=[bng])
        wo = c.sb([128, 4, D], es); bwo = Buf()
        p.dma("sp", wo[:], w_out.rearrange("(c p) f -> p c f", p=128), writes=[bwo])
        qkv = [c.sb([128, 12, 128], es) for _ in range(2)]; qkt = [c.sb([128, 8, 128], es) for _ in range(2)]; bg_ = [c.sb([128, 16], es) for _ in range(2)]
        bin_ = [Buf(), Buf()]
        S = c.sb([128, 4, 128], es); bS = Buf()
        sm = c.sb([128, 32], es); bsm = Buf()
        sums = c.sb([128, 32], es); bsums = Buf()
        rbs = (c.sb([128, 4, 128], es), Buf())
        gc, egc, etot, kdw, bs_ = (sm[:, 4 * j:4 * j + 4] for j in range(5))
        banks = [(c.ps([128, 512], es), PBuf()) for _ in range(7)]
        bank_i = [0]

        def nb():
            b = banks[bank_i[0] % 7]
            bank_i[0] += 1
            return b

        def T3(name=None):
            return (c.sb([128, 4, 128], es), Buf())
        Kb, Vb, kd, abc, tA, tB, E1, E2, Eg, Lm, AT, qgT, LT, D0, D0T, ImD0T, O1T, O2T, O3T = (T3() for _ in range(19))
        D2, D2T, IpD2T, D4, D4T, IpD4T, IpD8, R1, R2, Xa, XaT, Xb, XbT, Gt, nwT, vn, osb = (T3() for _ in range(17))
        ofl = c.sb([128, 512], es); zl = c.sb([128, 512], es); bfl = Buf()
        s4 = c.sb([128, 12], es); bs4 = Buf()
        ynT = c.sb([128, 4, 128], es); bynT = Buf()
        ob = c.sb([128, D], es); bob = Buf()

        def f(tb_):
            return tb_[0][:].rearrange("p h d -> p (h d)")

        def mm4(dst, lhs, rhs, lhs_b, rhs_b):
            for hh in range(4):
                p.op("pe", lambda e: e.matmul(dst[0][:, hh * 128:(hh + 1) * 128], lhs[0][:, hh, :], rhs[0][:, hh, :], start=True, stop=True),
                     reads=[lhs_b, rhs_b], writes=[dst[1]])

        def ev(eng, dst, src_ps, add=None, sub_from=None):
            if sub_from is not None:
                p.op("dve", lambda e: e.tensor_sub(f(dst), f(sub_from), src_ps[0][:]), reads=[src_ps[1], sub_from[1]], writes=[dst[1]])
            elif add is not None:
                p.op("dve", lambda e: e.tensor_add(f(dst), src_ps[0][:], add), reads=[src_ps[1], bm4], writes=[dst[1]])
            elif eng == "act":
                p.op("act", lambda e: e.copy(f(dst), src_ps[0][:]), reads=[src_ps[1]], writes=[dst[1]])
            else:
                p.op("dve", lambda e: e.tensor_copy(f(dst), src_ps[0][:]), reads=[src_ps[1]], writes=[dst[1]])
        idf = id4[:].rearrange("p h d -> p (h d)")
        for d in range(2):
            order = [32, 33] + list(range(32)) if d == 0 else [33, 32] + list(range(31, -1, -1))
            if isinstance(stop, int):
                order = order[:stop]
            tri_d, MS_d, NM_d = mk[:, d, 0, :], mk[:, d, 1, :], mk[:, d, 2, :]
            p.op("dve", lambda e: e.memset(S[:], 0.0), reads=[bS], writes=[bS])
            if stop == ("B", 1):
                p.barrier()
                return
            for vi, t in enumerate(order):
                i = vi % 2
                sl = slice(t * 128, (t + 1) * 128)
                p.dma("sp", qkv[i][:], QKV[sl, :].rearrange("p (k d) -> p k d", k=12), reads=[bQ[t]], writes=[bin_[i]])
                for hq in range(8):
                    p.dma("sp", qkt[i][:, hq, :], QKT[hq, :, sl], reads=[bQ[t]], writes=[bin_[i]])
                p.dma("sp", bg_[i][:], BG[sl, :], reads=[bZS[t]], writes=[bin_[i]])
                beta = bg_[i][:, d * 4:(d + 1) * 4]
                g = bg_[i][:, 8 + d * 4:8 + (d + 1) * 4]
                qv = (qkv[i], bin_[i]); kT = qkt[i]
                pa_ = nb()
                gcol = slice(8 + d * 4, 8 + (d + 1) * 4)
                tcol = slice(16 + 8 + d * 4, 16 + 8 + (d + 1) * 4)
                p.op("pe", lambda e: e.matmul(pa_[0][:, 0:16], tri_t[d][:], bg_[i][:, 0:16], start=True, stop=True), reads=[bmk, bin_[i]], writes=[pa_[1]])
                p.op("pe", lambda e: e.matmul(pa_[0][:, 16:32], ones[:], bg_[i][:, 0:16], start=True, stop=True), reads=[bon, bin_[i]], writes=[pa_[1]])
                p.op("act", lambda e: e.copy(sums[:], pa_[0][:, 0:32]), reads=[pa_[1]], writes=[bsums])
                p.op("dve", lambda e: e.tensor_copy(gc, sums[:, gcol]), reads=[bsums], writes=[bsm])
                p.op("act", lambda e: e.activation(egc, sums[:, gcol], AF.Exp), reads=[bsums], writes=[bsm])
                p.op("act", lambda e: e.activation(etot, sums[:, tcol], AF.Exp), reads=[bsums], writes=[bsm])
                p.op("dve", lambda e: e.tensor_tensor(kdw, sums[:, tcol], gc, ALU.subtract), reads=[bsums, bsm], writes=[bsm])
                p.op("act", lambda e: e.activation(kdw, kdw, AF.Exp), reads=[bsm], writes=[bsm])
                p.op("dve", lambda e: e.tensor_mul(bs_, beta, egc), reads=[bin_[i], bsm], writes=[bsm])
                prb, pkk, pqk = nb(), nb(), nb()
                for hh in range(4):
                    p.op("pool", lambda e: e.tensor_scalar(Kb[0][:, hh, :], qkv[i][:, 4 + hh, :], bs_[:, hh:hh + 1], 1.0, ALU.mult, ALU.mult), reads=[bin_[i], bsm], writes=[Kb[1]])
                    p.op("pool", lambda e: e.tensor_scalar(Vb[0][:, hh, :], qkv[i][:, 8 + hh, :], beta[:, hh:hh + 1], 1.0, ALU.mult, ALU.mult), reads=[bin_[i]], writes=[Vb[1]])
                    p.op("pool", lambda e: e.tensor_scalar(kd[0][:, hh, :], qkv[i][:, 4 + hh, :], kdw[:, hh:hh + 1], 1.0, ALU.mult, ALU.mult), reads=[bin_[i], bsm], writes=[kd[1]])
                    p.op("pool", lambda e: e.tensor_scalar(abc[0][:, hh, :], ones[:], g[:, hh:hh + 1], 1.0, ALU.mult, ALU.mult), reads=[bon, bin_[i]], writes=[abc[1]])
                    p.op("pe", lambda e: e.matmul(prb[0][:, hh * 128:(hh + 1) * 128], abc[0][:, hh, :], tri_t[d][:], start=True, stop=True), reads=[abc[1], bmk], writes=[prb[1]])
                    p.op("pe", lambda e: e.matmul(pkk[0][:, hh * 128:(hh + 1) * 128], kT[:, 4 + hh, :], kT[:, 4 + hh, :], start=True, stop=True), reads=[bin_[i]], writes=[pkk[1]])
                    p.op("pe", lambda e: e.matmul(pqk[0][:, hh * 128:(hh + 1) * 128], kT[:, 4 + hh, :], kT[:, hh, :], start=True, stop=True), reads=[bin_[i]], writes=[pqk[1]])
                    if stop == ("B", 2):
                        p.barrier()
                        return
                p.op("act", lambda e: e.copy(rbs[0][:].rearrange("p h d -> p (h d)"), prb[0][:]), reads=[prb[1]], writes=[rbs[1]])
                for hh in range(4):
                    p.op("dve", lambda e: e.scalar_tensor_tensor(tA[0][:, hh, :], rbs[0][:, hh, :], gc[:, hh:hh + 1], ms_t[d][:], ALU.subtract, ALU.max),
                         reads=[rbs[1], bsm, bmk], writes=[tA[1]])
                    p.op("dve", lambda e: e.scalar_tensor_tensor(tB[0][:, hh, :], rbs[0][:, hh, :], gc[:, hh:hh + 1], nm_t[d][:], ALU.subtract, ALU.min),
                         reads=[rbs[1], bsm, bmk], writes=[tB[1]])
                p.op("act", lambda e: e.activation(f(E1), f(tA), AF.Exp, scale=-1.0), reads=[tA[1]], writes=[E1[1]])
                p.op("act", lambda e: e.activation(f(E2), f(tB), AF.Exp), reads=[tB[1]], writes=[E2[1]])
                p.op("act", lambda e: e.activation(f(Eg), rbs[0][:].rearrange("p h d -> p (h d)"), AF.Exp), reads=[rbs[1]], writes=[Eg[1]])
                for hh in range(4):
                    p.op("dve", lambda e: e.scalar_tensor_tensor(Lm[0][:, hh, :], pkk[0][:, hh * 128:(hh + 1) * 128], beta[:, hh:hh + 1], E1[0][:, hh, :], ALU.mult, ALU.mult),
                         reads=[pkk[1], bin_[i], E1[1]], writes=[Lm[1]])
                p.op("dve", lambda e: e.tensor_mul(f(AT), pqk[0][:], f(E2)), reads=[pqk[1], E2[1]], writes=[AT[1]])
                p.op("pool", lambda e: e.tensor_mul(f(qgT), qkt[i][:, 0:4, :].rearrange("p h d -> p (h d)"), f(Eg)), reads=[bin_[i], Eg[1]], writes=[qgT[1]])
                if stop == ("B", 3):
                    p.barrier()
                    return
                plt = nb()
                for hh in range(4):
                    p.op("pe", lambda e: e.transpose(plt[0][:, hh * 128:(hh + 1) * 128], Lm[0][:, hh, :], K["ident"][:]), reads=[Lm[1], K["bident"]], writes=[plt[1]])
                ev("act", LT, plt)
                p.op("dve", lambda e: e.tensor_mul(f(D0), f(Lm), m4[:, 0].rearrange("p h d -> p (h d)")), reads=[Lm[1], bm4], writes=[D0[1]])
                p.op("pool", lambda e: e.tensor_mul(f(D0T), f(LT), m4[:, 0].rearrange("p h d -> p (h d)")), reads=[LT[1], bm4], writes=[D0T[1]])
                p.op("pool", lambda e: e.tensor_mul(f(O1T), f(LT), m4[:, 1].rearrange("p h d -> p (h d)")), reads=[LT[1], bm4], writes=[O1T[1]])
                p.op("dve", lambda e: e.tensor_mul(f(O2T), f(LT), m4[:, 2].rearrange("p h d -> p (h d)")), reads=[LT[1], bm4], writes=[O2T[1]])
                p.op("pool", lambda e: e.tensor_mul(f(O3T), f(LT), m4[:, 3].rearrange("p h d -> p (h d)")), reads=[LT[1], bm4], writes=[O3T[1]])
                p.op("pool", lambda e: e.tensor_sub(f(ImD0T), idf, f(D0T)), reads=[D0T[1], bm4], writes=[ImD0T[1]])
                if stop == ("B", 4):
                    p.barrier()
                    return
                b1 = nb(); mm4(b1, D0T, D0, D0T[1], D0[1]); ev("act", D2, b1)
                b2 = nb(); mm4(b2, D0, D0T, D0[1], D0T[1]); ev("act", D2T, b2); ev("dve", IpD2T, b2, add=idf)
                b3 = nb(); mm4(b3, D2T, D2, D2T[1], D2[1]); ev("act", D4, b3)
                b4 = nb(); mm4(b4, D2, D2T, D2[1], D2T[1]); ev("act", D4T, b4); ev("dve", IpD4T, b4, add=idf)
                b5 = nb(); mm4(b5, D4T, D4, D4T[1], D4[1]); ev("dve", IpD8, b5, add=idf)
                b6 = nb(); mm4(b6, IpD4T, IpD8, IpD4T[1], IpD8[1]); ev("act", R1, b6)
                b7 = nb(); mm4(b7, IpD2T, R1, IpD2T[1], R1[1]); ev("dve", R2, b7)
                b8 = nb(); mm4(b8, ImD0T, R2, ImD0T[1], R2[1]); ev("act", Xa, b8)
                b9 = nb()
                for hh in range(4):
                    p.op("pe", lambda e: e.transpose(b9[0][:, hh * 128:(hh + 1) * 128], Xa[0][:, hh, :], K["ident"][:]), reads=[Xa[1], K["bident"]], writes=[b9[1]])
                ev("dve", XaT, b9)
                if stop == ("B", 5):
                    p.barrier()
                    return
                Xc, XcT, Xn, XnT = Xa, XaT, Xb, XbT
                for lvl, OT_ in enumerate((O1T, O2T, O3T)):
                    bg1 = nb(); mm4(bg1, OT_, Xc, OT_[1], Xc[1]); ev("act", Gt, bg1)
                    if lvl < 2:
                        bp1 = nb(); mm4(bp1, XcT, Gt, XcT[1], Gt[1]); ev("dve", Xn, bp1, sub_from=Xc)
                    bp2 = nb(); mm4(bp2, Gt, XcT, Gt[1], XcT[1]); ev("dve", XnT, bp2, sub_from=XcT)
                    Xc, XcT, Xn, XnT = Xn, XnT, Xc, XcT
                TT = XcT
                if stop == ("B", 6):
                    p.barrier()
                    return
                bw = nb(); mm4(bw, Kb, TT, Kb[1], TT[1])
                p.op("dve", lambda e: e.tensor_scalar(f(nwT), bw[0][:], -1.0, None, ALU.mult), reads=[bw[1]], writes=[nwT[1]])
                bv = nb()
                for hh in range(4):
                    p.op("pe", lambda e: e.matmul(bv[0][:, hh * 128:(hh + 1) * 128], TT[0][:, hh, :], Vb[0][:, hh, :], start=True, stop=False), reads=[TT[1], Vb[1]], writes=[bv[1]])
                    p.op("pe", lambda e: e.matmul(bv[0][:, hh * 128:(hh + 1) * 128], nwT[0][:, hh, :], S[:, hh, :], start=False, stop=True), reads=[nwT[1], bS], writes=[bv[1]])
                ev("act", vn, bv)
                bo_ = nb()
                for hh in range(4):
                    p.op("pe", lambda e: e.matmul(bo_[0][:, hh * 128:(hh + 1) * 128], qgT[0][:, hh, :], S[:, hh, :], start=True, stop=False), reads=[qgT[1], bS], writes=[bo_[1]])
                    p.op("pe", lambda e: e.matmul(bo_[0][:, hh * 128:(hh + 1) * 128], AT[0][:, hh, :], vn[0][:, hh, :], start=False, stop=True), reads=[AT[1], vn[1]], writes=[bo_[1]])
                ev("dve", osb, bo_)
                bsn = nb(); mm4(bsn, kd, vn, kd[1], vn[1])
                for hh in range(4):
                    p.op("dve", lambda e: e.scalar_tensor_tensor(S[:, hh, :], S[:, hh, :], etot[:, hh:hh + 1], bsn[0][:, hh * 128:(hh + 1) * 128], ALU.mult, ALU.add),
                         reads=[bS, bsm, bsn[1]], writes=[bS])
                if stop == ("B", 7):
                    p.barrier()
                    return
                if d == 0:
                    p.dma("pool", OF[sl, :], f(osb), reads=[osb[1]], writes=[bOF[t]])
                    continue
                p.dma("pool", ofl[:], OF[sl, :], reads=[bOF[t]], writes=[bfl])
                p.dma("pool", zl[:], ZS[sl, :], reads=[bZS[t]], writes=[bfl])
                p.op("dve", lambda e: e.tensor_add(f(osb), f(osb), ofl[:]), reads=[osb[1], bfl], writes=[osb[1]])
                for hh in range(4):
                    p.op("act", lambda e: e.activation(K["junk"][:, 0:128], osb[0][:, hh, :], AF.Square, accum_out=s4[:, hh:hh + 1]), reads=[osb[1]], writes=[K["bjunk"], bs4])
                p.op("act", lambda e: e.activation(s4[:, 4:8], s4[:, 0:4], AF.Sqrt, bias=K["eps"][:, 0:1], scale=1.0 / 128), reads=[bs4], writes=[bs4])
                p.op("dve", lambda e: e.reciprocal(s4[:, 8:12], s4[:, 4:8]), reads=[bs4], writes=[bs4])
                for hh in range(4):
                    p.op("dve", lambda e: e.scalar_tensor_tensor(osb[0][:, hh, :], osb[0][:, hh, :], s4[:, 8 + hh:9 + hh], ngt[:], ALU.mult, ALU.mult),
                         reads=[osb[1], bs4, bng], writes=[osb[1]])
                p.op("dve", lambda e: e.tensor_mul(f(osb), f(osb), zl[:]), reads=[osb[1], bfl], writes=[osb[1]])
                for hh in range(4):
                    p.op("pe", lambda e: e.transpose(K["ptr"][:, hh * 128:(hh + 1) * 128], osb[0][:, hh, :], K["ident"][:]), reads=[osb[1], K["bident"]], writes=[K["bptr"]])
                p.op("act", lambda e: e.copy(ynT[:].rearrange("p h d -> p (h d)"), K["ptr"][:]), reads=[K["bptr"]], writes=[bynT])
                for half in range(2):
                    pc_ = nb()
                    for ch in range(4):
                        p.op("pe", lambda e: e.matmul(pc_[0][:], ynT[:, ch, :], wo[:, ch, half * 512:(half + 1) * 512], start=(ch == 0), stop=(ch == 3)),
                             reads=[bynT, bwo], writes=[pc_[1]])
                    p.op("dve", lambda e: e.tensor_copy(ob[:, half * 512:(half + 1) * 512], pc_[0][:]), reads=[pc_[1]], writes=[bob])
                p.dma("pool", P[sl, :], ob[:], reads=[bob], writes=[bP[t]])
        p.barrier()


def gdn_masks():
    i = np.arange(128)[:, None]; j = np.arange(128)[None, :]
    blk = lambda n: (i // n == j // n)
    mk16 = blk(16).astype(np.float32)
    mo1 = (blk(32) & ~blk(16)).astype(np.float32)
    mo2 = (blk(64) & ~blk(32)).astype(np.float32)
    mo3 = (~blk(64)).astype(np.float32)
    out = np.zeros((2, 7, 128, 128), np.float32)
    for d in range(2):
        before = (j < i) if d == 0 else (j > i)
        tri = (i <= j) if d == 0 else (i >= j)
        out[d, 0] = tri
        out[d, 1] = np.where(before, 0.0, 1e4)
        out[d, 2] = np.where(tri, 0.0, -1e4)
        out[d, 3], out[d, 4], out[d, 5], out[d, 6] = mk16, mo1, mo2, mo3
    return out


def gdn_weights(z, jl, h):
    w_in = z["gdn_w_in"][jl]
    f = np.ascontiguousarray
    cols = np.concatenate([h * 512 + np.arange(512), 1024 + h * 512 + np.arange(512), 2048 + h * 512 + np.arange(512)])
    cw = z["gdn_conv_w"][jl]
    convp = np.stack([cw[0, cols], cw[1, cols], cw[2, cols], np.zeros(1536, np.float32)], 1)
    hs = 4 * h + np.arange(4)
    bgc = np.concatenate([4096 + hs, 4096 + 8 + hs, 4112 + hs, 4112 + 8 + hs])
    return dict(w_cv=f(w_in[:, cols]), convp=f(convp.reshape(12, 128, 4).astype(np.float32)), w_z=f(w_in[:, 3072 + h * 512:3072 + (h + 1) * 512]),
                w_bg=f(w_in[:, bgc]), dtb=f(np.concatenate([z["gdn_dt_bias"][jl][0, hs], z["gdn_dt_bias"][jl][1, hs]])[None, :]),
                alog=f(np.concatenate([z["gdn_a_log"][jl][0, hs], z["gdn_a_log"][jl][1, hs]])[None, :]),
                ng=f(z["gdn_norm_g"][jl][None, :]), w_out=f(z["gdn_w_out"][jl][h * 512:(h + 1) * 512]), masks=gdn_masks())


def build_ada():
    nc = new_nc()
    es = ExitStack()
    with es:
        c = Ctx(nc, es); p = c.p
        K = common_consts(c, c.din("ident", [128, 128]))
        cvec = c.din("cvec", [5, D]); aw = c.din("aw", [D, 3072]); ab = c.din("ab", [1, 3072])
        out = c.dout("m", [5, 3072])
        craw = c.sb([40, 128]); bcraw = Buf()
        p.dma("pool", craw[:], cvec.rearrange("k (c p) -> (k c) p", p=128), writes=[bcraw])
        sc = c.sb([128, 8, 5]); bsc = Buf()
        p.op("pe", lambda e: e.transpose(K["ptr"][:, 0:40], craw[:], K["ident"][0:40, 0:40]), reads=[bcraw, K["bident"]], writes=[K["bptr"]])
        p.op("act", lambda e: e.activation(sc[:].rearrange("p c k -> p k c"), K["ptr"][:, 0:40].rearrange("p (k c) -> p k c", k=5), AF.Silu),
             reads=[K["bptr"]], writes=[bsc])
        abt = c.sb([5, 3072]); babt = Buf()
        p.dma("pool", abt[:], ab.partition_broadcast(5), writes=[babt])
        wt = [c.sb([128, 8, 512]) for _ in range(2)]; bwt = [Buf(), Buf()]
        pa = [c.ps([128, 512]) for _ in range(2)]; bpa = [PBuf(), PBuf()]
        orow = c.sb([5, 3072]); borow = Buf()
        for g in range(6):
            j = g % 2
            p.dma("sp", wt[j][:], aw[:, g * 512:(g + 1) * 512].rearrange("(c p) f -> p c f", p=128), writes=[bwt[j]])
            for ch in range(8):
                p.op("pe", lambda e: e.matmul(pa[j][0:5, :], sc[:, ch, :], wt[j][:, ch, :], start=(ch == 0), stop=(ch == 7)), reads=[bsc, bwt[j]], writes=[bpa[j]])
            p.op("dve", lambda e: e.tensor_add(orow[:, g * 512:(g + 1) * 512], pa[j][0:5, :], abt[:, g * 512:(g + 1) * 512]), reads=[bpa[j], babt], writes=[borow])
        bo = Buf()
        p.dma("pool", out, orow[:], reads=[borow], writes=[bo])
        p.finish([bo])
    return nc


_STAGES = {}


def _stage(kind, **kw):
    key = (kind, tuple(sorted(kw.items())))
    if key not in _STAGES:
        _STAGES[key] = build_ada() if kind == "ada" else build_stage(kind, **kw)
    return _STAGES[key]


def kernel_unfused(**z):
    z = {k: np.asarray(v) for k, v in z.items()}
    f = np.ascontiguousarray
    cores = list(range(8))
    ident = np.eye(128, dtype=np.float32)
    cvec = f(np.concatenate([z["c"], z["c_ctx"][None]], 0).astype(np.float32))
    ins = [dict(ident=ident, cvec=cvec, aw=f(z["ada_w"][k // 2][:, (k % 2) * 3072:(k % 2 + 1) * 3072]),
                ab=f(z["ada_b"][k // 2][None, (k % 2) * 3072:(k % 2 + 1) * 3072])) for k in cores]
    r = run_bass_kernel_spmd(_stage("ada"), ins, core_ids=cores).results
    mods = np.stack([np.concatenate([r[2 * i]["m"], r[2 * i + 1]["m"]], 1) for i in range(4)]).reshape(4, 5, 6, D)
    zero_v = np.zeros(D, np.float32)

    def vec_for(li, b, pgx, pgc):
        return make_vec(mods[li, b], mods[li, 4], z["norm1_g"][li], z["norm2_g"][li], z["final_norm_g"], pgx, pgc)

    xprev = [f(np.concatenate([z["x"][b], z["ctx"][b]], 0)) for b in range(4)]
    part = [np.zeros((NTOK, D), np.float32) for _ in cores]
    ss = None
    pg = [(zero_v, zero_v) for _ in range(4)]
    plan = []
    for li in range(4):
        plan.append((("gdn", "ssd", "mla")[li % 3], li))
        plan.append(("moe", li))
    plan.append(("final", 3))
    for kind, li in plan:
        with_ss = ss is not None
        nc = _stage(kind, with_ss=True) if with_ss else _stage(kind)
        ins = []
        for k in cores:
            b, h = divmod(k, 2)
            d = dict(ident=ident, xprev=xprev[b], pa=part[2 * b], pb=part[2 * b + 1], vec=vec_for(li, b, *pg[b]))
            if with_ss:
                d.update(ssa=ss[2 * b], ssb=ss[2 * b + 1])
            if kind == "gdn":
                d.update(gdn_weights(z, li // 3, h))
            elif kind == "ssd":
                d.update(ssd_weights(z, h))
            elif kind == "mla":
                d.update(mla_weights(z["mla_w_in"][0], z["mla_w_uq"][0], z["mla_w_ukv"][0], z["mla_w_out"][0],
                                     z["mla_q_norm_g"][0], z["mla_kv_norm_g"][0], h))
            elif kind == "moe":
                perm = list(range(8 * h, 8 * h + 8)) + list(range(8 * (1 - h), 8 * (1 - h) + 8))
                d.update(wr=f(z["router_w"][li][:, perm]), wg=f(z["moe_w_gate"][li][8 * h:8 * h + 8].reshape(8 * D, D)),
                         wu=f(z["moe_w_up"][li][8 * h:8 * h + 8].reshape(8 * D, D)), wd=f(z["moe_w_down"][li][8 * h:8 * h + 8].reshape(8 * D, D)))
            ins.append(d)
        r = run_bass_kernel_spmd(nc, ins, core_ids=cores).results
        if kind == "final":
            return f(np.stack([r[2 * b]["p"][:4096] for b in range(4)]).astype(np.float32))
        xprev = [r[2 * b]["xo"] for b in range(4)]
        part = [r[k]["p"] for k in cores]
        ss = [r[k]["ss"] for k in cores] if kind == "ssd" else None
        gi = 5 if kind == "moe" else 2
        pg = [(mods[li, b, gi], mods[li, 4, gi]) for b in range(4)]


def decl_weights(c, kind, pre):
    d = lambda n, sh: c.din(pre + n, sh)
    if kind == "moe":
        return dict(wr=d("wr", [D, NE]), wg=d("wg", [NEH * D, D]), wu=d("wu", [NEH * D, D]), wd=d("wd", [NEH * D, D]))
    if kind == "mla":
        return dict(w_in=d("w_in", [D, 1088]), w_uqn=d("w_uqn", [768, 512]), w_uqr=d("w_uqr", [768, 256]), w_uqs=d("w_uqs", [768, 256]),
                    w_ukn=d("w_ukn", [256, 512]), w_ukv=d("w_ukv", [256, 512]), w_out=d("w_out", [512, D]), gq=d("gq", [1, 768]), gkv=d("gkv", [1, 256]),
                    cosT=d("cosT", [32, 4096]), sinT=d("sinT", [32, 4096]), cos_tm=d("cos_tm", [4096, 32]), sin_tm=d("sin_tm", [4096, 32]))
    if kind == "ssd":
        return dict(w_cv=d("w_cv", [D, 2048]), convp=d("convp", [16, 128, 4]), w_z=d("w_z", [D, D]), w_dt=d("w_dt", [D, 32]), dtb=d("dtb", [1, 32]),
                    alog=d("alog", [1, 32]), dvec=d("dvec", [1, D]), ng=d("ng", [1, D]), w_out=d("w_out", [D, D]), triF=d("triF", [128, 128]), triB=d("triB", [128, 128]))
    if kind == "gdn":
        return dict(w_cv=d("w_cv", [D, 1536]), convp=d("convp", [12, 128, 4]), w_z=d("w_z", [D, 512]), w_bg=d("w_bg", [D, 16]), dtb=d("dtb", [1, 8]),
                    alog=d("alog", [1, 8]), ng=d("ng", [1, 128]), w_out=d("w_out", [512, D]), masks=d("masks", [2, 7, 128, 128]))
    raise ValueError(kind)


FUSED_PLAN = [("gdn", 0), ("moe", 0), ("ssd", 1), ("moe", 1), ("mla", 2), ("moe", 2), ("gdn", 3), ("moe", 3)]


def build_fused():
    nc = new_nc()
    es = ExitStack()
    with es:
        c = Ctx(nc, es); p = c.p
        K = common_consts(c, c.din("ident", [128, 128]))
        xin = c.din("xin", [NTOK, D]); cvec = c.din("cvec", [2, D])
        ada_w = c.din("ada_w", [4 * D, 6 * D]); ada_b = c.din("ada_b", [4, 6 * D]); gains = c.din("gains", [9, D])
        out = c.dout("out", [4096, D])
        vecs, bv = emit_ada(c, K, cvec, ada_w, Buf(), ada_b)
        Xc = dram(c, [NTOK, D]); bXc = [Buf() for _ in range(NT)]
        for t in range(NT):
            p.dma("sp", Xc[t * 128:(t + 1) * 128, :], xin[t * 128:(t + 1) * 128, :], writes=[bXc[t]])
        prev = None

        def make_vec(li):
            vs = dram(c, [NVEC, D]); bvs = Buf()
            rows = [vecs[li, 0:1, j * D:(j + 1) * D] for j in range(6)] + [vecs[li, 1:2, j * D:(j + 1) * D] for j in range(6)]
            rows += [gains[li:li + 1, :], gains[4 + li:5 + li, :], gains[8:9, :]]
            if prev is not None:
                pli, gi = prev[8], prev[9]
                rows += [vecs[pli, 0:1, gi * D:(gi + 1) * D], vecs[pli, 1:2, gi * D:(gi + 1) * D]]
            else:
                rows += [gains[8:9, :], gains[8:9, :]]
            for r_, src in enumerate(rows):
                p.dma("sp", vs[r_:r_ + 1, :], src, reads=[bv], writes=[bvs])
            return vs, bvs

        def fold(li):
            nonlocal Xc, bXc
            vs, bvs = make_vec(li)
            c.vec_reads = [bvs]
            if prev is not None:
                Pa, bPa, Pb, bPb, SSa, bSSa, SSb, bSSb = prev[:8]
                Xn = dram(c, [NTOK, D]); bXn = [Buf() for _ in range(NT)]
                c.res_reads = list(bXc) + list(bPa) + list(bPb) + (list(bSSa) + list(bSSb) if SSa is not None else [])
                emit_residual_in(c, K, Xc, Pa, Pb, vs, Xn, bXn, None, SSa, SSb)
                c.res_reads = []
                Xc, bXc = Xn, bXn
            return vs

        for kind, li in FUSED_PLAN:
            vs = fold(li)
            Ps = []
            for h in range(2):
                w = decl_weights(c, kind, f"L{li}h{h}_")
                P = dram(c, [NTOK, D]); bP = [Buf() for _ in range(NT)]
                SS = bSS = None
                es1 = ExitStack()
                with es1:
                    if kind == "moe":
                        mods = mods_from_vec(c, es1, vs, 3, 13)
                        emit_moe(c, K, Xc, bXc, mods, w["wr"], w["wg"], w["wu"], w["wd"], Buf(), P, bP)
                    else:
                        mods = mods_from_vec(c, es1, vs, 0, 12)
                        if kind == "ssd":
                            SS = dram(c, [NTOK, 1]); bSS = [Buf() for _ in range(NT)]
                            emit_ssd(c, K, Xc, bXc, mods, P=P, bP=bP, SS=SS, bSS=bSS, **w)
                        elif kind == "mla":
                            emit_mla(c, K, Xc, bXc, mods, P=P, bP=bP, **w)
                        else:
                            emit_gdn(c, K, Xc, bXc, mods, P=P, bP=bP, **w)
                    p.barrier()
                Ps.append((P, bP, SS, bSS))
            prev = (Ps[0][0], Ps[0][1], Ps[1][0], Ps[1][1], Ps[0][2], Ps[0][3], Ps[1][2], Ps[1][3], li, 5 if kind == "moe" else 2)
        fold(3)
        es1 = ExitStack()
        bo = []
        with es1:
            fg = bc_tile(c, es1, gains[8:9, :])
            xt = [c.sb([128, D], es1) for _ in range(2)]; bxt = [Buf(), Buf()]
            ot = [c.sb([128, D], es1) for _ in range(2)]; bot = [Buf(), Buf()]
            for t in range(32):
                i = t % 2
                p.dma("sp", xt[i][:], Xc[t * 128:(t + 1) * 128, :], reads=[bXc[t]], writes=[bxt[i]])
                norm_mod_T(c, K, xt[i][:], bxt[i], fg, None, None, None)
                p.op("act", lambda e: e.copy(ot[i][:], K["h"][:]), reads=[K["bh"]], writes=[bot[i]])
                b = Buf(); bo.append(b)
                p.dma("pool", out[t * 128:(t + 1) * 128, :], ot[i][:], reads=[bot[i]], writes=[b])
        p.finish(bo)
        print("fused ninst", p.ninst, "nsem", p.nsem)
    return nc


def fused_inputs(z, b):
    f = np.ascontiguousarray
    d = dict(ident=np.eye(128, dtype=np.float32), xin=f(np.concatenate([z["x"][b], z["ctx"][b]], 0)),
             cvec=f(np.stack([z["c"][b], z["c_ctx"]]).astype(np.float32)), ada_w=f(z["ada_w"].reshape(4 * D, 6 * D)), ada_b=f(z["ada_b"]),
             gains=f(np.concatenate([z["norm1_g"], z["norm2_g"], z["final_norm_g"][None]], 0).astype(np.float32)))
    for kind, li in FUSED_PLAN:
        for h in range(2):
            pre = f"L{li}h{h}_"
            if kind == "gdn":
                w = gdn_weights(z, li // 3, h)
            elif kind == "ssd":
                w = ssd_weights(z, h)
            elif kind == "mla":
                w = mla_weights(z["mla_w_in"][0], z["mla_w_uq"][0], z["mla_w_ukv"][0], z["mla_w_out"][0], z["mla_q_norm_g"][0], z["mla_kv_norm_g"][0], h)
            else:
                perm = list(range(8 * h, 8 * h + 8)) + list(range(8 * (1 - h), 8 * (1 - h) + 8))
                w = dict(wr=f(z["router_w"][li][:, perm]), wg=f(z["moe_w_gate"][li][8 * h:8 * h + 8].reshape(8 * D, D)),
                         wu=f(z["moe_w_up"][li][8 * h:8 * h + 8].reshape(8 * D, D)), wd=f(z["moe_w_down"][li][8 * h:8 * h + 8].reshape(8 * D, D)))
            d.update({pre + k: v for k, v in w.items()})
    return d


def kernel_fused_dup(**z):
    z = {k: np.asarray(v) for k, v in z.items()}
    nc = _stage_fused()
    per_sample = [fused_inputs(z, b) for b in range(4)]
    ins = [per_sample[k // 2] for k in range(8)]
    r = run_bass_kernel_spmd(nc, ins, core_ids=list(range(8))).results
    return np.ascontiguousarray(np.stack([r[2 * b]["out"] for b in range(4)]).astype(np.float32))


def _stage_fused():
    if "fused" not in _STAGES:
        _STAGES["fused"] = build_fused()
    return _STAGES["fused"]


CH_ROWS = 256
NCHUNK = NTOK // CH_ROWS


def build_fused_pair():
    nc = new_nc()
    es = ExitStack()
    with es:
        c = Ctx(nc, es); p = c.p
        K = common_consts(c, c.din("ident", [128, 128]))
        xin = c.din("xin", [NTOK, D]); cvec = c.din("cvec", [2, D])
        ada_w = c.din("ada_w", [4 * D, 6 * D]); ada_b = c.din("ada_b", [4, 6 * D]); gains = c.din("gains", [9, D])
        out = c.dout("out", [4096, D])
        vecs, bv = emit_ada(c, K, cvec, ada_w, Buf(), ada_b)
        Xc = dram(c, [NTOK, D]); bXc = [Buf() for _ in range(NT)]
        for t in range(NT):
            p.dma("sp", Xc[t * 128:(t + 1) * 128, :], xin[t * 128:(t + 1) * 128, :], writes=[bXc[t]])
        prev = None

        def make_vec(li):
            vs = dram(c, [NVEC, D]); bvs = Buf()
            rows = [vecs[li, 0:1, j * D:(j + 1) * D] for j in range(6)] + [vecs[li, 1:2, j * D:(j + 1) * D] for j in range(6)]
            rows += [gains[li:li + 1, :], gains[4 + li:5 + li, :], gains[8:9, :]]
            if prev is not None:
                pli, gi = prev[4], prev[5]
                rows += [vecs[pli, 0:1, gi * D:(gi + 1) * D], vecs[pli, 1:2, gi * D:(gi + 1) * D]]
            else:
                rows += [gains[8:9, :], gains[8:9, :]]
            for r_, src in enumerate(rows):
                p.dma("sp", vs[r_:r_ + 1, :], src, reads=[bv], writes=[bvs])
            return vs, bvs

        def fold(li):
            nonlocal Xc, bXc
            vs, bvs = make_vec(li)
            c.vec_reads = [bvs]
            if prev is not None:
                G, bG, GSS, bGSS = prev[:4]
                Xn = dram(c, [NTOK, D]); bXn = [Buf() for _ in range(NT)]
                c.res_reads = list(bXc) + list(bG) + ([bGSS] if GSS is not None else [])

                def tile_aps(t):
                    j, off = divmod(t * 128, CH_ROWS)
                    return G[j][off:off + 128, :], G[j][CH_ROWS + off:CH_ROWS + off + 128, :]
                c.res_tile_aps = tile_aps
                ssa = GSS[0:NTOK, :] if GSS is not None else None
                ssb = GSS[NTOK:2 * NTOK, :] if GSS is not None else None
                emit_residual_in(c, K, Xc, None, None, vs, Xn, bXn, None, ssa, ssb)
                c.res_reads = []
                c.res_tile_aps = None
                Xc, bXc = Xn, bXn
            return vs

        for kind, li in FUSED_PLAN:
            vs = fold(li)
            w = decl_weights(c, kind, f"L{li}_")
            P = dram(c, [NTOK, D]); bP = [Buf() for _ in range(NT)]
            SS = bSS = None
            es1 = ExitStack()
            with es1:
                if kind == "moe":
                    mods = mods_from_vec(c, es1, vs, 3, 13)
                    emit_moe(c, K, Xc, bXc, mods, w["wr"], w["wg"], w["wu"], w["wd"], Buf(), P, bP, skip_ctx=(li == 3))
                else:
                    mods = mods_from_vec(c, es1, vs, 0, 12)
                    if kind == "ssd":
                        SS = dram(c, [NTOK, 1]); bSS = [Buf() for _ in range(NT)]
                        emit_ssd(c, K, Xc, bXc, mods, P=P, bP=bP, SS=SS, bSS=bSS, **w)
                    elif kind == "mla":
                        emit_mla(c, K, Xc, bXc, mods, P=P, bP=bP, **w)
                    else:
                        emit_gdn(c, K, Xc, bXc, mods, P=P, bP=bP, **w)
                p.barrier()
            G, bG = [], []
            for j in range(NCHUNK):
                src = dram(c, [CH_ROWS, D]); bs = Buf()
                p.dma("sp", src, P[j * CH_ROWS:(j + 1) * CH_ROWS, :], reads=[bP[2 * j], bP[2 * j + 1]], writes=[bs])
                g = dram(c, [2 * CH_ROWS, D]); bg = Buf()
                allgather(c, src, g, GRP_PAIR, [bs], [bg])
                G.append(g); bG.append(bg)
            GSS = bGSS = None
            if SS is not None:
                GSS = dram(c, [2 * NTOK, 1]); bGSS = Buf()
                allgather(c, SS, GSS, GRP_PAIR, list(bSS), [bGSS])
            prev = (G, bG, GSS, bGSS, li, 5 if kind == "moe" else 2)
        fold(3)
        es1 = ExitStack()
        bo = []
        with es1:
            fg = bc_tile(c, es1, gains[8:9, :])
            xt = [c.sb([128, D], es1) for _ in range(2)]; bxt = [Buf(), Buf()]
            ot = [c.sb([128, D], es1) for _ in range(2)]; bot = [Buf(), Buf()]
            for t in range(32):
                i = t % 2
                p.dma("sp", xt[i][:], Xc[t * 128:(t + 1) * 128, :], reads=[bXc[t]], writes=[bxt[i]])
                norm_mod_T(c, K, xt[i][:], bxt[i], fg, None, None, None)
                p.op("act", lambda e: e.copy(ot[i][:], K["h"][:]), reads=[K["bh"]], writes=[bot[i]])
                b = Buf(); bo.append(b)
                p.dma("pool", out[t * 128:(t + 1) * 128, :], ot[i][:], reads=[bot[i]], writes=[b])
        p.finish(bo)
        print("fused-pair ninst", p.ninst, "nsem", p.nsem)
    return nc


def fused_pair_inputs(z, b, h):
    f = np.ascontiguousarray
    d = dict(ident=np.eye(128, dtype=np.float32), xin=f(np.concatenate([z["x"][b], z["ctx"][b]], 0)),
             cvec=f(np.stack([z["c"][b], z["c_ctx"]]).astype(np.float32)), ada_w=f(z["ada_w"].reshape(4 * D, 6 * D)), ada_b=f(z["ada_b"]),
             gains=f(np.concatenate([z["norm1_g"], z["norm2_g"], z["final_norm_g"][None]], 0).astype(np.float32)))
    for kind, li in FUSED_PLAN:
        pre = f"L{li}_"
        if kind == "gdn":
            w = gdn_weights(z, li // 3, h)
        elif kind == "ssd":
            w = ssd_weights(z, h)
        elif kind == "mla":
            w = mla_weights(z["mla_w_in"][0], z["mla_w_uq"][0], z["mla_w_ukv"][0], z["mla_w_out"][0], z["mla_q_norm_g"][0], z["mla_kv_norm_g"][0], h)
        else:
            perm = list(range(8 * h, 8 * h + 8)) + list(range(8 * (1 - h), 8 * (1 - h) + 8))
            w = dict(wr=f(z["router_w"][li][:, perm]), wg=f(z["moe_w_gate"][li][8 * h:8 * h + 8].reshape(8 * D, D)),
                     wu=f(z["moe_w_up"][li][8 * h:8 * h + 8].reshape(8 * D, D)), wd=f(z["moe_w_down"][li][8 * h:8 * h + 8].reshape(8 * D, D)))
        d.update({pre + k: v for k, v in w.items()})
    return d


def kernel(**z):
    z = {k: np.asarray(v) for k, v in z.items()}
    if "fused_pair" not in _STAGES:
        _STAGES["fused_pair"] = build_fused_pair()
    ins = [fused_pair_inputs(z, k // 2, k % 2) for k in range(8)]
    r = run_bass_kernel_spmd(_STAGES["fused_pair"], ins, core_ids=list(range(8))).results
    return np.ascontiguousarray(np.stack([r[2 * b]["out"] for b in range(4)]).astype(np.float32))
```
